# Optimizing a Trainium2 kernel written in Bass

```python
import jax
import jax.numpy as jnp
from jax import lax
import numpy as np

D_MODEL = 1024
BATCH = 8
SEQ = 4096
DEPTH = 2

GRID_W = 64
CTX_LEN = 256
N_MOD = 6
D_MIX = D_MODEL
EPS = 1e-6
ML_H = 4
ML_DH = 64
ML_W = ML_H * ML_DH
ML_CHUNK = 64
ML_CONV = 3
NA_H = 6
NA_DH = 64
NA_W = NA_H * NA_DH
NA_WR = 8
NA_WC = 16
MLA_H = 6
MLA_NOPE = 64
MLA_ROPE = 32
MLA_V = 64
MLA_W = MLA_H * MLA_V
Q_LORA = 512
KV_LORA = 256
ATT_BLOCK = 128
ROPE_THETA = 10000.0
ML_COLS = 4 * ML_W + 4 * ML_H
NA_COLS = 3 * NA_W
MLA_COLS = Q_LORA + KV_LORA + MLA_ROPE
OFF_NA = ML_COLS
OFF_MLA = OFF_NA + NA_COLS
N_IN = OFF_MLA + MLA_COLS
D_FF = 2816
N_EXPERTS = 8
TOP_K = 2
D_FF_EXPERT = 2816
MOE_BLOCK = 256
N_DENSE = (DEPTH + 1) // 2
N_MOE = DEPTH // 2
F32 = jnp.float32

kernel_name = "hybrid_mlstm_natten_mla_moe_dit"


def rmsnorm(x, w):
    xf = x.astype(F32)
    y = xf * lax.rsqrt(jnp.mean(xf * xf, axis=-1, keepdims=True) + EPS) * w.astype(F32)
    return y.astype(x.dtype)


def modulate(h, shift, scale):
    return h * (1.0 + scale) + shift


def short_conv(x, w):
    k_w = w.shape[0]
    left = k_w // 2
    t_ = x.shape[1]
    xp = jnp.pad(x, ((0, 0), (left, k_w - 1 - left), (0, 0)))
    acc = xp[:, 0:t_] * w[0]
    for j in range(1, k_w):
        acc = acc + xp[:, j:j + t_] * w[j]
    return acc


def axial_rope_angles(t_):
    t = jnp.arange(t_, dtype=jnp.int32)
    row = (t // GRID_W).astype(F32)
    col = (t % GRID_W).astype(F32)
    half = MLA_ROPE // 2
    inv = ROPE_THETA ** (-jnp.arange(0, half, 2, dtype=F32) / half)
    return row[:, None] * inv, col[:, None] * inv


def _rot(x, ang):
    m = x.shape[-1] // 2
    x1, x2 = x[..., :m], x[..., m:]
    cs, sn = jnp.cos(ang), jnp.sin(ang)
    return jnp.concatenate([x1 * cs - x2 * sn, x2 * cs + x1 * sn], axis=-1)


def apply_axial_rope(x, ang_r, ang_c):
    xf = x.astype(F32)
    half = x.shape[-1] // 2
    return jnp.concatenate([_rot(xf[..., :half], ang_r), _rot(xf[..., half:], ang_c)], axis=-1).astype(x.dtype)


def softmax_attend(q, k, v):
    s = jnp.einsum('bhqd,bhkd->bhqk', q, k).astype(F32)
    return jnp.einsum('bhqk,bhkd->bhqd', jax.nn.softmax(s, axis=-1).astype(v.dtype), v)


def mlstm_chunkwise(q, k, v, li, lf, state, with_out):
    b_, h_, t_, dh = q.shape
    nc = t_ // ML_CHUNK
    qc = q.reshape(b_, h_, nc, ML_CHUNK, dh)
    kc = k.reshape(b_, h_, nc, ML_CHUNK, dh)
    vc = v.reshape(b_, h_, nc, ML_CHUNK, dh)
    lic = li.reshape(b_, h_, nc, ML_CHUNK)
    bcum = jnp.cumsum(lf.reshape(b_, h_, nc, ML_CHUNK), axis=-1)
    g = bcum[..., -1]
    a = g[..., None] - bcum + lic
    m_loc = jnp.max(a, axis=-1)
    w = jnp.exp(a - m_loc[..., None])
    c_loc = jnp.einsum('bhnl,bhnld,bhnle->bhnde', w, vc, kc)
    n_loc = jnp.einsum('bhnl,bhnle->bhne', w, kc)

    def step(carry, inp):
        c_st, n_st, m_st = carry
        g_j, c_j, n_j, m_j = inp
        m_new = jnp.maximum(g_j + m_st, m_j)
        w_old = jnp.exp(g_j + m_st - m_new)
        w_new = jnp.exp(m_j - m_new)
        c_nx = w_old[..., None, None] * c_st + w_new[..., None, None] * c_j
        n_nx = w_old[..., None] * n_st + w_new[..., None] * n_j
        return (c_nx, n_nx, m_new), carry

    xs = (jnp.moveaxis(g, 2, 0), jnp.moveaxis(c_loc, 2, 0), jnp.moveaxis(n_loc, 2, 0), jnp.moveaxis(m_loc, 2, 0))
    final, entering = lax.scan(step, state, xs)
    if not with_out:
        return None, final
    c_in = jnp.moveaxis(entering[0], 0, 2)
    n_in = jnp.moveaxis(entering[1], 0, 2)
    m_in = jnp.moveaxis(entering[2], 0, 2)
    inter = bcum + m_in[..., None]
    causal = jnp.tril(jnp.ones((ML_CHUNK, ML_CHUNK), dtype=bool))
    dmat = jnp.where(causal, bcum[..., :, None] - bcum[..., None, :] + lic[..., None, :], -jnp.inf)
    m_t = jnp.maximum(inter, jnp.max(dmat, axis=-1))
    s = jnp.einsum('bhntd,bhnsd->bhnts', qc, kc) * jnp.exp(dmat - m_t[..., None])
    w_inter = jnp.exp(inter - m_t)
    num = jnp.einsum('bhnts,bhnsd->bhntd', s, vc) + w_inter[..., None] * jnp.einsum('bhnde,bhnte->bhntd', c_in, qc)
    den = jnp.sum(s, axis=-1) + w_inter * jnp.einsum('bhne,bhnte->bhnt', n_in, qc)
    h = num / jnp.maximum(jnp.abs(den), jnp.exp(-m_t))[..., None]
    return h.reshape(b_, h_, t_, dh), final


def mlstm_prep(p, conv_w, ig_b, fg_b):
    b_, t_, _ = p.shape
    qk = jax.nn.silu(short_conv(p[..., :2 * ML_W], conv_w))

    def heads(a):
        return a.reshape(b_, t_, ML_H, ML_DH).transpose(0, 2, 1, 3).astype(F32)

    q = heads(qk[..., :ML_W])
    k = heads(qk[..., ML_W:]) * (ML_DH ** -0.5)
    v = heads(p[..., 2 * ML_W:3 * ML_W])
    o = p[..., 3 * ML_W:4 * ML_W]
    gates = p[..., 4 * ML_W:].astype(F32).reshape(b_, t_, 2, 2, ML_H)
    li = (gates[:, :, 0] + ig_b.astype(F32)).transpose(2, 0, 3, 1)
    lf = jax.nn.log_sigmoid(gates[:, :, 1] + fg_b.astype(F32)).transpose(2, 0, 3, 1)
    return q, k, v, o, li, lf


def mlstm_out(h, o, norm_w):
    b_, h_, t_, dh = h.shape
    ht = h.transpose(0, 2, 1, 3)
    hn = ht * lax.rsqrt(jnp.mean(ht * ht, axis=-1, keepdims=True) + EPS) * norm_w.astype(F32).reshape(h_, dh)
    return (hn.reshape(b_, t_, h_ * dh) * jax.nn.sigmoid(o.astype(F32))).astype(o.dtype)


def mlstm_mixer(p, pc, conv_w, ig_b, fg_b, norm_w, with_ctx_out):
    q, k, v, o, li, lf = mlstm_prep(p, conv_w, ig_b, fg_b)
    qc, kc, vc, oc, lic, lfc = mlstm_prep(pc, conv_w, ig_b, fg_b)
    b_, h_, _, dh = q.shape
    zero = (jnp.zeros((b_, h_, dh, dh), F32), jnp.zeros((b_, h_, dh), F32), jnp.full((b_, h_), -jnp.inf, F32))
    h_lat = []
    h_ctx = []
    for d in range(2):
        fl = (lambda z: jnp.flip(z, axis=2)) if d == 1 else (lambda z: z)
        hc_d, st = mlstm_chunkwise(fl(qc), fl(kc), fl(vc), fl(lic[d]), fl(lfc[d]), zero, with_ctx_out)
        h_d, _ = mlstm_chunkwise(fl(q), fl(k), fl(v), fl(li[d]), fl(lf[d]), st, True)
        h_lat.append(fl(h_d))
        if with_ctx_out:
            h_ctx.append(fl(hc_d))
    out = mlstm_out(h_lat[0] + h_lat[1], o, norm_w)
    if not with_ctx_out:
        return out, None
    return out, mlstm_out(h_ctx[0] + h_ctx[1], oc, norm_w)


def na_mixer(p, pc, rpb, with_ctx_out):
    b_, t_, _ = p.shape
    rows = t_ // GRID_W
    wr = min(NA_WR, rows)
    nband = wr * GRID_W
    scale = NA_DH ** -0.5

    def heads(a):
        return a.reshape(a.shape[0], a.shape[1], NA_H, NA_DH).transpose(0, 2, 1, 3)

    q = heads(p[..., :NA_W]) * scale
    k = heads(p[..., NA_W:2 * NA_W])
    v = heads(p[..., 2 * NA_W:])
    qc = heads(pc[..., :NA_W]) * scale
    kc = heads(pc[..., NA_W:2 * NA_W])
    vc = heads(pc[..., 2 * NA_W:])
    kg = k.reshape(b_, NA_H, rows, GRID_W, NA_DH)
    vg = v.reshape(b_, NA_H, rows, GRID_W, NA_DH)
    qg = jnp.moveaxis(q.reshape(b_, NA_H, rows, GRID_W, NA_DH), 2, 0)
    col = jnp.arange(GRID_W)
    c0 = jnp.clip(col - NA_WC // 2, 0, GRID_W - NA_WC)
    col_mask = (col[None, :] >= c0[:, None]) & (col[None, :] < c0[:, None] + NA_WC)
    col_idx = jnp.clip(col[None, :] - col[:, None], 1 - NA_WC, NA_WC - 1) + NA_WC - 1
    rpb_cols = rpb.astype(F32)[:, :, col_idx]

    def row_block(args):
        r, q_r = args
        r0 = jnp.clip(r - wr // 2, 0, rows - wr)
        k_b = lax.dynamic_slice_in_dim(kg, r0, wr, axis=2)
        v_b = lax.dynamic_slice_in_dim(vg, r0, wr, axis=2)
        bias = rpb_cols[:, r0 + jnp.arange(wr) - r + NA_WR - 1].transpose(0, 2, 1, 3)
        s = jnp.einsum('bhqd,bhrkd->bhqrk', q_r, k_b).astype(F32) + bias
        s = jnp.where(col_mask[:, None, :], s, -jnp.inf).reshape(b_, NA_H, GRID_W, nband)
        s_c = jnp.einsum('bhqd,bhcd->bhqc', q_r, kc).astype(F32)
        pr = jax.nn.softmax(jnp.concatenate([s, s_c], axis=-1), axis=-1).astype(v.dtype)
        o_b = jnp.einsum('bhqrk,bhrkd->bhqd', pr[..., :nband].reshape(b_, NA_H, GRID_W, wr, GRID_W), v_b)
        return o_b + jnp.einsum('bhqc,bhcd->bhqd', pr[..., nband:], vc)

    og = lax.map(row_block, (jnp.arange(rows), qg))
    out = og.transpose(1, 0, 3, 2, 4).reshape(b_, t_, NA_W)
    if not with_ctx_out:
        return out, None
    oc = softmax_attend(qc, kc, vc).transpose(0, 2, 1, 3).reshape(b_, pc.shape[1], NA_W)
    return out, oc


def mla_mixer(p, pc, q_norm_w, kv_norm_w, w_uq, w_ukv, ang_r, ang_c, with_ctx_out):
    scale = (MLA_NOPE + MLA_ROPE) ** -0.5

    def project(a, rotate):
        b_, t_, _ = a.shape
        cq = rmsnorm(a[..., :Q_LORA], q_norm_w)
        ckv = rmsnorm(a[..., Q_LORA:Q_LORA + KV_LORA], kv_norm_w)
        kr = a[..., Q_LORA + KV_LORA:]
        qf = (cq @ w_uq).reshape(b_, t_, MLA_H, MLA_NOPE + MLA_ROPE)
        kv = (ckv @ w_ukv).reshape(b_, t_, MLA_H, MLA_NOPE + MLA_V)
        qn, qr = qf[..., :MLA_NOPE], qf[..., MLA_NOPE:]
        kn, vv = kv[..., :MLA_NOPE], kv[..., MLA_NOPE:]
        if rotate:
            qr = apply_axial_rope(qr, ang_r[:, None], ang_c[:, None])
            kr = apply_axial_rope(kr, ang_r, ang_c)
        return (qn.transpose(0, 2, 1, 3) * scale, qr.transpose(0, 2, 1, 3) * scale,
                kn.transpose(0, 2, 1, 3), kr, vv.transpose(0, 2, 1, 3))

    qn, qr, kn, kr, v = project(p, True)
    qnc, qrc, knc, krc, vc = project(pc, False)
    b_, h_, t_, _ = qn.shape
    nb = t_ // ATT_BLOCK

    def blocks(z):
        return jnp.moveaxis(z.reshape(b_, h_, nb, ATT_BLOCK, z.shape[-1]), 2, 0)

    def attend(args):
        qn_b, qr_b = args
        s_l = jnp.einsum('bhqd,bhkd->bhqk', qn_b, kn) + jnp.einsum('bhqr,bkr->bhqk', qr_b, kr)
        s_c = jnp.einsum('bhqd,bhkd->bhqk', qn_b, knc) + jnp.einsum('bhqr,bkr->bhqk', qr_b, krc)
        pr = jax.nn.softmax(jnp.concatenate([s_l, s_c], axis=-1).astype(F32), axis=-1).astype(v.dtype)
        return jnp.einsum('bhqk,bhkd->bhqd', pr[..., :t_], v) + jnp.einsum('bhqk,bhkd->bhqd', pr[..., t_:], vc)

    og = lax.map(attend, (blocks(qn), blocks(qr)))
    out = og.transpose(1, 0, 3, 2, 4).reshape(b_, t_, MLA_W)
    if not with_ctx_out:
        return out, None
    s = (jnp.einsum('bhqd,bhkd->bhqk', qnc, knc) + jnp.einsum('bhqr,bkr->bhqk', qrc, krc)).astype(F32)
    oc = jnp.einsum('bhqk,bhkd->bhqd', jax.nn.softmax(s, axis=-1).astype(vc.dtype), vc)
    return out, oc.transpose(0, 2, 1, 3).reshape(b_, pc.shape[1], MLA_W)


def swiglu(x, w1, w3, w2):
    return (jax.nn.silu(x @ w1) * (x @ w3)) @ w2


def moe_ffn(x, router_w, w1, w3, w2):
    n_tok, d_ = x.shape
    logits = (x @ router_w).astype(F32)
    top_val, top_idx = lax.top_k(logits, TOP_K)
    gates = jax.nn.softmax(top_val, axis=-1)
    n_asg = n_tok * TOP_K
    e_flat = top_idx.reshape(n_asg)
    g_flat = gates.reshape(n_asg)
    tok_flat = jnp.arange(n_asg, dtype=jnp.int32) // TOP_K
    order = jnp.argsort(e_flat)
    e_s, tok_s, g_s = e_flat[order], tok_flat[order], g_flat[order]
    counts = jnp.zeros((N_EXPERTS,), jnp.int32).at[e_flat].add(1)
    padded = (counts + MOE_BLOCK - 1) // MOE_BLOCK * MOE_BLOCK
    pad_end = jnp.cumsum(padded)
    pad_start = pad_end - padded
    start = jnp.cumsum(counts) - counts
    dest = pad_start[e_s] + jnp.arange(n_asg, dtype=jnp.int32) - start[e_s]
    n_blocks = -(-n_asg // MOE_BLOCK) + N_EXPERTS
    n_rows = n_blocks * MOE_BLOCK
    buf_tok = jnp.full((n_rows,), n_tok, jnp.int32).at[dest].set(tok_s)
    buf_gate = jnp.zeros((n_rows,), F32).at[dest].set(g_s)
    blk_expert = jnp.minimum(jnp.searchsorted(pad_end, jnp.arange(n_blocks) * MOE_BLOCK, side='right'), N_EXPERTS - 1)
    x_pad = jnp.concatenate([x, jnp.zeros((1, d_), x.dtype)], axis=0)
    xb = x_pad[buf_tok].reshape(n_blocks, MOE_BLOCK, d_)

    def expert_block(args):
        xb_i, e = args
        return swiglu(xb_i, w1[e], w3[e], w2[e])

    yb = lax.map(expert_block, (xb, blk_expert)).reshape(n_rows, d_)
    y = jax.ops.segment_sum(yb * buf_gate[:, None].astype(yb.dtype), buf_tok, num_segments=n_tok + 1)
    return y[:n_tok]


def setup_inputs(seed: int = 0) -> dict:
    key = jax.random.key(seed)
    ks = list(jax.random.split(key, 32))
    d_ = D_MODEL

    def nrm(i, shape, scale):
        return jax.random.normal(ks[i], shape, F32) * scale

    return {
        'x': nrm(0, (BATCH, SEQ, d_), 1.0),
        'c': nrm(1, (BATCH, d_), 1.0),
        'ctx': nrm(2, (BATCH, CTX_LEN, d_), 1.0),
        'c_ctx': nrm(3, (d_,), 1.0),
        'ada_w': nrm(4, (DEPTH, d_, N_MOD * d_), 0.5 * d_ ** -0.5),
        'ada_b': nrm(5, (DEPTH, N_MOD * d_), 0.01),
        'norm1_w': 1.0 + nrm(6, (DEPTH, d_), 0.02),
        'norm2_w': 1.0 + nrm(7, (DEPTH, d_), 0.02),
        'w_in': nrm(8, (DEPTH, d_, N_IN), d_ ** -0.5),
        'w_out': nrm(9, (DEPTH, D_MIX, d_), D_MIX ** -0.5),
        'mlstm_conv_w': nrm(10, (DEPTH, ML_CONV, 2 * ML_W), ML_CONV ** -0.5),
        'mlstm_ig_b': nrm(11, (DEPTH, 2, ML_H), 0.1),
        'mlstm_fg_b': jnp.linspace(3.0, 6.0, ML_H, dtype=F32) + nrm(12, (DEPTH, 2, ML_H), 0.1),
        'mlstm_norm_w': 1.0 + nrm(13, (DEPTH, ML_W), 0.02),
        'na_rpb': nrm(14, (DEPTH, NA_H, 2 * NA_WR - 1, 2 * NA_WC - 1), 0.1),
        'mla_q_norm_w': 1.0 + nrm(15, (DEPTH, Q_LORA), 0.02),
        'mla_kv_norm_w': 1.0 + nrm(16, (DEPTH, KV_LORA), 0.02),
        'mla_w_uq': nrm(17, (DEPTH, Q_LORA, MLA_H * (MLA_NOPE + MLA_ROPE)), Q_LORA ** -0.5),
        'mla_w_ukv': nrm(18, (DEPTH, KV_LORA, MLA_H * (MLA_NOPE + MLA_V)), KV_LORA ** -0.5),
        'ffn_w1': nrm(19, (N_DENSE, d_, D_FF), d_ ** -0.5),
        'ffn_w3': nrm(20, (N_DENSE, d_, D_FF), d_ ** -0.5),
        'ffn_w2': nrm(21, (N_DENSE, D_FF, d_), D_FF ** -0.5),
        'moe_router_w': nrm(22, (N_MOE, d_, N_EXPERTS), d_ ** -0.5),
        'moe_w1': nrm(23, (N_MOE, N_EXPERTS, d_, D_FF_EXPERT), d_ ** -0.5),
        'moe_w3': nrm(24, (N_MOE, N_EXPERTS, d_, D_FF_EXPERT), d_ ** -0.5),
        'moe_w2': nrm(25, (N_MOE, N_EXPERTS, D_FF_EXPERT, d_), D_FF_EXPERT ** -0.5),
        'final_norm_w': 1.0 + nrm(26, (d_,), 0.02),
    }


def reference(x, c, ctx, c_ctx, ada_w, ada_b, norm1_w, norm2_w, w_in, w_out,
              mlstm_conv_w, mlstm_ig_b, mlstm_fg_b, mlstm_norm_w, na_rpb,
              mla_q_norm_w, mla_kv_norm_w, mla_w_uq, mla_w_ukv,
              ffn_w1, ffn_w3, ffn_w2, moe_router_w, moe_w1, moe_w3, moe_w2, final_norm_w):
    b_, t_, d_ = x.shape
    n_ctx = ctx.shape[1]
    ang_r, ang_c = axial_rope_angles(t_)
    xc = ctx
    sc_lat = jax.nn.silu(c)
    sc_ctx = jax.nn.silu(c_ctx)
    for l in range(DEPTH):
        last = l == DEPTH - 1
        mod = (sc_lat @ ada_w[l] + ada_b[l]).reshape(b_, N_MOD, 1, d_)
        modc = (sc_ctx @ ada_w[l] + ada_b[l]).reshape(N_MOD, d_)
        p = modulate(rmsnorm(x, norm1_w[l]), mod[:, 0], mod[:, 1]) @ w_in[l]
        pc = modulate(rmsnorm(xc, norm1_w[l]), modc[0], modc[1]) @ w_in[l]
        ml, mlc = mlstm_mixer(p[..., :OFF_NA], pc[..., :OFF_NA], mlstm_conv_w[l], mlstm_ig_b[l], mlstm_fg_b[l],
                              mlstm_norm_w[l], not last)
        na, nac = na_mixer(p[..., OFF_NA:OFF_MLA], pc[..., OFF_NA:OFF_MLA], na_rpb[l], not last)
        mla, mlac = mla_mixer(p[..., OFF_MLA:], pc[..., OFF_MLA:], mla_q_norm_w[l], mla_kv_norm_w[l],
                              mla_w_uq[l], mla_w_ukv[l], ang_r, ang_c, not last)
        x = x + mod[:, 2] * (jnp.concatenate([ml, na, mla], axis=-1) @ w_out[l]).astype(x.dtype)
        if not last:
            xc = xc + modc[2] * (jnp.concatenate([mlc, nac, mlac], axis=-1) @ w_out[l]).astype(xc.dtype)
        h = modulate(rmsnorm(x, norm2_w[l]), mod[:, 3], mod[:, 4])
        j = l // 2
        if l % 2 == 0:
            x = x + mod[:, 5] * swiglu(h, ffn_w1[j], ffn_w3[j], ffn_w2[j])
            if not last:
                hc = modulate(rmsnorm(xc, norm2_w[l]), modc[3], modc[4])
                xc = xc + modc[5] * swiglu(hc, ffn_w1[j], ffn_w3[j], ffn_w2[j])
        else:
            if last:
                y = moe_ffn(h.reshape(b_ * t_, d_), moe_router_w[j], moe_w1[j], moe_w3[j], moe_w2[j])
            else:
                hc = modulate(rmsnorm(xc, norm2_w[l]), modc[3], modc[4])
                tok = jnp.concatenate([h.reshape(b_ * t_, d_), hc.reshape(b_ * n_ctx, d_)], axis=0)
                y_all = moe_ffn(tok, moe_router_w[j], moe_w1[j], moe_w3[j], moe_w2[j])
                y = y_all[:b_ * t_]
                xc = xc + modc[5] * y_all[b_ * t_:].reshape(b_, n_ctx, d_).astype(xc.dtype)
            x = x + mod[:, 5] * y.reshape(b_, t_, d_).astype(x.dtype)
    return rmsnorm(x, final_norm_w)
```

```python
import numpy as np
from contextlib import ExitStack
import concourse.bass as bass
import concourse.mybir as mybir
from concourse.bass_utils import run_bass_kernel_spmd

F32 = mybir.dt.float32
BF16 = mybir.dt.bfloat16
U8 = mybir.dt.uint8
AF = mybir.ActivationFunctionType
ALU = mybir.AluOpType
AX = mybir.AxisListType
DTSIZE = {F32: 4, BF16: 2, mybir.dt.int32: 4}
COMPUTE = ('pe', 'act', 'dve', 'pool')
QUEUES = ('sp', 'act', 'pool')
NDSEM = 12
ARENA = 207 * 1024

NT = 4352
NTILE = 34
DM = 1024
NIN = 2992
DFF = 2816
EPS = 1e-6
DEBUG = False
NBLK = 24
NSLOT = NBLK * 512
I32 = mybir.dt.int32


class Op:
    __slots__ = ('eng', 'fn', 'deps', 'isdma', 'needs_inc', 'count', 'semi', 'semval', 'idx', 'q')


class V:
    __slots__ = ('buf', 'ap')

    def __init__(self, buf, ap):
        self.buf = buf
        self.ap = ap

    def __getitem__(self, k):
        return V(self.buf, self.ap[k])

    def rearrange(self, pattern_, **kw):
        return V(self.buf, self.ap.rearrange(pattern_, **kw))

    def unsqueeze(self, a):
        return V(self.buf, self.ap.unsqueeze(a))

    def to_broadcast(self, shp):
        return V(self.buf, self.ap.to_broadcast(list(shp)))

    def bitcast(self, dt):
        return V(self.buf, self.ap.bitcast(dt))

    def partition_broadcast(self, n):
        return V(self.buf, self.ap.partition_broadcast(n))


class Buf:
    def __init__(self, ap, name, off=0, size=0):
        self.ap = ap
        self.name = name
        self.w = None
        self.rc = {}
        self.rd = {}
        self.inh = []
        self.multiw = False
        self.wl = {}
        self.war = []
        self.off = off
        self.size = size

    def __getitem__(self, k):
        return V(self, self.ap[k])

    def v(self):
        return V(self, self.ap)

    def rearrange(self, pattern_, **kw):
        return V(self, self.ap.rearrange(pattern_, **kw))


class Sched:
    def __init__(self, nc):
        self.nc = nc
        self.ops = {e: [] for e in ('pe', 'act', 'dve', 'pool', 'sp')}
        self.nops = 0
        self.dsem_use = {q: [0] * NDSEM for q in QUEUES}
        self.dsem_rr = {q: 0 for q in QUEUES}
        self.arena = nc.alloc_sbuf_tensor("arena", [128, ARENA], U8)
        self.live = []
        self.dead = []
        self.psum = []
        for i in range(8):
            h = nc.alloc_psum_tensor(f"psb{i}", [128, 512], F32)
            self.psum.append(Buf(h[:, :], f"psb{i}"))

    def alloc(self, name, shape, dtype):
        n = int(np.prod(shape[1:])) * DTSIZE[dtype]
        n_al = (n + 63) // 64 * 64
        self.live.sort(key=lambda b: b.off)
        off = 0
        for b in self.live:
            if b.off - off >= n_al:
                break
            off = max(off, b.off + b.size)
        if off + n_al > ARENA:
            raise RuntimeError(f"SBUF arena OOM allocating {name} {shape}: need {n_al} at {off}; live="
                               + str([(b.name, b.off, b.size) for b in self.live]))
        ap = self.arena[0:shape[0], off:off + n].bitcast(dtype)
        if len(shape) > 2:
            names = ' '.join(f'd{i}' for i in range(1, len(shape)))
            kw = {f'd{i}': shape[i] for i in range(1, len(shape))}
            ap = ap.rearrange(f'p ({names}) -> p {names}', **kw)
        buf = Buf(ap, name, off, n_al)
        keep = []
        for d in self.dead:
            if d.off < off + n_al and off < d.off + d.size:
                acc = list(d.inh)
                if d.w is not None:
                    acc.append(d.w)
                acc.extend(d.rc.values())
                acc.extend(d.rd.values())
                buf.inh.extend(acc)
                if not (off <= d.off and d.off + d.size <= off + n_al):
                    keep.append(d)
            else:
                keep.append(d)
        self.dead = keep
        best = {}
        for o in buf.inh:
            key = (o.q, o.semi) if o.isdma else o.eng
            if key not in best or best[key].idx < o.idx:
                best[key] = o
        buf.inh = list(best.values())
        self.live.append(buf)
        return buf

    def free(self, *bufs):
        for b in bufs:
            self.live.remove(b)
            self.dead.append(b)

    def dram(self, name, shape, dtype, kind="Internal"):
        h = self.nc.dram_tensor(name, list(shape), dtype, kind=kind)
        return Buf(h.ap(), name)

    def _mk(self, eng, fn, reads, writes, isdma, q=None):
        op = Op()
        op.eng = eng
        op.fn = fn
        op.isdma = isdma
        op.needs_inc = False
        op.count = 0
        op.idx = self.nops
        op.q = q
        op.semi = -1
        self.nops += 1
        if isdma:
            i = self.dsem_rr[q]
            self.dsem_rr[q] = (i + 1) % NDSEM
            self.dsem_use[q][i] += 1
            op.semi = i
            op.semval = 16 * self.dsem_use[q][i]
        deps = []
        for b in reads:
            if b.w is not None:
                deps.append((b.w, True))
            for o in b.wl.values():
                deps.append((o, True))
        for b in writes:
            mw = b.multiw and isdma
            if mw:
                if b.rc or b.rd:
                    b.war = list(b.rc.values()) + list(b.rd.values())
                    b.wl = {}
                    b.rc = {}
                    b.rd = {}
                for o in b.war:
                    deps.append((o, False))
                if b.w is not None:
                    deps.append((b.w, False))
            else:
                if b.w is not None:
                    deps.append((b.w, False))
                for o in b.wl.values():
                    deps.append((o, False))
                for o in b.rc.values():
                    deps.append((o, False))
                for o in b.rd.values():
                    deps.append((o, False))
            for o in b.inh:
                deps.append((o, False))
        fin = []
        seen = set()
        for p, raw in deps:
            if p is op or id(p) in seen:
                continue
            if (not p.isdma) and (not isdma) and p.eng == eng and eng == 'pe':
                continue
            seen.add(id(p))
            fin.append(p)
            p.needs_inc = True
        op.deps = fin
        for b in reads:
            if isdma:
                b.rd[(q, op.semi)] = op
            else:
                b.rc[eng] = op
        for b in writes:
            if b.multiw and isdma:
                b.wl[(q, op.semi)] = op
                b.inh = []
            else:
                b.w = op
                b.wl = {}
                b.war = []
                b.rc = {}
                b.rd = {}
                b.inh = []
        self.ops[eng].append(op)
        return op

    def op(self, eng, fn, reads=(), writes=()):
        return self._mk(eng, fn, reads, writes, False)

    def dma(self, q, out, in_, **kw):
        oa, ia = out.ap, in_.ap

        def fn(e):
            return e.dma_start(out=oa, in_=ia, **kw)
        return self._mk(q, fn, [in_.buf], [out.buf], True, q=q)

    def emit(self):
        nc = self.nc
        with ExitStack() as es:
            self.csems = {e: es.enter_context(nc.semaphore(f"c_{e}")) for e in COMPUTE}
            self.dsems = {q: [es.enter_context(nc.semaphore(f"d_{q}{i}")) for i in range(NDSEM)] for q in QUEUES}
            for e in COMPUTE:
                cnt = 0
                for op in self.ops[e]:
                    if not op.isdma and op.needs_inc:
                        cnt += 1
                    op.count = cnt
            block = es.enter_context(nc.Block())

            @block.tensor
            def _(eng):
                self._emit_engine('pe', eng)

            @block.scalar
            def _(eng):
                self._emit_engine('act', eng)

            @block.vector
            def _(eng):
                self._emit_engine('dve', eng)

            @block.gpsimd
            def _(eng):
                self._emit_engine('pool', eng)

            @block.sync
            def _(eng):
                self._emit_engine('sp', eng, final=True)

    def _emit_engine(self, e, eng, final=False):
        waited = {}

        def wait(key, sem, val):
            if waited.get(key, 0) >= val:
                return
            eng.wait_ge(sem, val)
            waited[key] = val

        for op in self.ops[e]:
            for p in op.deps:
                if p.isdma:
                    wait(('d', p.q, p.semi), self.dsems[p.q][p.semi], p.semval)
                else:
                    wait(('c', p.eng), self.csems[p.eng], p.count)
            if op.isdma:
                sem = self.dsems[op.q][op.semi]
                if op.semval > 16:
                    wait(('d', op.q, op.semi), sem, op.semval - 16)
                ins = op.fn(eng)
                ins.then_inc(sem, 16)
            else:
                ins = op.fn(eng)
                if op.needs_inc:
                    ins.then_inc(self.csems[e], 1)
        if final:
            for q in QUEUES:
                for i in range(NDSEM):
                    v = 16 * self.dsem_use[q][i]
                    if v > 0:
                        wait(('d', q, i), self.dsems[q][i], v)
            for ce in COMPUTE:
                last = None
                for op in self.ops[ce]:
                    if not op.isdma and op.needs_inc:
                        last = op
                if last is not None:
                    wait(('c', ce), self.csems[ce], last.count)


def _bufs(*xs):
    out = []
    for x in xs:
        if isinstance(x, V) and x.buf not in out:
            out.append(x.buf)
    return out


def _a(x):
    return x.ap if isinstance(x, V) else x


class K:
    def __init__(self, nc):
        self.nc = nc
        self.S = Sched(nc)
        self.defctx = {'banks': [0, 1, 2, 3], 'accs': [4, 5, 6, 7], 'i': 0, 'j': 0}
        self.cur = self.defctx

    def ps(self):
        c = self.cur
        b = self.S.psum[c['banks'][c['i'] % len(c['banks'])]]
        c['i'] += 1
        return b

    def psacc(self):
        c = self.cur
        b = self.S.psum[c['accs'][c['j'] % len(c['accs'])]]
        c['j'] += 1
        return b

    def interleave(self, gens):
        live = list(gens)
        while live:
            for item in list(live):
                g, ctx, wgt = item
                self.cur = ctx
                for _ in range(wgt):
                    try:
                        next(g)
                    except StopIteration:
                        live.remove(item)
                        break
        self.cur = self.defctx

    def mm(self, out, lhsT, rhs, start=True, stop=True):
        oa, la, ra = out.ap, lhsT.ap, rhs.ap
        self.S.op('pe', lambda e: e.matmul(oa, lhsT=la, rhs=ra, start=start, stop=stop),
                  reads=_bufs(lhsT, rhs), writes=_bufs(out))

    def tr(self, out, in_, ident):
        oa, ia, da = out.ap, in_.ap, ident.ap
        self.S.op('pe', lambda e: e.transpose(oa, ia, da), reads=_bufs(in_, ident), writes=_bufs(out))

    def act(self, out, in_, func, bias=None, scale=None, accum=None):
        kw = {}
        if bias is not None:
            kw['bias'] = _a(bias)
        if scale is not None:
            kw['scale'] = _a(scale)
        if accum is not None:
            kw['accum_out'] = accum.ap
        oa, ia = out.ap, in_.ap
        self.S.op('act', lambda e: e.activation(out=oa, in_=ia, func=func, **kw),
                  reads=_bufs(in_, bias, scale), writes=_bufs(out, accum))

    def tt(self, out, in0, in1, op, eng='dve'):
        oa, a0, a1 = out.ap, in0.ap, in1.ap
        self.S.op(eng, lambda e: e.tensor_tensor(out=oa, in0=a0, in1=a1, op=op), reads=_bufs(in0, in1), writes=_bufs(out))

    def ts(self, out, in0, s1, op0, s2=None, op1=None, eng='dve'):
        oa, a0 = out.ap, in0.ap
        x1, x2 = _a(s1), _a(s2)
        if op1 is None:
            self.S.op(eng, lambda e: e.tensor_scalar(out=oa, in0=a0, scalar1=x1, scalar2=None, op0=op0),
                      reads=_bufs(in0, s1), writes=_bufs(out))
        else:
            self.S.op(eng, lambda e: e.tensor_scalar(out=oa, in0=a0, scalar1=x1, scalar2=x2, op0=op0, op1=op1),
                      reads=_bufs(in0, s1, s2), writes=_bufs(out))

    def stt(self, out, in0, scalar, in1, op0, op1, eng='dve'):
        oa, a0, a1, sc = out.ap, in0.ap, in1.ap, _a(scalar)
        self.S.op(eng, lambda e: e.scalar_tensor_tensor(out=oa, in0=a0, scalar=sc, in1=a1, op0=op0, op1=op1),
                  reads=_bufs(in0, in1, scalar), writes=_bufs(out))

    def cp(self, out, in_, eng='dve'):
        oa, ia = out.ap, in_.ap
        if eng == 'act':
            self.S.op('act', lambda e: e.copy(out=oa, in_=ia), reads=_bufs(in_), writes=_bufs(out))
        else:
            self.S.op(eng, lambda e: e.tensor_copy(out=oa, in_=ia), reads=_bufs(in_), writes=_bufs(out))

    def memset(self, out, val, eng='dve'):
        oa = out.ap
        self.S.op(eng, lambda e: e.memset(oa, val), writes=_bufs(out))

    def red(self, out, in_, op, negate=False):
        oa, ia = out.ap, in_.ap
        self.S.op('dve', lambda e: e.tensor_reduce(out=oa, in_=ia, axis=AX.X, op=op, negate=negate),
                  reads=_bufs(in_), writes=_bufs(out))

    def recip(self, out, in_):
        oa, ia = out.ap, in_.ap
        self.S.op('dve', lambda e: e.reciprocal(out=oa, in_=ia), reads=_bufs(in_), writes=_bufs(out))

    def dma(self, q, out, in_, **kw):
        self.S.dma(q, out, in_, **kw)

    def dma_ind(self, out, in_, idx, scatter, bound):
        oa, ia, xa = out.ap, in_.ap, idx.ap

        def fn(e):
            off = bass.IndirectOffsetOnAxis(ap=xa, axis=0)
            if scatter:
                return e.indirect_dma_start(out=oa, out_offset=off, in_=ia, in_offset=None, bounds_check=None, oob_is_err=False)
            return e.indirect_dma_start(out=oa, out_offset=None, in_=ia, in_offset=off, bounds_check=None, oob_is_err=False)
        self.S._mk('pool', fn, [in_.buf, idx.buf], [out.buf], True, q='pool')

    def bcast(self, name, row_view, n):
        b = self.S.alloc(name, [128, n], F32)
        self.dma('sp', b[:, :], row_view.partition_broadcast(128))
        return b

    def rstd(self, ss, n, name):
        self.act(ss, ss, AF.Sqrt, bias=self.eps[0:ss.ap.shape[0], 0:1], scale=1.0 / n)
        self.recip(ss, ss)

    def build(self):
        S = self.S
        d = {}
        self.d = d
        import os
        self.flags = set(os.environ.get('KFLAGS', '').split(','))
        om = 'only_moe' in self.flags

        def inp(name, shape):
            d[name] = S.dram(name, shape, F32, kind="ExternalInput")
        inp('xin', [NT, DM])
        inp('cc', [128, 16])
        inp('ada_w', [2, DM, 6 * DM])
        inp('ada_b', [2, 6 * DM])
        inp('norm1_w', [2, DM])
        inp('norm2_w', [2, DM])
        inp('w_in', [2, DM, NIN])
        inp('w_out', [2, DM, DM])
        inp('convT', [2, 512, 3])
        inp('ig_b', [2, 8])
        inp('fg_b', [2, 8])
        inp('ml_nw', [2, 256])
        inp('na_bm', [2, 6, 20, 128, 512])
        inp('q_nw', [2, 512])
        inp('kv_nw', [2, 256])
        inp('w_uq', [2, 512, 576])
        inp('w_ukv', [2, 256, 768])
        inp('ffn_w1', [DM, DFF])
        inp('ffn_w3', [DM, DFF])
        inp('ffn_w2', [DFF, DM])
        inp('router', [DM, 8])
        inp('moe_w1', [8 * 11 * 128, 2048])
        inp('moe_w3', [8 * 11 * 128, 2048])
        inp('moe_w2', [8 * 11 * 128, 2048])
        inp('c_th', [128, 16])
        inp('c_j512', [128, NBLK])
        inp('c_gp', [128, 11])
        inp('fin_w', [DM])
        inp('ident', [128, 128])
        inp('tri0', [128, 128])
        inp('tri1', [128, 128])
        inp('rope_cs', [4096, 16])
        inp('rope_sn', [4096, 16])
        d['out'] = S.dram('out', [4096, DM], F32, kind="ExternalOutput")
        kind = "ExternalOutput" if DEBUG else "Internal"
        d['mod'] = S.dram('mod', [2, 2, 6 * DM], F32, kind=('ExternalInput' if om else kind))
        d['pT_ml'] = S.dram('pT_ml', [512, NT], F32, kind=kind)
        d['pT_na'] = S.dram('pT_na', [768, NT], BF16, kind=kind)
        d['p_tok'] = S.dram('p_tok', [NT, 1712], F32, kind=kind)
        d['cat'] = S.dram('cat', [DM, NT], BF16, kind=kind)
        d['x1'] = S.dram('x1', [NT, DM], F32, kind=('ExternalInput' if om else kind))
        d['x2'] = S.dram('x2', [NT, DM], F32, kind=kind)
        d['h2T'] = S.dram('h2T', [DM, NT], BF16, kind=kind)
        d['gates'] = S.dram('gatesd', [NT, 8], F32, kind=('ExternalInput' if om else kind))
        d['sel1'] = S.dram('sel1d', [NT, 8], F32, kind=('ExternalInput' if om else kind))
        d['h2'] = S.dram('h2d', [NT, DM], BF16, kind=('ExternalInput' if om else kind))
        d['hs'] = S.dram('hsd', [NSLOT, DM], BF16, kind=kind)
        d['yb'] = S.dram('ybd', [NSLOT, DM], F32, kind=kind)

        for nm in ('pT_ml', 'pT_na', 'p_tok', 'cat', 'x1', 'x2', 'h2T', 'h2', 'gates', 'sel1', 'out', 'yb'):
            d[nm].multiw = True
        self.identb = S.alloc('identb', [128, 128], BF16)
        self.dma('pool', self.identb[:, :], d['ident'][:, :])
        self.identf = S.alloc('identf', [128, 128], F32)
        self.dma('sp', self.identf[:, :], d['ident'][:, :])
        self.tri0 = S.alloc('tri0', [128, 128], F32)
        self.dma('sp', self.tri0[:, :], d['tri0'][:, :])
        self.tri1 = S.alloc('tri1', [128, 128], F32)
        self.dma('sp', self.tri1[:, :], d['tri1'][:, :])
        self.onesf = S.alloc('onesf', [128, 128], F32)
        self.memset(self.onesf[:, :], 1.0)
        self.eps = S.alloc('eps', [128, 1], F32)
        self.memset(self.eps[:, :], EPS)

        stop = getattr(self, 'stop_after', None)
        xs = d['xin']
        if 'only_moe' in self.flags:
            self.stage_moe(1)
            S.emit()
            return
        for l in range(2):
            last = l == 1
            if 'only_mla' not in self.flags:
                self.adaln(l)
                if stop == ('adaln', l):
                    break
                self.stage_in(l, xs)
                if stop == ('in', l):
                    break
                if 'inter' not in self.flags:
                    for _ in self.stage_mlstm(l, last):
                        pass
                    if stop == ('ml', l):
                        break
                    for _ in self.stage_na(l, last):
                        pass
                else:
                    cm = {'banks': [3], 'accs': [4, 5], 'i': 0, 'j': 0}
                    cn = {'banks': [0, 1, 2], 'accs': [6, 7], 'i': 0, 'j': 0, 'depth': 2}
                    self.interleave([(self.stage_mlstm(l, last), cm, 1), (self.stage_na(l, last), cn, 4)])
            if stop == ('na', l):
                break
            self.stage_mla(l, last)
            if stop in (('mla', l), ('mla1', l)):
                break
            self.stage_out(l, xs, last)
            if stop == ('out', l):
                break
            if last:
                self.stage_moe(l)
            else:
                self.stage_ffn(l, last)
            if stop == ('ffn', l):
                break
            xs = d['x2']
        S.emit()

    def adaln(self, l):
        S, d = self.S, self.d
        cc = S.alloc('cc', [128, 16], F32)
        self.dma('sp', cc[:, :], d['cc'][:, :])
        scT = S.alloc('scT', [128, 16], BF16)
        self.act(scT[:, :], cc[:, :], AF.Silu)
        sc3 = scT[:, :].rearrange('p (k r) -> p k r', r=2)
        adab = S.alloc('adab', [2, 6 * DM], F32)
        self.dma('sp', adab[:, :], d['ada_b'][l, :].partition_broadcast(2))
        modrow = S.alloc('modrow', [2, 6 * DM], F32)
        wts = [S.alloc(f'adaw{i}', [128, 8, 512], BF16) for i in range(2)]
        wsrc = d['ada_w'][l].rearrange('(k p) n -> p k n', p=128)
        for j in range(12):
            wt = wts[j % 2]
            self.dma('pool', wt[:, :, :], wsrc[:, :, j * 512:(j + 1) * 512])
            ps = self.ps()
            for k in range(8):
                self.mm(ps[0:2, 0:512], sc3[:, k, :], wt[:, k, :], start=(k == 0), stop=(k == 7))
            self.tt(modrow[:, j * 512:(j + 1) * 512], ps[0:2, 0:512], adab[:, j * 512:(j + 1) * 512], ALU.add)
        self.dma('pool', d['mod'][l], modrow[:, :])
        S.free(cc, scT, adab, modrow, *wts)

    def mod_ab(self, l, which, nw_name, tag):
        S, d = self.S, self.d
        nwb = self.bcast(f'nwb{tag}', d[nw_name][l, :], DM)
        res = []
        for r in range(2):
            sc = self.bcast(f'sc{tag}{r}', d['mod'][l, r, (3 * which + 1) * DM:(3 * which + 2) * DM], DM)
            self.stt(sc[:, :], sc[:, :], 1.0, nwb[:, :], ALU.add, ALU.mult)
            sh = self.bcast(f'sh{tag}{r}', d['mod'][l, r, (3 * which) * DM:(3 * which + 1) * DM], DM)
            res.append((sc, sh))
        S.free(nwb)
        return res

    def norm_mod(self, xt, A, B, hb, tmp, ss):
        self.act(tmp[:, :], xt[:, :], AF.Square, accum=ss[:, 0:1])
        self.rstd(ss[:, 0:1], DM, 'r')
        self.stt(tmp[:, :], xt[:, :], ss[:, 0:1], A[:, :], ALU.mult, ALU.mult)
        self.tt(hb[:, :], tmp[:, :], B[:, :], ALU.add)

    def transpose8(self, hb, dst3):
        ps = self.ps()
        psb = ps[:, :].bitcast(BF16)
        for k in range(8):
            self.tr(psb[:, k * 128:(k + 1) * 128], hb[:, k * 128:(k + 1) * 128], self.identb[:, :])
        self.cp(dst3, psb[:, :].rearrange('p (k t) -> p k t', k=8), eng='act')

    def stage_in(self, l, xs):
        S, d = self.S, self.d
        w = S.alloc('w_in', [128, 8, NIN], BF16)
        wsrc = d['w_in'][l].rearrange('(k p) n -> p k n', p=128)
        for k in range(8):
            self.dma('pool', w[:, k, :], wsrc[:, k, :])
        ab = self.mod_ab(l, 0, 'norm1_w', 'a')
        hTs = [S.alloc(f'hT{i}', [128, 8, 512], BF16) for i in range(2)]
        xts = [S.alloc(f'xt{i}', [128, DM], F32) for i in range(2)]
        tmps = [S.alloc(f'tmp{i}', [128, DM], F32) for i in range(2)]
        hbs = [S.alloc(f'hb{i}', [128, DM], BF16) for i in range(2)]
        sss = [S.alloc(f'ss{i}', [128, 1], F32) for i in range(2)]
        fms = [S.alloc(f'fm{i}', [128, 512], F32) for i in range(2)]
        fnb = [S.alloc(f'fnb{i}', [128, 512], BF16) for i in range(2)]
        pts = [S.alloc(f'pt{i}', [128, 1712], F32) for i in range(2)]
        groups = [(0, 2)] + [(2 + 4 * i, 4) for i in range(8)]
        fcols = [0, 128, 256, 384] + [1040 + 128 * i for i in range(6)]
        tcols = [(512, 1024, 0), (1024, 1040, 512), (1808, 2320, 528), (2320, 2832, 1040), (2832, 2992, 1552)]
        ti = 0
        for gi, (t0, nt) in enumerate(groups):
            hT = hTs[gi % 2]
            G = nt * 128
            A, B = ab[1] if gi == 0 else ab[0]
            for j in range(nt):
                n = t0 + j
                xt = xts[ti % 2]
                hb = hbs[ti % 2]
                ss = sss[ti % 2]
                tmp = tmps[ti % 2]
                ti += 1
                self.dma('sp', xt[:, :], xs[n * 128:(n + 1) * 128, :])
                self.norm_mod(xt, A, B, hb, tmp, ss)
                self.transpose8(hb, hT[:, :, j * 128:(j + 1) * 128])
            for ci, c0 in enumerate(fcols):
                ps = self.ps()
                for k in range(8):
                    self.mm(ps[:, 0:G], w[:, k, c0:c0 + 128], hT[:, k, 0:G], start=(k == 0), stop=(k == 7))
                if ci < 4:
                    fm = fms[ci % 2]
                    self.cp(fm[:, 0:G], ps[:, 0:G], eng='act')
                    self.dma('pool', d['pT_ml'][ci * 128:(ci + 1) * 128, t0 * 128:t0 * 128 + G], fm[:, 0:G])
                else:
                    fb = fnb[ci % 2]
                    if ci < 7:
                        self.ts(fb[:, 0:G], ps[:, 0:G], 0.125, ALU.mult)
                    else:
                        self.cp(fb[:, 0:G], ps[:, 0:G], eng='act')
                    self.dma('pool', d['pT_na'][(ci - 4) * 128:(ci - 3) * 128, t0 * 128:t0 * 128 + G], fb[:, 0:G])
            for j in range(nt):
                n = t0 + j
                pt = pts[n % 2]
                for ii, (c0, c1, o0) in enumerate(tcols):
                    ps = self.ps()
                    for k in range(8):
                        self.mm(ps[:, 0:c1 - c0], hT[:, k, j * 128:(j + 1) * 128], w[:, k, c0:c1], start=(k == 0), stop=(k == 7))
                    if ii % 2 == 0:
                        self.cp(pt[:, o0:o0 + c1 - c0], ps[:, 0:c1 - c0], eng='dve')
                    else:
                        self.cp(pt[:, o0:o0 + c1 - c0], ps[:, 0:c1 - c0], eng='act')
                self.dma('pool', d['p_tok'][n * 128:(n + 1) * 128, :], pt[:, :])
        S.free(w, *tmps, *hTs, *xts, *hbs, *sss, *fms, *fnb, *pts)
        for a, b in ab:
            S.free(a, b)

    def stage_mlstm(self, l, last):
        S, d = self.S, self.d

        def tcol(n):
            return 1 + n * 128 if n < 2 else 2 + n * 128
        gt = S.alloc('gt', [128, NTILE, 16], F32)
        self.dma('sp', gt[:, :, :], d['p_tok'][:, 512:528].rearrange('(n p) c -> p n c', p=128))
        igb = self.bcast('igb', d['ig_b'][l, :], 8)
        fgb = self.bcast('fgb', d['fg_b'][l, :], 8)
        LI = S.alloc('LI', [128, NTILE, 8], F32)
        LF = S.alloc('LF', [128, NTILE, 8], F32)
        self.tt(LI[:, :, :], gt[:, :, 0:8], igb[:, :].unsqueeze(1).to_broadcast([128, NTILE, 8]), ALU.add)
        self.tt(LF[:, :, :], gt[:, :, 8:16], fgb[:, :].unsqueeze(1).to_broadcast([128, NTILE, 8]), ALU.add)
        self.act(LF[:, :, :], LF[:, :, :], AF.Exp, scale=-1.0)
        self.act(LF[:, :, :], LF[:, :, :], AF.Ln, bias=self.onesf[:, 0:1], scale=1.0)
        self.ts(LF[:, :, :], LF[:, :, :], -1.0, ALU.mult)
        LF2 = LF[:, :, :].rearrange('p n c -> p (n c)')
        NC = NTILE * 8
        Bc = S.alloc('Bc', [128, NTILE, 2, 4], F32)
        EB = S.alloc('EB', [128, NTILE, 2, 4], F32)
        ES = S.alloc('ES', [128, NTILE, 2, 4], F32)
        EG = S.alloc('EG', [128, NTILE, 2, 4], F32)
        psA = self.ps()
        self.mm(psA[:, 0:NC], self.tri0[:, :], LF2)
        self.cp(Bc[:, :, 0, :], psA[:, 0:NC].rearrange('p (n d h) -> p n d h', d=2, h=4)[:, :, 0, :])
        psB = self.ps()
        self.mm(psB[:, 0:NC], self.tri1[:, :], LF2)
        self.cp(Bc[:, :, 1, :], psB[:, 0:NC].rearrange('p (n d h) -> p n d h', d=2, h=4)[:, :, 1, :])
        psC = self.ps()
        self.mm(psC[:, 0:NC], self.onesf[:, :], LF2)
        self.act(EG[:, :, :, :].rearrange('p n d h -> p (n d h)'), psC[:, 0:NC], AF.Exp)
        Bc2 = Bc[:, :, :, :].rearrange('p n d h -> p (n d h)')
        self.act(EB[:, :, :, :].rearrange('p n d h -> p (n d h)'), Bc2, AF.Exp)
        ES2 = ES[:, :, :, :].rearrange('p n d h -> p (n d h)')
        self.tt(ES2, LI[:, :, :].rearrange('p n c -> p (n c)'), Bc2, ALU.subtract)
        self.act(ES2, ES2, AF.Exp)
        yield
        S.free(gt, igb, fgb, LI, LF, Bc)

        convw = S.alloc('convw', [128, 4, 3], F32)
        self.dma('sp', convw[:, :, :], d['convT'][l].rearrange('(c p) j -> p c j', p=128))
        W = NT + 3
        qkT = S.alloc('qkT', [128, 4, W], BF16)
        x32 = S.alloc('x32', [128, W], F32)
        acc = S.alloc('acc', [128, W], F32)
        for c in (0, 257, W - 1):
            self.memset(x32[:, c:c + 1], 0.0)
        for fc in range(4):
            self.dma('sp', x32[:, 1:257], d['pT_ml'][fc * 128:(fc + 1) * 128, 0:256])
            self.dma('sp', x32[:, 258:W - 1], d['pT_ml'][fc * 128:(fc + 1) * 128, 256:NT])
            a = acc[:, 1:W - 1]
            self.ts(a, x32[:, 0:W - 2], convw[:, fc, 0:1], ALU.mult)
            self.stt(a, x32[:, 1:W - 1], convw[:, fc, 1:2], a, ALU.mult, ALU.add)
            self.stt(a, x32[:, 2:W], convw[:, fc, 2:3], a, ALU.mult, ALU.add)
            if fc < 2:
                self.act(qkT[:, fc, 1:W - 1], a, AF.Silu)
            else:
                self.act(a, a, AF.Silu)
                self.ts(qkT[:, fc, 1:W - 1], a, 0.125, ALU.mult)
            yield
        S.free(x32, acc, convw)

        ktok = S.alloc('ktok', [128, NTILE, 256], BF16)
        for n in range(NTILE):
            ps = self.ps()
            psb = ps[:, :].bitcast(BF16)
            for kc in range(2):
                self.tr(psb[:, kc * 128:(kc + 1) * 128], qkT[:, 2 + kc, tcol(n):tcol(n) + 128], self.identb[:, :])
            self.cp(ktok[:, n, :], psb[:, 0:256], eng=('act' if n % 2 else 'dve'))
            if n % 4 == 3:
                yield

        Vp = S.alloc('Vp', [128, NTILE, 2, 4, 65], BF16)
        Xb = S.alloc('Xb', [128, NTILE, 2, 4, 65], BF16)
        v32 = S.alloc('v32', [128, 9, 256], F32)
        for t0 in range(0, NTILE, 9):
            tn = min(9, NTILE - t0)
            self.dma('sp', v32[:, 0:tn, :], d['p_tok'][t0 * 128:(t0 + tn) * 128, 0:256].rearrange('(n p) c -> p n c', p=128))
            for dd in range(2):
                self.tt(Vp[:, t0:t0 + tn, dd, :, 0:64], v32[:, 0:tn, :].rearrange('p n (h e) -> p n h e', h=4),
                        ES[:, t0:t0 + tn, dd, :].unsqueeze(3).to_broadcast([128, tn, 4, 64]), ALU.mult)
            yield
        for dd in range(2):
            self.cp(Vp[:, :, dd, :, 64:65], ES[:, :, dd, :].unsqueeze(3))
        S.free(v32, ES)

        Xf = S.alloc('Xf', [128, 2, 4, 65], F32)
        self.memset(Xf[:, :, :, :].rearrange('p d h e -> p (d h e)'), 0.0)
        order = [list(range(NTILE)), [1, 0] + list(range(NTILE - 1, 1, -1))]
        for step in range(NTILE):
            for dd in range(2):
                n = order[dd][step]
                self.cp(Xb[:, n, dd, :, :], Xf[:, dd, :, :], eng='act')
                ps = self.ps()
                psv = ps[:, 0:260].rearrange('p (h e) -> p h e', h=4)
                for h in range(4):
                    self.mm(psv[:, h, :], ktok[:, n, (h // 2) * 128:(h // 2) * 128 + 128], Vp[:, n, dd, h, :])
                self.tt(Xf[:, dd, :, :], Xf[:, dd, :, :], psv, ALU.add)
                self.tt(Xf[:, dd, :, :], Xf[:, dd, :, :], EG[:, n, dd, :].unsqueeze(2).to_broadcast([128, 4, 65]), ALU.mult)
            yield
        S.free(Xf, EG, ktok)

        mlnw = self.bcast('mlnw', d['ml_nw'][l, :], 256)
        Sms = [S.alloc(f'Sm{i}', [128, 128], BF16) for i in range(4)]
        Hd = [S.alloc(f'Hd{i}', [128, 4, 65], F32) for i in range(2)]
        den = S.alloc('den', [128, 2, 4], F32)
        hs = S.alloc('hs', [128, 4, 64], F32)
        ht = S.alloc('ht', [128, 4, 64], F32)
        ssq = S.alloc('ssq', [128, 4], F32)
        o32 = [S.alloc(f'o32{i}', [128, 256], F32) for i in range(2)]
        res = [S.alloc(f'mres{i}', [128, 256], BF16) for i in range(2)]
        resT = [S.alloc(f'mresT{i}', [128, 2, 128], BF16) for i in range(2)]
        smi = 0
        for n in range(2 if last else 0, NTILE):
            psO = [self.psacc(), self.psacc()]
            psOv = [p[:, 0:260].rearrange('p (h e) -> p h e', h=4) for p in psO]
            c0 = tcol(n)
            for h in range(4):
                pb = (h % 2) * 64
                qT = qkT[pb:pb + 64, h // 2, c0:c0 + 128]
                kT = qkT[pb:pb + 64, 2 + h // 2, c0:c0 + 128]
                psS = self.ps()
                self.mm(psS[:, 0:128], kT, qT)
                for dd in range(2):
                    Sm = Sms[smi % 4]
                    smi += 1
                    self.tt(Sm[:, :], psS[:, 0:128], (self.tri0 if dd == 0 else self.tri1)[:, :], ALU.mult)
                    self.mm(psOv[dd][:, h, :], Sm[:, :], Vp[:, n, dd, h, :], start=True, stop=False)
                    self.mm(psOv[dd][:, h, :], qT, Xb[pb:pb + 64, n, dd, h, :], start=False, stop=True)
                yield
            for dd in range(2):
                self.tt(Hd[dd][:, :, :], psOv[dd], EB[:, n, dd, :].unsqueeze(2).to_broadcast([128, 4, 65]), ALU.mult)
                self.stt(den[:, dd, :], Hd[dd][:, :, 64], -1.0, Hd[dd][:, :, 64], ALU.mult, ALU.max)
            self.ts(den[:, :, :], den[:, :, :], 1.0, ALU.max)
            self.recip(den[:, :, :], den[:, :, :])
            self.tt(hs[:, :, :], Hd[0][:, :, 0:64], den[:, 0, :].unsqueeze(2).to_broadcast([128, 4, 64]), ALU.mult)
            self.tt(ht[:, :, :], Hd[1][:, :, 0:64], den[:, 1, :].unsqueeze(2).to_broadcast([128, 4, 64]), ALU.mult)
            self.tt(hs[:, :, :], hs[:, :, :], ht[:, :, :], ALU.add)
            self.tt(ht[:, :, :], hs[:, :, :], hs[:, :, :], ALU.mult)
            self.red(ssq[:, :], ht[:, :, :], ALU.add)
            self.act(ssq[:, :], ssq[:, :], AF.Sqrt, bias=self.eps[:, 0:1], scale=1.0 / 64)
            self.recip(ssq[:, :], ssq[:, :])
            self.tt(hs[:, :, :], hs[:, :, :], ssq[:, :].unsqueeze(2).to_broadcast([128, 4, 64]), ALU.mult)
            hs2 = hs[:, :, :].rearrange('p h e -> p (h e)')
            self.tt(hs2, hs2, mlnw[:, :], ALU.mult)
            o = o32[n % 2]
            self.dma('sp', o[:, :], d['p_tok'][n * 128:(n + 1) * 128, 256:512])
            self.act(o[:, :], o[:, :], AF.Sigmoid)
            r = res[n % 2]
            self.tt(r[:, :], hs2, o[:, :], ALU.mult)
            pst = self.ps()
            pstb = pst[:, :].bitcast(BF16)
            for c in range(2):
                self.tr(pstb[:, c * 128:(c + 1) * 128], r[:, c * 128:(c + 1) * 128], self.identb[:, :])
            rT = resT[n % 2]
            self.cp(rT[:, :, :], pstb[:, 0:256].rearrange('p (c t) -> p c t', c=2), eng='act')
            self.dma('pool', d['cat'][0:256, n * 128:(n + 1) * 128].rearrange('(c p) t -> p c t', p=128), rT[:, :, :])
            yield
        S.free(mlnw, den, hs, ht, ssq, qkT, Vp, Xb, EB, *Sms, *Hd, *o32, *res, *resT)

    def attn_block(self, kT_of, qTv, nq, keys, Va_of, out_dram, bm_of=None):
        psO = self.psacc()
        nk = len(keys)

        def issue_s(i):
            kt, cfg = keys[i]
            psS = self.ps()
            self.mm(psS[:, 0:nq], kT_of(kt), qTv, start=True, stop=True)
            return psS
        D = self.cur.get('depth', 2)
        pend = [issue_s(i) for i in range(min(D, nk))]
        for i in range(nk):
            psS = pend.pop(0)
            if i + D < nk:
                pend.append(issue_s(i + D))
            P = self.Ps[self.pi % len(self.Ps)]
            self.pi += 1
            self.act(P[:, 0:nq], psS[:, 0:nq], AF.Exp)
            if keys[i][1] is not None:
                self.tt(P[:, 0:nq], P[:, 0:nq], bm_of(keys[i][1]), ALU.mult)
            self.mm(psO[:, 0:nq], Va_of(keys[i][0]), P[:, 0:nq], start=(i == 0), stop=(i == nk - 1))
            yield
        r = self.pi % 2
        rsb, rinv, ob = self.rsbs[r], self.rinvs[r], self.obs[r]
        self.cp(rsb[64:65, 0:nq], psO[64:65, 0:nq], eng='act')
        psR = self.ps()
        self.mm(psR[0:64, 0:nq], self.onesf[64:65, 0:64], rsb[64:65, 0:nq])
        self.recip(rinv[:, 0:nq], psR[0:64, 0:nq])
        self.tt(ob[:, 0:nq], psO[0:64, 0:nq], rinv[:, 0:nq], ALU.mult)
        self.dma('pool', out_dram, ob[:, 0:nq])
        yield

    def attn_bufs(self):
        S = self.S
        self.Ps = [S.alloc(f'P{i}', [128, 512], BF16) for i in range(3)]
        self.rsbs = [S.alloc(f'rsb{i}', [128, 512], F32) for i in range(2)]
        self.rinvs = [S.alloc(f'rinv{i}', [64, 512], F32) for i in range(2)]
        self.obs = [S.alloc(f'ob{i}', [64, 512], BF16) for i in range(2)]
        self.pi = 0
        return [*self.Ps, *self.rsbs, *self.rinvs, *self.obs]

    def stage_na(self, l, last):
        S, d = self.S, self.d
        ab = self.attn_bufs()
        qTs = [S.alloc(f'naq{i}', [64, NT], BF16) for i in range(2)]
        kTs = [S.alloc(f'nak{i}', [64, NT], BF16) for i in range(2)]
        Vas = [S.alloc(f'nav{i}', [128, NTILE, 128], BF16) for i in range(2)]
        bms = [S.alloc(f'nabm{i}', [128, 20, 512], BF16) for i in range(2)]
        ebms = [S.alloc(f'naebm{i}', [128, 20, 512], BF16) for i in range(2)]
        vst = S.alloc('vst', [128, NTILE, 64], F32)
        for Va in Vas:
            self.memset(Va[:, :, :].rearrange('p n e -> p (n e)'), 0.0)
            self.memset(Va[:, :, 64:65], 1.0)

        def prologue(h):
            qT, kT, Va, bm, ebm = qTs[h % 2], kTs[h % 2], Vas[h % 2], bms[h % 2], ebms[h % 2]
            self.dma('sp', qT[:, :], d['pT_na'][h * 64:(h + 1) * 64, :])
            self.dma('sp', kT[:, :], d['pT_na'][384 + h * 64:384 + (h + 1) * 64, :])
            self.dma('sp', vst[:, :, :], d['p_tok'][:, 528 + h * 64:528 + (h + 1) * 64].rearrange('(n p) c -> p n c', p=128))
            self.cp(Va[:, :, 0:64], vst[:, :, :], eng='act')
            for cfg in range(20):
                self.dma('pool', bm[:, cfg, :], d['na_bm'][l, h, cfg])
            self.act(ebm[:, :, :].rearrange('p c q -> p (c q)'), bm[:, :, :].rearrange('p c q -> p (c q)'), AF.Exp)

        prologue(0)
        for h in range(6):
            qT, kT, Va, ebm = qTs[h % 2], kTs[h % 2], Vas[h % 2], ebms[h % 2]
            blocks = []
            if not last:
                blocks.append((0, 256, [(0, None), (1, None)]))
            for qb in range(8):
                if qb == 0:
                    lat = [(2 + j, 8 + j) for j in range(6)]
                elif qb == 7:
                    lat = [(2 + 26 + j, 14 + j) for j in range(6)]
                else:
                    lat = [(2 + 4 * qb - 2 + i, i) for i in range(8)]
                blocks.append((256 + qb * 512, 512, [(0, None), (1, None)] + lat))
            for bi, (q0, nq, keys) in enumerate(blocks):
                if bi == 3 and h + 1 < 6:
                    prologue(h + 1)
                yield from self.attn_block(lambda kt: kT[:, kt * 128:(kt + 1) * 128], qT[:, q0:q0 + nq], nq, keys,
                                           lambda kt: Va[:, kt, :], d['cat'][256 + h * 64:256 + (h + 1) * 64, q0:q0 + nq],
                                           bm_of=lambda cfg: ebm[:, cfg, :])
        S.free(vst, *qTs, *kTs, *Vas, *bms, *ebms, *ab)

    def rope(self, xv, nh, cs, sn, tmp):
        x5 = xv.rearrange('p h (a b f) -> p h a b f', a=2, b=2)
        x1 = x5[:, :, :, 0, :]
        x2 = x5[:, :, :, 1, :]
        csb = cs[:, :].rearrange('p (a f) -> p a f', a=2).unsqueeze(1).to_broadcast([128, nh, 2, 8])
        snb = sn[:, :].rearrange('p (a f) -> p a f', a=2).unsqueeze(1).to_broadcast([128, nh, 2, 8])
        t = [tmp[:, i, 0:nh * 16].rearrange('p (h a f) -> p h a f', h=nh, a=2) for i in range(4)]
        self.tt(t[0], x1, csb, ALU.mult)
        self.tt(t[1], x2, snb, ALU.mult)
        self.tt(t[2], x2, csb, ALU.mult)
        self.tt(t[3], x1, snb, ALU.mult)
        self.tt(x1, t[0], t[1], ALU.subtract)
        self.tt(x2, t[2], t[3], ALU.add)

    def stage_mla(self, l, last):
        S, d = self.S, self.d
        wuq = S.alloc('wuq', [128, 4, 576], BF16)
        self.dma('pool', wuq[:, :, :], d['w_uq'][l].rearrange('(k p) n -> p k n', p=128))
        wukv = S.alloc('wukv', [128, 2, 768], BF16)
        self.dma('pool', wukv[:, :, :], d['w_ukv'][l].rearrange('(k p) n -> p k n', p=128))
        qnw = self.bcast('qnw', d['q_nw'][l, :], 512)
        kvnw = self.bcast('kvnw', d['kv_nw'][l, :], 256)
        qT = S.alloc('mqT', [96, 6, NT], BF16)
        kT = S.alloc('mkT', [96, 6, NT], BF16)
        Va = S.alloc('mVa', [128, NTILE, 6, 128], BF16)
        self.memset(Va[:, :, :, :].rearrange('p n h e -> p (n h e)'), 0.0)
        self.memset(Va[:, :, :, 64:65], 1.0)
        pms = [S.alloc(f'pm{i}', [128, 800], F32) for i in range(2)]
        rings = []
        for nm, shp, dt in (('junk', [128, 512], F32), ('ssa', [128, 2], F32), ('cn', [128, 768], BF16), ('cT', [128, 6, 128], BF16),
                            ('qf32', [128, 576], F32), ('qb16', [128, 6, 128], BF16), ('kfull', [128, 6, 128], BF16),
                            ('krr', [128, 32], F32), ('rtmp', [128, 4, 96], F32)):
            rings.append([S.alloc(f'{nm}{i}', shp, dt) for i in range(2)])
        for i in range(2):
            self.memset(rings[5][i][:, :, :].rearrange('p h e -> p (h e)'), 0.0)
            self.memset(rings[6][i][:, :, :].rearrange('p h e -> p (h e)'), 0.0)
        css = [S.alloc(f'cs{i}', [128, 16], F32) for i in range(2)]
        sns = [S.alloc(f'sn{i}', [128, 16], F32) for i in range(2)]
        scale = 96 ** -0.5
        for n in range(3 if 'mla_short' in self.flags else NTILE):
            pm = pms[n % 2]
            junk, ssa, cn, cT, qf32, qb16, kfull, krr, rtmp = [rg[n % 2] for rg in rings]
            self.dma('sp', pm[:, :], d['p_tok'][n * 128:(n + 1) * 128, 912:1712])
            cs, sn = css[n % 2], sns[n % 2]
            if n >= 2:
                self.dma('sp', cs[:, :], d['rope_cs'][(n - 2) * 128:(n - 1) * 128, :])
                self.dma('sp', sn[:, :], d['rope_sn'][(n - 2) * 128:(n - 1) * 128, :])
            self.act(junk[:, 0:512], pm[:, 0:512], AF.Square, accum=ssa[:, 0:1])
            self.act(junk[:, 0:256], pm[:, 512:768], AF.Square, accum=ssa[:, 1:2])
            self.rstd(ssa[:, 0:1], 512, 'q')
            self.rstd(ssa[:, 1:2], 256, 'kv')
            self.stt(cn[:, 0:512], pm[:, 0:512], ssa[:, 0:1], qnw[:, :], ALU.mult, ALU.mult)
            self.stt(cn[:, 512:768], pm[:, 512:768], ssa[:, 1:2], kvnw[:, :], ALU.mult, ALU.mult)
            ps = self.ps()
            psb = ps[:, :].bitcast(BF16)
            for k in range(6):
                self.tr(psb[:, k * 128:(k + 1) * 128], cn[:, k * 128:(k + 1) * 128], self.identb[:, :])
            self.cp(cT[:, :, :], psb[:, 0:768].rearrange('p (k t) -> p k t', k=6), eng='act')
            p1, p2 = self.ps(), self.ps()
            for k in range(4):
                self.mm(p1[:, 0:512], cT[:, k, :], wuq[:, k, 0:512], start=(k == 0), stop=(k == 3))
            for k in range(4):
                self.mm(p2[:, 0:64], cT[:, k, :], wuq[:, k, 512:576], start=(k == 0), stop=(k == 3))
            self.ts(qf32[:, 0:512], p1[:, 0:512], scale, ALU.mult)
            self.ts(qf32[:, 512:576], p2[:, 0:64], scale, ALU.mult)
            if n >= 2 and 'norope' not in self.flags:
                self.rope(qf32[:, :].rearrange('p (h e) -> p h e', h=6)[:, :, 64:96], 6, cs, sn, rtmp)
            self.cp(qb16[:, :, 0:96], qf32[:, :].rearrange('p (h e) -> p h e', h=6), eng='act')
            ps = self.ps()
            psb = ps[:, :].bitcast(BF16)
            if 'no_qtr' not in self.flags:
                for h in range(6):
                    self.tr(psb[:, h * 128:(h + 1) * 128], qb16[:, h, :], self.identb[:, :])
                for (pa_, pb2) in ((0, 64), (64, 96)):
                    self.cp(qT[pa_:pb2, :, n * 128:(n + 1) * 128], psb[pa_:pb2, 0:768].rearrange('p (h t) -> p h t', h=6), eng='act')
            k1, k2 = self.ps(), self.ps()
            for k in range(2):
                self.mm(k1[:, 0:512], cT[:, 4 + k, :], wukv[:, k, 0:512], start=(k == 0), stop=(k == 1))
            for k in range(2):
                self.mm(k2[:, 0:256], cT[:, 4 + k, :], wukv[:, k, 512:768], start=(k == 0), stop=(k == 1))
            k1v = k1[:, 0:512].rearrange('p (h e) -> p h e', h=4)
            k2v = k2[:, 0:256].rearrange('p (h e) -> p h e', h=2)
            self.cp(kfull[:, 0:4, 0:64], k1v[:, :, 0:64])
            self.cp(kfull[:, 4:6, 0:64], k2v[:, :, 0:64])
            self.cp(Va[:, n, 0:4, 0:64], k1v[:, :, 64:128], eng='act')
            self.cp(Va[:, n, 4:6, 0:64], k2v[:, :, 64:128], eng='act')
            self.cp(krr[:, :], pm[:, 768:800])
            if n >= 2 and 'norope' not in self.flags:
                self.rope(krr[:, :].unsqueeze(1), 1, cs, sn, rtmp)
            self.cp(kfull[:, :, 64:96], krr[:, :].unsqueeze(1).to_broadcast([128, 6, 32]))
            ps = self.ps()
            psb = ps[:, :].bitcast(BF16)
            if 'no_ktr' not in self.flags:
                for h in range(6):
                    self.tr(psb[:, h * 128:(h + 1) * 128], kfull[:, h, :], self.identb[:, :])
                for (pa_, pb2) in ((0, 64), (64, 96)):
                    self.cp(kT[pa_:pb2, :, n * 128:(n + 1) * 128], psb[pa_:pb2, 0:768].rearrange('p (h t) -> p h t', h=6), eng='dve')
        S.free(wuq, wukv, qnw, kvnw, *pms, *css, *sns, *[b for rg in rings for b in rg])

        if getattr(self, 'stop_after', None) == ('mla1', l):
            S.free(qT, kT, Va)
            return
        ab = self.attn_bufs()
        allk = [(kt, None) for kt in range(NTILE)]
        for h in range(6):
            if not last:
                for _ in self.attn_block(lambda kt: kT[:, h, kt * 128:(kt + 1) * 128], qT[:, h, 0:256], 256, [(0, None), (1, None)],
                                         lambda kt: Va[:, kt, h, :], d['cat'][640 + h * 64:640 + (h + 1) * 64, 0:256]):
                    pass
            for b in range(8):
                q0 = 256 + b * 512
                for _ in self.attn_block(lambda kt: kT[:, h, kt * 128:(kt + 1) * 128], qT[:, h, q0:q0 + 512], 512, allk,
                                         lambda kt: Va[:, kt, h, :], d['cat'][640 + h * 64:640 + (h + 1) * 64, q0:q0 + 512]):
                    pass
        S.free(qT, kT, Va, *ab)

    def stage_out(self, l, xs, last):
        S, d = self.S, self.d
        w = S.alloc('w_out', [128, 8, DM], BF16)
        wsrc = d['w_out'][l].rearrange('(k p) n -> p k n', p=128)
        for k in range(8):
            self.dma('pool', w[:, k, :], wsrc[:, k, :])
        ab = self.mod_ab(l, 1, 'norm2_w', 'b')
        g1 = [self.bcast(f'g1{r}', d['mod'][l, r, 2 * DM:3 * DM], DM) for r in range(2)]
        if last:
            rw = S.alloc('rw', [128, 8, 8], F32)
            self.dma('sp', rw[:, :, :], d['router'].rearrange('(k p) e -> p k e', p=128))
            hf = S.alloc('hf', [128, DM], F32)
            hT32 = S.alloc('hT32', [128, 8, 128], F32)
            lg = S.alloc('lg', [128, 8], F32)
            l2 = S.alloc('l2', [128, 8], F32)
            mx = S.alloc('mx', [128, 4], F32)
            gsb = S.alloc('gsb', [128, 8], F32)
            m1m = S.alloc('m1m', [128, 8], F32)
        catTs = [S.alloc(f'catT{i}', [128, 8, 128], BF16) for i in range(2)]
        xts = [S.alloc(f'xo{i}', [128, DM], F32) for i in range(2)]
        xn = [S.alloc(f'xn{i}', [128, DM], F32) for i in range(2)]
        tmps = [S.alloc(f'tmpo{i}', [128, DM], F32) for i in range(2)]
        hbos = [S.alloc(f'hbo{i}', [128, DM], BF16) for i in range(2)]
        hT = [S.alloc(f'hTo{i}', [128, 8, 128], BF16) for i in range(2)]
        sso = [S.alloc(f'sso{i}', [128, 1], F32) for i in range(2)]
        for n in range(NTILE):
            if last and n < 2:
                continue
            r = 1 if n < 2 else 0
            catT, tmp, hb, ss = catTs[n % 2], tmps[n % 2], hbos[n % 2], sso[n % 2]
            self.dma('sp', catT[:, :, :], d['cat'].rearrange('(k p) t -> p k t', p=128)[:, :, n * 128:(n + 1) * 128])
            xt = xts[n % 2]
            self.dma('sp', xt[:, :], xs[n * 128:(n + 1) * 128, :])
            x1 = xn[n % 2]
            for half in range(2):
                ps = self.ps()
                for k in range(8):
                    self.mm(ps[:, 0:512], catT[:, k, :], w[:, k, half * 512:(half + 1) * 512], start=(k == 0), stop=(k == 7))
                sl = slice(half * 512, (half + 1) * 512)
                self.tt(x1[:, sl], ps[:, 0:512], g1[r][:, sl], ALU.mult)
                self.tt(x1[:, sl], x1[:, sl], xt[:, sl], ALU.add)
            self.dma('pool', d['x1'][n * 128:(n + 1) * 128, :], x1[:, :])
            A, B = ab[r]
            if last:
                self.norm_mod(x1, A, B, hf, tmp, ss)
                self.cp(hb[:, :], hf[:, :], eng='act')
                self.dma('pool', d['h2'][n * 128:(n + 1) * 128, :], hb[:, :])
                pa, pb_ = self.ps(), self.ps()
                for k in range(8):
                    pp = pa if k < 4 else pb_
                    self.tr(pp[:, (k % 4) * 128:(k % 4 + 1) * 128], hf[:, k * 128:(k + 1) * 128], self.identf[:, :])
                self.cp(hT32[:, 0:4, :], pa[:, :].rearrange('p (k t) -> p k t', k=4))
                self.cp(hT32[:, 4:8, :], pb_[:, :].rearrange('p (k t) -> p k t', k=4), eng='act')
                pl = self.ps()
                for k in range(8):
                    self.mm(pl[:, 0:8], hT32[:, k, :], rw[:, k, :], start=(k == 0), stop=(k == 7))
                self.cp(lg[:, :], pl[:, 0:8])
                self.red(mx[:, 0:1], lg[:, :], ALU.max)
                self.ts(m1m[:, :], lg[:, :], mx[:, 0:1], ALU.is_equal)
                self.dma('pool', d['sel1'][n * 128:(n + 1) * 128, :], m1m[:, :])
                self.stt(l2[:, :], m1m[:, :], -1e30, lg[:, :], ALU.mult, ALU.add)
                self.red(mx[:, 1:2], l2[:, :], ALU.max)
                self.ts(l2[:, :], lg[:, :], mx[:, 1:2], ALU.is_ge)
                self.ts(mx[:, 2:3], mx[:, 0:1], -1.0, ALU.mult)
                self.act(gsb[:, :], lg[:, :], AF.Exp, bias=mx[:, 2:3], scale=1.0)
                self.tt(gsb[:, :], gsb[:, :], l2[:, :], ALU.mult)
                self.red(mx[:, 3:4], gsb[:, :], ALU.add)
                self.recip(mx[:, 3:4], mx[:, 3:4])
                self.ts(gsb[:, :], gsb[:, :], mx[:, 3:4], ALU.mult)
                self.dma('pool', d['gates'][n * 128:(n + 1) * 128, :], gsb[:, :])
            else:
                self.norm_mod(x1, A, B, hb, tmp, ss)
            ht = hT[n % 2]
            self.transpose8(hb, ht[:, :, :])
            self.dma('pool', d['h2T'].rearrange('(k p) t -> p k t', p=128)[:, :, n * 128:(n + 1) * 128], ht[:, :, :])
        fr = [w, *catTs, *tmps, *hbos, *sso, *xts, *xn, *hT, *g1]
        if last:
            fr += [rw, hf, hT32, lg, l2, mx, gsb, m1m]
        S.free(*fr)
        for a, b in ab:
            S.free(a, b)

    def stage_ffn(self, l, last):
        S, d = self.S, self.d
        g2 = [self.bcast(f'g2{r}', d['mod'][l, r, 5 * DM:6 * DM], DM) for r in range(2)]
        if last:
            fw = self.bcast('fw', d['fin_w'][:], DM)
            sbs = [(256 + i * 1024, 1024) for i in range(4)]
            experts = [(d['moe_w1'][e], d['moe_w3'][e], d['moe_w2'][e]) for e in range(8)]
        else:
            sbs = [(0, 256)] + [(256 + i * 1024, 1024) for i in range(4)]
            experts = [(d['ffn_w1'], d['ffn_w3'], d['ffn_w2'])]
        hTb = S.alloc('hTb', [128, 8, 1024], BF16)
        acc = S.alloc('facc', [128, 8, DM], F32)
        uT = S.alloc('uT', [128, 4, 1024], BF16)
        us = [S.alloc(f'us{i}', [128, 512], F32) for i in range(2)]
        w1s = [S.alloc(f'w1g{i}', [128, 8, 512], BF16) for i in range(2)]
        w3s = [S.alloc(f'w3g{i}', [128, 8, 512], BF16) for i in range(2)]
        w2s = [S.alloc(f'w2g{i}', [128, 4, DM], BF16) for i in range(2)]
        gt = S.alloc('fgt', [128, 8, 8], F32)
        xts = [S.alloc(f'xf{i}', [128, DM], F32) for i in range(2)]
        tmp = S.alloc('ftmp', [128, DM], F32)
        ss = S.alloc('fss', [128, 1], F32)
        groups = [(g * 512, 512) for g in range(5)] + [(2560, 256)]
        wi = 0
        for (t0, ntok) in sbs:
            ntile = ntok // 128
            self.dma('sp', hTb[:, :, 0:ntok], d['h2T'].rearrange('(k p) t -> p k t', p=128)[:, :, t0:t0 + ntok])
            if last:
                self.dma('sp', gt[:, 0:ntile, :], d['gates'][t0:t0 + ntok, :].rearrange('(n p) e -> p n e', p=128))
            first = True
            for ei, (W1, W3, W2) in enumerate(experts):
                for (g0, gs) in groups:
                    nfc = gs // 128
                    w1, w3, w2 = w1s[wi % 2], w3s[wi % 2], w2s[wi % 2]
                    wi += 1
                    self.dma('pool', w1[:, :, 0:gs], W1.rearrange('(k p) n -> p k n', p=128)[:, :, g0:g0 + gs])
                    self.dma('pool', w3[:, :, 0:gs], W3.rearrange('(k p) n -> p k n', p=128)[:, :, g0:g0 + gs])
                    self.dma('pool', w2[:, 0:nfc, :], W2[g0:g0 + gs, :].rearrange('(c p) n -> p c n', p=128))
                    for tb in range((ntok + 511) // 512):
                        tw = min(512, ntok - tb * 512)
                        for fc in range(nfc):
                            pa, pb_ = self.ps(), self.ps()
                            for k in range(8):
                                self.mm(pa[:, 0:tw], w1[:, k, fc * 128:(fc + 1) * 128], hTb[:, k, tb * 512:tb * 512 + tw], start=(k == 0), stop=(k == 7))
                            for k in range(8):
                                self.mm(pb_[:, 0:tw], w3[:, k, fc * 128:(fc + 1) * 128], hTb[:, k, tb * 512:tb * 512 + tw], start=(k == 0), stop=(k == 7))
                            u = us[fc % 2]
                            self.act(u[:, 0:tw], pa[:, 0:tw], AF.Silu)
                            self.tt(uT[:, fc, tb * 512:tb * 512 + tw], u[:, 0:tw], pb_[:, 0:tw], ALU.mult)
                    for tt_ in range(ntile):
                        for half in range(2):
                            ps = self.ps()
                            for fc in range(nfc):
                                self.mm(ps[:, 0:512], uT[:, fc, tt_ * 128:(tt_ + 1) * 128], w2[:, fc, half * 512:(half + 1) * 512],
                                        start=(fc == 0), stop=(fc == nfc - 1))
                            a = acc[:, tt_, half * 512:(half + 1) * 512]
                            if last:
                                gsc = gt[:, tt_, ei:ei + 1]
                                if first:
                                    self.ts(a, ps[:, 0:512], gsc, ALU.mult)
                                else:
                                    self.stt(a, ps[:, 0:512], gsc, a, ALU.mult, ALU.add)
                            else:
                                if first:
                                    self.cp(a, ps[:, 0:512], eng='act')
                                else:
                                    self.tt(a, a, ps[:, 0:512], ALU.add)
                    first = False
            for tt_ in range(ntile):
                n = t0 // 128 + tt_
                r = 1 if n < 2 else 0
                xt = xts[tt_ % 2]
                self.dma('sp', xt[:, :], d['x1'][n * 128:(n + 1) * 128, :])
                a = acc[:, tt_, :]
                self.tt(a, a, g2[r][:, :], ALU.mult)
                self.tt(xt[:, :], xt[:, :], a, ALU.add)
                if last:
                    self.act(tmp[:, :], xt[:, :], AF.Square, accum=ss[:, 0:1])
                    self.rstd(ss[:, 0:1], DM, 'f')
                    self.stt(xt[:, :], xt[:, :], ss[:, 0:1], fw[:, :], ALU.mult, ALU.mult)
                    self.dma('pool', d['out'][(n - 2) * 128:(n - 1) * 128, :], xt[:, :])
                else:
                    self.dma('pool', d['x2'][n * 128:(n + 1) * 128, :], xt[:, :])
        fr = [hTb, acc, uT, gt, tmp, ss, *us, *w1s, *w3s, *w2s, *xts, *g2]
        if last:
            fr.append(fw)
        S.free(*fr)


    def stage_moe(self, l):
        S, d = self.S, self.d
        NL = 32
        g2 = self.bcast('g2m', d['mod'][l, 0, 5 * DM:6 * DM], DM)
        fw = self.bcast('fwm', d['fin_w'][:], DM)
        G3 = S.alloc('G3', [128, NL, 8], F32)
        M1 = S.alloc('M1', [128, NL, 8], F32)
        self.dma('sp', G3[:, :, :], d['gates'][256:NT, :].rearrange('(n p) e -> p n e', p=128))
        self.dma('sp', M1[:, :, :], d['sel1'][256:NT, :].rearrange('(n p) e -> p n e', p=128))
        Ms = S.alloc('Ms', [128, NL, 8], F32)
        self.ts(Ms[:, :, :], G3[:, :, :], 0.0, ALU.is_gt)
        M2 = S.alloc('M2', [128, NL, 8], F32)
        self.tt(M2[:, :, :], Ms[:, :, :], M1[:, :, :], ALU.subtract)
        triS = S.alloc('triS', [128, 128], F32)
        self.tt(triS[:, :], self.tri0[:, :], self.identf[:, :], ALU.subtract)
        Ms2 = Ms[:, :, :].rearrange('p n e -> p (n e)')
        pc_, pr_ = self.ps(), self.ps()
        self.mm(pc_[:, 0:256], self.onesf[:, :], Ms2)
        self.mm(pr_[:, 0:256], triS[:, :], Ms2)
        CS = S.alloc('CS', [128, NL, 8], F32)
        self.cp(CS[:, :, :].rearrange('p n e -> p (n e)'), pc_[:, 0:256])
        pos = S.alloc('pos', [128, NL, 8], F32)
        self.cp(pos[:, :, :].rearrange('p n e -> p (n e)'), pr_[:, 0:256], eng='act')
        TB = S.alloc('TB', [128, NL + 1, 8], F32)
        self.memset(TB[:, 0, :], 0.0)
        for n in range(NL):
            self.tt(TB[:, n + 1, :], TB[:, n, :], CS[:, n, :], ALU.add)
        cth = S.alloc('cth', [128, 16], F32)
        self.dma('sp', cth[:, :], d['c_th'][:, :])
        cj = S.alloc('cj', [128, NBLK], F32)
        self.dma('sp', cj[:, :], d['c_j512'][:, :])
        cgp = S.alloc('cgp', [128, 11], F32)
        self.dma('sp', cgp[:, :], d['c_gp'][:, :])
        cmp_ = S.alloc('cmp', [128, 8, 16], F32)
        self.tt(cmp_[:, :, :], cth[:, :].unsqueeze(1).to_broadcast([128, 8, 16]),
                TB[:, NL, :].unsqueeze(2).to_broadcast([128, 8, 16]), ALU.is_lt)
        pcn = S.alloc('pcn', [128, 8], F32)
        self.red(pcn[:, :], cmp_[:, :, :], ALU.add)
        self.ts(pcn[:, :], pcn[:, :], 512.0, ALU.mult)
        end = S.alloc('end', [128, 8], F32)
        self.cp(end[:, 0:1], pcn[:, 0:1])
        for e in range(1, 8):
            self.tt(end[:, e:e + 1], end[:, e - 1:e], pcn[:, e:e + 1], ALU.add)
        st = S.alloc('st', [128, 8], F32)
        self.tt(st[:, :], end[:, :], pcn[:, :], ALU.subtract)
        self.tt(pos[:, :, :], pos[:, :, :], TB[:, 0:NL, :], ALU.add)
        self.tt(pos[:, :, :], pos[:, :, :], st[:, :].unsqueeze(1).to_broadcast([128, NL, 8]), ALU.add)
        tmp3 = S.alloc('tmp3', [128, NL, 8], F32)
        slotf = S.alloc('slotf', [128, 2, NL], F32)
        gk = S.alloc('gk', [128, 2, NL], F32)
        for k, Mk in enumerate((M1, M2)):
            self.tt(tmp3[:, :, :], Mk[:, :, :], pos[:, :, :], ALU.mult)
            self.red(slotf[:, k, :], tmp3[:, :, :], ALU.add)
            self.tt(tmp3[:, :, :], Mk[:, :, :], G3[:, :, :], ALU.mult)
            self.red(gk[:, k, :], tmp3[:, :, :], ALU.add)
        self.ts(slotf[:, :, :], slotf[:, :, :], float(NSLOT - 1), ALU.min)
        sloti = S.alloc('sloti', [128, 2, NL], I32)
        self.cp(sloti[:, :, :].rearrange('p k n -> p (k n)'), slotf[:, :, :].rearrange('p k n -> p (k n)'))
        bj = S.alloc('bj', [128, NBLK, 8], F32)
        self.tt(bj[:, :, :], end[:, :].unsqueeze(1).to_broadcast([128, NBLK, 8]),
                cj[:, :].unsqueeze(2).to_broadcast([128, NBLK, 8]), ALU.is_le)
        ej = S.alloc('ej', [128, NBLK], F32)
        self.red(ej[:, :], bj[:, :, :], ALU.add)
        self.ts(ej[:, :], ej[:, :], 7.0, ALU.min, 1408.0, ALU.mult)
        idxf = S.alloc('idxf', [128, NBLK, 11], F32)
        self.tt(idxf[:, :, :], ej[:, :].unsqueeze(2).to_broadcast([128, NBLK, 11]),
                cgp[:, :].unsqueeze(1).to_broadcast([128, NBLK, 11]), ALU.add)
        idxi = S.alloc('idxi', [128, NBLK, 11], I32)
        self.cp(idxi[:, :, :].rearrange('p j g -> p (j g)'), idxf[:, :, :].rearrange('p j g -> p (j g)'))
        S.free(G3, M1, Ms, M2, triS, CS, pos, TB, cth, cj, cgp, cmp_, pcn, end, st, tmp3, slotf, bj, ej, idxf)

        zt = S.alloc('zt', [128, 24, DM], BF16)
        self.memset(zt[:, :, :].rearrange('p a c -> p (a c)'), 0.0)
        for j in range(NBLK // 6):
            self.dma('sp', d['hs'][j * 3072:(j + 1) * 3072, :].rearrange('(a p) c -> p a c', p=128), zt[:, :, :])
        hts = [S.alloc(f'hsrc{i}', [128, DM], BF16) for i in range(2)]
        for n in range(NL):
            ht = hts[n % 2]
            self.dma('sp', ht[:, :], d['h2'][(n + 2) * 128:(n + 3) * 128, :])
            for k in range(2):
                self.dma_ind(d['hs'][:, :], ht[:, :], sloti[:, k, n:n + 1], True, NSLOT - 1)
        S.free(zt, *hts)

        hbs = [S.alloc(f'hsb{i}', [128, 4, DM], BF16) for i in range(2)]
        hTs = [S.alloc(f'hTm{i}', [128, 8, 512], BF16) for i in range(2)]
        accs = [S.alloc(f'macc{i}', [128, 4, DM], F32) for i in range(2)]
        uTs = [S.alloc(f'uTm{i}', [128, 2, 512], BF16) for i in range(2)]
        us = [S.alloc(f'usm{i}', [128, 512], F32) for i in range(2)]
        w1s = [S.alloc(f'w1m{i}', [128, 8, 256], BF16) for i in range(3)]
        w3s = [S.alloc(f'w3m{i}', [128, 8, 256], BF16) for i in range(3)]
        w2s = [S.alloc(f'w2m{i}', [128, 2, DM], BF16) for i in range(3)]
        wi = 0
        nrow = 8 * 11 * 128
        for j in range(NBLK):
            hb, hT, acc = hbs[j % 2], hTs[j % 2], accs[j % 2]
            self.dma('sp', hb[:, :, :], d['hs'][j * 512:(j + 1) * 512, :].rearrange('(a p) c -> p a c', p=128))
            for a in range(4):
                ps = self.ps()
                psb = ps[:, :].bitcast(BF16)
                for k in range(8):
                    self.tr(psb[:, k * 128:(k + 1) * 128], hb[:, a, k * 128:(k + 1) * 128], self.identb[:, :])
                self.cp(hT[:, :, a * 128:(a + 1) * 128], psb[:, :].rearrange('p (k t) -> p k t', k=8), eng=('act' if a % 2 else 'dve'))
            for g in range(11):
                w1, w3, w2 = w1s[wi % 3], w3s[wi % 3], w2s[wi % 3]
                wi += 1
                ix = idxi[:, j, g:g + 1]
                self.dma_ind(w1[:, :, :].rearrange('p k c -> p (k c)'), d['moe_w1'][:, :], ix, False, nrow - 1)
                self.dma_ind(w3[:, :, :].rearrange('p k c -> p (k c)'), d['moe_w3'][:, :], ix, False, nrow - 1)
                self.dma_ind(w2[:, :, :].rearrange('p k c -> p (k c)'), d['moe_w2'][:, :], ix, False, nrow - 1)
                uT = uTs[g % 2]
                for fc in range(2):
                    pa, pb_ = self.ps(), self.ps()
                    for k in range(8):
                        self.mm(pa[:, 0:512], w1[:, k, fc * 128:(fc + 1) * 128], hT[:, k, :], start=(k == 0), stop=(k == 7))
                    for k in range(8):
                        self.mm(pb_[:, 0:512], w3[:, k, fc * 128:(fc + 1) * 128], hT[:, k, :], start=(k == 0), stop=(k == 7))
                    u = us[fc % 2]
                    self.act(u[:, :], pa[:, 0:512], AF.Silu)
                    self.tt(uT[:, fc, :], u[:, :], pb_[:, 0:512], ALU.mult)
                for a in range(4):
                    for half in range(2):
                        ps = self.psacc()
                        for fc in range(2):
                            self.mm(ps[:, 0:512], uT[:, fc, a * 128:(a + 1) * 128], w2[:, fc, half * 512:(half + 1) * 512],
                                    start=(fc == 0), stop=(fc == 1))
                        av = acc[:, a, half * 512:(half + 1) * 512]
                        if g == 0:
                            self.cp(av, ps[:, 0:512], eng='act')
                        else:
                            self.tt(av, av, ps[:, 0:512], ALU.add)
            self.dma('sp', d['yb'][j * 512:(j + 1) * 512, :].rearrange('(a p) c -> p a c', p=128), acc[:, :, :])
        S.free(*hbs, *hTs, *accs, *uTs, *us, *w1s, *w3s, *w2s, idxi)

        y1s = [S.alloc(f'y1_{i}', [128, DM], F32) for i in range(2)]
        y2s = [S.alloc(f'y2_{i}', [128, DM], F32) for i in range(2)]
        xts = [S.alloc(f'xm{i}', [128, DM], F32) for i in range(2)]
        tmp = S.alloc('mtmp', [128, DM], F32)
        ss = S.alloc('mss', [128, 1], F32)
        outs = [S.alloc(f'mout{i}', [128, DM], F32) for i in range(2)]
        def loads(n):
            y1, y2, xt = y1s[n % 2], y2s[n % 2], xts[n % 2]
            self.dma_ind(y1[:, :], d['yb'][:, :], sloti[:, 0, n:n + 1], False, NSLOT - 1)
            self.dma_ind(y2[:, :], d['yb'][:, :], sloti[:, 1, n:n + 1], False, NSLOT - 1)
            self.dma('sp', xt[:, :], d['x1'][(n + 2) * 128:(n + 3) * 128, :])
        loads(0)
        for n in range(NL):
            y1, y2, xt = y1s[n % 2], y2s[n % 2], xts[n % 2]
            ob = outs[n % 2]
            if n + 1 < NL:
                loads(n + 1)
            self.ts(y1[:, :], y1[:, :], gk[:, 0, n:n + 1], ALU.mult)
            self.stt(y1[:, :], y2[:, :], gk[:, 1, n:n + 1], y1[:, :], ALU.mult, ALU.add)
            self.tt(y1[:, :], y1[:, :], g2[:, :], ALU.mult)
            self.tt(xt[:, :], xt[:, :], y1[:, :], ALU.add)
            self.act(tmp[:, :], xt[:, :], AF.Square, accum=ss[:, 0:1])
            self.rstd(ss[:, 0:1], DM, 'f')
            self.stt(ob[:, :], xt[:, :], ss[:, 0:1], fw[:, :], ALU.mult, ALU.mult)
            self.dma('sp', d['out'][n * 128:(n + 1) * 128, :], ob[:, :])
        S.free(*outs)
        S.free(g2, fw, gk, sloti, tmp, ss, *y1s, *y2s, *xts)


def _na_bias_tiles(rpb):
    L, H = rpb.shape[0], rpb.shape[1]
    NEG = np.float32(-30000.0)
    col = np.arange(64)
    c0 = np.clip(col - 8, 0, 48)
    ck = col[:, None]
    cq = col[None, :]
    cvalid = (ck >= c0[None, :]) & (ck < c0[None, :] + 16)
    cidx = np.clip(ck - cq, -15, 15) + 15
    out = np.full((L, H, 20, 128, 512), NEG, dtype=np.float32)
    cfgs = []
    for i in range(8):
        cfgs.append((8, 4 + 2 * i))
    for j in range(6):
        cfgs.append((0, 2 * j))
    for j in range(6):
        cfgs.append((56, 52 + 2 * j))
    for ci, (R, kr0) in enumerate(cfgs):
        for a in range(2):
            kr = kr0 + a
            for b in range(8):
                r = R + b
                r0 = min(max(r - 4, 0), 56)
                if not (r0 <= kr <= r0 + 7) or kr > 63:
                    continue
                drow = kr - r + 7
                g = rpb[:, :, drow, :][:, :, cidx]
                blk = np.where(cvalid[None, None], g, NEG)
                out[:, :, ci, a * 64:(a + 1) * 64, b * 64:(b + 1) * 64] = blk
    return out


def _rope_tables():
    t = np.arange(4096)
    row = (t // 64).astype(np.float32)
    colv = (t % 64).astype(np.float32)
    inv = (np.float32(10000.0) ** (-np.arange(0, 16, 2, dtype=np.float32) / np.float32(16))).astype(np.float32)
    ang = np.concatenate([row[:, None] * inv, colv[:, None] * inv], axis=1).astype(np.float32)
    return np.cos(ang).astype(np.float32), np.sin(ang).astype(np.float32)


def _w13_layout(w):
    w = w.reshape(8, 8, 128, 11, 256)
    return np.ascontiguousarray(w.transpose(0, 3, 2, 1, 4)).reshape(8 * 11 * 128, 2048)


def _w2_layout(w):
    w = w.reshape(8, 11, 2, 128, 1024)
    return np.ascontiguousarray(w.transpose(0, 1, 3, 2, 4)).reshape(8 * 11 * 128, 2048)


_NC_CACHE = {}


def _get_nc(stop_after=None):
    key = (stop_after, DEBUG)
    if key not in _NC_CACHE:
        nc = bass.Bass("TRN2", target_bir_lowering=False)
        k = K(nc)
        k.stop_after = stop_after
        k.build()
        _NC_CACHE[key] = nc
    return _NC_CACHE[key]


def make_in_maps(inputs):
    f = lambda a: np.ascontiguousarray(np.asarray(a, dtype=np.float32))
    x, c, ctx, c_ctx = f(inputs['x']), f(inputs['c']), f(inputs['ctx']), f(inputs['c_ctx'])
    cs, sn = _rope_tables()
    tri0 = np.triu(np.ones((128, 128), np.float32))
    tri1 = np.tril(np.ones((128, 128), np.float32))
    shared = {
        'ada_w': f(inputs['ada_w']), 'ada_b': f(inputs['ada_b']),
        'norm1_w': f(inputs['norm1_w']), 'norm2_w': f(inputs['norm2_w']),
        'w_in': f(inputs['w_in']), 'w_out': f(inputs['w_out']),
        'convT': f(np.transpose(np.asarray(inputs['mlstm_conv_w']), (0, 2, 1))),
        'ig_b': f(np.asarray(inputs['mlstm_ig_b']).reshape(2, 8)),
        'fg_b': f(np.asarray(inputs['mlstm_fg_b']).reshape(2, 8)),
        'ml_nw': f(inputs['mlstm_norm_w']),
        'na_bm': _na_bias_tiles(f(inputs['na_rpb'])),
        'q_nw': f(inputs['mla_q_norm_w']), 'kv_nw': f(inputs['mla_kv_norm_w']),
        'w_uq': f(inputs['mla_w_uq']), 'w_ukv': f(inputs['mla_w_ukv']),
        'ffn_w1': f(inputs['ffn_w1'])[0], 'ffn_w3': f(inputs['ffn_w3'])[0], 'ffn_w2': f(inputs['ffn_w2'])[0],
        'router': f(inputs['moe_router_w'])[0],
        'moe_w1': _w13_layout(f(inputs['moe_w1'])[0]), 'moe_w3': _w13_layout(f(inputs['moe_w3'])[0]),
        'moe_w2': _w2_layout(f(inputs['moe_w2'])[0]),
        'c_th': np.ascontiguousarray(np.broadcast_to(np.arange(16, dtype=np.float32) * 512, (128, 16))),
        'c_j512': np.ascontiguousarray(np.broadcast_to(np.arange(NBLK, dtype=np.float32) * 512, (128, NBLK))),
        'c_gp': np.ascontiguousarray((np.arange(11, dtype=np.float32)[None, :] * 128 + np.arange(128, dtype=np.float32)[:, None])),
        'fin_w': f(inputs['final_norm_w']),
        'ident': np.eye(128, dtype=np.float32), 'tri0': tri0, 'tri1': tri1,
        'rope_cs': cs, 'rope_sn': sn,
    }
    maps = []
    for b in range(8):
        m = dict(shared)
        m['xin'] = np.ascontiguousarray(np.concatenate([ctx[b], x[b]], axis=0))
        cc = np.stack([c[b].reshape(8, 128).T, c_ctx.reshape(8, 128).T], axis=2)
        m['cc'] = np.ascontiguousarray(cc.reshape(128, 16))
        maps.append(m)
    return maps


def kernel(**inputs):
    nc = _get_nc()
    maps = make_in_maps(inputs)
    res = run_bass_kernel_spmd(nc, maps, core_ids=list(range(8)))
    return np.stack([np.asarray(r['out'], dtype=np.float32) for r in res.results], axis=0)
```

```python
import numpy as np
from contextlib import ExitStack
import concourse.bass as bass
import concourse.mybir as mybir
from concourse.bass_utils import run_bass_kernel_spmd

F32 = mybir.dt.float32
BF16 = mybir.dt.bfloat16
U8 = mybir.dt.uint8
AF = mybir.ActivationFunctionType
ALU = mybir.AluOpType
AX = mybir.AxisListType
DTSIZE = {F32: 4, BF16: 2, mybir.dt.int32: 4}
COMPUTE = ('pe', 'act', 'dve', 'pool')
QUEUES = ('sp', 'act', 'pool')
NDSEM = 12
ARENA = 207 * 1024

NT = 4352
NTILE = 34
DM = 1024
NIN = 2992
DFF = 2816
EPS = 1e-6
DEBUG = False
NBLK = 24
NSLOT = NBLK * 512
I32 = mybir.dt.int32


class Op:
    __slots__ = ('eng', 'fn', 'deps', 'isdma', 'needs_inc', 'count', 'semi', 'semval', 'idx', 'q')


class V:
    __slots__ = ('buf', 'ap')

    def __init__(self, buf, ap):
        self.buf = buf
        self.ap = ap

    def __getitem__(self, k):
        return V(self.buf, self.ap[k])

    def rearrange(self, pattern_, **kw):
        return V(self.buf, self.ap.rearrange(pattern_, **kw))

    def unsqueeze(self, a):
        return V(self.buf, self.ap.unsqueeze(a))

    def to_broadcast(self, shp):
        return V(self.buf, self.ap.to_broadcast(list(shp)))

    def bitcast(self, dt):
        return V(self.buf, self.ap.bitcast(dt))

    def partition_broadcast(self, n):
        return V(self.buf, self.ap.partition_broadcast(n))


class Buf:
    def __init__(self, ap, name, off=0, size=0):
        self.ap = ap
        self.name = name
        self.w = None
        self.rc = {}
        self.rd = {}
        self.inh = []
        self.multiw = False
        self.wl = {}
        self.war = []
        self.off = off
        self.size = size

    def __getitem__(self, k):
        return V(self, self.ap[k])

    def v(self):
        return V(self, self.ap)

    def rearrange(self, pattern_, **kw):
        return V(self, self.ap.rearrange(pattern_, **kw))


class Sched:
    def __init__(self, nc):
        self.nc = nc
        self.ops = {e: [] for e in ('pe', 'act', 'dve', 'pool', 'sp')}
        self.nops = 0
        self.dsem_use = {q: [0] * NDSEM for q in QUEUES}
        self.dsem_rr = {q: 0 for q in QUEUES}
        self.arena = nc.alloc_sbuf_tensor("arena", [128, ARENA], U8)
        self.live = []
        self.dead = []
        self.psum = []
        for i in range(8):
            h = nc.alloc_psum_tensor(f"psb{i}", [128, 512], F32)
            self.psum.append(Buf(h[:, :], f"psb{i}"))

    def alloc(self, name, shape, dtype):
        n = int(np.prod(shape[1:])) * DTSIZE[dtype]
        n_al = (n + 63) // 64 * 64
        self.live.sort(key=lambda b: b.off)
        off = 0
        for b in self.live:
            if b.off - off >= n_al:
                break
            off = max(off, b.off + b.size)
        if off + n_al > ARENA:
            raise RuntimeError(f"SBUF arena OOM allocating {name} {shape}: need {n_al} at {off}; live="
                               + str([(b.name, b.off, b.size) for b in self.live]))
        ap = self.arena[0:shape[0], off:off + n].bitcast(dtype)
        if len(shape) > 2:
            names = ' '.join(f'd{i}' for i in range(1, len(shape)))
            kw = {f'd{i}': shape[i] for i in range(1, len(shape))}
            ap = ap.rearrange(f'p ({names}) -> p {names}', **kw)
        buf = Buf(ap, name, off, n_al)
        keep = []
        for d in self.dead:
            if d.off < off + n_al and off < d.off + d.size:
                acc = list(d.inh)
                if d.w is not None:
                    acc.append(d.w)
                acc.extend(d.rc.values())
                acc.extend(d.rd.values())
                buf.inh.extend(acc)
                if not (off <= d.off and d.off + d.size <= off + n_al):
                    keep.append(d)
            else:
                keep.append(d)
        self.dead = keep
        best = {}
        for o in buf.inh:
            key = (o.q, o.semi) if o.isdma else o.eng
            if key not in best or best[key].idx < o.idx:
                best[key] = o
        buf.inh = list(best.values())
        self.live.append(buf)
        return buf

    def free(self, *bufs):
        for b in bufs:
            self.live.remove(b)
            self.dead.append(b)

    def dram(self, name, shape, dtype, kind="Internal"):
        h = self.nc.dram_tensor(name, list(shape), dtype, kind=kind)
        return Buf(h.ap(), name)

    def _mk(self, eng, fn, reads, writes, isdma, q=None):
        op = Op()
        op.eng = eng
        op.fn = fn
        op.isdma = isdma
        op.needs_inc = False
        op.count = 0
        op.idx = self.nops
        op.q = q
        op.semi = -1
        self.nops += 1
        if isdma:
            i = self.dsem_rr[q]
            self.dsem_rr[q] = (i + 1) % NDSEM
            self.dsem_use[q][i] += 1
            op.semi = i
            op.semval = 16 * self.dsem_use[q][i]
        deps = []
        for b in reads:
            if b.w is not None:
                deps.append((b.w, True))
            for o in b.wl.values():
                deps.append((o, True))
        for b in writes:
            mw = b.multiw and isdma
            if mw:
                if b.rc or b.rd:
                    b.war = list(b.rc.values()) + list(b.rd.values())
                    b.wl = {}
                    b.rc = {}
                    b.rd = {}
                for o in b.war:
                    deps.append((o, False))
                if b.w is not None:
                    deps.append((b.w, False))
            else:
                if b.w is not None:
                    deps.append((b.w, False))
                for o in b.wl.values():
                    deps.append((o, False))
                for o in b.rc.values():
                    deps.append((o, False))
                for o in b.rd.values():
                    deps.append((o, False))
            for o in b.inh:
                deps.append((o, False))
        fin = []
        seen = set()
        for p, raw in deps:
            if p is op or id(p) in seen:
                continue
            if (not p.isdma) and (not isdma) and p.eng == eng and eng == 'pe':
                continue
            seen.add(id(p))
            fin.append(p)
            p.needs_inc = True
        op.deps = fin
        for b in reads:
            if isdma:
                b.rd[(q, op.semi)] = op
            else:
                b.rc[eng] = op
        for b in writes:
            if b.multiw and isdma:
                b.wl[(q, op.semi)] = op
                b.inh = []
            else:
                b.w = op
                b.wl = {}
                b.war = []
                b.rc = {}
                b.rd = {}
                b.inh = []
        self.ops[eng].append(op)
        return op

    def op(self, eng, fn, reads=(), writes=()):
        return self._mk(eng, fn, reads, writes, False)

    def dma(self, q, out, in_, **kw):
        oa, ia = out.ap, in_.ap

        def fn(e):
            return e.dma_start(out=oa, in_=ia, **kw)
        return self._mk(q, fn, [in_.buf], [out.buf], True, q=q)

    def emit(self):
        nc = self.nc
        with ExitStack() as es:
            self.csems = {e: es.enter_context(nc.semaphore(f"c_{e}")) for e in COMPUTE}
            self.dsems = {q: [es.enter_context(nc.semaphore(f"d_{q}{i}")) for i in range(NDSEM)] for q in QUEUES}
            for e in COMPUTE:
                cnt = 0
                for op in self.ops[e]:
                    if not op.isdma and op.needs_inc:
                        cnt += 1
                    op.count = cnt
            block = es.enter_context(nc.Block())

            @block.tensor
            def _(eng):
                self._emit_engine('pe', eng)

            @block.scalar
            def _(eng):
                self._emit_engine('act', eng)

            @block.vector
            def _(eng):
                self._emit_engine('dve', eng)

            @block.gpsimd
            def _(eng):
                self._emit_engine('pool', eng)

            @block.sync
            def _(eng):
                self._emit_engine('sp', eng, final=True)

    def _emit_engine(self, e, eng, final=False):
        waited = {}

        def wait(key, sem, val):
            if waited.get(key, 0) >= val:
                return
            eng.wait_ge(sem, val)
            waited[key] = val

        for op in self.ops[e]:
            for p in op.deps:
                if p.isdma:
                    wait(('d', p.q, p.semi), self.dsems[p.q][p.semi], p.semval)
                else:
                    wait(('c', p.eng), self.csems[p.eng], p.count)
            if op.isdma:
                sem = self.dsems[op.q][op.semi]
                if op.semval > 16:
                    wait(('d', op.q, op.semi), sem, op.semval - 16)
                ins = op.fn(eng)
                ins.then_inc(sem, 16)
            else:
                ins = op.fn(eng)
                if op.needs_inc:
                    ins.then_inc(self.csems[e], 1)
        if final:
            for q in QUEUES:
                for i in range(NDSEM):
                    v = 16 * self.dsem_use[q][i]
                    if v > 0:
                        wait(('d', q, i), self.dsems[q][i], v)
            for ce in COMPUTE:
                last = None
                for op in self.ops[ce]:
                    if not op.isdma and op.needs_inc:
                        last = op
                if last is not None:
                    wait(('c', ce), self.csems[ce], last.count)


def _bufs(*xs):
    out = []
    for x in xs:
        if isinstance(x, V) and x.buf not in out:
            out.append(x.buf)
    return out


def _a(x):
    return x.ap if isinstance(x, V) else x


class K:
    def __init__(self, nc):
        self.nc = nc
        self.S = Sched(nc)
        self.defctx = {'banks': [0, 1, 2, 3], 'accs': [4, 5, 6, 7], 'i': 0, 'j': 0}
        self.cur = self.defctx

    def ps(self):
        c = self.cur
        b = self.S.psum[c['banks'][c['i'] % len(c['banks'])]]
        c['i'] += 1
        return b

    def psacc(self):
        c = self.cur
        b = self.S.psum[c['accs'][c['j'] % len(c['accs'])]]
        c['j'] += 1
        return b

    def interleave(self, gens):
        live = list(gens)
        while live:
            for item in list(live):
                g, ctx, wgt = item
                self.cur = ctx
                for _ in range(wgt):
                    try:
                        next(g)
                    except StopIteration:
                        live.remove(item)
                        break
        self.cur = self.defctx

    def mm(self, out, lhsT, rhs, start=True, stop=True):
        oa, la, ra = out.ap, lhsT.ap, rhs.ap
        self.S.op('pe', lambda e: e.matmul(oa, lhsT=la, rhs=ra, start=start, stop=stop),
                  reads=_bufs(lhsT, rhs), writes=_bufs(out))

    def tr(self, out, in_, ident):
        oa, ia, da = out.ap, in_.ap, ident.ap
        self.S.op('pe', lambda e: e.transpose(oa, ia, da), reads=_bufs(in_, ident), writes=_bufs(out))

    def act(self, out, in_, func, bias=None, scale=None, accum=None):
        kw = {}
        if bias is not None:
            kw['bias'] = _a(bias)
        if scale is not None:
            kw['scale'] = _a(scale)
        if accum is not None:
            kw['accum_out'] = accum.ap
        oa, ia = out.ap, in_.ap
        self.S.op('act', lambda e: e.activation(out=oa, in_=ia, func=func, **kw),
                  reads=_bufs(in_, bias, scale), writes=_bufs(out, accum))

    def tt(self, out, in0, in1, op, eng='dve'):
        oa, a0, a1 = out.ap, in0.ap, in1.ap
        self.S.op(eng, lambda e: e.tensor_tensor(out=oa, in0=a0, in1=a1, op=op), reads=_bufs(in0, in1), writes=_bufs(out))

    def ts(self, out, in0, s1, op0, s2=None, op1=None, eng='dve'):
        oa, a0 = out.ap, in0.ap
        x1, x2 = _a(s1), _a(s2)
        if op1 is None:
            self.S.op(eng, lambda e: e.tensor_scalar(out=oa, in0=a0, scalar1=x1, scalar2=None, op0=op0),
                      reads=_bufs(in0, s1), writes=_bufs(out))
        else:
            self.S.op(eng, lambda e: e.tensor_scalar(out=oa, in0=a0, scalar1=x1, scalar2=x2, op0=op0, op1=op1),
                      reads=_bufs(in0, s1, s2), writes=_bufs(out))

    def stt(self, out, in0, scalar, in1, op0, op1, eng='dve'):
        oa, a0, a1, sc = out.ap, in0.ap, in1.ap, _a(scalar)
        self.S.op(eng, lambda e: e.scalar_tensor_tensor(out=oa, in0=a0, scalar=sc, in1=a1, op0=op0, op1=op1),
                  reads=_bufs(in0, in1, scalar), writes=_bufs(out))

    def cp(self, out, in_, eng='dve'):
        oa, ia = out.ap, in_.ap
        if eng == 'act':
            self.S.op('act', lambda e: e.copy(out=oa, in_=ia), reads=_bufs(in_), writes=_bufs(out))
        else:
            self.S.op(eng, lambda e: e.tensor_copy(out=oa, in_=ia), reads=_bufs(in_), writes=_bufs(out))

    def memset(self, out, val, eng='dve'):
        oa = out.ap
        self.S.op(eng, lambda e: e.memset(oa, val), writes=_bufs(out))

    def red(self, out, in_, op, negate=False):
        oa, ia = out.ap, in_.ap
        self.S.op('dve', lambda e: e.tensor_reduce(out=oa, in_=ia, axis=AX.X, op=op, negate=negate),
                  reads=_bufs(in_), writes=_bufs(out))

    def recip(self, out, in_):
        oa, ia = out.ap, in_.ap
        self.S.op('dve', lambda e: e.reciprocal(out=oa, in_=ia), reads=_bufs(in_), writes=_bufs(out))

    def dma(self, q, out, in_, **kw):
        self.S.dma(q, out, in_, **kw)

    def dma_ind(self, out, in_, idx, scatter, bound):
        oa, ia, xa = out.ap, in_.ap, idx.ap

        def fn(e):
            off = bass.IndirectOffsetOnAxis(ap=xa, axis=0)
            if scatter:
                return e.indirect_dma_start(out=oa, out_offset=off, in_=ia, in_offset=None, bounds_check=None, oob_is_err=False)
            return e.indirect_dma_start(out=oa, out_offset=None, in_=ia, in_offset=off, bounds_check=None, oob_is_err=False)
        self.S._mk('pool', fn, [in_.buf, idx.buf], [out.buf], True, q='pool')

    def bcast(self, name, row_view, n):
        b = self.S.alloc(name, [128, n], F32)
        self.dma('sp', b[:, :], row_view.partition_broadcast(128))
        return b

    def rstd(self, ss, n, name):
        self.act(ss, ss, AF.Sqrt, bias=self.eps[0:ss.ap.shape[0], 0:1], scale=1.0 / n)
        self.recip(ss, ss)

    def build(self):
        S = self.S
        d = {}
        self.d = d
        import os
        self.flags = set(os.environ.get('KFLAGS', '').split(','))
        om = 'only_moe' in self.flags

        def inp(name, shape):
            d[name] = S.dram(name, shape, F32, kind="ExternalInput")
        inp('xin', [NT, DM])
        inp('cc', [128, 16])
        inp('ada_w', [2, DM, 6 * DM])
        inp('ada_b', [2, 6 * DM])
        inp('norm1_w', [2, DM])
        inp('norm2_w', [2, DM])
        inp('w_in', [2, DM, NIN])
        inp('w_out', [2, DM, DM])
        inp('convT', [2, 512, 3])
        inp('ig_b', [2, 8])
        inp('fg_b', [2, 8])
        inp('ml_nw', [2, 256])
        inp('na_bm', [2, 6, 14, 128, 256])
        inp('q_nw', [2, 512])
        inp('kv_nw', [2, 256])
        inp('w_uq', [2, 512, 576])
        inp('w_ukv', [2, 256, 768])
        inp('ffn_w1', [DM, DFF])
        inp('ffn_w3', [DM, DFF])
        inp('ffn_w2', [DFF, DM])
        inp('router', [DM, 8])
        inp('moe_w1', [8 * 11 * 128, 2048])
        inp('moe_w3', [8 * 11 * 128, 2048])
        inp('moe_w2', [8 * 11 * 128, 2048])
        inp('c_th', [128, 16])
        inp('c_j512', [128, NBLK])
        inp('c_gp', [128, 11])
        inp('fin_w', [DM])
        inp('ident', [128, 128])
        inp('tri0', [128, 128])
        inp('tri1', [128, 128])
        inp('rope_cs', [4096, 16])
        inp('rope_sn', [4096, 16])
        d['out'] = S.dram('out', [4096, DM], F32, kind="ExternalOutput")
        kind = "ExternalOutput" if DEBUG else "Internal"
        d['mod'] = S.dram('mod', [2, 2, 6 * DM], F32, kind=('ExternalInput' if om else kind))
        d['pT_ml'] = S.dram('pT_ml', [512, NT], F32, kind=kind)
        d['pT_na'] = S.dram('pT_na', [768, NT], BF16, kind=kind)
        d['p_tok'] = S.dram('p_tok', [NT, 1712], F32, kind=kind)
        d['cat'] = S.dram('cat', [DM, NT], BF16, kind=kind)
        d['x1'] = S.dram('x1', [NT, DM], F32, kind=('ExternalInput' if om else kind))
        d['x2'] = S.dram('x2', [NT, DM], F32, kind=kind)
        d['h2T'] = S.dram('h2T', [DM, NT], BF16, kind=kind)
        d['gates'] = S.dram('gatesd', [NT, 8], F32, kind=('ExternalInput' if om else kind))
        d['sel1'] = S.dram('sel1d', [NT, 8], F32, kind=('ExternalInput' if om else kind))
        d['h2'] = S.dram('h2d', [NT, DM], BF16, kind=('ExternalInput' if om else kind))
        d['hs'] = S.dram('hsd', [NSLOT, DM], BF16, kind=kind)
        d['yb'] = S.dram('ybd', [NSLOT, DM], F32, kind=kind)

        for nm in ('pT_ml', 'pT_na', 'p_tok', 'cat', 'x1', 'x2', 'h2T', 'h2', 'gates', 'sel1', 'out', 'yb'):
            d[nm].multiw = True
        self.identb = S.alloc('identb', [128, 128], BF16)
        self.dma('pool', self.identb[:, :], d['ident'][:, :])
        self.identf = S.alloc('identf', [128, 128], F32)
        self.dma('sp', self.identf[:, :], d['ident'][:, :])
        self.tri0 = S.alloc('tri0', [128, 128], F32)
        self.dma('sp', self.tri0[:, :], d['tri0'][:, :])
        self.tri1 = S.alloc('tri1', [128, 128], F32)
        self.dma('sp', self.tri1[:, :], d['tri1'][:, :])
        self.onesf = S.alloc('onesf', [128, 128], F32)
        self.memset(self.onesf[:, :], 1.0)
        self.eps = S.alloc('eps', [128, 1], F32)
        self.memset(self.eps[:, :], EPS)

        stop = getattr(self, 'stop_after', None)
        xs = d['xin']
        if 'only_moe' in self.flags:
            self.stage_moe(1)
            S.emit()
            return
        for l in range(2):
            last = l == 1
            if 'only_mla' not in self.flags:
                self.adaln(l)
                if stop == ('adaln', l):
                    break
                self.stage_in(l, xs)
                if stop == ('in', l):
                    break
                if 'inter' not in self.flags:
                    for _ in self.stage_mlstm(l, last):
                        pass
                    if stop == ('ml', l):
                        break
                    for _ in self.stage_na(l, last):
                        pass
                else:
                    cm = {'banks': [3], 'accs': [4, 5], 'i': 0, 'j': 0}
                    cn = {'banks': [0, 1, 2], 'accs': [6, 7], 'i': 0, 'j': 0, 'depth': 2}
                    self.interleave([(self.stage_mlstm(l, last), cm, 1), (self.stage_na(l, last), cn, 4)])
            if stop == ('na', l):
                break
            self.stage_mla(l, last)
            if stop in (('mla', l), ('mla1', l)):
                break
            self.stage_out(l, xs, last)
            if stop == ('out', l):
                break
            if last:
                self.stage_moe(l)
            else:
                self.stage_ffn(l, last)
            if stop == ('ffn', l):
                break
            xs = d['x2']
        S.emit()

    def adaln(self, l):
        S, d = self.S, self.d
        cc = S.alloc('cc', [128, 16], F32)
        self.dma('sp', cc[:, :], d['cc'][:, :])
        scT = S.alloc('scT', [128, 16], BF16)
        self.act(scT[:, :], cc[:, :], AF.Silu)
        sc3 = scT[:, :].rearrange('p (k r) -> p k r', r=2)
        adab = S.alloc('adab', [2, 6 * DM], F32)
        self.dma('sp', adab[:, :], d['ada_b'][l, :].partition_broadcast(2))
        modrow = S.alloc('modrow', [2, 6 * DM], F32)
        wts = [S.alloc(f'adaw{i}', [128, 8, 512], BF16) for i in range(2)]
        wsrc = d['ada_w'][l].rearrange('(k p) n -> p k n', p=128)
        for j in range(12):
            wt = wts[j % 2]
            self.dma('pool', wt[:, :, :], wsrc[:, :, j * 512:(j + 1) * 512])
            ps = self.ps()
            for k in range(8):
                self.mm(ps[0:2, 0:512], sc3[:, k, :], wt[:, k, :], start=(k == 0), stop=(k == 7))
            self.tt(modrow[:, j * 512:(j + 1) * 512], ps[0:2, 0:512], adab[:, j * 512:(j + 1) * 512], ALU.add)
        self.dma('pool', d['mod'][l], modrow[:, :])
        S.free(cc, scT, adab, modrow, *wts)

    def mod_ab(self, l, which, nw_name, tag):
        S, d = self.S, self.d
        nwb = self.bcast(f'nwb{tag}', d[nw_name][l, :], DM)
        res = []
        for r in range(2):
            sc = self.bcast(f'sc{tag}{r}', d['mod'][l, r, (3 * which + 1) * DM:(3 * which + 2) * DM], DM)
            self.stt(sc[:, :], sc[:, :], 1.0, nwb[:, :], ALU.add, ALU.mult)
            sh = self.bcast(f'sh{tag}{r}', d['mod'][l, r, (3 * which) * DM:(3 * which + 1) * DM], DM)
            res.append((sc, sh))
        S.free(nwb)
        return res

    def norm_mod(self, xt, A, B, hb, tmp, ss):
        self.act(tmp[:, :], xt[:, :], AF.Square, accum=ss[:, 0:1])
        self.rstd(ss[:, 0:1], DM, 'r')
        self.stt(tmp[:, :], xt[:, :], ss[:, 0:1], A[:, :], ALU.mult, ALU.mult)
        self.tt(hb[:, :], tmp[:, :], B[:, :], ALU.add)

    def transpose8(self, hb, dst3):
        ps = self.ps()
        psb = ps[:, :].bitcast(BF16)
        for k in range(8):
            self.tr(psb[:, k * 128:(k + 1) * 128], hb[:, k * 128:(k + 1) * 128], self.identb[:, :])
        self.cp(dst3, psb[:, :].rearrange('p (k t) -> p k t', k=8), eng='act')

    def stage_in(self, l, xs):
        S, d = self.S, self.d
        w = S.alloc('w_in', [128, 8, NIN], BF16)
        wsrc = d['w_in'][l].rearrange('(k p) n -> p k n', p=128)
        for k in range(8):
            self.dma('pool', w[:, k, :], wsrc[:, k, :])
        ab = self.mod_ab(l, 0, 'norm1_w', 'a')
        hTs = [S.alloc(f'hT{i}', [128, 8, 512], BF16) for i in range(2)]
        xts = [S.alloc(f'xt{i}', [128, DM], F32) for i in range(2)]
        tmps = [S.alloc(f'tmp{i}', [128, DM], F32) for i in range(2)]
        hbs = [S.alloc(f'hb{i}', [128, DM], BF16) for i in range(2)]
        sss = [S.alloc(f'ss{i}', [128, 1], F32) for i in range(2)]
        fms = [S.alloc(f'fm{i}', [128, 512], F32) for i in range(2)]
        fnb = [S.alloc(f'fnb{i}', [128, 512], BF16) for i in range(2)]
        pts = [S.alloc(f'pt{i}', [128, 1712], F32) for i in range(2)]
        groups = [(0, 2)] + [(2 + 4 * i, 4) for i in range(8)]
        fcols = [0, 128, 256, 384] + [1040 + 128 * i for i in range(6)]
        tcols = [(512, 1024, 0), (1024, 1040, 512), (1808, 2320, 528), (2320, 2832, 1040), (2832, 2992, 1552)]
        ti = 0
        for gi, (t0, nt) in enumerate(groups):
            hT = hTs[gi % 2]
            G = nt * 128
            A, B = ab[1] if gi == 0 else ab[0]
            for j in range(nt):
                n = t0 + j
                xt = xts[ti % 2]
                hb = hbs[ti % 2]
                ss = sss[ti % 2]
                tmp = tmps[ti % 2]
                ti += 1
                self.dma('sp', xt[:, :], xs[n * 128:(n + 1) * 128, :])
                self.norm_mod(xt, A, B, hb, tmp, ss)
                self.transpose8(hb, hT[:, :, j * 128:(j + 1) * 128])
            for ci, c0 in enumerate(fcols):
                ps = self.ps()
                for k in range(8):
                    self.mm(ps[:, 0:G], w[:, k, c0:c0 + 128], hT[:, k, 0:G], start=(k == 0), stop=(k == 7))
                if ci < 4:
                    fm = fms[ci % 2]
                    self.cp(fm[:, 0:G], ps[:, 0:G], eng='act')
                    self.dma('pool', d['pT_ml'][ci * 128:(ci + 1) * 128, t0 * 128:t0 * 128 + G], fm[:, 0:G])
                else:
                    fb = fnb[ci % 2]
                    if ci < 7:
                        self.ts(fb[:, 0:G], ps[:, 0:G], 0.125, ALU.mult)
                    else:
                        self.cp(fb[:, 0:G], ps[:, 0:G], eng='act')
                    self.dma('pool', d['pT_na'][(ci - 4) * 128:(ci - 3) * 128, t0 * 128:t0 * 128 + G], fb[:, 0:G])
            for j in range(nt):
                n = t0 + j
                pt = pts[n % 2]
                for ii, (c0, c1, o0) in enumerate(tcols):
                    ps = self.ps()
                    for k in range(8):
                        self.mm(ps[:, 0:c1 - c0], hT[:, k, j * 128:(j + 1) * 128], w[:, k, c0:c1], start=(k == 0), stop=(k == 7))
                    if ii % 2 == 0:
                        self.cp(pt[:, o0:o0 + c1 - c0], ps[:, 0:c1 - c0], eng='dve')
                    else:
                        self.cp(pt[:, o0:o0 + c1 - c0], ps[:, 0:c1 - c0], eng='act')
                self.dma('pool', d['p_tok'][n * 128:(n + 1) * 128, :], pt[:, :])
        S.free(w, *tmps, *hTs, *xts, *hbs, *sss, *fms, *fnb, *pts)
        for a, b in ab:
            S.free(a, b)

    def stage_mlstm(self, l, last):
        S, d = self.S, self.d

        def tcol(n):
            return 1 + n * 128 if n < 2 else 2 + n * 128
        gt = S.alloc('gt', [128, NTILE, 16], F32)
        self.dma('sp', gt[:, :, :], d['p_tok'][:, 512:528].rearrange('(n p) c -> p n c', p=128))
        igb = self.bcast('igb', d['ig_b'][l, :], 8)
        fgb = self.bcast('fgb', d['fg_b'][l, :], 8)
        LI = S.alloc('LI', [128, NTILE, 8], F32)
        LF = S.alloc('LF', [128, NTILE, 8], F32)
        self.tt(LI[:, :, :], gt[:, :, 0:8], igb[:, :].unsqueeze(1).to_broadcast([128, NTILE, 8]), ALU.add)
        self.tt(LF[:, :, :], gt[:, :, 8:16], fgb[:, :].unsqueeze(1).to_broadcast([128, NTILE, 8]), ALU.add)
        self.act(LF[:, :, :], LF[:, :, :], AF.Exp, scale=-1.0)
        self.act(LF[:, :, :], LF[:, :, :], AF.Ln, bias=self.onesf[:, 0:1], scale=1.0)
        self.ts(LF[:, :, :], LF[:, :, :], -1.0, ALU.mult)
        LF2 = LF[:, :, :].rearrange('p n c -> p (n c)')
        NC = NTILE * 8
        Bc = S.alloc('Bc', [128, NTILE, 2, 4], F32)
        EB = S.alloc('EB', [128, NTILE, 2, 4], F32)
        ES = S.alloc('ES', [128, NTILE, 2, 4], F32)
        EG = S.alloc('EG', [128, NTILE, 2, 4], F32)
        psA = self.ps()
        self.mm(psA[:, 0:NC], self.tri0[:, :], LF2)
        self.cp(Bc[:, :, 0, :], psA[:, 0:NC].rearrange('p (n d h) -> p n d h', d=2, h=4)[:, :, 0, :])
        psB = self.ps()
        self.mm(psB[:, 0:NC], self.tri1[:, :], LF2)
        self.cp(Bc[:, :, 1, :], psB[:, 0:NC].rearrange('p (n d h) -> p n d h', d=2, h=4)[:, :, 1, :])
        psC = self.ps()
        self.mm(psC[:, 0:NC], self.onesf[:, :], LF2)
        self.act(EG[:, :, :, :].rearrange('p n d h -> p (n d h)'), psC[:, 0:NC], AF.Exp)
        Bc2 = Bc[:, :, :, :].rearrange('p n d h -> p (n d h)')
        self.act(EB[:, :, :, :].rearrange('p n d h -> p (n d h)'), Bc2, AF.Exp)
        ES2 = ES[:, :, :, :].rearrange('p n d h -> p (n d h)')
        self.tt(ES2, LI[:, :, :].rearrange('p n c -> p (n c)'), Bc2, ALU.subtract)
        self.act(ES2, ES2, AF.Exp)
        yield
        S.free(gt, igb, fgb, LI, LF, Bc)

        convw = S.alloc('convw', [128, 4, 3], F32)
        self.dma('sp', convw[:, :, :], d['convT'][l].rearrange('(c p) j -> p c j', p=128))
        W = NT + 3
        qkT = S.alloc('qkT', [128, 4, W], BF16)
        x32 = S.alloc('x32', [128, W], F32)
        acc = S.alloc('acc', [128, W], F32)
        for c in (0, 257, W - 1):
            self.memset(x32[:, c:c + 1], 0.0)
        for fc in range(4):
            self.dma('sp', x32[:, 1:257], d['pT_ml'][fc * 128:(fc + 1) * 128, 0:256])
            self.dma('sp', x32[:, 258:W - 1], d['pT_ml'][fc * 128:(fc + 1) * 128, 256:NT])
            a = acc[:, 1:W - 1]
            self.ts(a, x32[:, 0:W - 2], convw[:, fc, 0:1], ALU.mult)
            self.stt(a, x32[:, 1:W - 1], convw[:, fc, 1:2], a, ALU.mult, ALU.add)
            self.stt(a, x32[:, 2:W], convw[:, fc, 2:3], a, ALU.mult, ALU.add)
            if fc < 2:
                self.act(qkT[:, fc, 1:W - 1], a, AF.Silu)
            else:
                self.act(a, a, AF.Silu)
                self.ts(qkT[:, fc, 1:W - 1], a, 0.125, ALU.mult)
            yield
        S.free(x32, acc, convw)

        ktok = S.alloc('ktok', [128, NTILE, 256], BF16)
        for n in range(NTILE):
            ps = self.ps()
            psb = ps[:, :].bitcast(BF16)
            for kc in range(2):
                self.tr(psb[:, kc * 128:(kc + 1) * 128], qkT[:, 2 + kc, tcol(n):tcol(n) + 128], self.identb[:, :])
            self.cp(ktok[:, n, :], psb[:, 0:256], eng=('act' if n % 2 else 'dve'))
            if n % 4 == 3:
                yield

        Vp = S.alloc('Vp', [128, NTILE, 2, 4, 65], BF16)
        Xb = S.alloc('Xb', [128, NTILE, 2, 4, 65], BF16)
        v32 = S.alloc('v32', [128, 9, 256], F32)
        for t0 in range(0, NTILE, 9):
            tn = min(9, NTILE - t0)
            self.dma('sp', v32[:, 0:tn, :], d['p_tok'][t0 * 128:(t0 + tn) * 128, 0:256].rearrange('(n p) c -> p n c', p=128))
            for dd in range(2):
                self.tt(Vp[:, t0:t0 + tn, dd, :, 0:64], v32[:, 0:tn, :].rearrange('p n (h e) -> p n h e', h=4),
                        ES[:, t0:t0 + tn, dd, :].unsqueeze(3).to_broadcast([128, tn, 4, 64]), ALU.mult)
            yield
        for dd in range(2):
            self.cp(Vp[:, :, dd, :, 64:65], ES[:, :, dd, :].unsqueeze(3))
        S.free(v32, ES)

        Xf = S.alloc('Xf', [128, 2, 4, 65], F32)
        self.memset(Xf[:, :, :, :].rearrange('p d h e -> p (d h e)'), 0.0)
        order = [list(range(NTILE)), [1, 0] + list(range(NTILE - 1, 1, -1))]
        for step in range(NTILE):
            for dd in range(2):
                n = order[dd][step]
                self.cp(Xb[:, n, dd, :, :], Xf[:, dd, :, :], eng='act')
                ps = self.ps()
                psv = ps[:, 0:260].rearrange('p (h e) -> p h e', h=4)
                for h in range(4):
                    self.mm(psv[:, h, :], ktok[:, n, (h // 2) * 128:(h // 2) * 128 + 128], Vp[:, n, dd, h, :])
                self.tt(Xf[:, dd, :, :], Xf[:, dd, :, :], psv, ALU.add)
                self.tt(Xf[:, dd, :, :], Xf[:, dd, :, :], EG[:, n, dd, :].unsqueeze(2).to_broadcast([128, 4, 65]), ALU.mult)
            yield
        S.free(Xf, EG, ktok)

        mlnw = self.bcast('mlnw', d['ml_nw'][l, :], 256)
        Sms = [S.alloc(f'Sm{i}', [128, 128], BF16) for i in range(4)]
        Hd = [S.alloc(f'Hd{i}', [128, 4, 65], F32) for i in range(2)]
        den = S.alloc('den', [128, 2, 4], F32)
        hs = S.alloc('hs', [128, 4, 64], F32)
        ht = S.alloc('ht', [128, 4, 64], F32)
        ssq = S.alloc('ssq', [128, 4], F32)
        o32 = [S.alloc(f'o32{i}', [128, 256], F32) for i in range(2)]
        res = [S.alloc(f'mres{i}', [128, 256], BF16) for i in range(2)]
        resT = [S.alloc(f'mresT{i}', [128, 2, 128], BF16) for i in range(2)]
        smi = 0
        for n in range(2 if last else 0, NTILE):
            psO = [self.psacc(), self.psacc()]
            psOv = [p[:, 0:260].rearrange('p (h e) -> p h e', h=4) for p in psO]
            c0 = tcol(n)
            for h in range(4):
                pb = (h % 2) * 64
                qT = qkT[pb:pb + 64, h // 2, c0:c0 + 128]
                kT = qkT[pb:pb + 64, 2 + h // 2, c0:c0 + 128]
                psS = self.ps()
                self.mm(psS[:, 0:128], kT, qT)
                for dd in range(2):
                    Sm = Sms[smi % 4]
                    smi += 1
                    self.tt(Sm[:, :], psS[:, 0:128], (self.tri0 if dd == 0 else self.tri1)[:, :], ALU.mult)
                    self.mm(psOv[dd][:, h, :], Sm[:, :], Vp[:, n, dd, h, :], start=True, stop=False)
                    self.mm(psOv[dd][:, h, :], qT, Xb[pb:pb + 64, n, dd, h, :], start=False, stop=True)
                yield
            for dd in range(2):
                self.tt(Hd[dd][:, :, :], psOv[dd], EB[:, n, dd, :].unsqueeze(2).to_broadcast([128, 4, 65]), ALU.mult)
                self.stt(den[:, dd, :], Hd[dd][:, :, 64], -1.0, Hd[dd][:, :, 64], ALU.mult, ALU.max)
            self.ts(den[:, :, :], den[:, :, :], 1.0, ALU.max)
            self.recip(den[:, :, :], den[:, :, :])
            self.tt(hs[:, :, :], Hd[0][:, :, 0:64], den[:, 0, :].unsqueeze(2).to_broadcast([128, 4, 64]), ALU.mult)
            self.tt(ht[:, :, :], Hd[1][:, :, 0:64], den[:, 1, :].unsqueeze(2).to_broadcast([128, 4, 64]), ALU.mult)
            self.tt(hs[:, :, :], hs[:, :, :], ht[:, :, :], ALU.add)
            self.tt(ht[:, :, :], hs[:, :, :], hs[:, :, :], ALU.mult)
            self.red(ssq[:, :], ht[:, :, :], ALU.add)
            self.act(ssq[:, :], ssq[:, :], AF.Sqrt, bias=self.eps[:, 0:1], scale=1.0 / 64)
            self.recip(ssq[:, :], ssq[:, :])
            self.tt(hs[:, :, :], hs[:, :, :], ssq[:, :].unsqueeze(2).to_broadcast([128, 4, 64]), ALU.mult)
            hs2 = hs[:, :, :].rearrange('p h e -> p (h e)')
            self.tt(hs2, hs2, mlnw[:, :], ALU.mult)
            o = o32[n % 2]
            self.dma('sp', o[:, :], d['p_tok'][n * 128:(n + 1) * 128, 256:512])
            self.act(o[:, :], o[:, :], AF.Sigmoid)
            r = res[n % 2]
            self.tt(r[:, :], hs2, o[:, :], ALU.mult)
            pst = self.ps()
            pstb = pst[:, :].bitcast(BF16)
            for c in range(2):
                self.tr(pstb[:, c * 128:(c + 1) * 128], r[:, c * 128:(c + 1) * 128], self.identb[:, :])
            rT = resT[n % 2]
            self.cp(rT[:, :, :], pstb[:, 0:256].rearrange('p (c t) -> p c t', c=2), eng='act')
            self.dma('pool', d['cat'][0:256, n * 128:(n + 1) * 128].rearrange('(c p) t -> p c t', p=128), rT[:, :, :])
            yield
        S.free(mlnw, den, hs, ht, ssq, qkT, Vp, Xb, EB, *Sms, *Hd, *o32, *res, *resT)

    def attn_block(self, kT_of, qTv, nq, keys, Va_of, out_dram, bm_of=None):
        psO = self.psacc()
        nk = len(keys)

        def issue_s(i):
            kt, cfg = keys[i]
            psS = self.ps()
            self.mm(psS[:, 0:nq], kT_of(kt), qTv, start=True, stop=True)
            return psS
        D = self.cur.get('depth', 2)
        pend = [issue_s(i) for i in range(min(D, nk))]
        for i in range(nk):
            psS = pend.pop(0)
            if i + D < nk:
                pend.append(issue_s(i + D))
            P = self.Ps[self.pi % len(self.Ps)]
            self.pi += 1
            self.act(P[:, 0:nq], psS[:, 0:nq], AF.Exp)
            if keys[i][1] is not None:
                self.tt(P[:, 0:nq], P[:, 0:nq], bm_of(keys[i][1]), ALU.mult)
            self.mm(psO[:, 0:nq], Va_of(keys[i][0]), P[:, 0:nq], start=(i == 0), stop=(i == nk - 1))
            yield
        r = self.pi % 2
        rsb, rinv, ob = self.rsbs[r], self.rinvs[r], self.obs[r]
        self.act(rsb[64:65, 0:nq], psO[64:65, 0:nq], AF.Ln)
        self.act(rsb[64:65, 0:nq], rsb[64:65, 0:nq], AF.Exp, scale=-1.0)
        psR = self.ps()
        self.mm(psR[0:64, 0:nq], self.onesf[64:65, 0:64], rsb[64:65, 0:nq])
        self.cp(rinv[:, 0:nq], psR[0:64, 0:nq], eng='act')
        self.tt(ob[:, 0:nq], psO[0:64, 0:nq], rinv[:, 0:nq], ALU.mult)
        self.dma('pool', out_dram, ob[:, 0:nq])
        yield

    def attn_bufs(self):
        S = self.S
        self.Ps = [S.alloc(f'P{i}', [128, 512], BF16) for i in range(3)]
        self.rsbs = [S.alloc(f'rsb{i}', [128, 512], F32) for i in range(2)]
        self.rinvs = [S.alloc(f'rinv{i}', [64, 512], F32) for i in range(2)]
        self.obs = [S.alloc(f'ob{i}', [64, 512], BF16) for i in range(2)]
        self.pi = 0
        return [*self.Ps, *self.rsbs, *self.rinvs, *self.obs]

    def stage_na(self, l, last):
        S, d = self.S, self.d
        ab = self.attn_bufs()
        qT = S.alloc('naq', [64, NT], BF16)
        kT = S.alloc('nak', [64, NT], BF16)
        Va = S.alloc('nav', [128, NTILE, 128], BF16)
        self.memset(Va[:, :, :].rearrange('p n e -> p (n e)'), 0.0)
        bm = S.alloc('nabm', [128, 14, 256], BF16)
        ebm = S.alloc('naebm', [128, 14, 256], BF16)
        vst = S.alloc('vst', [128, NTILE, 64], F32)
        for h in range(6):
            self.dma('sp', qT[:, :], d['pT_na'][h * 64:(h + 1) * 64, :])
            self.dma('sp', kT[:, :], d['pT_na'][384 + h * 64:384 + (h + 1) * 64, :])
            self.memset(Va[:, :, 64:65], 1.0)
            self.dma('sp', vst[:, :, :], d['p_tok'][:, 528 + h * 64:528 + (h + 1) * 64].rearrange('(n p) c -> p n c', p=128))
            self.cp(Va[:, :, 0:64], vst[:, :, :], eng='act')
            for cfg in range(14):
                self.dma('pool', bm[:, cfg, :], d['na_bm'][l, h, cfg])
            self.act(ebm[:, :, :].rearrange('p c q -> p (c q)'), bm[:, :, :].rearrange('p c q -> p (c q)'), AF.Exp)
            blocks = []
            if not last:
                blocks.append((0, [(0, None), (1, None)]))
            for qb in range(16):
                if qb == 0:
                    lat = [(2 + j, 6 + j) for j in range(4)]
                elif qb == 15:
                    lat = [(2 + 28 + j, 10 + j) for j in range(4)]
                else:
                    lat = [(2 + 2 * qb - 2 + i, i) for i in range(6)]
                blocks.append((256 + qb * 256, [(0, None), (1, None)] + lat))
            for q0, keys in blocks:
                yield from self.attn_block(lambda kt: kT[:, kt * 128:(kt + 1) * 128], qT[:, q0:q0 + 256], 256, keys,
                                           lambda kt: Va[:, kt, :], d['cat'][256 + h * 64:256 + (h + 1) * 64, q0:q0 + 256],
                                           bm_of=lambda cfg: ebm[:, cfg, :])
        S.free(vst, qT, kT, Va, bm, ebm, *ab)

    def rope(self, xv, nh, cs, sn, tmp):
        x5 = xv.rearrange('p h (a b f) -> p h a b f', a=2, b=2)
        x1 = x5[:, :, :, 0, :]
        x2 = x5[:, :, :, 1, :]
        csb = cs[:, :].rearrange('p (a f) -> p a f', a=2).unsqueeze(1).to_broadcast([128, nh, 2, 8])
        snb = sn[:, :].rearrange('p (a f) -> p a f', a=2).unsqueeze(1).to_broadcast([128, nh, 2, 8])
        t = [tmp[:, i, 0:nh * 16].rearrange('p (h a f) -> p h a f', h=nh, a=2) for i in range(4)]
        self.tt(t[0], x1, csb, ALU.mult)
        self.tt(t[1], x2, snb, ALU.mult)
        self.tt(t[2], x2, csb, ALU.mult)
        self.tt(t[3], x1, snb, ALU.mult)
        self.tt(x1, t[0], t[1], ALU.subtract)
        self.tt(x2, t[2], t[3], ALU.add)

    def stage_mla(self, l, last):
        S, d = self.S, self.d
        wuq = S.alloc('wuq', [128, 4, 576], BF16)
        self.dma('pool', wuq[:, :, :], d['w_uq'][l].rearrange('(k p) n -> p k n', p=128))
        wukv = S.alloc('wukv', [128, 2, 768], BF16)
        self.dma('pool', wukv[:, :, :], d['w_ukv'][l].rearrange('(k p) n -> p k n', p=128))
        qnw = self.bcast('qnw', d['q_nw'][l, :], 512)
        kvnw = self.bcast('kvnw', d['kv_nw'][l, :], 256)
        qT = S.alloc('mqT', [96, 6, NT], BF16)
        kT = S.alloc('mkT', [96, 6, NT], BF16)
        Va = S.alloc('mVa', [128, NTILE, 6, 128], BF16)
        self.memset(Va[:, :, :, :].rearrange('p n h e -> p (n h e)'), 0.0)
        self.memset(Va[:, :, :, 64:65], 1.0)
        pms = [S.alloc(f'pm{i}', [128, 800], F32) for i in range(2)]
        rings = []
        for nm, shp, dt in (('junk', [128, 512], F32), ('ssa', [128, 2], F32), ('cn', [128, 768], BF16), ('cT', [128, 6, 128], BF16),
                            ('qf32', [128, 576], F32), ('qb16', [128, 6, 128], BF16), ('kfull', [128, 6, 128], BF16),
                            ('krr', [128, 32], F32), ('rtmp', [128, 4, 96], F32)):
            rings.append([S.alloc(f'{nm}{i}', shp, dt) for i in range(2)])
        for i in range(2):
            self.memset(rings[5][i][:, :, :].rearrange('p h e -> p (h e)'), 0.0)
            self.memset(rings[6][i][:, :, :].rearrange('p h e -> p (h e)'), 0.0)
        css = [S.alloc(f'cs{i}', [128, 16], F32) for i in range(2)]
        sns = [S.alloc(f'sn{i}', [128, 16], F32) for i in range(2)]
        scale = 96 ** -0.5
        for n in range(3 if 'mla_short' in self.flags else NTILE):
            pm = pms[n % 2]
            junk, ssa, cn, cT, qf32, qb16, kfull, krr, rtmp = [rg[n % 2] for rg in rings]
            self.dma('sp', pm[:, :], d['p_tok'][n * 128:(n + 1) * 128, 912:1712])
            cs, sn = css[n % 2], sns[n % 2]
            if n >= 2:
                self.dma('sp', cs[:, :], d['rope_cs'][(n - 2) * 128:(n - 1) * 128, :])
                self.dma('sp', sn[:, :], d['rope_sn'][(n - 2) * 128:(n - 1) * 128, :])
            self.act(junk[:, 0:512], pm[:, 0:512], AF.Square, accum=ssa[:, 0:1])
            self.act(junk[:, 0:256], pm[:, 512:768], AF.Square, accum=ssa[:, 1:2])
            self.rstd(ssa[:, 0:1], 512, 'q')
            self.rstd(ssa[:, 1:2], 256, 'kv')
            self.stt(cn[:, 0:512], pm[:, 0:512], ssa[:, 0:1], qnw[:, :], ALU.mult, ALU.mult)
            self.stt(cn[:, 512:768], pm[:, 512:768], ssa[:, 1:2], kvnw[:, :], ALU.mult, ALU.mult)
            ps = self.ps()
            psb = ps[:, :].bitcast(BF16)
            for k in range(6):
                self.tr(psb[:, k * 128:(k + 1) * 128], cn[:, k * 128:(k + 1) * 128], self.identb[:, :])
            self.cp(cT[:, :, :], psb[:, 0:768].rearrange('p (k t) -> p k t', k=6), eng='act')
            p1, p2 = self.ps(), self.ps()
            for k in range(4):
                self.mm(p1[:, 0:512], cT[:, k, :], wuq[:, k, 0:512], start=(k == 0), stop=(k == 3))
            for k in range(4):
                self.mm(p2[:, 0:64], cT[:, k, :], wuq[:, k, 512:576], start=(k == 0), stop=(k == 3))
            self.ts(qf32[:, 0:512], p1[:, 0:512], scale, ALU.mult)
            self.ts(qf32[:, 512:576], p2[:, 0:64], scale, ALU.mult)
            if n >= 2 and 'norope' not in self.flags:
                self.rope(qf32[:, :].rearrange('p (h e) -> p h e', h=6)[:, :, 64:96], 6, cs, sn, rtmp)
            self.cp(qb16[:, :, 0:96], qf32[:, :].rearrange('p (h e) -> p h e', h=6), eng='act')
            ps = self.ps()
            psb = ps[:, :].bitcast(BF16)
            if 'no_qtr' not in self.flags:
                for h in range(6):
                    self.tr(psb[:, h * 128:(h + 1) * 128], qb16[:, h, :], self.identb[:, :])
                for (pa_, pb2) in ((0, 64), (64, 96)):
                    self.cp(qT[pa_:pb2, :, n * 128:(n + 1) * 128], psb[pa_:pb2, 0:768].rearrange('p (h t) -> p h t', h=6), eng='act')
            k1, k2 = self.ps(), self.ps()
            for k in range(2):
                self.mm(k1[:, 0:512], cT[:, 4 + k, :], wukv[:, k, 0:512], start=(k == 0), stop=(k == 1))
            for k in range(2):
                self.mm(k2[:, 0:256], cT[:, 4 + k, :], wukv[:, k, 512:768], start=(k == 0), stop=(k == 1))
            k1v = k1[:, 0:512].rearrange('p (h e) -> p h e', h=4)
            k2v = k2[:, 0:256].rearrange('p (h e) -> p h e', h=2)
            self.cp(kfull[:, 0:4, 0:64], k1v[:, :, 0:64])
            self.cp(kfull[:, 4:6, 0:64], k2v[:, :, 0:64])
            self.cp(Va[:, n, 0:4, 0:64], k1v[:, :, 64:128], eng='act')
            self.cp(Va[:, n, 4:6, 0:64], k2v[:, :, 64:128], eng='act')
            self.cp(krr[:, :], pm[:, 768:800])
            if n >= 2 and 'norope' not in self.flags:
                self.rope(krr[:, :].unsqueeze(1), 1, cs, sn, rtmp)
            self.cp(kfull[:, :, 64:96], krr[:, :].unsqueeze(1).to_broadcast([128, 6, 32]))
            ps = self.ps()
            psb = ps[:, :].bitcast(BF16)
            if 'no_ktr' not in self.flags:
                for h in range(6):
                    self.tr(psb[:, h * 128:(h + 1) * 128], kfull[:, h, :], self.identb[:, :])
                for (pa_, pb2) in ((0, 64), (64, 96)):
                    self.cp(kT[pa_:pb2, :, n * 128:(n + 1) * 128], psb[pa_:pb2, 0:768].rearrange('p (h t) -> p h t', h=6), eng='dve')
        S.free(wuq, wukv, qnw, kvnw, *pms, *css, *sns, *[b for rg in rings for b in rg])

        if getattr(self, 'stop_after', None) == ('mla1', l):
            S.free(qT, kT, Va)
            return
        ab = self.attn_bufs()
        allk = [(kt, None) for kt in range(NTILE)]
        for h in range(6):
            if not last:
                for _ in self.attn_block(lambda kt: kT[:, h, kt * 128:(kt + 1) * 128], qT[:, h, 0:256], 256, [(0, None), (1, None)],
                                         lambda kt: Va[:, kt, h, :], d['cat'][640 + h * 64:640 + (h + 1) * 64, 0:256]):
                    pass
            for b in range(8):
                q0 = 256 + b * 512
                for _ in self.attn_block(lambda kt: kT[:, h, kt * 128:(kt + 1) * 128], qT[:, h, q0:q0 + 512], 512, allk,
                                         lambda kt: Va[:, kt, h, :], d['cat'][640 + h * 64:640 + (h + 1) * 64, q0:q0 + 512]):
                    pass
        S.free(qT, kT, Va, *ab)

    def stage_out(self, l, xs, last):
        S, d = self.S, self.d
        w = S.alloc('w_out', [128, 8, DM], BF16)
        wsrc = d['w_out'][l].rearrange('(k p) n -> p k n', p=128)
        for k in range(8):
            self.dma('pool', w[:, k, :], wsrc[:, k, :])
        ab = self.mod_ab(l, 1, 'norm2_w', 'b')
        g1 = [self.bcast(f'g1{r}', d['mod'][l, r, 2 * DM:3 * DM], DM) for r in range(2)]
        if last:
            rw = S.alloc('rw', [128, 8, 8], F32)
            self.dma('sp', rw[:, :, :], d['router'].rearrange('(k p) e -> p k e', p=128))
            hf = S.alloc('hf', [128, DM], F32)
            hT32 = S.alloc('hT32', [128, 8, 128], F32)
            lg = S.alloc('lg', [128, 8], F32)
            l2 = S.alloc('l2', [128, 8], F32)
            mx = S.alloc('mx', [128, 4], F32)
            gsb = S.alloc('gsb', [128, 8], F32)
            m1m = S.alloc('m1m', [128, 8], F32)
        catTs = [S.alloc(f'catT{i}', [128, 8, 128], BF16) for i in range(2)]
        xts = [S.alloc(f'xo{i}', [128, DM], F32) for i in range(2)]
        xn = [S.alloc(f'xn{i}', [128, DM], F32) for i in range(2)]
        tmps = [S.alloc(f'tmpo{i}', [128, DM], F32) for i in range(2)]
        hbos = [S.alloc(f'hbo{i}', [128, DM], BF16) for i in range(2)]
        hT = [S.alloc(f'hTo{i}', [128, 8, 128], BF16) for i in range(2)]
        sso = [S.alloc(f'sso{i}', [128, 1], F32) for i in range(2)]
        for n in range(NTILE):
            if last and n < 2:
                continue
            r = 1 if n < 2 else 0
            catT, tmp, hb, ss = catTs[n % 2], tmps[n % 2], hbos[n % 2], sso[n % 2]
            self.dma('sp', catT[:, :, :], d['cat'].rearrange('(k p) t -> p k t', p=128)[:, :, n * 128:(n + 1) * 128])
            xt = xts[n % 2]
            self.dma('sp', xt[:, :], xs[n * 128:(n + 1) * 128, :])
            x1 = xn[n % 2]
            for half in range(2):
                ps = self.ps()
                for k in range(8):
                    self.mm(ps[:, 0:512], catT[:, k, :], w[:, k, half * 512:(half + 1) * 512], start=(k == 0), stop=(k == 7))
                sl = slice(half * 512, (half + 1) * 512)
                self.tt(x1[:, sl], ps[:, 0:512], g1[r][:, sl], ALU.mult)
                self.tt(x1[:, sl], x1[:, sl], xt[:, sl], ALU.add)
            self.dma('pool', d['x1'][n * 128:(n + 1) * 128, :], x1[:, :])
            A, B = ab[r]
            if last:
                self.norm_mod(x1, A, B, hf, tmp, ss)
                self.cp(hb[:, :], hf[:, :], eng='act')
                self.dma('pool', d['h2'][n * 128:(n + 1) * 128, :], hb[:, :])
                pa, pb_ = self.ps(), self.ps()
                for k in range(8):
                    pp = pa if k < 4 else pb_
                    self.tr(pp[:, (k % 4) * 128:(k % 4 + 1) * 128], hf[:, k * 128:(k + 1) * 128], self.identf[:, :])
                self.cp(hT32[:, 0:4, :], pa[:, :].rearrange('p (k t) -> p k t', k=4))
                self.cp(hT32[:, 4:8, :], pb_[:, :].rearrange('p (k t) -> p k t', k=4), eng='act')
                pl = self.ps()
                for k in range(8):
                    self.mm(pl[:, 0:8], hT32[:, k, :], rw[:, k, :], start=(k == 0), stop=(k == 7))
                self.cp(lg[:, :], pl[:, 0:8])
                self.red(mx[:, 0:1], lg[:, :], ALU.max)
                self.ts(m1m[:, :], lg[:, :], mx[:, 0:1], ALU.is_equal)
                self.dma('pool', d['sel1'][n * 128:(n + 1) * 128, :], m1m[:, :])
                self.stt(l2[:, :], m1m[:, :], -1e30, lg[:, :], ALU.mult, ALU.add)
                self.red(mx[:, 1:2], l2[:, :], ALU.max)
                self.ts(l2[:, :], lg[:, :], mx[:, 1:2], ALU.is_ge)
                self.ts(mx[:, 2:3], mx[:, 0:1], -1.0, ALU.mult)
                self.act(gsb[:, :], lg[:, :], AF.Exp, bias=mx[:, 2:3], scale=1.0)
                self.tt(gsb[:, :], gsb[:, :], l2[:, :], ALU.mult)
                self.red(mx[:, 3:4], gsb[:, :], ALU.add)
                self.recip(mx[:, 3:4], mx[:, 3:4])
                self.ts(gsb[:, :], gsb[:, :], mx[:, 3:4], ALU.mult)
                self.dma('pool', d['gates'][n * 128:(n + 1) * 128, :], gsb[:, :])
            else:
                self.norm_mod(x1, A, B, hb, tmp, ss)
            ht = hT[n % 2]
            self.transpose8(hb, ht[:, :, :])
            self.dma('pool', d['h2T'].rearrange('(k p) t -> p k t', p=128)[:, :, n * 128:(n + 1) * 128], ht[:, :, :])
        fr = [w, *catTs, *tmps, *hbos, *sso, *xts, *xn, *hT, *g1]
        if last:
            fr += [rw, hf, hT32, lg, l2, mx, gsb, m1m]
        S.free(*fr)
        for a, b in ab:
            S.free(a, b)

    def stage_ffn(self, l, last):
        S, d = self.S, self.d
        g2 = [self.bcast(f'g2{r}', d['mod'][l, r, 5 * DM:6 * DM], DM) for r in range(2)]
        if last:
            fw = self.bcast('fw', d['fin_w'][:], DM)
            sbs = [(256 + i * 1024, 1024) for i in range(4)]
            experts = [(d['moe_w1'][e], d['moe_w3'][e], d['moe_w2'][e]) for e in range(8)]
        else:
            sbs = [(0, 256)] + [(256 + i * 1024, 1024) for i in range(4)]
            experts = [(d['ffn_w1'], d['ffn_w3'], d['ffn_w2'])]
        hTb = S.alloc('hTb', [128, 8, 1024], BF16)
        acc = S.alloc('facc', [128, 8, DM], F32)
        uT = S.alloc('uT', [128, 4, 1024], BF16)
        us = [S.alloc(f'us{i}', [128, 512], F32) for i in range(2)]
        w1s = [S.alloc(f'w1g{i}', [128, 8, 512], BF16) for i in range(2)]
        w3s = [S.alloc(f'w3g{i}', [128, 8, 512], BF16) for i in range(2)]
        w2s = [S.alloc(f'w2g{i}', [128, 4, DM], BF16) for i in range(2)]
        gt = S.alloc('fgt', [128, 8, 8], F32)
        xts = [S.alloc(f'xf{i}', [128, DM], F32) for i in range(2)]
        tmp = S.alloc('ftmp', [128, DM], F32)
        ss = S.alloc('fss', [128, 1], F32)
        groups = [(g * 512, 512) for g in range(5)] + [(2560, 256)]
        wi = 0
        for (t0, ntok) in sbs:
            ntile = ntok // 128
            self.dma('sp', hTb[:, :, 0:ntok], d['h2T'].rearrange('(k p) t -> p k t', p=128)[:, :, t0:t0 + ntok])
            if last:
                self.dma('sp', gt[:, 0:ntile, :], d['gates'][t0:t0 + ntok, :].rearrange('(n p) e -> p n e', p=128))
            first = True
            for ei, (W1, W3, W2) in enumerate(experts):
                for (g0, gs) in groups:
                    nfc = gs // 128
                    w1, w3, w2 = w1s[wi % 2], w3s[wi % 2], w2s[wi % 2]
                    wi += 1
                    self.dma('pool', w1[:, :, 0:gs], W1.rearrange('(k p) n -> p k n', p=128)[:, :, g0:g0 + gs])
                    self.dma('pool', w3[:, :, 0:gs], W3.rearrange('(k p) n -> p k n', p=128)[:, :, g0:g0 + gs])
                    self.dma('pool', w2[:, 0:nfc, :], W2[g0:g0 + gs, :].rearrange('(c p) n -> p c n', p=128))
                    for tb in range((ntok + 511) // 512):
                        tw = min(512, ntok - tb * 512)
                        for fc in range(nfc):
                            pa, pb_ = self.ps(), self.ps()
                            for k in range(8):
                                self.mm(pa[:, 0:tw], w1[:, k, fc * 128:(fc + 1) * 128], hTb[:, k, tb * 512:tb * 512 + tw], start=(k == 0), stop=(k == 7))
                            for k in range(8):
                                self.mm(pb_[:, 0:tw], w3[:, k, fc * 128:(fc + 1) * 128], hTb[:, k, tb * 512:tb * 512 + tw], start=(k == 0), stop=(k == 7))
                            u = us[fc % 2]
                            self.act(u[:, 0:tw], pa[:, 0:tw], AF.Silu)
                            self.tt(uT[:, fc, tb * 512:tb * 512 + tw], u[:, 0:tw], pb_[:, 0:tw], ALU.mult)
                    for tt_ in range(ntile):
                        for half in range(2):
                            ps = self.ps()
                            for fc in range(nfc):
                                self.mm(ps[:, 0:512], uT[:, fc, tt_ * 128:(tt_ + 1) * 128], w2[:, fc, half * 512:(half + 1) * 512],
                                        start=(fc == 0), stop=(fc == nfc - 1))
                            a = acc[:, tt_, half * 512:(half + 1) * 512]
                            if last:
                                gsc = gt[:, tt_, ei:ei + 1]
                                if first:
                                    self.ts(a, ps[:, 0:512], gsc, ALU.mult)
                                else:
                                    self.stt(a, ps[:, 0:512], gsc, a, ALU.mult, ALU.add)
                            else:
                                if first:
                                    self.cp(a, ps[:, 0:512], eng='act')
                                else:
                                    self.tt(a, a, ps[:, 0:512], ALU.add)
                    first = False
            for tt_ in range(ntile):
                n = t0 // 128 + tt_
                r = 1 if n < 2 else 0
                xt = xts[tt_ % 2]
                self.dma('sp', xt[:, :], d['x1'][n * 128:(n + 1) * 128, :])
                a = acc[:, tt_, :]
                self.tt(a, a, g2[r][:, :], ALU.mult)
                self.tt(xt[:, :], xt[:, :], a, ALU.add)
                if last:
                    self.act(tmp[:, :], xt[:, :], AF.Square, accum=ss[:, 0:1])
                    self.rstd(ss[:, 0:1], DM, 'f')
                    self.stt(xt[:, :], xt[:, :], ss[:, 0:1], fw[:, :], ALU.mult, ALU.mult)
                    self.dma('pool', d['out'][(n - 2) * 128:(n - 1) * 128, :], xt[:, :])
                else:
                    self.dma('pool', d['x2'][n * 128:(n + 1) * 128, :], xt[:, :])
        fr = [hTb, acc, uT, gt, tmp, ss, *us, *w1s, *w3s, *w2s, *xts, *g2]
        if last:
            fr.append(fw)
        S.free(*fr)


    def stage_moe(self, l):
        S, d = self.S, self.d
        NL = 32
        g2 = self.bcast('g2m', d['mod'][l, 0, 5 * DM:6 * DM], DM)
        fw = self.bcast('fwm', d['fin_w'][:], DM)
        G3 = S.alloc('G3', [128, NL, 8], F32)
        M1 = S.alloc('M1', [128, NL, 8], F32)
        self.dma('sp', G3[:, :, :], d['gates'][256:NT, :].rearrange('(n p) e -> p n e', p=128))
        self.dma('sp', M1[:, :, :], d['sel1'][256:NT, :].rearrange('(n p) e -> p n e', p=128))
        Ms = S.alloc('Ms', [128, NL, 8], F32)
        self.ts(Ms[:, :, :], G3[:, :, :], 0.0, ALU.is_gt)
        M2 = S.alloc('M2', [128, NL, 8], F32)
        self.tt(M2[:, :, :], Ms[:, :, :], M1[:, :, :], ALU.subtract)
        triS = S.alloc('triS', [128, 128], F32)
        self.tt(triS[:, :], self.tri0[:, :], self.identf[:, :], ALU.subtract)
        Ms2 = Ms[:, :, :].rearrange('p n e -> p (n e)')
        pc_, pr_ = self.ps(), self.ps()
        self.mm(pc_[:, 0:256], self.onesf[:, :], Ms2)
        self.mm(pr_[:, 0:256], triS[:, :], Ms2)
        CS = S.alloc('CS', [128, NL, 8], F32)
        self.cp(CS[:, :, :].rearrange('p n e -> p (n e)'), pc_[:, 0:256])
        pos = S.alloc('pos', [128, NL, 8], F32)
        self.cp(pos[:, :, :].rearrange('p n e -> p (n e)'), pr_[:, 0:256], eng='act')
        TB = S.alloc('TB', [128, NL + 1, 8], F32)
        self.memset(TB[:, 0, :], 0.0)
        for n in range(NL):
            self.tt(TB[:, n + 1, :], TB[:, n, :], CS[:, n, :], ALU.add)
        cth = S.alloc('cth', [128, 16], F32)
        self.dma('sp', cth[:, :], d['c_th'][:, :])
        cj = S.alloc('cj', [128, NBLK], F32)
        self.dma('sp', cj[:, :], d['c_j512'][:, :])
        cgp = S.alloc('cgp', [128, 11], F32)
        self.dma('sp', cgp[:, :], d['c_gp'][:, :])
        cmp_ = S.alloc('cmp', [128, 8, 16], F32)
        self.tt(cmp_[:, :, :], cth[:, :].unsqueeze(1).to_broadcast([128, 8, 16]),
                TB[:, NL, :].unsqueeze(2).to_broadcast([128, 8, 16]), ALU.is_lt)
        pcn = S.alloc('pcn', [128, 8], F32)
        self.red(pcn[:, :], cmp_[:, :, :], ALU.add)
        self.ts(pcn[:, :], pcn[:, :], 512.0, ALU.mult)
        end = S.alloc('end', [128, 8], F32)
        self.cp(end[:, 0:1], pcn[:, 0:1])
        for e in range(1, 8):
            self.tt(end[:, e:e + 1], end[:, e - 1:e], pcn[:, e:e + 1], ALU.add)
        st = S.alloc('st', [128, 8], F32)
        self.tt(st[:, :], end[:, :], pcn[:, :], ALU.subtract)
        self.tt(pos[:, :, :], pos[:, :, :], TB[:, 0:NL, :], ALU.add)
        self.tt(pos[:, :, :], pos[:, :, :], st[:, :].unsqueeze(1).to_broadcast([128, NL, 8]), ALU.add)
        tmp3 = S.alloc('tmp3', [128, NL, 8], F32)
        slotf = S.alloc('slotf', [128, 2, NL], F32)
        gk = S.alloc('gk', [128, 2, NL], F32)
        for k, Mk in enumerate((M1, M2)):
            self.tt(tmp3[:, :, :], Mk[:, :, :], pos[:, :, :], ALU.mult)
            self.red(slotf[:, k, :], tmp3[:, :, :], ALU.add)
            self.tt(tmp3[:, :, :], Mk[:, :, :], G3[:, :, :], ALU.mult)
            self.red(gk[:, k, :], tmp3[:, :, :], ALU.add)
        self.ts(slotf[:, :, :], slotf[:, :, :], float(NSLOT - 1), ALU.min)
        sloti = S.alloc('sloti', [128, 2, NL], I32)
        self.cp(sloti[:, :, :].rearrange('p k n -> p (k n)'), slotf[:, :, :].rearrange('p k n -> p (k n)'))
        bj = S.alloc('bj', [128, NBLK, 8], F32)
        self.tt(bj[:, :, :], end[:, :].unsqueeze(1).to_broadcast([128, NBLK, 8]),
                cj[:, :].unsqueeze(2).to_broadcast([128, NBLK, 8]), ALU.is_le)
        ej = S.alloc('ej', [128, NBLK], F32)
        self.red(ej[:, :], bj[:, :, :], ALU.add)
        self.ts(ej[:, :], ej[:, :], 7.0, ALU.min, 1408.0, ALU.mult)
        idxf = S.alloc('idxf', [128, NBLK, 11], F32)
        self.tt(idxf[:, :, :], ej[:, :].unsqueeze(2).to_broadcast([128, NBLK, 11]),
                cgp[:, :].unsqueeze(1).to_broadcast([128, NBLK, 11]), ALU.add)
        idxi = S.alloc('idxi', [128, NBLK, 11], I32)
        self.cp(idxi[:, :, :].rearrange('p j g -> p (j g)'), idxf[:, :, :].rearrange('p j g -> p (j g)'))
        S.free(G3, M1, Ms, M2, triS, CS, pos, TB, cth, cj, cgp, cmp_, pcn, end, st, tmp3, slotf, bj, ej, idxf)

        zt = S.alloc('zt', [128, 24, DM], BF16)
        self.memset(zt[:, :, :].rearrange('p a c -> p (a c)'), 0.0)
        for j in range(NBLK // 6):
            self.dma('sp', d['hs'][j * 3072:(j + 1) * 3072, :].rearrange('(a p) c -> p a c', p=128), zt[:, :, :])
        hts = [S.alloc(f'hsrc{i}', [128, DM], BF16) for i in range(2)]
        for n in range(NL):
            ht = hts[n % 2]
            self.dma('sp', ht[:, :], d['h2'][(n + 2) * 128:(n + 3) * 128, :])
            for k in range(2):
                self.dma_ind(d['hs'][:, :], ht[:, :], sloti[:, k, n:n + 1], True, NSLOT - 1)
        S.free(zt, *hts)

        hbs = [S.alloc(f'hsb{i}', [128, 4, DM], BF16) for i in range(2)]
        hTs = [S.alloc(f'hTm{i}', [128, 8, 512], BF16) for i in range(2)]
        accs = [S.alloc(f'macc{i}', [128, 4, DM], F32) for i in range(2)]
        uTs = [S.alloc(f'uTm{i}', [128, 2, 512], BF16) for i in range(2)]
        us = [S.alloc(f'usm{i}', [128, 512], F32) for i in range(2)]
        w1s = [S.alloc(f'w1m{i}', [128, 8, 256], BF16) for i in range(3)]
        w3s = [S.alloc(f'w3m{i}', [128, 8, 256], BF16) for i in range(3)]
        w2s = [S.alloc(f'w2m{i}', [128, 2, DM], BF16) for i in range(3)]
        wi = 0
        nrow = 8 * 11 * 128
        for j in range(NBLK):
            hb, hT, acc = hbs[j % 2], hTs[j % 2], accs[j % 2]
            self.dma('sp', hb[:, :, :], d['hs'][j * 512:(j + 1) * 512, :].rearrange('(a p) c -> p a c', p=128))
            for a in range(4):
                ps = self.ps()
                psb = ps[:, :].bitcast(BF16)
                for k in range(8):
                    self.tr(psb[:, k * 128:(k + 1) * 128], hb[:, a, k * 128:(k + 1) * 128], self.identb[:, :])
                self.cp(hT[:, :, a * 128:(a + 1) * 128], psb[:, :].rearrange('p (k t) -> p k t', k=8), eng=('act' if a % 2 else 'dve'))
            for g in range(11):
                w1, w3, w2 = w1s[wi % 3], w3s[wi % 3], w2s[wi % 3]
                wi += 1
                ix = idxi[:, j, g:g + 1]
                self.dma_ind(w1[:, :, :].rearrange('p k c -> p (k c)'), d['moe_w1'][:, :], ix, False, nrow - 1)
                self.dma_ind(w3[:, :, :].rearrange('p k c -> p (k c)'), d['moe_w3'][:, :], ix, False, nrow - 1)
                self.dma_ind(w2[:, :, :].rearrange('p k c -> p (k c)'), d['moe_w2'][:, :], ix, False, nrow - 1)
                uT = uTs[g % 2]
                for fc in range(2):
                    pa, pb_ = self.ps(), self.ps()
                    for k in range(8):
                        self.mm(pa[:, 0:512], w1[:, k, fc * 128:(fc + 1) * 128], hT[:, k, :], start=(k == 0), stop=(k == 7))
                    for k in range(8):
                        self.mm(pb_[:, 0:512], w3[:, k, fc * 128:(fc + 1) * 128], hT[:, k, :], start=(k == 0), stop=(k == 7))
                    u = us[fc % 2]
                    self.act(u[:, :], pa[:, 0:512], AF.Silu)
                    self.tt(uT[:, fc, :], u[:, :], pb_[:, 0:512], ALU.mult)
                for a in range(4):
                    for half in range(2):
                        ps = self.psacc()
                        for fc in range(2):
                            self.mm(ps[:, 0:512], uT[:, fc, a * 128:(a + 1) * 128], w2[:, fc, half * 512:(half + 1) * 512],
                                    start=(fc == 0), stop=(fc == 1))
                        av = acc[:, a, half * 512:(half + 1) * 512]
                        if g == 0:
                            self.cp(av, ps[:, 0:512], eng='act')
                        else:
                            self.tt(av, av, ps[:, 0:512], ALU.add)
            self.dma('sp', d['yb'][j * 512:(j + 1) * 512, :].rearrange('(a p) c -> p a c', p=128), acc[:, :, :])
        S.free(*hbs, *hTs, *accs, *uTs, *us, *w1s, *w3s, *w2s, idxi)

        y1s = [S.alloc(f'y1_{i}', [128, DM], F32) for i in range(2)]
        y2s = [S.alloc(f'y2_{i}', [128, DM], F32) for i in range(2)]
        xts = [S.alloc(f'xm{i}', [128, DM], F32) for i in range(2)]
        tmp = S.alloc('mtmp', [128, DM], F32)
        ss = S.alloc('mss', [128, 1], F32)
        outs = [S.alloc(f'mout{i}', [128, DM], F32) for i in range(2)]
        def loads(n):
            y1, y2, xt = y1s[n % 2], y2s[n % 2], xts[n % 2]
            self.dma_ind(y1[:, :], d['yb'][:, :], sloti[:, 0, n:n + 1], False, NSLOT - 1)
            self.dma_ind(y2[:, :], d['yb'][:, :], sloti[:, 1, n:n + 1], False, NSLOT - 1)
            self.dma('sp', xt[:, :], d['x1'][(n + 2) * 128:(n + 3) * 128, :])
        loads(0)
        for n in range(NL):
            y1, y2, xt = y1s[n % 2], y2s[n % 2], xts[n % 2]
            ob = outs[n % 2]
            if n + 1 < NL:
                loads(n + 1)
            self.ts(y1[:, :], y1[:, :], gk[:, 0, n:n + 1], ALU.mult)
            self.stt(y1[:, :], y2[:, :], gk[:, 1, n:n + 1], y1[:, :], ALU.mult, ALU.add)
            self.tt(y1[:, :], y1[:, :], g2[:, :], ALU.mult)
            self.tt(xt[:, :], xt[:, :], y1[:, :], ALU.add)
            self.act(tmp[:, :], xt[:, :], AF.Square, accum=ss[:, 0:1])
            self.rstd(ss[:, 0:1], DM, 'f')
            self.stt(ob[:, :], xt[:, :], ss[:, 0:1], fw[:, :], ALU.mult, ALU.mult)
            self.dma('sp', d['out'][n * 128:(n + 1) * 128, :], ob[:, :])
        S.free(*outs)
        S.free(g2, fw, gk, sloti, tmp, ss, *y1s, *y2s, *xts)


def _na_bias_tiles(rpb):
    L, H = rpb.shape[0], rpb.shape[1]
    NEG = np.float32(-30000.0)
    col = np.arange(64)
    c0 = np.clip(col - 8, 0, 48)
    ck = col[:, None]
    cq = col[None, :]
    cvalid = (ck >= c0[None, :]) & (ck < c0[None, :] + 16)
    cidx = np.clip(ck - cq, -15, 15) + 15
    out = np.full((L, H, 14, 128, 256), NEG, dtype=np.float32)
    cfgs = []
    for i in range(6):
        cfgs.append((4, 4 - 4 + 2 * i))
    for j in range(4):
        cfgs.append((0, 2 * j))
    for j in range(4):
        cfgs.append((60, 56 + 2 * j))
    for ci, (R, kr0) in enumerate(cfgs):
        for a in range(2):
            kr = kr0 + a
            for b in range(4):
                r = R + b
                r0 = min(max(r - 4, 0), 56)
                if not (r0 <= kr <= r0 + 7) or kr > 63:
                    continue
                drow = kr - r + 7
                g = rpb[:, :, drow, :][:, :, cidx]
                blk = np.where(cvalid[None, None], g, NEG)
                out[:, :, ci, a * 64:(a + 1) * 64, b * 64:(b + 1) * 64] = blk
    return out


def _rope_tables():
    t = np.arange(4096)
    row = (t // 64).astype(np.float32)
    colv = (t % 64).astype(np.float32)
    inv = (np.float32(10000.0) ** (-np.arange(0, 16, 2, dtype=np.float32) / np.float32(16))).astype(np.float32)
    ang = np.concatenate([row[:, None] * inv, colv[:, None] * inv], axis=1).astype(np.float32)
    return np.cos(ang).astype(np.float32), np.sin(ang).astype(np.float32)


def _w13_layout(w):
    w = w.reshape(8, 8, 128, 11, 256)
    return np.ascontiguousarray(w.transpose(0, 3, 2, 1, 4)).reshape(8 * 11 * 128, 2048)


def _w2_layout(w):
    w = w.reshape(8, 11, 2, 128, 1024)
    return np.ascontiguousarray(w.transpose(0, 1, 3, 2, 4)).reshape(8 * 11 * 128, 2048)


_NC_CACHE = {}


def _get_nc(stop_after=None):
    key = (stop_after, DEBUG)
    if key not in _NC_CACHE:
        nc = bass.Bass("TRN2", target_bir_lowering=False)
        k = K(nc)
        k.stop_after = stop_after
        k.build()
        _NC_CACHE[key] = nc
    return _NC_CACHE[key]


def make_in_maps(inputs):
    f = lambda a: np.ascontiguousarray(np.asarray(a, dtype=np.float32))
    x, c, ctx, c_ctx = f(inputs['x']), f(inputs['c']), f(inputs['ctx']), f(inputs['c_ctx'])
    cs, sn = _rope_tables()
    tri0 = np.triu(np.ones((128, 128), np.float32))
    tri1 = np.tril(np.ones((128, 128), np.float32))
    shared = {
        'ada_w': f(inputs['ada_w']), 'ada_b': f(inputs['ada_b']),
        'norm1_w': f(inputs['norm1_w']), 'norm2_w': f(inputs['norm2_w']),
        'w_in': f(inputs['w_in']), 'w_out': f(inputs['w_out']),
        'convT': f(np.transpose(np.asarray(inputs['mlstm_conv_w']), (0, 2, 1))),
        'ig_b': f(np.asarray(inputs['mlstm_ig_b']).reshape(2, 8)),
        'fg_b': f(np.asarray(inputs['mlstm_fg_b']).reshape(2, 8)),
        'ml_nw': f(inputs['mlstm_norm_w']),
        'na_bm': _na_bias_tiles(f(inputs['na_rpb'])),
        'q_nw': f(inputs['mla_q_norm_w']), 'kv_nw': f(inputs['mla_kv_norm_w']),
        'w_uq': f(inputs['mla_w_uq']), 'w_ukv': f(inputs['mla_w_ukv']),
        'ffn_w1': f(inputs['ffn_w1'])[0], 'ffn_w3': f(inputs['ffn_w3'])[0], 'ffn_w2': f(inputs['ffn_w2'])[0],
        'router': f(inputs['moe_router_w'])[0],
        'moe_w1': _w13_layout(f(inputs['moe_w1'])[0]), 'moe_w3': _w13_layout(f(inputs['moe_w3'])[0]),
        'moe_w2': _w2_layout(f(inputs['moe_w2'])[0]),
        'c_th': np.ascontiguousarray(np.broadcast_to(np.arange(16, dtype=np.float32) * 512, (128, 16))),
        'c_j512': np.ascontiguousarray(np.broadcast_to(np.arange(NBLK, dtype=np.float32) * 512, (128, NBLK))),
        'c_gp': np.ascontiguousarray((np.arange(11, dtype=np.float32)[None, :] * 128 + np.arange(128, dtype=np.float32)[:, None])),
        'fin_w': f(inputs['final_norm_w']),
        'ident': np.eye(128, dtype=np.float32), 'tri0': tri0, 'tri1': tri1,
        'rope_cs': cs, 'rope_sn': sn,
    }
    maps = []
    for b in range(8):
        m = dict(shared)
        m['xin'] = np.ascontiguousarray(np.concatenate([ctx[b], x[b]], axis=0))
        cc = np.stack([c[b].reshape(8, 128).T, c_ctx.reshape(8, 128).T], axis=2)
        m['cc'] = np.ascontiguousarray(cc.reshape(128, 16))
        maps.append(m)
    return maps


def kernel(**inputs):
    nc = _get_nc()
    maps = make_in_maps(inputs)
    res = run_bass_kernel_spmd(nc, maps, core_ids=list(range(8)))
    return np.stack([np.asarray(r['out'], dtype=np.float32) for r in res.results], axis=0)
```

```python
import numpy as np
from contextlib import ExitStack
import concourse.bass as bass
import concourse.mybir as mybir
from concourse.bass_utils import run_bass_kernel_spmd

F32 = mybir.dt.float32
BF16 = mybir.dt.bfloat16
U8 = mybir.dt.uint8
AF = mybir.ActivationFunctionType
ALU = mybir.AluOpType
AX = mybir.AxisListType
DTSIZE = {F32: 4, BF16: 2, mybir.dt.int32: 4}
COMPUTE = ('pe', 'act', 'dve', 'pool')
QUEUES = ('sp', 'act', 'pool')
NDSEM = 12
ARENA = 207 * 1024

NT = 4352
NTILE = 34
DM = 1024
NIN = 2992
DFF = 2816
EPS = 1e-6
DEBUG = False
NBLK = 24
NSLOT = NBLK * 512
I32 = mybir.dt.int32


class Op:
    __slots__ = ('eng', 'fn', 'deps', 'isdma', 'needs_inc', 'count', 'semi', 'semval', 'idx', 'q')


class V:
    __slots__ = ('buf', 'ap')

    def __init__(self, buf, ap):
        self.buf = buf
        self.ap = ap

    def __getitem__(self, k):
        return V(self.buf, self.ap[k])

    def rearrange(self, pattern_, **kw):
        return V(self.buf, self.ap.rearrange(pattern_, **kw))

    def unsqueeze(self, a):
        return V(self.buf, self.ap.unsqueeze(a))

    def to_broadcast(self, shp):
        return V(self.buf, self.ap.to_broadcast(list(shp)))

    def bitcast(self, dt):
        return V(self.buf, self.ap.bitcast(dt))

    def partition_broadcast(self, n):
        return V(self.buf, self.ap.partition_broadcast(n))


class Buf:
    def __init__(self, ap, name, off=0, size=0):
        self.ap = ap
        self.name = name
        self.w = None
        self.rc = {}
        self.rd = {}
        self.inh = []
        self.multiw = False
        self.wl = {}
        self.war = []
        self.off = off
        self.size = size

    def __getitem__(self, k):
        return V(self, self.ap[k])

    def v(self):
        return V(self, self.ap)

    def rearrange(self, pattern_, **kw):
        return V(self, self.ap.rearrange(pattern_, **kw))


class Sched:
    def __init__(self, nc):
        self.nc = nc
        self.ops = {e: [] for e in ('pe', 'act', 'dve', 'pool', 'sp')}
        self.nops = 0
        self.dsem_use = {q: [0] * NDSEM for q in QUEUES}
        self.dsem_rr = {q: 0 for q in QUEUES}
        self.arena = nc.alloc_sbuf_tensor("arena", [128, ARENA], U8)
        self.live = []
        self.dead = []
        self.psum = []
        for i in range(8):
            h = nc.alloc_psum_tensor(f"psb{i}", [128, 512], F32)
            self.psum.append(Buf(h[:, :], f"psb{i}"))

    def alloc(self, name, shape, dtype):
        n = int(np.prod(shape[1:])) * DTSIZE[dtype]
        n_al = (n + 63) // 64 * 64
        self.live.sort(key=lambda b: b.off)
        off = 0
        for b in self.live:
            if b.off - off >= n_al:
                break
            off = max(off, b.off + b.size)
        if off + n_al > ARENA:
            raise RuntimeError(f"SBUF arena OOM allocating {name} {shape}: need {n_al} at {off}; live="
                               + str([(b.name, b.off, b.size) for b in self.live]))
        ap = self.arena[0:shape[0], off:off + n].bitcast(dtype)
        if len(shape) > 2:
            names = ' '.join(f'd{i}' for i in range(1, len(shape)))
            kw = {f'd{i}': shape[i] for i in range(1, len(shape))}
            ap = ap.rearrange(f'p ({names}) -> p {names}', **kw)
        buf = Buf(ap, name, off, n_al)
        keep = []
        for d in self.dead:
            if d.off < off + n_al and off < d.off + d.size:
                acc = list(d.inh)
                if d.w is not None:
                    acc.append(d.w)
                acc.extend(d.rc.values())
                acc.extend(d.rd.values())
                buf.inh.extend(acc)
                if not (off <= d.off and d.off + d.size <= off + n_al):
                    keep.append(d)
            else:
                keep.append(d)
        self.dead = keep
        best = {}
        for o in buf.inh:
            key = (o.q, o.semi) if o.isdma else o.eng
            if key not in best or best[key].idx < o.idx:
                best[key] = o
        buf.inh = list(best.values())
        self.live.append(buf)
        return buf

    def free(self, *bufs):
        for b in bufs:
            self.live.remove(b)
            self.dead.append(b)

    def dram(self, name, shape, dtype, kind="Internal"):
        h = self.nc.dram_tensor(name, list(shape), dtype, kind=kind)
        return Buf(h.ap(), name)

    def _mk(self, eng, fn, reads, writes, isdma, q=None):
        op = Op()
        op.eng = eng
        op.fn = fn
        op.isdma = isdma
        op.needs_inc = False
        op.count = 0
        op.idx = self.nops
        op.q = q
        op.semi = -1
        self.nops += 1
        if isdma:
            i = self.dsem_rr[q]
            self.dsem_rr[q] = (i + 1) % NDSEM
            self.dsem_use[q][i] += 1
            op.semi = i
            op.semval = 16 * self.dsem_use[q][i]
        deps = []
        for b in reads:
            if b.w is not None:
                deps.append((b.w, True))
            for o in b.wl.values():
                deps.append((o, True))
        for b in writes:
            mw = b.multiw and isdma
            if mw:
                if b.rc or b.rd:
                    b.war = list(b.rc.values()) + list(b.rd.values())
                    b.wl = {}
                    b.rc = {}
                    b.rd = {}
                for o in b.war:
                    deps.append((o, False))
                if b.w is not None:
                    deps.append((b.w, False))
            else:
                if b.w is not None:
                    deps.append((b.w, False))
                for o in b.wl.values():
                    deps.append((o, False))
                for o in b.rc.values():
                    deps.append((o, False))
                for o in b.rd.values():
                    deps.append((o, False))
            for o in b.inh:
                deps.append((o, False))
        fin = []
        seen = set()
        for p, raw in deps:
            if p is op or id(p) in seen:
                continue
            if (not p.isdma) and (not isdma) and p.eng == eng and eng == 'pe':
                continue
            seen.add(id(p))
            fin.append(p)
            p.needs_inc = True
        op.deps = fin
        for b in reads:
            if isdma:
                b.rd[(q, op.semi)] = op
            else:
                b.rc[eng] = op
        for b in writes:
            if b.multiw and isdma:
                b.wl[(q, op.semi)] = op
                b.inh = []
            else:
                b.w = op
                b.wl = {}
                b.war = []
                b.rc = {}
                b.rd = {}
                b.inh = []
        self.ops[eng].append(op)
        return op

    def op(self, eng, fn, reads=(), writes=()):
        return self._mk(eng, fn, reads, writes, False)

    def dma(self, q, out, in_, **kw):
        oa, ia = out.ap, in_.ap

        def fn(e):
            return e.dma_start(out=oa, in_=ia, **kw)
        return self._mk(q, fn, [in_.buf], [out.buf], True, q=q)

    def emit(self):
        nc = self.nc
        with ExitStack() as es:
            self.csems = {e: es.enter_context(nc.semaphore(f"c_{e}")) for e in COMPUTE}
            self.dsems = {q: [es.enter_context(nc.semaphore(f"d_{q}{i}")) for i in range(NDSEM)] for q in QUEUES}
            for e in COMPUTE:
                cnt = 0
                for op in self.ops[e]:
                    if not op.isdma and op.needs_inc:
                        cnt += 1
                    op.count = cnt
            block = es.enter_context(nc.Block())

            @block.tensor
            def _(eng):
                self._emit_engine('pe', eng)

            @block.scalar
            def _(eng):
                self._emit_engine('act', eng)

            @block.vector
            def _(eng):
                self._emit_engine('dve', eng)

            @block.gpsimd
            def _(eng):
                self._emit_engine('pool', eng)

            @block.sync
            def _(eng):
                self._emit_engine('sp', eng, final=True)

    def _emit_engine(self, e, eng, final=False):
        waited = {}

        def wait(key, sem, val):
            if waited.get(key, 0) >= val:
                return
            eng.wait_ge(sem, val)
            waited[key] = val

        for op in self.ops[e]:
            for p in op.deps:
                if p.isdma:
                    wait(('d', p.q, p.semi), self.dsems[p.q][p.semi], p.semval)
                else:
                    wait(('c', p.eng), self.csems[p.eng], p.count)
            if op.isdma:
                sem = self.dsems[op.q][op.semi]
                if op.semval > 16:
                    wait(('d', op.q, op.semi), sem, op.semval - 16)
                ins = op.fn(eng)
                ins.then_inc(sem, 16)
            else:
                ins = op.fn(eng)
                if op.needs_inc:
                    ins.then_inc(self.csems[e], 1)
        if final:
            for q in QUEUES:
                for i in range(NDSEM):
                    v = 16 * self.dsem_use[q][i]
                    if v > 0:
                        wait(('d', q, i), self.dsems[q][i], v)
            for ce in COMPUTE:
                last = None
                for op in self.ops[ce]:
                    if not op.isdma and op.needs_inc:
                        last = op
                if last is not None:
                    wait(('c', ce), self.csems[ce], last.count)


def _bufs(*xs):
    out = []
    for x in xs:
        if isinstance(x, V) and x.buf not in out:
            out.append(x.buf)
    return out


def _a(x):
    return x.ap if isinstance(x, V) else x


class K:
    def __init__(self, nc):
        self.nc = nc
        self.S = Sched(nc)
        self.defctx = {'banks': [0, 1, 2, 3], 'accs': [4, 5, 6, 7], 'i': 0, 'j': 0}
        self.cur = self.defctx

    def ps(self):
        c = self.cur
        b = self.S.psum[c['banks'][c['i'] % len(c['banks'])]]
        c['i'] += 1
        return b

    def psacc(self):
        c = self.cur
        b = self.S.psum[c['accs'][c['j'] % len(c['accs'])]]
        c['j'] += 1
        return b

    def interleave(self, gens):
        live = list(gens)
        while live:
            for item in list(live):
                g, ctx, wgt = item
                self.cur = ctx
                for _ in range(wgt):
                    try:
                        next(g)
                    except StopIteration:
                        live.remove(item)
                        break
        self.cur = self.defctx

    def mm(self, out, lhsT, rhs, start=True, stop=True):
        oa, la, ra = out.ap, lhsT.ap, rhs.ap
        self.S.op('pe', lambda e: e.matmul(oa, lhsT=la, rhs=ra, start=start, stop=stop),
                  reads=_bufs(lhsT, rhs), writes=_bufs(out))

    def tr(self, out, in_, ident):
        oa, ia, da = out.ap, in_.ap, ident.ap
        self.S.op('pe', lambda e: e.transpose(oa, ia, da), reads=_bufs(in_, ident), writes=_bufs(out))

    def act(self, out, in_, func, bias=None, scale=None, accum=None):
        kw = {}
        if bias is not None:
            kw['bias'] = _a(bias)
        if scale is not None:
            kw['scale'] = _a(scale)
        if accum is not None:
            kw['accum_out'] = accum.ap
        oa, ia = out.ap, in_.ap
        self.S.op('act', lambda e: e.activation(out=oa, in_=ia, func=func, **kw),
                  reads=_bufs(in_, bias, scale), writes=_bufs(out, accum))

    def tt(self, out, in0, in1, op, eng='dve'):
        oa, a0, a1 = out.ap, in0.ap, in1.ap
        self.S.op(eng, lambda e: e.tensor_tensor(out=oa, in0=a0, in1=a1, op=op), reads=_bufs(in0, in1), writes=_bufs(out))

    def ts(self, out, in0, s1, op0, s2=None, op1=None, eng='dve'):
        oa, a0 = out.ap, in0.ap
        x1, x2 = _a(s1), _a(s2)
        if op1 is None:
            self.S.op(eng, lambda e: e.tensor_scalar(out=oa, in0=a0, scalar1=x1, scalar2=None, op0=op0),
                      reads=_bufs(in0, s1), writes=_bufs(out))
        else:
            self.S.op(eng, lambda e: e.tensor_scalar(out=oa, in0=a0, scalar1=x1, scalar2=x2, op0=op0, op1=op1),
                      reads=_bufs(in0, s1, s2), writes=_bufs(out))

    def stt(self, out, in0, scalar, in1, op0, op1, eng='dve'):
        oa, a0, a1, sc = out.ap, in0.ap, in1.ap, _a(scalar)
        self.S.op(eng, lambda e: e.scalar_tensor_tensor(out=oa, in0=a0, scalar=sc, in1=a1, op0=op0, op1=op1),
                  reads=_bufs(in0, in1, scalar), writes=_bufs(out))

    def cp(self, out, in_, eng='dve'):
        oa, ia = out.ap, in_.ap
        if eng == 'act':
            self.S.op('act', lambda e: e.copy(out=oa, in_=ia), reads=_bufs(in_), writes=_bufs(out))
        else:
            self.S.op(eng, lambda e: e.tensor_copy(out=oa, in_=ia), reads=_bufs(in_), writes=_bufs(out))

    def memset(self, out, val, eng='dve'):
        oa = out.ap
        self.S.op(eng, lambda e: e.memset(oa, val), writes=_bufs(out))

    def red(self, out, in_, op, negate=False):
        oa, ia = out.ap, in_.ap
        self.S.op('dve', lambda e: e.tensor_reduce(out=oa, in_=ia, axis=AX.X, op=op, negate=negate),
                  reads=_bufs(in_), writes=_bufs(out))

    def recip(self, out, in_):
        oa, ia = out.ap, in_.ap
        self.S.op('dve', lambda e: e.reciprocal(out=oa, in_=ia), reads=_bufs(in_), writes=_bufs(out))

    def dma(self, q, out, in_, **kw):
        self.S.dma(q, out, in_, **kw)

    def dma_ind(self, out, in_, idx, scatter, bound):
        oa, ia, xa = out.ap, in_.ap, idx.ap

        def fn(e):
            off = bass.IndirectOffsetOnAxis(ap=xa, axis=0)
            if scatter:
                return e.indirect_dma_start(out=oa, out_offset=off, in_=ia, in_offset=None, bounds_check=None, oob_is_err=False)
            return e.indirect_dma_start(out=oa, out_offset=None, in_=ia, in_offset=off, bounds_check=None, oob_is_err=False)
        self.S._mk('pool', fn, [in_.buf, idx.buf], [out.buf], True, q='pool')

    def bcast(self, name, row_view, n):
        b = self.S.alloc(name, [128, n], F32)
        self.dma('sp', b[:, :], row_view.partition_broadcast(128))
        return b

    def rstd(self, ss, n, name):
        self.act(ss, ss, AF.Sqrt, bias=self.eps[0:ss.ap.shape[0], 0:1], scale=1.0 / n)
        self.recip(ss, ss)

    def build(self):
        S = self.S
        d = {}
        self.d = d
        import os
        self.flags = set(os.environ.get('KFLAGS', '').split(','))
        om = 'only_moe' in self.flags

        def inp(name, shape):
            d[name] = S.dram(name, shape, F32, kind="ExternalInput")
        inp('xin', [NT, DM])
        inp('cc', [128, 16])
        inp('ada_w', [2, DM, 6 * DM])
        inp('ada_b', [2, 6 * DM])
        inp('norm1_w', [2, DM])
        inp('norm2_w', [2, DM])
        inp('w_in', [2, DM, NIN])
        inp('w_out', [2, DM, DM])
        inp('convT', [2, 512, 3])
        inp('ig_b', [2, 8])
        inp('fg_b', [2, 8])
        inp('ml_nw', [2, 256])
        inp('na_bm', [2, 6, 14, 128, 256])
        inp('q_nw', [2, 512])
        inp('kv_nw', [2, 256])
        inp('w_uq', [2, 512, 576])
        inp('w_ukv', [2, 256, 768])
        inp('ffn_w1', [DM, DFF])
        inp('ffn_w3', [DM, DFF])
        inp('ffn_w2', [DFF, DM])
        inp('router', [DM, 8])
        inp('moe_w1', [8 * 11 * 128, 2048])
        inp('moe_w3', [8 * 11 * 128, 2048])
        inp('moe_w2', [8 * 11 * 128, 2048])
        inp('c_th', [128, 16])
        inp('c_j512', [128, NBLK])
        inp('c_gp', [128, 11])
        inp('fin_w', [DM])
        inp('ident', [128, 128])
        inp('tri0', [128, 128])
        inp('tri1', [128, 128])
        inp('rope_cs', [4096, 16])
        inp('rope_sn', [4096, 16])
        d['out'] = S.dram('out', [4096, DM], F32, kind="ExternalOutput")
        kind = "ExternalOutput" if DEBUG else "Internal"
        d['mod'] = S.dram('mod', [2, 2, 6 * DM], F32, kind=('ExternalInput' if om else kind))
        d['pT_ml'] = S.dram('pT_ml', [512, NT], F32, kind=kind)
        d['pT_na'] = S.dram('pT_na', [768, NT], BF16, kind=kind)
        d['p_tok'] = S.dram('p_tok', [NT, 1712], F32, kind=kind)
        d['cat'] = S.dram('cat', [DM, NT], BF16, kind=kind)
        d['x1'] = S.dram('x1', [NT, DM], F32, kind=('ExternalInput' if om else kind))
        d['x2'] = S.dram('x2', [NT, DM], F32, kind=kind)
        d['h2T'] = S.dram('h2T', [DM, NT], BF16, kind=kind)
        d['gates'] = S.dram('gatesd', [NT, 8], F32, kind=('ExternalInput' if om else kind))
        d['sel1'] = S.dram('sel1d', [NT, 8], F32, kind=('ExternalInput' if om else kind))
        d['h2'] = S.dram('h2d', [NT, DM], BF16, kind=('ExternalInput' if om else kind))
        d['hs'] = S.dram('hsd', [NSLOT, DM], BF16, kind=kind)
        d['yb'] = S.dram('ybd', [NSLOT, DM], F32, kind=kind)

        for nm in ('pT_ml', 'pT_na', 'p_tok', 'cat', 'x1', 'x2', 'h2T', 'h2', 'gates', 'sel1', 'out', 'yb'):
            d[nm].multiw = True
        self.identb = S.alloc('identb', [128, 128], BF16)
        self.dma('pool', self.identb[:, :], d['ident'][:, :])
        self.identf = S.alloc('identf', [128, 128], F32)
        self.dma('sp', self.identf[:, :], d['ident'][:, :])
        self.tri0 = S.alloc('tri0', [128, 128], F32)
        self.dma('sp', self.tri0[:, :], d['tri0'][:, :])
        self.tri1 = S.alloc('tri1', [128, 128], F32)
        self.dma('sp', self.tri1[:, :], d['tri1'][:, :])
        self.onesf = S.alloc('onesf', [128, 128], F32)
        self.memset(self.onesf[:, :], 1.0)
        self.eps = S.alloc('eps', [128, 1], F32)
        self.memset(self.eps[:, :], EPS)

        stop = getattr(self, 'stop_after', None)
        xs = d['xin']
        if 'only_moe' in self.flags:
            self.stage_moe(1)
            S.emit()
            return
        for l in range(2):
            last = l == 1
            if 'only_mla' not in self.flags:
                self.adaln(l)
                if stop == ('adaln', l):
                    break
                self.stage_in(l, xs)
                if stop == ('in', l):
                    break
                if 'inter' not in self.flags:
                    for _ in self.stage_mlstm(l, last):
                        pass
                    if stop == ('ml', l):
                        break
                    for _ in self.stage_na(l, last):
                        pass
                else:
                    cm = {'banks': [3], 'accs': [4, 5], 'i': 0, 'j': 0}
                    cn = {'banks': [0, 1, 2], 'accs': [6, 7], 'i': 0, 'j': 0, 'depth': 2}
                    self.interleave([(self.stage_mlstm(l, last), cm, 1), (self.stage_na(l, last), cn, 4)])
            if stop == ('na', l):
                break
            self.stage_mla(l, last)
            if stop in (('mla', l), ('mla1', l)):
                break
            self.stage_out(l, xs, last)
            if stop == ('out', l):
                break
            if last:
                self.stage_moe(l)
            else:
                self.stage_ffn(l, last)
            if stop == ('ffn', l):
                break
            xs = d['x2']
        S.emit()

    def adaln(self, l):
        S, d = self.S, self.d
        cc = S.alloc('cc', [128, 16], F32)
        self.dma('sp', cc[:, :], d['cc'][:, :])
        scT = S.alloc('scT', [128, 16], BF16)
        self.act(scT[:, :], cc[:, :], AF.Silu)
        sc3 = scT[:, :].rearrange('p (k r) -> p k r', r=2)
        adab = S.alloc('adab', [2, 6 * DM], F32)
        self.dma('sp', adab[:, :], d['ada_b'][l, :].partition_broadcast(2))
        modrow = S.alloc('modrow', [2, 6 * DM], F32)
        wts = [S.alloc(f'adaw{i}', [128, 8, 512], BF16) for i in range(2)]
        wsrc = d['ada_w'][l].rearrange('(k p) n -> p k n', p=128)
        for j in range(12):
            wt = wts[j % 2]
            self.dma('pool', wt[:, :, :], wsrc[:, :, j * 512:(j + 1) * 512])
            ps = self.ps()
            for k in range(8):
                self.mm(ps[0:2, 0:512], sc3[:, k, :], wt[:, k, :], start=(k == 0), stop=(k == 7))
            self.tt(modrow[:, j * 512:(j + 1) * 512], ps[0:2, 0:512], adab[:, j * 512:(j + 1) * 512], ALU.add)
        self.dma('pool', d['mod'][l], modrow[:, :])
        S.free(cc, scT, adab, modrow, *wts)

    def mod_ab(self, l, which, nw_name, tag):
        S, d = self.S, self.d
        nwb = self.bcast(f'nwb{tag}', d[nw_name][l, :], DM)
        res = []
        for r in range(2):
            sc = self.bcast(f'sc{tag}{r}', d['mod'][l, r, (3 * which + 1) * DM:(3 * which + 2) * DM], DM)
            self.stt(sc[:, :], sc[:, :], 1.0, nwb[:, :], ALU.add, ALU.mult)
            sh = self.bcast(f'sh{tag}{r}', d['mod'][l, r, (3 * which) * DM:(3 * which + 1) * DM], DM)
            res.append((sc, sh))
        S.free(nwb)
        return res

    def norm_mod(self, xt, A, B, hb, tmp, ss):
        self.act(tmp[:, :], xt[:, :], AF.Square, accum=ss[:, 0:1])
        self.rstd(ss[:, 0:1], DM, 'r')
        self.stt(tmp[:, :], xt[:, :], ss[:, 0:1], A[:, :], ALU.mult, ALU.mult)
        self.tt(hb[:, :], tmp[:, :], B[:, :], ALU.add)

    def transpose8(self, hb, dst3):
        ps = self.ps()
        psb = ps[:, :].bitcast(BF16)
        for k in range(8):
            self.tr(psb[:, k * 128:(k + 1) * 128], hb[:, k * 128:(k + 1) * 128], self.identb[:, :])
        self.cp(dst3, psb[:, :].rearrange('p (k t) -> p k t', k=8), eng='act')

    def stage_in(self, l, xs):
        S, d = self.S, self.d
        w = S.alloc('w_in', [128, 8, NIN], BF16)
        wsrc = d['w_in'][l].rearrange('(k p) n -> p k n', p=128)
        for k in range(8):
            self.dma('pool', w[:, k, :], wsrc[:, k, :])
        ab = self.mod_ab(l, 0, 'norm1_w', 'a')
        hTs = [S.alloc(f'hT{i}', [128, 8, 512], BF16) for i in range(2)]
        xts = [S.alloc(f'xt{i}', [128, DM], F32) for i in range(2)]
        tmps = [S.alloc(f'tmp{i}', [128, DM], F32) for i in range(2)]
        hbs = [S.alloc(f'hb{i}', [128, DM], BF16) for i in range(2)]
        sss = [S.alloc(f'ss{i}', [128, 1], F32) for i in range(2)]
        fms = [S.alloc(f'fm{i}', [128, 512], F32) for i in range(2)]
        fnb = [S.alloc(f'fnb{i}', [128, 512], BF16) for i in range(2)]
        pts = [S.alloc(f'pt{i}', [128, 1712], F32) for i in range(2)]
        groups = [(0, 2)] + [(2 + 4 * i, 4) for i in range(8)]
        fcols = [0, 128, 256, 384] + [1040 + 128 * i for i in range(6)]
        tcols = [(512, 1024, 0), (1024, 1040, 512), (1808, 2320, 528), (2320, 2832, 1040), (2832, 2992, 1552)]
        ti = 0
        for gi, (t0, nt) in enumerate(groups):
            hT = hTs[gi % 2]
            G = nt * 128
            A, B = ab[1] if gi == 0 else ab[0]
            for j in range(nt):
                n = t0 + j
                xt = xts[ti % 2]
                hb = hbs[ti % 2]
                ss = sss[ti % 2]
                tmp = tmps[ti % 2]
                ti += 1
                self.dma('sp', xt[:, :], xs[n * 128:(n + 1) * 128, :])
                self.norm_mod(xt, A, B, hb, tmp, ss)
                self.transpose8(hb, hT[:, :, j * 128:(j + 1) * 128])
            for ci, c0 in enumerate(fcols):
                ps = self.ps()
                for k in range(8):
                    self.mm(ps[:, 0:G], w[:, k, c0:c0 + 128], hT[:, k, 0:G], start=(k == 0), stop=(k == 7))
                if ci < 4:
                    fm = fms[ci % 2]
                    self.cp(fm[:, 0:G], ps[:, 0:G], eng='act')
                    self.dma('pool', d['pT_ml'][ci * 128:(ci + 1) * 128, t0 * 128:t0 * 128 + G], fm[:, 0:G])
                else:
                    fb = fnb[ci % 2]
                    if ci < 7:
                        self.ts(fb[:, 0:G], ps[:, 0:G], 0.125, ALU.mult)
                    else:
                        self.cp(fb[:, 0:G], ps[:, 0:G], eng='act')
                    self.dma('pool', d['pT_na'][(ci - 4) * 128:(ci - 3) * 128, t0 * 128:t0 * 128 + G], fb[:, 0:G])
            for j in range(nt):
                n = t0 + j
                pt = pts[n % 2]
                for ii, (c0, c1, o0) in enumerate(tcols):
                    ps = self.ps()
                    for k in range(8):
                        self.mm(ps[:, 0:c1 - c0], hT[:, k, j * 128:(j + 1) * 128], w[:, k, c0:c1], start=(k == 0), stop=(k == 7))
                    if ii % 2 == 0:
                        self.cp(pt[:, o0:o0 + c1 - c0], ps[:, 0:c1 - c0], eng='dve')
                    else:
                        self.cp(pt[:, o0:o0 + c1 - c0], ps[:, 0:c1 - c0], eng='act')
                self.dma('pool', d['p_tok'][n * 128:(n + 1) * 128, :], pt[:, :])
        S.free(w, *tmps, *hTs, *xts, *hbs, *sss, *fms, *fnb, *pts)
        for a, b in ab:
            S.free(a, b)

    def stage_mlstm(self, l, last):
        S, d = self.S, self.d

        def tcol(n):
            return 1 + n * 128 if n < 2 else 2 + n * 128
        gt = S.alloc('gt', [128, NTILE, 16], F32)
        self.dma('sp', gt[:, :, :], d['p_tok'][:, 512:528].rearrange('(n p) c -> p n c', p=128))
        igb = self.bcast('igb', d['ig_b'][l, :], 8)
        fgb = self.bcast('fgb', d['fg_b'][l, :], 8)
        LI = S.alloc('LI', [128, NTILE, 8], F32)
        LF = S.alloc('LF', [128, NTILE, 8], F32)
        self.tt(LI[:, :, :], gt[:, :, 0:8], igb[:, :].unsqueeze(1).to_broadcast([128, NTILE, 8]), ALU.add)
        self.tt(LF[:, :, :], gt[:, :, 8:16], fgb[:, :].unsqueeze(1).to_broadcast([128, NTILE, 8]), ALU.add)
        self.act(LF[:, :, :], LF[:, :, :], AF.Exp, scale=-1.0)
        self.act(LF[:, :, :], LF[:, :, :], AF.Ln, bias=self.onesf[:, 0:1], scale=1.0)
        self.ts(LF[:, :, :], LF[:, :, :], -1.0, ALU.mult)
        LF2 = LF[:, :, :].rearrange('p n c -> p (n c)')
        NC = NTILE * 8
        Bc = S.alloc('Bc', [128, NTILE, 2, 4], F32)
        EB = S.alloc('EB', [128, NTILE, 2, 4], F32)
        ES = S.alloc('ES', [128, NTILE, 2, 4], F32)
        EG = S.alloc('EG', [128, NTILE, 2, 4], F32)
        psA = self.ps()
        self.mm(psA[:, 0:NC], self.tri0[:, :], LF2)
        self.cp(Bc[:, :, 0, :], psA[:, 0:NC].rearrange('p (n d h) -> p n d h', d=2, h=4)[:, :, 0, :])
        psB = self.ps()
        self.mm(psB[:, 0:NC], self.tri1[:, :], LF2)
        self.cp(Bc[:, :, 1, :], psB[:, 0:NC].rearrange('p (n d h) -> p n d h', d=2, h=4)[:, :, 1, :])
        psC = self.ps()
        self.mm(psC[:, 0:NC], self.onesf[:, :], LF2)
        self.act(EG[:, :, :, :].rearrange('p n d h -> p (n d h)'), psC[:, 0:NC], AF.Exp)
        Bc2 = Bc[:, :, :, :].rearrange('p n d h -> p (n d h)')
        self.act(EB[:, :, :, :].rearrange('p n d h -> p (n d h)'), Bc2, AF.Exp)
        ES2 = ES[:, :, :, :].rearrange('p n d h -> p (n d h)')
        self.tt(ES2, LI[:, :, :].rearrange('p n c -> p (n c)'), Bc2, ALU.subtract)
        self.act(ES2, ES2, AF.Exp)
        yield
        S.free(gt, igb, fgb, LI, LF, Bc)

        convw = S.alloc('convw', [128, 4, 3], F32)
        self.dma('sp', convw[:, :, :], d['convT'][l].rearrange('(c p) j -> p c j', p=128))
        W = NT + 3
        qkT = S.alloc('qkT', [128, 4, W], BF16)
        x32 = S.alloc('x32', [128, W], F32)
        acc = S.alloc('acc', [128, W], F32)
        for c in (0, 257, W - 1):
            self.memset(x32[:, c:c + 1], 0.0)
        for fc in range(4):
            self.dma('sp', x32[:, 1:257], d['pT_ml'][fc * 128:(fc + 1) * 128, 0:256])
            self.dma('sp', x32[:, 258:W - 1], d['pT_ml'][fc * 128:(fc + 1) * 128, 256:NT])
            a = acc[:, 1:W - 1]
            self.ts(a, x32[:, 0:W - 2], convw[:, fc, 0:1], ALU.mult)
            self.stt(a, x32[:, 1:W - 1], convw[:, fc, 1:2], a, ALU.mult, ALU.add)
            self.stt(a, x32[:, 2:W], convw[:, fc, 2:3], a, ALU.mult, ALU.add)
            if fc < 2:
                self.act(qkT[:, fc, 1:W - 1], a, AF.Silu)
            else:
                self.act(a, a, AF.Silu)
                self.ts(qkT[:, fc, 1:W - 1], a, 0.125, ALU.mult)
            yield
        S.free(x32, acc, convw)

        ktok = S.alloc('ktok', [128, NTILE, 256], BF16)
        for n in range(NTILE):
            ps = self.ps()
            psb = ps[:, :].bitcast(BF16)
            for kc in range(2):
                self.tr(psb[:, kc * 128:(kc + 1) * 128], qkT[:, 2 + kc, tcol(n):tcol(n) + 128], self.identb[:, :])
            self.cp(ktok[:, n, :], psb[:, 0:256], eng=('act' if n % 2 else 'dve'))
            if n % 4 == 3:
                yield

        Vp = S.alloc('Vp', [128, NTILE, 2, 4, 65], BF16)
        Xb = S.alloc('Xb', [128, NTILE, 2, 4, 65], BF16)
        v32 = S.alloc('v32', [128, 9, 256], F32)
        for t0 in range(0, NTILE, 9):
            tn = min(9, NTILE - t0)
            self.dma('sp', v32[:, 0:tn, :], d['p_tok'][t0 * 128:(t0 + tn) * 128, 0:256].rearrange('(n p) c -> p n c', p=128))
            for dd in range(2):
                self.tt(Vp[:, t0:t0 + tn, dd, :, 0:64], v32[:, 0:tn, :].rearrange('p n (h e) -> p n h e', h=4),
                        ES[:, t0:t0 + tn, dd, :].unsqueeze(3).to_broadcast([128, tn, 4, 64]), ALU.mult)
            yield
        for dd in range(2):
            self.cp(Vp[:, :, dd, :, 64:65], ES[:, :, dd, :].unsqueeze(3))
        S.free(v32, ES)

        Xf = S.alloc('Xf', [128, 2, 4, 65], F32)
        self.memset(Xf[:, :, :, :].rearrange('p d h e -> p (d h e)'), 0.0)
        order = [list(range(NTILE)), [1, 0] + list(range(NTILE - 1, 1, -1))]
        for step in range(NTILE):
            for dd in range(2):
                n = order[dd][step]
                self.cp(Xb[:, n, dd, :, :], Xf[:, dd, :, :], eng='act')
                ps = self.ps()
                psv = ps[:, 0:260].rearrange('p (h e) -> p h e', h=4)
                for h in range(4):
                    self.mm(psv[:, h, :], ktok[:, n, (h // 2) * 128:(h // 2) * 128 + 128], Vp[:, n, dd, h, :])
                self.tt(Xf[:, dd, :, :], Xf[:, dd, :, :], psv, ALU.add)
                self.tt(Xf[:, dd, :, :], Xf[:, dd, :, :], EG[:, n, dd, :].unsqueeze(2).to_broadcast([128, 4, 65]), ALU.mult)
            yield
        S.free(Xf, EG, ktok)

        mlnw = self.bcast('mlnw', d['ml_nw'][l, :], 256)
        Sms = [S.alloc(f'Sm{i}', [128, 128], BF16) for i in range(4)]
        Hd = [S.alloc(f'Hd{i}', [128, 4, 65], F32) for i in range(2)]
        den = S.alloc('den', [128, 2, 4], F32)
        hs = S.alloc('hs', [128, 4, 64], F32)
        ht = S.alloc('ht', [128, 4, 64], F32)
        ssq = S.alloc('ssq', [128, 4], F32)
        o32 = [S.alloc(f'o32{i}', [128, 256], F32) for i in range(2)]
        res = [S.alloc(f'mres{i}', [128, 256], BF16) for i in range(2)]
        resT = [S.alloc(f'mresT{i}', [128, 2, 128], BF16) for i in range(2)]
        smi = 0
        for n in range(2 if last else 0, NTILE):
            psO = [self.psacc(), self.psacc()]
            psOv = [p[:, 0:260].rearrange('p (h e) -> p h e', h=4) for p in psO]
            c0 = tcol(n)
            for h in range(4):
                pb = (h % 2) * 64
                qT = qkT[pb:pb + 64, h // 2, c0:c0 + 128]
                kT = qkT[pb:pb + 64, 2 + h // 2, c0:c0 + 128]
                psS = self.ps()
                self.mm(psS[:, 0:128], kT, qT)
                for dd in range(2):
                    Sm = Sms[smi % 4]
                    smi += 1
                    self.tt(Sm[:, :], psS[:, 0:128], (self.tri0 if dd == 0 else self.tri1)[:, :], ALU.mult)
                    self.mm(psOv[dd][:, h, :], Sm[:, :], Vp[:, n, dd, h, :], start=True, stop=False)
                    self.mm(psOv[dd][:, h, :], qT, Xb[pb:pb + 64, n, dd, h, :], start=False, stop=True)
                yield
            for dd in range(2):
                self.tt(Hd[dd][:, :, :], psOv[dd], EB[:, n, dd, :].unsqueeze(2).to_broadcast([128, 4, 65]), ALU.mult)
                self.stt(den[:, dd, :], Hd[dd][:, :, 64], -1.0, Hd[dd][:, :, 64], ALU.mult, ALU.max)
            self.ts(den[:, :, :], den[:, :, :], 1.0, ALU.max)
            self.recip(den[:, :, :], den[:, :, :])
            self.tt(hs[:, :, :], Hd[0][:, :, 0:64], den[:, 0, :].unsqueeze(2).to_broadcast([128, 4, 64]), ALU.mult)
            self.tt(ht[:, :, :], Hd[1][:, :, 0:64], den[:, 1, :].unsqueeze(2).to_broadcast([128, 4, 64]), ALU.mult)
            self.tt(hs[:, :, :], hs[:, :, :], ht[:, :, :], ALU.add)
            self.tt(ht[:, :, :], hs[:, :, :], hs[:, :, :], ALU.mult)
            self.red(ssq[:, :], ht[:, :, :], ALU.add)
            self.act(ssq[:, :], ssq[:, :], AF.Sqrt, bias=self.eps[:, 0:1], scale=1.0 / 64)
            self.recip(ssq[:, :], ssq[:, :])
            self.tt(hs[:, :, :], hs[:, :, :], ssq[:, :].unsqueeze(2).to_broadcast([128, 4, 64]), ALU.mult)
            hs2 = hs[:, :, :].rearrange('p h e -> p (h e)')
            self.tt(hs2, hs2, mlnw[:, :], ALU.mult)
            o = o32[n % 2]
            self.dma('sp', o[:, :], d['p_tok'][n * 128:(n + 1) * 128, 256:512])
            self.act(o[:, :], o[:, :], AF.Sigmoid)
            r = res[n % 2]
            self.tt(r[:, :], hs2, o[:, :], ALU.mult)
            pst = self.ps()
            pstb = pst[:, :].bitcast(BF16)
            for c in range(2):
                self.tr(pstb[:, c * 128:(c + 1) * 128], r[:, c * 128:(c + 1) * 128], self.identb[:, :])
            rT = resT[n % 2]
            self.cp(rT[:, :, :], pstb[:, 0:256].rearrange('p (c t) -> p c t', c=2), eng='act')
            self.dma('pool', d['cat'][0:256, n * 128:(n + 1) * 128].rearrange('(c p) t -> p c t', p=128), rT[:, :, :])
            yield
        S.free(mlnw, den, hs, ht, ssq, qkT, Vp, Xb, EB, *Sms, *Hd, *o32, *res, *resT)

    def attn_block(self, kT_of, qTv, nq, keys, Va_of, out_dram, bm_of=None):
        psO = self.psacc()
        nk = len(keys)

        def issue_s(i):
            kt, cfg = keys[i]
            psS = self.ps()
            self.mm(psS[:, 0:nq], kT_of(kt), qTv, start=True, stop=True)
            return psS
        D = self.cur.get('depth', 2)
        pend = [issue_s(i) for i in range(min(D, nk))]
        for i in range(nk):
            psS = pend.pop(0)
            if i + D < nk:
                pend.append(issue_s(i + D))
            P = self.Ps[self.pi % len(self.Ps)]
            self.pi += 1
            self.act(P[:, 0:nq], psS[:, 0:nq], AF.Exp)
            if keys[i][1] is not None:
                self.tt(P[:, 0:nq], P[:, 0:nq], bm_of(keys[i][1]), ALU.mult)
            self.mm(psO[:, 0:nq], Va_of(keys[i][0]), P[:, 0:nq], start=(i == 0), stop=(i == nk - 1))
            yield
        r = self.pi % 2
        rsb, rinv, ob = self.rsbs[r], self.rinvs[r], self.obs[r]
        self.act(rsb[64:65, 0:nq], psO[64:65, 0:nq], AF.Ln)
        self.act(rsb[64:65, 0:nq], rsb[64:65, 0:nq], AF.Exp, scale=-1.0)
        psR = self.ps()
        self.mm(psR[0:64, 0:nq], self.onesf[64:65, 0:64], rsb[64:65, 0:nq])
        self.cp(rinv[:, 0:nq], psR[0:64, 0:nq], eng='act')
        self.tt(ob[:, 0:nq], psO[0:64, 0:nq], rinv[:, 0:nq], ALU.mult)
        self.dma('pool', out_dram, ob[:, 0:nq])
        yield

    def attn_bufs(self):
        S = self.S
        self.Ps = [S.alloc(f'P{i}', [128, 512], BF16) for i in range(3)]
        self.rsbs = [S.alloc(f'rsb{i}', [128, 512], F32) for i in range(2)]
        self.rinvs = [S.alloc(f'rinv{i}', [64, 512], F32) for i in range(2)]
        self.obs = [S.alloc(f'ob{i}', [64, 512], BF16) for i in range(2)]
        self.pi = 0
        return [*self.Ps, *self.rsbs, *self.rinvs, *self.obs]

    def stage_na(self, l, last):
        S, d = self.S, self.d
        ab = self.attn_bufs()
        qT = S.alloc('naq', [64, NT], BF16)
        kT = S.alloc('nak', [64, NT], BF16)
        Va = S.alloc('nav', [128, NTILE, 128], BF16)
        self.memset(Va[:, :, :].rearrange('p n e -> p (n e)'), 0.0)
        bm = S.alloc('nabm', [128, 14, 256], BF16)
        ebm = S.alloc('naebm', [128, 14, 256], BF16)
        vst = S.alloc('vst', [128, NTILE, 64], F32)
        for h in range(6):
            self.dma('sp', qT[:, :], d['pT_na'][h * 64:(h + 1) * 64, :])
            self.dma('sp', kT[:, :], d['pT_na'][384 + h * 64:384 + (h + 1) * 64, :])
            self.memset(Va[:, :, 64:65], 1.0)
            self.dma('sp', vst[:, :, :], d['p_tok'][:, 528 + h * 64:528 + (h + 1) * 64].rearrange('(n p) c -> p n c', p=128))
            self.cp(Va[:, :, 0:64], vst[:, :, :], eng='act')
            for cfg in range(14):
                self.dma('pool', bm[:, cfg, :], d['na_bm'][l, h, cfg])
            self.act(ebm[:, :, :].rearrange('p c q -> p (c q)'), bm[:, :, :].rearrange('p c q -> p (c q)'), AF.Exp)
            blocks = []
            if not last:
                blocks.append((0, [(0, None), (1, None)]))
            for qb in range(16):
                if qb == 0:
                    lat = [(2 + j, 6 + j) for j in range(4)]
                elif qb == 15:
                    lat = [(2 + 28 + j, 10 + j) for j in range(4)]
                else:
                    lat = [(2 + 2 * qb - 2 + i, i) for i in range(6)]
                blocks.append((256 + qb * 256, [(0, None), (1, None)] + lat))
            for q0, keys in blocks:
                yield from self.attn_block(lambda kt: kT[:, kt * 128:(kt + 1) * 128], qT[:, q0:q0 + 256], 256, keys,
                                           lambda kt: Va[:, kt, :], d['cat'][256 + h * 64:256 + (h + 1) * 64, q0:q0 + 256],
                                           bm_of=lambda cfg: ebm[:, cfg, :])
        S.free(vst, qT, kT, Va, bm, ebm, *ab)

    def rope(self, xv, nh, cs, sn, tmp):
        x5 = xv.rearrange('p h (a b f) -> p h a b f', a=2, b=2)
        x1 = x5[:, :, :, 0, :]
        x2 = x5[:, :, :, 1, :]
        csb = cs[:, :].rearrange('p (a f) -> p a f', a=2).unsqueeze(1).to_broadcast([128, nh, 2, 8])
        snb = sn[:, :].rearrange('p (a f) -> p a f', a=2).unsqueeze(1).to_broadcast([128, nh, 2, 8])
        t = [tmp[:, i, 0:nh * 16].rearrange('p (h a f) -> p h a f', h=nh, a=2) for i in range(4)]
        self.tt(t[0], x1, csb, ALU.mult)
        self.tt(t[1], x2, snb, ALU.mult)
        self.tt(t[2], x2, csb, ALU.mult)
        self.tt(t[3], x1, snb, ALU.mult)
        self.tt(x1, t[0], t[1], ALU.subtract)
        self.tt(x2, t[2], t[3], ALU.add)

    def stage_mla(self, l, last):
        S, d = self.S, self.d
        wuq = S.alloc('wuq', [128, 4, 576], BF16)
        self.dma('pool', wuq[:, :, :], d['w_uq'][l].rearrange('(k p) n -> p k n', p=128))
        wukv = S.alloc('wukv', [128, 2, 768], BF16)
        self.dma('pool', wukv[:, :, :], d['w_ukv'][l].rearrange('(k p) n -> p k n', p=128))
        qnw = self.bcast('qnw', d['q_nw'][l, :], 512)
        kvnw = self.bcast('kvnw', d['kv_nw'][l, :], 256)
        qT = S.alloc('mqT', [96, 6, NT], BF16)
        kT = S.alloc('mkT', [96, 6, NT], BF16)
        Va = S.alloc('mVa', [128, NTILE, 6, 128], BF16)
        self.memset(Va[:, :, :, :].rearrange('p n h e -> p (n h e)'), 0.0)
        self.memset(Va[:, :, :, 64:65], 1.0)
        pms = [S.alloc(f'pm{i}', [128, 800], F32) for i in range(2)]
        rings = []
        for nm, shp, dt in (('junk', [128, 512], F32), ('ssa', [128, 2], F32), ('cn', [128, 768], BF16), ('cT', [128, 6, 128], BF16),
                            ('qf32', [128, 576], F32), ('qb16', [128, 6, 128], BF16), ('kfull', [128, 6, 128], BF16),
                            ('krr', [128, 32], F32), ('rtmp', [128, 4, 96], F32)):
            rings.append([S.alloc(f'{nm}{i}', shp, dt) for i in range(2)])
        for i in range(2):
            self.memset(rings[5][i][:, :, :].rearrange('p h e -> p (h e)'), 0.0)
            self.memset(rings[6][i][:, :, :].rearrange('p h e -> p (h e)'), 0.0)
        css = [S.alloc(f'cs{i}', [128, 16], F32) for i in range(2)]
        sns = [S.alloc(f'sn{i}', [128, 16], F32) for i in range(2)]
        scale = 96 ** -0.5
        for n in range(3 if 'mla_short' in self.flags else NTILE):
            pm = pms[n % 2]
            junk, ssa, cn, cT, qf32, qb16, kfull, krr, rtmp = [rg[n % 2] for rg in rings]
            self.dma('sp', pm[:, :], d['p_tok'][n * 128:(n + 1) * 128, 912:1712])
            cs, sn = css[n % 2], sns[n % 2]
            if n >= 2:
                self.dma('sp', cs[:, :], d['rope_cs'][(n - 2) * 128:(n - 1) * 128, :])
                self.dma('sp', sn[:, :], d['rope_sn'][(n - 2) * 128:(n - 1) * 128, :])
            self.act(junk[:, 0:512], pm[:, 0:512], AF.Square, accum=ssa[:, 0:1])
            self.act(junk[:, 0:256], pm[:, 512:768], AF.Square, accum=ssa[:, 1:2])
            self.rstd(ssa[:, 0:1], 512, 'q')
            self.rstd(ssa[:, 1:2], 256, 'kv')
            self.stt(cn[:, 0:512], pm[:, 0:512], ssa[:, 0:1], qnw[:, :], ALU.mult, ALU.mult)
            self.stt(cn[:, 512:768], pm[:, 512:768], ssa[:, 1:2], kvnw[:, :], ALU.mult, ALU.mult)
            ps = self.ps()
            psb = ps[:, :].bitcast(BF16)
            for k in range(6):
                self.tr(psb[:, k * 128:(k + 1) * 128], cn[:, k * 128:(k + 1) * 128], self.identb[:, :])
            self.cp(cT[:, :, :], psb[:, 0:768].rearrange('p (k t) -> p k t', k=6), eng='act')
            p1, p2 = self.ps(), self.ps()
            for k in range(4):
                self.mm(p1[:, 0:512], cT[:, k, :], wuq[:, k, 0:512], start=(k == 0), stop=(k == 3))
            for k in range(4):
                self.mm(p2[:, 0:64], cT[:, k, :], wuq[:, k, 512:576], start=(k == 0), stop=(k == 3))
            self.ts(qf32[:, 0:512], p1[:, 0:512], scale, ALU.mult)
            self.ts(qf32[:, 512:576], p2[:, 0:64], scale, ALU.mult)
            if n >= 2 and 'norope' not in self.flags:
                self.rope(qf32[:, :].rearrange('p (h e) -> p h e', h=6)[:, :, 64:96], 6, cs, sn, rtmp)
            self.cp(qb16[:, :, 0:96], qf32[:, :].rearrange('p (h e) -> p h e', h=6), eng='act')
            ps = self.ps()
            psb = ps[:, :].bitcast(BF16)
            if 'no_qtr' not in self.flags:
                for h in range(6):
                    self.tr(psb[:, h * 128:(h + 1) * 128], qb16[:, h, :], self.identb[:, :])
                for (pa_, pb2) in ((0, 64), (64, 96)):
                    self.cp(qT[pa_:pb2, :, n * 128:(n + 1) * 128], psb[pa_:pb2, 0:768].rearrange('p (h t) -> p h t', h=6), eng='act')
            k1, k2 = self.ps(), self.ps()
            for k in range(2):
                self.mm(k1[:, 0:512], cT[:, 4 + k, :], wukv[:, k, 0:512], start=(k == 0), stop=(k == 1))
            for k in range(2):
                self.mm(k2[:, 0:256], cT[:, 4 + k, :], wukv[:, k, 512:768], start=(k == 0), stop=(k == 1))
            k1v = k1[:, 0:512].rearrange('p (h e) -> p h e', h=4)
            k2v = k2[:, 0:256].rearrange('p (h e) -> p h e', h=2)
            self.cp(kfull[:, 0:4, 0:64], k1v[:, :, 0:64])
            self.cp(kfull[:, 4:6, 0:64], k2v[:, :, 0:64])
            self.cp(Va[:, n, 0:4, 0:64], k1v[:, :, 64:128], eng='act')
            self.cp(Va[:, n, 4:6, 0:64], k2v[:, :, 64:128], eng='act')
            self.cp(krr[:, :], pm[:, 768:800])
            if n >= 2 and 'norope' not in self.flags:
                self.rope(krr[:, :].unsqueeze(1), 1, cs, sn, rtmp)
            self.cp(kfull[:, :, 64:96], krr[:, :].unsqueeze(1).to_broadcast([128, 6, 32]))
            ps = self.ps()
            psb = ps[:, :].bitcast(BF16)
            if 'no_ktr' not in self.flags:
                for h in range(6):
                    self.tr(psb[:, h * 128:(h + 1) * 128], kfull[:, h, :], self.identb[:, :])
                for (pa_, pb2) in ((0, 64), (64, 96)):
                    self.cp(kT[pa_:pb2, :, n * 128:(n + 1) * 128], psb[pa_:pb2, 0:768].rearrange('p (h t) -> p h t', h=6), eng='dve')
        S.free(wuq, wukv, qnw, kvnw, *pms, *css, *sns, *[b for rg in rings for b in rg])

        if getattr(self, 'stop_after', None) == ('mla1', l):
            S.free(qT, kT, Va)
            return
        ab = self.attn_bufs()
        allk = [(kt, None) for kt in range(NTILE)]
        for h in range(6):
            if not last:
                for _ in self.attn_block(lambda kt: kT[:, h, kt * 128:(kt + 1) * 128], qT[:, h, 0:256], 256, [(0, None), (1, None)],
                                         lambda kt: Va[:, kt, h, :], d['cat'][640 + h * 64:640 + (h + 1) * 64, 0:256]):
                    pass
            for b in range(8):
                q0 = 256 + b * 512
                for _ in self.attn_block(lambda kt: kT[:, h, kt * 128:(kt + 1) * 128], qT[:, h, q0:q0 + 512], 512, allk,
                                         lambda kt: Va[:, kt, h, :], d['cat'][640 + h * 64:640 + (h + 1) * 64, q0:q0 + 512]):
                    pass
        S.free(qT, kT, Va, *ab)

    def stage_out(self, l, xs, last):
        S, d = self.S, self.d
        w = S.alloc('w_out', [128, 8, DM], BF16)
        wsrc = d['w_out'][l].rearrange('(k p) n -> p k n', p=128)
        for k in range(8):
            self.dma('pool', w[:, k, :], wsrc[:, k, :])
        ab = self.mod_ab(l, 1, 'norm2_w', 'b')
        g1 = [self.bcast(f'g1{r}', d['mod'][l, r, 2 * DM:3 * DM], DM) for r in range(2)]
        if last:
            rw = S.alloc('rw', [128, 8, 8], F32)
            self.dma('sp', rw[:, :, :], d['router'].rearrange('(k p) e -> p k e', p=128))
            hf = S.alloc('hf', [128, DM], F32)
            hT32 = S.alloc('hT32', [128, 8, 128], F32)
            lg = S.alloc('lg', [128, 8], F32)
            l2 = S.alloc('l2', [128, 8], F32)
            mx = S.alloc('mx', [128, 4], F32)
            gsb = S.alloc('gsb', [128, 8], F32)
            m1m = S.alloc('m1m', [128, 8], F32)
        catTs = [S.alloc(f'catT{i}', [128, 8, 128], BF16) for i in range(2)]
        xts = [S.alloc(f'xo{i}', [128, DM], F32) for i in range(2)]
        xn = [S.alloc(f'xn{i}', [128, DM], F32) for i in range(2)]
        tmps = [S.alloc(f'tmpo{i}', [128, DM], F32) for i in range(2)]
        hbos = [S.alloc(f'hbo{i}', [128, DM], BF16) for i in range(2)]
        hT = [S.alloc(f'hTo{i}', [128, 8, 128], BF16) for i in range(2)]
        sso = [S.alloc(f'sso{i}', [128, 1], F32) for i in range(2)]
        for n in range(NTILE):
            if last and n < 2:
                continue
            r = 1 if n < 2 else 0
            catT, tmp, hb, ss = catTs[n % 2], tmps[n % 2], hbos[n % 2], sso[n % 2]
            self.dma('sp', catT[:, :, :], d['cat'].rearrange('(k p) t -> p k t', p=128)[:, :, n * 128:(n + 1) * 128])
            xt = xts[n % 2]
            self.dma('sp', xt[:, :], xs[n * 128:(n + 1) * 128, :])
            x1 = xn[n % 2]
            for half in range(2):
                ps = self.ps()
                for k in range(8):
                    self.mm(ps[:, 0:512], catT[:, k, :], w[:, k, half * 512:(half + 1) * 512], start=(k == 0), stop=(k == 7))
                sl = slice(half * 512, (half + 1) * 512)
                self.tt(x1[:, sl], ps[:, 0:512], g1[r][:, sl], ALU.mult)
                self.tt(x1[:, sl], x1[:, sl], xt[:, sl], ALU.add)
            self.dma('pool', d['x1'][n * 128:(n + 1) * 128, :], x1[:, :])
            A, B = ab[r]
            if last:
                self.norm_mod(x1, A, B, hf, tmp, ss)
                self.cp(hb[:, :], hf[:, :], eng='act')
                self.dma('pool', d['h2'][n * 128:(n + 1) * 128, :], hb[:, :])
                pa, pb_ = self.ps(), self.ps()
                for k in range(8):
                    pp = pa if k < 4 else pb_
                    self.tr(pp[:, (k % 4) * 128:(k % 4 + 1) * 128], hf[:, k * 128:(k + 1) * 128], self.identf[:, :])
                self.cp(hT32[:, 0:4, :], pa[:, :].rearrange('p (k t) -> p k t', k=4))
                self.cp(hT32[:, 4:8, :], pb_[:, :].rearrange('p (k t) -> p k t', k=4), eng='act')
                pl = self.ps()
                for k in range(8):
                    self.mm(pl[:, 0:8], hT32[:, k, :], rw[:, k, :], start=(k == 0), stop=(k == 7))
                self.cp(lg[:, :], pl[:, 0:8])
                self.red(mx[:, 0:1], lg[:, :], ALU.max)
                self.ts(m1m[:, :], lg[:, :], mx[:, 0:1], ALU.is_equal)
                self.dma('pool', d['sel1'][n * 128:(n + 1) * 128, :], m1m[:, :])
                self.stt(l2[:, :], m1m[:, :], -1e30, lg[:, :], ALU.mult, ALU.add)
                self.red(mx[:, 1:2], l2[:, :], ALU.max)
                self.ts(l2[:, :], lg[:, :], mx[:, 1:2], ALU.is_ge)
                self.ts(mx[:, 2:3], mx[:, 0:1], -1.0, ALU.mult)
                self.act(gsb[:, :], lg[:, :], AF.Exp, bias=mx[:, 2:3], scale=1.0)
                self.tt(gsb[:, :], gsb[:, :], l2[:, :], ALU.mult)
                self.red(mx[:, 3:4], gsb[:, :], ALU.add)
                self.recip(mx[:, 3:4], mx[:, 3:4])
                self.ts(gsb[:, :], gsb[:, :], mx[:, 3:4], ALU.mult)
                self.dma('pool', d['gates'][n * 128:(n + 1) * 128, :], gsb[:, :])
            else:
                self.norm_mod(x1, A, B, hb, tmp, ss)
            ht = hT[n % 2]
            self.transpose8(hb, ht[:, :, :])
            self.dma('pool', d['h2T'].rearrange('(k p) t -> p k t', p=128)[:, :, n * 128:(n + 1) * 128], ht[:, :, :])
        fr = [w, *catTs, *tmps, *hbos, *sso, *xts, *xn, *hT, *g1]
        if last:
            fr += [rw, hf, hT32, lg, l2, mx, gsb, m1m]
        S.free(*fr)
        for a, b in ab:
            S.free(a, b)

    def stage_ffn(self, l, last):
        S, d = self.S, self.d
        g2 = [self.bcast(f'g2{r}', d['mod'][l, r, 5 * DM:6 * DM], DM) for r in range(2)]
        if last:
            fw = self.bcast('fw', d['fin_w'][:], DM)
            sbs = [(256 + i * 1024, 1024) for i in range(4)]
            experts = [(d['moe_w1'][e], d['moe_w3'][e], d['moe_w2'][e]) for e in range(8)]
        else:
            sbs = [(0, 256)] + [(256 + i * 1024, 1024) for i in range(4)]
            experts = [(d['ffn_w1'], d['ffn_w3'], d['ffn_w2'])]
        hTb = S.alloc('hTb', [128, 8, 1024], BF16)
        acc = S.alloc('facc', [128, 8, DM], F32)
        uT = S.alloc('uT', [128, 4, 1024], BF16)
        us = [S.alloc(f'us{i}', [128, 512], F32) for i in range(2)]
        w1s = [S.alloc(f'w1g{i}', [128, 8, 512], BF16) for i in range(2)]
        w3s = [S.alloc(f'w3g{i}', [128, 8, 512], BF16) for i in range(2)]
        w2s = [S.alloc(f'w2g{i}', [128, 4, DM], BF16) for i in range(2)]
        gt = S.alloc('fgt', [128, 8, 8], F32)
        xts = [S.alloc(f'xf{i}', [128, DM], F32) for i in range(2)]
        tmp = S.alloc('ftmp', [128, DM], F32)
        ss = S.alloc('fss', [128, 1], F32)
        groups = [(g * 512, 512) for g in range(5)] + [(2560, 256)]
        wi = 0
        for (t0, ntok) in sbs:
            ntile = ntok // 128
            self.dma('sp', hTb[:, :, 0:ntok], d['h2T'].rearrange('(k p) t -> p k t', p=128)[:, :, t0:t0 + ntok])
            if last:
                self.dma('sp', gt[:, 0:ntile, :], d['gates'][t0:t0 + ntok, :].rearrange('(n p) e -> p n e', p=128))
            first = True
            for ei, (W1, W3, W2) in enumerate(experts):
                for (g0, gs) in groups:
                    nfc = gs // 128
                    w1, w3, w2 = w1s[wi % 2], w3s[wi % 2], w2s[wi % 2]
                    wi += 1
                    self.dma('pool', w1[:, :, 0:gs], W1.rearrange('(k p) n -> p k n', p=128)[:, :, g0:g0 + gs])
                    self.dma('pool', w3[:, :, 0:gs], W3.rearrange('(k p) n -> p k n', p=128)[:, :, g0:g0 + gs])
                    self.dma('pool', w2[:, 0:nfc, :], W2[g0:g0 + gs, :].rearrange('(c p) n -> p c n', p=128))
                    for tb in range((ntok + 511) // 512):
                        tw = min(512, ntok - tb * 512)
                        for fc in range(nfc):
                            pa, pb_ = self.ps(), self.ps()
                            for k in range(8):
                                self.mm(pa[:, 0:tw], w1[:, k, fc * 128:(fc + 1) * 128], hTb[:, k, tb * 512:tb * 512 + tw], start=(k == 0), stop=(k == 7))
                            for k in range(8):
                                self.mm(pb_[:, 0:tw], w3[:, k, fc * 128:(fc + 1) * 128], hTb[:, k, tb * 512:tb * 512 + tw], start=(k == 0), stop=(k == 7))
                            u = us[fc % 2]
                            self.act(u[:, 0:tw], pa[:, 0:tw], AF.Silu)
                            self.tt(uT[:, fc, tb * 512:tb * 512 + tw], u[:, 0:tw], pb_[:, 0:tw], ALU.mult)
                    for tt_ in range(ntile):
                        for half in range(2):
                            ps = self.ps()
                            for fc in range(nfc):
                                self.mm(ps[:, 0:512], uT[:, fc, tt_ * 128:(tt_ + 1) * 128], w2[:, fc, half * 512:(half + 1) * 512],
                                        start=(fc == 0), stop=(fc == nfc - 1))
                            a = acc[:, tt_, half * 512:(half + 1) * 512]
                            if last:
                                gsc = gt[:, tt_, ei:ei + 1]
                                if first:
                                    self.ts(a, ps[:, 0:512], gsc, ALU.mult)
                                else:
                                    self.stt(a, ps[:, 0:512], gsc, a, ALU.mult, ALU.add)
                            else:
                                if first:
                                    self.cp(a, ps[:, 0:512], eng='act')
                                else:
                                    self.tt(a, a, ps[:, 0:512], ALU.add)
                    first = False
            for tt_ in range(ntile):
                n = t0 // 128 + tt_
                r = 1 if n < 2 else 0
                xt = xts[tt_ % 2]
                self.dma('sp', xt[:, :], d['x1'][n * 128:(n + 1) * 128, :])
                a = acc[:, tt_, :]
                self.tt(a, a, g2[r][:, :], ALU.mult)
                self.tt(xt[:, :], xt[:, :], a, ALU.add)
                if last:
                    self.act(tmp[:, :], xt[:, :], AF.Square, accum=ss[:, 0:1])
                    self.rstd(ss[:, 0:1], DM, 'f')
                    self.stt(xt[:, :], xt[:, :], ss[:, 0:1], fw[:, :], ALU.mult, ALU.mult)
                    self.dma('pool', d['out'][(n - 2) * 128:(n - 1) * 128, :], xt[:, :])
                else:
                    self.dma('pool', d['x2'][n * 128:(n + 1) * 128, :], xt[:, :])
        fr = [hTb, acc, uT, gt, tmp, ss, *us, *w1s, *w3s, *w2s, *xts, *g2]
        if last:
            fr.append(fw)
        S.free(*fr)


    def stage_moe(self, l):
        S, d = self.S, self.d
        NL = 32
        g2 = self.bcast('g2m', d['mod'][l, 0, 5 * DM:6 * DM], DM)
        fw = self.bcast('fwm', d['fin_w'][:], DM)
        G3 = S.alloc('G3', [128, NL, 8], F32)
        M1 = S.alloc('M1', [128, NL, 8], F32)
        self.dma('sp', G3[:, :, :], d['gates'][256:NT, :].rearrange('(n p) e -> p n e', p=128))
        self.dma('sp', M1[:, :, :], d['sel1'][256:NT, :].rearrange('(n p) e -> p n e', p=128))
        Ms = S.alloc('Ms', [128, NL, 8], F32)
        self.ts(Ms[:, :, :], G3[:, :, :], 0.0, ALU.is_gt)
        M2 = S.alloc('M2', [128, NL, 8], F32)
        self.tt(M2[:, :, :], Ms[:, :, :], M1[:, :, :], ALU.subtract)
        triS = S.alloc('triS', [128, 128], F32)
        self.tt(triS[:, :], self.tri0[:, :], self.identf[:, :], ALU.subtract)
        Ms2 = Ms[:, :, :].rearrange('p n e -> p (n e)')
        pc_, pr_ = self.ps(), self.ps()
        self.mm(pc_[:, 0:256], self.onesf[:, :], Ms2)
        self.mm(pr_[:, 0:256], triS[:, :], Ms2)
        CS = S.alloc('CS', [128, NL, 8], F32)
        self.cp(CS[:, :, :].rearrange('p n e -> p (n e)'), pc_[:, 0:256])
        pos = S.alloc('pos', [128, NL, 8], F32)
        self.cp(pos[:, :, :].rearrange('p n e -> p (n e)'), pr_[:, 0:256], eng='act')
        TB = S.alloc('TB', [128, NL + 1, 8], F32)
        self.memset(TB[:, 0, :], 0.0)
        for n in range(NL):
            self.tt(TB[:, n + 1, :], TB[:, n, :], CS[:, n, :], ALU.add)
        cth = S.alloc('cth', [128, 16], F32)
        self.dma('sp', cth[:, :], d['c_th'][:, :])
        cj = S.alloc('cj', [128, NBLK], F32)
        self.dma('sp', cj[:, :], d['c_j512'][:, :])
        cgp = S.alloc('cgp', [128, 11], F32)
        self.dma('sp', cgp[:, :], d['c_gp'][:, :])
        cmp_ = S.alloc('cmp', [128, 8, 16], F32)
        self.tt(cmp_[:, :, :], cth[:, :].unsqueeze(1).to_broadcast([128, 8, 16]),
                TB[:, NL, :].unsqueeze(2).to_broadcast([128, 8, 16]), ALU.is_lt)
        pcn = S.alloc('pcn', [128, 8], F32)
        self.red(pcn[:, :], cmp_[:, :, :], ALU.add)
        self.ts(pcn[:, :], pcn[:, :], 512.0, ALU.mult)
        end = S.alloc('end', [128, 8], F32)
        self.cp(end[:, 0:1], pcn[:, 0:1])
        for e in range(1, 8):
            self.tt(end[:, e:e + 1], end[:, e - 1:e], pcn[:, e:e + 1], ALU.add)
        st = S.alloc('st', [128, 8], F32)
        self.tt(st[:, :], end[:, :], pcn[:, :], ALU.subtract)
        self.tt(pos[:, :, :], pos[:, :, :], TB[:, 0:NL, :], ALU.add)
        self.tt(pos[:, :, :], pos[:, :, :], st[:, :].unsqueeze(1).to_broadcast([128, NL, 8]), ALU.add)
        tmp3 = S.alloc('tmp3', [128, NL, 8], F32)
        slotf = S.alloc('slotf', [128, 2, NL], F32)
        gk = S.alloc('gk', [128, 2, NL], F32)
        for k, Mk in enumerate((M1, M2)):
            self.tt(tmp3[:, :, :], Mk[:, :, :], pos[:, :, :], ALU.mult)
            self.red(slotf[:, k, :], tmp3[:, :, :], ALU.add)
            self.tt(tmp3[:, :, :], Mk[:, :, :], G3[:, :, :], ALU.mult)
            self.red(gk[:, k, :], tmp3[:, :, :], ALU.add)
        self.ts(slotf[:, :, :], slotf[:, :, :], float(NSLOT - 1), ALU.min)
        sloti = S.alloc('sloti', [128, 2, NL], I32)
        self.cp(sloti[:, :, :].rearrange('p k n -> p (k n)'), slotf[:, :, :].rearrange('p k n -> p (k n)'))
        bj = S.alloc('bj', [128, NBLK, 8], F32)
        self.tt(bj[:, :, :], end[:, :].unsqueeze(1).to_broadcast([128, NBLK, 8]),
                cj[:, :].unsqueeze(2).to_broadcast([128, NBLK, 8]), ALU.is_le)
        ej = S.alloc('ej', [128, NBLK], F32)
        self.red(ej[:, :], bj[:, :, :], ALU.add)
        self.ts(ej[:, :], ej[:, :], 7.0, ALU.min, 1408.0, ALU.mult)
        idxf = S.alloc('idxf', [128, NBLK, 11], F32)
        self.tt(idxf[:, :, :], ej[:, :].unsqueeze(2).to_broadcast([128, NBLK, 11]),
                cgp[:, :].unsqueeze(1).to_broadcast([128, NBLK, 11]), ALU.add)
        idxi = S.alloc('idxi', [128, NBLK, 11], I32)
        self.cp(idxi[:, :, :].rearrange('p j g -> p (j g)'), idxf[:, :, :].rearrange('p j g -> p (j g)'))
        S.free(G3, M1, Ms, M2, triS, CS, pos, TB, cth, cj, cgp, cmp_, pcn, end, st, tmp3, slotf, bj, ej, idxf)

        zt = S.alloc('zt', [128, 24, DM], BF16)
        self.memset(zt[:, :, :].rearrange('p a c -> p (a c)'), 0.0)
        for j in range(NBLK // 6):
            self.dma('sp', d['hs'][j * 3072:(j + 1) * 3072, :].rearrange('(a p) c -> p a c', p=128), zt[:, :, :])
        hts = [S.alloc(f'hsrc{i}', [128, DM], BF16) for i in range(2)]
        for n in range(NL):
            ht = hts[n % 2]
            self.dma('sp', ht[:, :], d['h2'][(n + 2) * 128:(n + 3) * 128, :])
            for k in range(2):
                self.dma_ind(d['hs'][:, :], ht[:, :], sloti[:, k, n:n + 1], True, NSLOT - 1)
        S.free(zt, *hts)

        hbs = [S.alloc(f'hsb{i}', [128, 4, DM], BF16) for i in range(2)]
        hTs = [S.alloc(f'hTm{i}', [128, 8, 512], BF16) for i in range(2)]
        accs = [S.alloc(f'macc{i}', [128, 4, DM], F32) for i in range(2)]
        uTs = [S.alloc(f'uTm{i}', [128, 2, 512], BF16) for i in range(2)]
        us = [S.alloc(f'usm{i}', [128, 512], F32) for i in range(2)]
        w1s = [S.alloc(f'w1m{i}', [128, 8, 256], BF16) for i in range(4)]
        w3s = [S.alloc(f'w3m{i}', [128, 8, 256], BF16) for i in range(4)]
        w2s = [S.alloc(f'w2m{i}', [128, 2, DM], BF16) for i in range(4)]
        wi = 0
        nrow = 8 * 11 * 128
        for j in range(NBLK):
            hb, hT, acc = hbs[j % 2], hTs[j % 2], accs[j % 2]
            self.dma('sp', hb[:, :, :], d['hs'][j * 512:(j + 1) * 512, :].rearrange('(a p) c -> p a c', p=128))
            for a in range(4):
                ps = self.ps()
                psb = ps[:, :].bitcast(BF16)
                for k in range(8):
                    self.tr(psb[:, k * 128:(k + 1) * 128], hb[:, a, k * 128:(k + 1) * 128], self.identb[:, :])
                self.cp(hT[:, :, a * 128:(a + 1) * 128], psb[:, :].rearrange('p (k t) -> p k t', k=8), eng=('act' if a % 2 else 'dve'))
            for gp in range(0, 11, 2):
                ws = []
                for g in [x for x in (gp, gp + 1) if x < 11]:
                    w1, w3, w2 = w1s[wi % 4], w3s[wi % 4], w2s[wi % 4]
                    wi += 1
                    ix = idxi[:, j, g:g + 1]
                    self.dma_ind(w1[:, :, :].rearrange('p k c -> p (k c)'), d['moe_w1'][:, :], ix, False, nrow - 1)
                    self.dma_ind(w3[:, :, :].rearrange('p k c -> p (k c)'), d['moe_w3'][:, :], ix, False, nrow - 1)
                    self.dma_ind(w2[:, :, :].rearrange('p k c -> p (k c)'), d['moe_w2'][:, :], ix, False, nrow - 1)
                    uT = uTs[g % 2]
                    for fc in range(2):
                        pa, pb_ = self.ps(), self.ps()
                        for k in range(8):
                            self.mm(pa[:, 0:512], w1[:, k, fc * 128:(fc + 1) * 128], hT[:, k, :], start=(k == 0), stop=(k == 7))
                        for k in range(8):
                            self.mm(pb_[:, 0:512], w3[:, k, fc * 128:(fc + 1) * 128], hT[:, k, :], start=(k == 0), stop=(k == 7))
                        u = us[fc % 2]
                        self.act(u[:, :], pa[:, 0:512], AF.Silu)
                        self.tt(uT[:, fc, :], u[:, :], pb_[:, 0:512], ALU.mult)
                    ws.append((uT, w2))
                for a in range(4):
                    for half in range(2):
                        ps = self.psacc()
                        nmm = 2 * len(ws)
                        mi = 0
                        for (uT, w2) in ws:
                            for fc in range(2):
                                self.mm(ps[:, 0:512], uT[:, fc, a * 128:(a + 1) * 128], w2[:, fc, half * 512:(half + 1) * 512],
                                        start=(mi == 0), stop=(mi == nmm - 1))
                                mi += 1
                        av = acc[:, a, half * 512:(half + 1) * 512]
                        if gp == 0:
                            self.cp(av, ps[:, 0:512], eng='act')
                        else:
                            self.tt(av, av, ps[:, 0:512], ALU.add)
            self.dma('sp', d['yb'][j * 512:(j + 1) * 512, :].rearrange('(a p) c -> p a c', p=128), acc[:, :, :])
        S.free(*hbs, *hTs, *accs, *uTs, *us, *w1s, *w3s, *w2s, idxi)

        y1s = [S.alloc(f'y1_{i}', [128, DM], F32) for i in range(2)]
        y2s = [S.alloc(f'y2_{i}', [128, DM], F32) for i in range(2)]
        xts = [S.alloc(f'xm{i}', [128, DM], F32) for i in range(2)]
        tmp = S.alloc('mtmp', [128, DM], F32)
        ss = S.alloc('mss', [128, 1], F32)
        outs = [S.alloc(f'mout{i}', [128, DM], F32) for i in range(2)]
        def loads(n):
            y1, y2, xt = y1s[n % 2], y2s[n % 2], xts[n % 2]
            self.dma_ind(y1[:, :], d['yb'][:, :], sloti[:, 0, n:n + 1], False, NSLOT - 1)
            self.dma_ind(y2[:, :], d['yb'][:, :], sloti[:, 1, n:n + 1], False, NSLOT - 1)
            self.dma('sp', xt[:, :], d['x1'][(n + 2) * 128:(n + 3) * 128, :])
        loads(0)
        for n in range(NL):
            y1, y2, xt = y1s[n % 2], y2s[n % 2], xts[n % 2]
            ob = outs[n % 2]
            if n + 1 < NL:
                loads(n + 1)
            self.ts(y1[:, :], y1[:, :], gk[:, 0, n:n + 1], ALU.mult)
            self.stt(y1[:, :], y2[:, :], gk[:, 1, n:n + 1], y1[:, :], ALU.mult, ALU.add)
            self.tt(y1[:, :], y1[:, :], g2[:, :], ALU.mult)
            self.tt(xt[:, :], xt[:, :], y1[:, :], ALU.add)
            self.act(tmp[:, :], xt[:, :], AF.Square, accum=ss[:, 0:1])
            self.rstd(ss[:, 0:1], DM, 'f')
            self.stt(ob[:, :], xt[:, :], ss[:, 0:1], fw[:, :], ALU.mult, ALU.mult)
            self.dma('sp', d['out'][n * 128:(n + 1) * 128, :], ob[:, :])
        S.free(*outs)
        S.free(g2, fw, gk, sloti, tmp, ss, *y1s, *y2s, *xts)


def _na_bias_tiles(rpb):
    L, H = rpb.shape[0], rpb.shape[1]
    NEG = np.float32(-30000.0)
    col = np.arange(64)
    c0 = np.clip(col - 8, 0, 48)
    ck = col[:, None]
    cq = col[None, :]
    cvalid = (ck >= c0[None, :]) & (ck < c0[None, :] + 16)
    cidx = np.clip(ck - cq, -15, 15) + 15
    out = np.full((L, H, 14, 128, 256), NEG, dtype=np.float32)
    cfgs = []
    for i in range(6):
        cfgs.append((4, 4 - 4 + 2 * i))
    for j in range(4):
        cfgs.append((0, 2 * j))
    for j in range(4):
        cfgs.append((60, 56 + 2 * j))
    for ci, (R, kr0) in enumerate(cfgs):
        for a in range(2):
            kr = kr0 + a
            for b in range(4):
                r = R + b
                r0 = min(max(r - 4, 0), 56)
                if not (r0 <= kr <= r0 + 7) or kr > 63:
                    continue
                drow = kr - r + 7
                g = rpb[:, :, drow, :][:, :, cidx]
                blk = np.where(cvalid[None, None], g, NEG)
                out[:, :, ci, a * 64:(a + 1) * 64, b * 64:(b + 1) * 64] = blk
    return out


def _rope_tables():
    t = np.arange(4096)
    row = (t // 64).astype(np.float32)
    colv = (t % 64).astype(np.float32)
    inv = (np.float32(10000.0) ** (-np.arange(0, 16, 2, dtype=np.float32) / np.float32(16))).astype(np.float32)
    ang = np.concatenate([row[:, None] * inv, colv[:, None] * inv], axis=1).astype(np.float32)
    return np.cos(ang).astype(np.float32), np.sin(ang).astype(np.float32)


def _w13_layout(w):
    w = w.reshape(8, 8, 128, 11, 256)
    return np.ascontiguousarray(w.transpose(0, 3, 2, 1, 4)).reshape(8 * 11 * 128, 2048)


def _w2_layout(w):
    w = w.reshape(8, 11, 2, 128, 1024)
    return np.ascontiguousarray(w.transpose(0, 1, 3, 2, 4)).reshape(8 * 11 * 128, 2048)


_NC_CACHE = {}


def _get_nc(stop_after=None):
    key = (stop_after, DEBUG)
    if key not in _NC_CACHE:
        nc = bass.Bass("TRN2", target_bir_lowering=False)
        k = K(nc)
        k.stop_after = stop_after
        k.build()
        _NC_CACHE[key] = nc
    return _NC_CACHE[key]


def make_in_maps(inputs):
    f = lambda a: np.ascontiguousarray(np.asarray(a, dtype=np.float32))
    x, c, ctx, c_ctx = f(inputs['x']), f(inputs['c']), f(inputs['ctx']), f(inputs['c_ctx'])
    cs, sn = _rope_tables()
    tri0 = np.triu(np.ones((128, 128), np.float32))
    tri1 = np.tril(np.ones((128, 128), np.float32))
    shared = {
        'ada_w': f(inputs['ada_w']), 'ada_b': f(inputs['ada_b']),
        'norm1_w': f(inputs['norm1_w']), 'norm2_w': f(inputs['norm2_w']),
        'w_in': f(inputs['w_in']), 'w_out': f(inputs['w_out']),
        'convT': f(np.transpose(np.asarray(inputs['mlstm_conv_w']), (0, 2, 1))),
        'ig_b': f(np.asarray(inputs['mlstm_ig_b']).reshape(2, 8)),
        'fg_b': f(np.asarray(inputs['mlstm_fg_b']).reshape(2, 8)),
        'ml_nw': f(inputs['mlstm_norm_w']),
        'na_bm': _na_bias_tiles(f(inputs['na_rpb'])),
        'q_nw': f(inputs['mla_q_norm_w']), 'kv_nw': f(inputs['mla_kv_norm_w']),
        'w_uq': f(inputs['mla_w_uq']), 'w_ukv': f(inputs['mla_w_ukv']),
        'ffn_w1': f(inputs['ffn_w1'])[0], 'ffn_w3': f(inputs['ffn_w3'])[0], 'ffn_w2': f(inputs['ffn_w2'])[0],
        'router': f(inputs['moe_router_w'])[0],
        'moe_w1': _w13_layout(f(inputs['moe_w1'])[0]), 'moe_w3': _w13_layout(f(inputs['moe_w3'])[0]),
        'moe_w2': _w2_layout(f(inputs['moe_w2'])[0]),
        'c_th': np.ascontiguousarray(np.broadcast_to(np.arange(16, dtype=np.float32) * 512, (128, 16))),
        'c_j512': np.ascontiguousarray(np.broadcast_to(np.arange(NBLK, dtype=np.float32) * 512, (128, NBLK))),
        'c_gp': np.ascontiguousarray((np.arange(11, dtype=np.float32)[None, :] * 128 + np.arange(128, dtype=np.float32)[:, None])),
        'fin_w': f(inputs['final_norm_w']),
        'ident': np.eye(128, dtype=np.float32), 'tri0': tri0, 'tri1': tri1,
        'rope_cs': cs, 'rope_sn': sn,
    }
    maps = []
    for b in range(8):
        m = dict(shared)
        m['xin'] = np.ascontiguousarray(np.concatenate([ctx[b], x[b]], axis=0))
        cc = np.stack([c[b].reshape(8, 128).T, c_ctx.reshape(8, 128).T], axis=2)
        m['cc'] = np.ascontiguousarray(cc.reshape(128, 16))
        maps.append(m)
    return maps


def kernel(**inputs):
    nc = _get_nc()
    maps = make_in_maps(inputs)
    res = run_bass_kernel_spmd(nc, maps, core_ids=list(range(8)))
    return np.stack([np.asarray(r['out'], dtype=np.float32) for r in res.results], axis=0)
```

```python
import numpy as np
from contextlib import ExitStack
import concourse.bass as bass
import concourse.mybir as mybir
from concourse.bass_utils import run_bass_kernel_spmd

F32 = mybir.dt.float32
BF16 = mybir.dt.bfloat16
U8 = mybir.dt.uint8
AF = mybir.ActivationFunctionType
ALU = mybir.AluOpType
AX = mybir.AxisListType
DTSIZE = {F32: 4, BF16: 2, mybir.dt.int32: 4}
COMPUTE = ('pe', 'act', 'dve', 'pool')
QUEUES = ('sp', 'act', 'pool')
NDSEM = 12
ARENA = 207 * 1024

NT = 4352
NTILE = 34
DM = 1024
NIN = 2992
DFF = 2816
EPS = 1e-6
DEBUG = False
NBLK = 24
NSLOT = NBLK * 512
I32 = mybir.dt.int32


class Op:
    __slots__ = ('eng', 'fn', 'deps', 'isdma', 'needs_inc', 'count', 'semi', 'semval', 'idx', 'q')


class V:
    __slots__ = ('buf', 'ap')

    def __init__(self, buf, ap):
        self.buf = buf
        self.ap = ap

    def __getitem__(self, k):
        return V(self.buf, self.ap[k])

    def rearrange(self, pattern_, **kw):
        return V(self.buf, self.ap.rearrange(pattern_, **kw))

    def unsqueeze(self, a):
        return V(self.buf, self.ap.unsqueeze(a))

    def to_broadcast(self, shp):
        return V(self.buf, self.ap.to_broadcast(list(shp)))

    def bitcast(self, dt):
        return V(self.buf, self.ap.bitcast(dt))

    def partition_broadcast(self, n):
        return V(self.buf, self.ap.partition_broadcast(n))


class Buf:
    def __init__(self, ap, name, off=0, size=0):
        self.ap = ap
        self.name = name
        self.w = None
        self.rc = {}
        self.rd = {}
        self.inh = []
        self.multiw = False
        self.wl = {}
        self.war = []
        self.off = off
        self.size = size

    def __getitem__(self, k):
        return V(self, self.ap[k])

    def v(self):
        return V(self, self.ap)

    def rearrange(self, pattern_, **kw):
        return V(self, self.ap.rearrange(pattern_, **kw))


class Sched:
    def __init__(self, nc):
        self.nc = nc
        self.ops = {e: [] for e in ('pe', 'act', 'dve', 'pool', 'sp')}
        self.nops = 0
        self.dsem_use = {q: [0] * NDSEM for q in QUEUES}
        self.dsem_rr = {q: 0 for q in QUEUES}
        self.arena = nc.alloc_sbuf_tensor("arena", [128, ARENA], U8)
        self.live = []
        self.dead = []
        self.psum = []
        for i in range(8):
            h = nc.alloc_psum_tensor(f"psb{i}", [128, 512], F32)
            self.psum.append(Buf(h[:, :], f"psb{i}"))

    def alloc(self, name, shape, dtype):
        n = int(np.prod(shape[1:])) * DTSIZE[dtype]
        n_al = (n + 63) // 64 * 64
        self.live.sort(key=lambda b: b.off)
        off = 0
        for b in self.live:
            if b.off - off >= n_al:
                break
            off = max(off, b.off + b.size)
        if off + n_al > ARENA:
            raise RuntimeError(f"SBUF arena OOM allocating {name} {shape}: need {n_al} at {off}; live="
                               + str([(b.name, b.off, b.size) for b in self.live]))
        ap = self.arena[0:shape[0], off:off + n].bitcast(dtype)
        if len(shape) > 2:
            names = ' '.join(f'd{i}' for i in range(1, len(shape)))
            kw = {f'd{i}': shape[i] for i in range(1, len(shape))}
            ap = ap.rearrange(f'p ({names}) -> p {names}', **kw)
        buf = Buf(ap, name, off, n_al)
        keep = []
        for d in self.dead:
            if d.off < off + n_al and off < d.off + d.size:
                acc = list(d.inh)
                if d.w is not None:
                    acc.append(d.w)
                acc.extend(d.rc.values())
                acc.extend(d.rd.values())
                buf.inh.extend(acc)
                if not (off <= d.off and d.off + d.size <= off + n_al):
                    keep.append(d)
            else:
                keep.append(d)
        self.dead = keep
        best = {}
        for o in buf.inh:
            key = (o.q, o.semi) if o.isdma else o.eng
            if key not in best or best[key].idx < o.idx:
                best[key] = o
        buf.inh = list(best.values())
        self.live.append(buf)
        return buf

    def free(self, *bufs):
        for b in bufs:
            self.live.remove(b)
            self.dead.append(b)

    def dram(self, name, shape, dtype, kind="Internal"):
        h = self.nc.dram_tensor(name, list(shape), dtype, kind=kind)
        return Buf(h.ap(), name)

    def _mk(self, eng, fn, reads, writes, isdma, q=None):
        op = Op()
        op.eng = eng
        op.fn = fn
        op.isdma = isdma
        op.needs_inc = False
        op.count = 0
        op.idx = self.nops
        op.q = q
        op.semi = -1
        self.nops += 1
        if isdma:
            i = self.dsem_rr[q]
            self.dsem_rr[q] = (i + 1) % NDSEM
            self.dsem_use[q][i] += 1
            op.semi = i
            op.semval = 16 * self.dsem_use[q][i]
        deps = []
        for b in reads:
            if b.w is not None:
                deps.append((b.w, True))
            for o in b.wl.values():
                deps.append((o, True))
        for b in writes:
            mw = b.multiw and isdma
            if mw:
                if b.rc or b.rd:
                    b.war = list(b.rc.values()) + list(b.rd.values())
                    b.wl = {}
                    b.rc = {}
                    b.rd = {}
                for o in b.war:
                    deps.append((o, False))
                if b.w is not None:
                    deps.append((b.w, False))
            else:
                if b.w is not None:
                    deps.append((b.w, False))
                for o in b.wl.values():
                    deps.append((o, False))
                for o in b.rc.values():
                    deps.append((o, False))
                for o in b.rd.values():
                    deps.append((o, False))
            for o in b.inh:
                deps.append((o, False))
        fin = []
        seen = set()
        for p, raw in deps:
            if p is op or id(p) in seen:
                continue
            if (not p.isdma) and (not isdma) and p.eng == eng and eng == 'pe':
                continue
            seen.add(id(p))
            fin.append(p)
            p.needs_inc = True
        op.deps = fin
        for b in reads:
            if isdma:
                b.rd[(q, op.semi)] = op
            else:
                b.rc[eng] = op
        for b in writes:
            if b.multiw and isdma:
                b.wl[(q, op.semi)] = op
                b.inh = []
            else:
                b.w = op
                b.wl = {}
                b.war = []
                b.rc = {}
                b.rd = {}
                b.inh = []
        self.ops[eng].append(op)
        return op

    def op(self, eng, fn, reads=(), writes=()):
        return self._mk(eng, fn, reads, writes, False)

    def dma(self, q, out, in_, **kw):
        oa, ia = out.ap, in_.ap

        def fn(e):
            return e.dma_start(out=oa, in_=ia, **kw)
        return self._mk(q, fn, [in_.buf], [out.buf], True, q=q)

    def emit(self):
        nc = self.nc
        with ExitStack() as es:
            self.csems = {e: es.enter_context(nc.semaphore(f"c_{e}")) for e in COMPUTE}
            self.dsems = {q: [es.enter_context(nc.semaphore(f"d_{q}{i}")) for i in range(NDSEM)] for q in QUEUES}
            for e in COMPUTE:
                cnt = 0
                for op in self.ops[e]:
                    if not op.isdma and op.needs_inc:
                        cnt += 1
                    op.count = cnt
            block = es.enter_context(nc.Block())

            @block.tensor
            def _(eng):
                self._emit_engine('pe', eng)

            @block.scalar
            def _(eng):
                self._emit_engine('act', eng)

            @block.vector
            def _(eng):
                self._emit_engine('dve', eng)

            @block.gpsimd
            def _(eng):
                self._emit_engine('pool', eng)

            @block.sync
            def _(eng):
                self._emit_engine('sp', eng, final=True)

    def _emit_engine(self, e, eng, final=False):
        waited = {}

        def wait(key, sem, val):
            if waited.get(key, 0) >= val:
                return
            eng.wait_ge(sem, val)
            waited[key] = val

        for op in self.ops[e]:
            for p in op.deps:
                if p.isdma:
                    wait(('d', p.q, p.semi), self.dsems[p.q][p.semi], p.semval)
                else:
                    wait(('c', p.eng), self.csems[p.eng], p.count)
            if op.isdma:
                sem = self.dsems[op.q][op.semi]
                if op.semval > 16:
                    wait(('d', op.q, op.semi), sem, op.semval - 16)
                ins = op.fn(eng)
                ins.then_inc(sem, 16)
            else:
                ins = op.fn(eng)
                if op.needs_inc:
                    ins.then_inc(self.csems[e], 1)
        if final:
            for q in QUEUES:
                for i in range(NDSEM):
                    v = 16 * self.dsem_use[q][i]
                    if v > 0:
                        wait(('d', q, i), self.dsems[q][i], v)
            for ce in COMPUTE:
                last = None
                for op in self.ops[ce]:
                    if not op.isdma and op.needs_inc:
                        last = op
                if last is not None:
                    wait(('c', ce), self.csems[ce], last.count)


def _bufs(*xs):
    out = []
    for x in xs:
        if isinstance(x, V) and x.buf not in out:
            out.append(x.buf)
    return out


def _a(x):
    return x.ap if isinstance(x, V) else x


class K:
    def __init__(self, nc):
        self.nc = nc
        self.S = Sched(nc)
        self.defctx = {'banks': [0, 1, 2, 3], 'accs': [4, 5, 6, 7], 'i': 0, 'j': 0}
        self.cur = self.defctx

    def ps(self):
        c = self.cur
        b = self.S.psum[c['banks'][c['i'] % len(c['banks'])]]
        c['i'] += 1
        return b

    def psacc(self):
        c = self.cur
        b = self.S.psum[c['accs'][c['j'] % len(c['accs'])]]
        c['j'] += 1
        return b

    def interleave(self, gens):
        live = list(gens)
        while live:
            for item in list(live):
                g, ctx, wgt = item
                self.cur = ctx
                for _ in range(wgt):
                    try:
                        next(g)
                    except StopIteration:
                        live.remove(item)
                        break
        self.cur = self.defctx

    def mm(self, out, lhsT, rhs, start=True, stop=True):
        oa, la, ra = out.ap, lhsT.ap, rhs.ap
        self.S.op('pe', lambda e: e.matmul(oa, lhsT=la, rhs=ra, start=start, stop=stop),
                  reads=_bufs(lhsT, rhs), writes=_bufs(out))

    def tr(self, out, in_, ident):
        oa, ia, da = out.ap, in_.ap, ident.ap
        self.S.op('pe', lambda e: e.transpose(oa, ia, da), reads=_bufs(in_, ident), writes=_bufs(out))

    def act(self, out, in_, func, bias=None, scale=None, accum=None):
        kw = {}
        if bias is not None:
            kw['bias'] = _a(bias)
        if scale is not None:
            kw['scale'] = _a(scale)
        if accum is not None:
            kw['accum_out'] = accum.ap
        oa, ia = out.ap, in_.ap
        self.S.op('act', lambda e: e.activation(out=oa, in_=ia, func=func, **kw),
                  reads=_bufs(in_, bias, scale), writes=_bufs(out, accum))

    def tt(self, out, in0, in1, op, eng='dve'):
        oa, a0, a1 = out.ap, in0.ap, in1.ap
        self.S.op(eng, lambda e: e.tensor_tensor(out=oa, in0=a0, in1=a1, op=op), reads=_bufs(in0, in1), writes=_bufs(out))

    def ts(self, out, in0, s1, op0, s2=None, op1=None, eng='dve'):
        oa, a0 = out.ap, in0.ap
        x1, x2 = _a(s1), _a(s2)
        if op1 is None:
            self.S.op(eng, lambda e: e.tensor_scalar(out=oa, in0=a0, scalar1=x1, scalar2=None, op0=op0),
                      reads=_bufs(in0, s1), writes=_bufs(out))
        else:
            self.S.op(eng, lambda e: e.tensor_scalar(out=oa, in0=a0, scalar1=x1, scalar2=x2, op0=op0, op1=op1),
                      reads=_bufs(in0, s1, s2), writes=_bufs(out))

    def stt(self, out, in0, scalar, in1, op0, op1, eng='dve'):
        oa, a0, a1, sc = out.ap, in0.ap, in1.ap, _a(scalar)
        self.S.op(eng, lambda e: e.scalar_tensor_tensor(out=oa, in0=a0, scalar=sc, in1=a1, op0=op0, op1=op1),
                  reads=_bufs(in0, in1, scalar), writes=_bufs(out))

    def cp(self, out, in_, eng='dve'):
        oa, ia = out.ap, in_.ap
        if eng == 'act':
            self.S.op('act', lambda e: e.copy(out=oa, in_=ia), reads=_bufs(in_), writes=_bufs(out))
        else:
            self.S.op(eng, lambda e: e.tensor_copy(out=oa, in_=ia), reads=_bufs(in_), writes=_bufs(out))

    def memset(self, out, val, eng='dve'):
        oa = out.ap
        self.S.op(eng, lambda e: e.memset(oa, val), writes=_bufs(out))

    def red(self, out, in_, op, negate=False):
        oa, ia = out.ap, in_.ap
        self.S.op('dve', lambda e: e.tensor_reduce(out=oa, in_=ia, axis=AX.X, op=op, negate=negate),
                  reads=_bufs(in_), writes=_bufs(out))

    def recip(self, out, in_):
        oa, ia = out.ap, in_.ap
        self.S.op('dve', lambda e: e.reciprocal(out=oa, in_=ia), reads=_bufs(in_), writes=_bufs(out))

    def dma(self, q, out, in_, **kw):
        self.S.dma(q, out, in_, **kw)

    def dma_ind(self, out, in_, idx, scatter, bound):
        oa, ia, xa = out.ap, in_.ap, idx.ap

        def fn(e):
            off = bass.IndirectOffsetOnAxis(ap=xa, axis=0)
            if scatter:
                return e.indirect_dma_start(out=oa, out_offset=off, in_=ia, in_offset=None, bounds_check=None, oob_is_err=False)
            return e.indirect_dma_start(out=oa, out_offset=None, in_=ia, in_offset=off, bounds_check=None, oob_is_err=False)
        self.S._mk('pool', fn, [in_.buf, idx.buf], [out.buf], True, q='pool')

    def bcast(self, name, row_view, n):
        b = self.S.alloc(name, [128, n], F32)
        self.dma('sp', b[:, :], row_view.partition_broadcast(128))
        return b

    def rstd(self, ss, n, name):
        self.act(ss, ss, AF.Sqrt, bias=self.eps[0:ss.ap.shape[0], 0:1], scale=1.0 / n)
        self.recip(ss, ss)

    def build(self):
        S = self.S
        d = {}
        self.d = d
        import os
        self.flags = set(os.environ.get('KFLAGS', '').split(','))
        om = 'only_moe' in self.flags

        def inp(name, shape):
            d[name] = S.dram(name, shape, F32, kind="ExternalInput")
        inp('xin', [NT, DM])
        inp('cc', [128, 16])
        inp('ada_w', [2, DM, 6 * DM])
        inp('ada_b', [2, 6 * DM])
        inp('norm1_w', [2, DM])
        inp('norm2_w', [2, DM])
        inp('w_in', [2, DM, NIN])
        inp('w_out', [2, DM, DM])
        inp('convT', [2, 512, 3])
        inp('ig_b', [2, 8])
        inp('fg_b', [2, 8])
        inp('ml_nw', [2, 256])
        inp('na_bm', [2, 6, 14, 128, 256])
        inp('q_nw', [2, 512])
        inp('kv_nw', [2, 256])
        inp('w_uq', [2, 512, 576])
        inp('w_ukv', [2, 256, 768])
        inp('ffn_w1', [DM, DFF])
        inp('ffn_w3', [DM, DFF])
        inp('ffn_w2', [DFF, DM])
        inp('router', [DM, 8])
        inp('moe_w1', [8 * 11 * 128, 2048])
        inp('moe_w3', [8 * 11 * 128, 2048])
        inp('moe_w2', [8 * 11 * 128, 2048])
        inp('c_th', [128, 16])
        inp('c_j512', [128, NBLK])
        inp('c_gp', [128, 11])
        inp('fin_w', [DM])
        inp('ident', [128, 128])
        inp('tri0', [128, 128])
        inp('tri1', [128, 128])
        inp('rope_cs', [4096, 16])
        inp('rope_sn', [4096, 16])
        d['out'] = S.dram('out', [4096, DM], F32, kind="ExternalOutput")
        kind = "ExternalOutput" if DEBUG else "Internal"
        d['mod'] = S.dram('mod', [2, 2, 6 * DM], F32, kind=('ExternalInput' if om else kind))
        d['pT_ml'] = S.dram('pT_ml', [512, NT], F32, kind=kind)
        d['pT_na'] = S.dram('pT_na', [768, NT], BF16, kind=kind)
        d['p_tok'] = S.dram('p_tok', [NT, 1712], F32, kind=kind)
        d['cat'] = S.dram('cat', [DM, NT], BF16, kind=kind)
        d['x1'] = S.dram('x1', [NT, DM], F32, kind=('ExternalInput' if om else kind))
        d['x2'] = S.dram('x2', [NT, DM], F32, kind=kind)
        d['h2T'] = S.dram('h2T', [DM, NT], BF16, kind=kind)
        d['gates'] = S.dram('gatesd', [NT, 8], F32, kind=('ExternalInput' if om else kind))
        d['sel1'] = S.dram('sel1d', [NT, 8], F32, kind=('ExternalInput' if om else kind))
        d['h2'] = S.dram('h2d', [NT, DM], BF16, kind=('ExternalInput' if om else kind))
        d['hs'] = S.dram('hsd', [NSLOT, DM], BF16, kind=kind)
        d['yb'] = S.dram('ybd', [NSLOT, DM], F32, kind=kind)

        for nm in ('pT_ml', 'pT_na', 'p_tok', 'cat', 'x1', 'x2', 'h2T', 'h2', 'gates', 'sel1', 'out', 'yb'):
            d[nm].multiw = True
        self.identb = S.alloc('identb', [128, 128], BF16)
        self.dma('pool', self.identb[:, :], d['ident'][:, :])
        self.identf = S.alloc('identf', [128, 128], F32)
        self.dma('sp', self.identf[:, :], d['ident'][:, :])
        self.tri0 = S.alloc('tri0', [128, 128], F32)
        self.dma('sp', self.tri0[:, :], d['tri0'][:, :])
        self.tri1 = S.alloc('tri1', [128, 128], F32)
        self.dma('sp', self.tri1[:, :], d['tri1'][:, :])
        self.onesf = S.alloc('onesf', [128, 128], F32)
        self.memset(self.onesf[:, :], 1.0)
        self.eps = S.alloc('eps', [128, 1], F32)
        self.memset(self.eps[:, :], EPS)

        stop = getattr(self, 'stop_after', None)
        xs = d['xin']
        if 'only_moe' in self.flags:
            self.stage_moe(1)
            S.emit()
            return
        for l in range(2):
            last = l == 1
            if 'only_mla' not in self.flags:
                self.adaln(l)
                if stop == ('adaln', l):
                    break
                self.stage_in(l, xs)
                if stop == ('in', l):
                    break
                if 'inter' not in self.flags:
                    for _ in self.stage_mlstm(l, last):
                        pass
                    if stop == ('ml', l):
                        break
                    for _ in self.stage_na(l, last):
                        pass
                else:
                    cm = {'banks': [3], 'accs': [4, 5], 'i': 0, 'j': 0}
                    cn = {'banks': [0, 1, 2], 'accs': [6, 7], 'i': 0, 'j': 0, 'depth': 2}
                    self.interleave([(self.stage_mlstm(l, last), cm, 1), (self.stage_na(l, last), cn, 4)])
            if stop == ('na', l):
                break
            self.stage_mla(l, last)
            if stop in (('mla', l), ('mla1', l)):
                break
            self.stage_out(l, xs, last)
            if stop == ('out', l):
                break
            if last:
                self.stage_moe(l)
            else:
                self.stage_ffn(l, last)
            if stop == ('ffn', l):
                break
            xs = d['x2']
        S.emit()

    def adaln(self, l):
        S, d = self.S, self.d
        cc = S.alloc('cc', [128, 16], F32)
        self.dma('sp', cc[:, :], d['cc'][:, :])
        scT = S.alloc('scT', [128, 16], BF16)
        self.act(scT[:, :], cc[:, :], AF.Silu)
        sc3 = scT[:, :].rearrange('p (k r) -> p k r', r=2)
        adab = S.alloc('adab', [2, 6 * DM], F32)
        self.dma('sp', adab[:, :], d['ada_b'][l, :].partition_broadcast(2))
        modrow = S.alloc('modrow', [2, 6 * DM], F32)
        wts = [S.alloc(f'adaw{i}', [128, 8, 512], BF16) for i in range(2)]
        wsrc = d['ada_w'][l].rearrange('(k p) n -> p k n', p=128)
        for j in range(12):
            wt = wts[j % 2]
            self.dma('pool', wt[:, :, :], wsrc[:, :, j * 512:(j + 1) * 512])
            ps = self.ps()
            for k in range(8):
                self.mm(ps[0:2, 0:512], sc3[:, k, :], wt[:, k, :], start=(k == 0), stop=(k == 7))
            self.tt(modrow[:, j * 512:(j + 1) * 512], ps[0:2, 0:512], adab[:, j * 512:(j + 1) * 512], ALU.add)
        self.dma('pool', d['mod'][l], modrow[:, :])
        S.free(cc, scT, adab, modrow, *wts)

    def mod_ab(self, l, which, nw_name, tag):
        S, d = self.S, self.d
        nwb = self.bcast(f'nwb{tag}', d[nw_name][l, :], DM)
        res = []
        for r in range(2):
            sc = self.bcast(f'sc{tag}{r}', d['mod'][l, r, (3 * which + 1) * DM:(3 * which + 2) * DM], DM)
            self.stt(sc[:, :], sc[:, :], 1.0, nwb[:, :], ALU.add, ALU.mult)
            sh = self.bcast(f'sh{tag}{r}', d['mod'][l, r, (3 * which) * DM:(3 * which + 1) * DM], DM)
            res.append((sc, sh))
        S.free(nwb)
        return res

    def norm_mod(self, xt, A, B, hb, tmp, ss):
        self.act(tmp[:, :], xt[:, :], AF.Square, accum=ss[:, 0:1])
        self.rstd(ss[:, 0:1], DM, 'r')
        self.stt(tmp[:, :], xt[:, :], ss[:, 0:1], A[:, :], ALU.mult, ALU.mult)
        self.tt(hb[:, :], tmp[:, :], B[:, :], ALU.add)

    def transpose8(self, hb, dst3):
        ps = self.ps()
        psb = ps[:, :].bitcast(BF16)
        for k in range(8):
            self.tr(psb[:, k * 128:(k + 1) * 128], hb[:, k * 128:(k + 1) * 128], self.identb[:, :])
        self.cp(dst3, psb[:, :].rearrange('p (k t) -> p k t', k=8), eng='act')

    def stage_in(self, l, xs):
        S, d = self.S, self.d
        w = S.alloc('w_in', [128, 8, NIN], BF16)
        wsrc = d['w_in'][l].rearrange('(k p) n -> p k n', p=128)
        for k in range(8):
            self.dma('pool', w[:, k, :], wsrc[:, k, :])
        ab = self.mod_ab(l, 0, 'norm1_w', 'a')
        hTs = [S.alloc(f'hT{i}', [128, 8, 512], BF16) for i in range(2)]
        xts = [S.alloc(f'xt{i}', [128, DM], F32) for i in range(2)]
        tmps = [S.alloc(f'tmp{i}', [128, DM], F32) for i in range(2)]
        hbs = [S.alloc(f'hb{i}', [128, DM], BF16) for i in range(2)]
        sss = [S.alloc(f'ss{i}', [128, 1], F32) for i in range(2)]
        fms = [S.alloc(f'fm{i}', [128, 512], F32) for i in range(2)]
        fnb = [S.alloc(f'fnb{i}', [128, 512], BF16) for i in range(2)]
        pts = [S.alloc(f'pt{i}', [128, 1712], F32) for i in range(2)]
        groups = [(0, 2)] + [(2 + 4 * i, 4) for i in range(8)]
        fcols = [0, 128, 256, 384] + [1040 + 128 * i for i in range(6)]
        tcols = [(512, 1024, 0), (1024, 1040, 512), (1808, 2320, 528), (2320, 2832, 1040), (2832, 2992, 1552)]
        ti = 0
        for gi, (t0, nt) in enumerate(groups):
            hT = hTs[gi % 2]
            G = nt * 128
            A, B = ab[1] if gi == 0 else ab[0]
            for j in range(nt):
                n = t0 + j
                xt = xts[ti % 2]
                hb = hbs[ti % 2]
                ss = sss[ti % 2]
                tmp = tmps[ti % 2]
                ti += 1
                self.dma('sp', xt[:, :], xs[n * 128:(n + 1) * 128, :])
                self.norm_mod(xt, A, B, hb, tmp, ss)
                self.transpose8(hb, hT[:, :, j * 128:(j + 1) * 128])
            for ci, c0 in enumerate(fcols):
                ps = self.ps()
                for k in range(8):
                    self.mm(ps[:, 0:G], w[:, k, c0:c0 + 128], hT[:, k, 0:G], start=(k == 0), stop=(k == 7))
                if ci < 4:
                    fm = fms[ci % 2]
                    self.cp(fm[:, 0:G], ps[:, 0:G], eng='act')
                    self.dma('pool', d['pT_ml'][ci * 128:(ci + 1) * 128, t0 * 128:t0 * 128 + G], fm[:, 0:G])
                else:
                    fb = fnb[ci % 2]
                    if ci < 7:
                        self.ts(fb[:, 0:G], ps[:, 0:G], 0.125, ALU.mult)
                    else:
                        self.cp(fb[:, 0:G], ps[:, 0:G], eng='act')
                    self.dma('pool', d['pT_na'][(ci - 4) * 128:(ci - 3) * 128, t0 * 128:t0 * 128 + G], fb[:, 0:G])
            for j in range(nt):
                n = t0 + j
                pt = pts[n % 2]
                for ii, (c0, c1, o0) in enumerate(tcols):
                    ps = self.ps()
                    for k in range(8):
                        self.mm(ps[:, 0:c1 - c0], hT[:, k, j * 128:(j + 1) * 128], w[:, k, c0:c1], start=(k == 0), stop=(k == 7))
                    if ii % 2 == 0:
                        self.cp(pt[:, o0:o0 + c1 - c0], ps[:, 0:c1 - c0], eng='dve')
                    else:
                        self.cp(pt[:, o0:o0 + c1 - c0], ps[:, 0:c1 - c0], eng='act')
                self.dma('pool', d['p_tok'][n * 128:(n + 1) * 128, :], pt[:, :])
        S.free(w, *tmps, *hTs, *xts, *hbs, *sss, *fms, *fnb, *pts)
        for a, b in ab:
            S.free(a, b)

    def stage_mlstm(self, l, last):
        S, d = self.S, self.d

        def tcol(n):
            return 1 + n * 128 if n < 2 else 2 + n * 128
        gt = S.alloc('gt', [128, NTILE, 16], F32)
        self.dma('sp', gt[:, :, :], d['p_tok'][:, 512:528].rearrange('(n p) c -> p n c', p=128))
        igb = self.bcast('igb', d['ig_b'][l, :], 8)
        fgb = self.bcast('fgb', d['fg_b'][l, :], 8)
        LI = S.alloc('LI', [128, NTILE, 8], F32)
        LF = S.alloc('LF', [128, NTILE, 8], F32)
        self.tt(LI[:, :, :], gt[:, :, 0:8], igb[:, :].unsqueeze(1).to_broadcast([128, NTILE, 8]), ALU.add)
        self.tt(LF[:, :, :], gt[:, :, 8:16], fgb[:, :].unsqueeze(1).to_broadcast([128, NTILE, 8]), ALU.add)
        self.act(LF[:, :, :], LF[:, :, :], AF.Exp, scale=-1.0)
        self.act(LF[:, :, :], LF[:, :, :], AF.Ln, bias=self.onesf[:, 0:1], scale=1.0)
        self.ts(LF[:, :, :], LF[:, :, :], -1.0, ALU.mult)
        LF2 = LF[:, :, :].rearrange('p n c -> p (n c)')
        NC = NTILE * 8
        Bc = S.alloc('Bc', [128, NTILE, 2, 4], F32)
        EB = S.alloc('EB', [128, NTILE, 2, 4], F32)
        ES = S.alloc('ES', [128, NTILE, 2, 4], F32)
        EG = S.alloc('EG', [128, NTILE, 2, 4], F32)
        psA = self.ps()
        self.mm(psA[:, 0:NC], self.tri0[:, :], LF2)
        self.cp(Bc[:, :, 0, :], psA[:, 0:NC].rearrange('p (n d h) -> p n d h', d=2, h=4)[:, :, 0, :])
        psB = self.ps()
        self.mm(psB[:, 0:NC], self.tri1[:, :], LF2)
        self.cp(Bc[:, :, 1, :], psB[:, 0:NC].rearrange('p (n d h) -> p n d h', d=2, h=4)[:, :, 1, :])
        psC = self.ps()
        self.mm(psC[:, 0:NC], self.onesf[:, :], LF2)
        self.act(EG[:, :, :, :].rearrange('p n d h -> p (n d h)'), psC[:, 0:NC], AF.Exp)
        Bc2 = Bc[:, :, :, :].rearrange('p n d h -> p (n d h)')
        self.act(EB[:, :, :, :].rearrange('p n d h -> p (n d h)'), Bc2, AF.Exp)
        ES2 = ES[:, :, :, :].rearrange('p n d h -> p (n d h)')
        self.tt(ES2, LI[:, :, :].rearrange('p n c -> p (n c)'), Bc2, ALU.subtract)
        self.act(ES2, ES2, AF.Exp)
        yield
        S.free(gt, igb, fgb, LI, LF, Bc)

        convw = S.alloc('convw', [128, 4, 3], F32)
        self.dma('sp', convw[:, :, :], d['convT'][l].rearrange('(c p) j -> p c j', p=128))
        W = NT + 3
        qkT = S.alloc('qkT', [128, 4, W], BF16)
        x32 = S.alloc('x32', [128, W], F32)
        acc = S.alloc('acc', [128, W], F32)
        for c in (0, 257, W - 1):
            self.memset(x32[:, c:c + 1], 0.0)
        for fc in range(4):
            self.dma('sp', x32[:, 1:257], d['pT_ml'][fc * 128:(fc + 1) * 128, 0:256])
            self.dma('sp', x32[:, 258:W - 1], d['pT_ml'][fc * 128:(fc + 1) * 128, 256:NT])
            a = acc[:, 1:W - 1]
            self.ts(a, x32[:, 0:W - 2], convw[:, fc, 0:1], ALU.mult)
            self.stt(a, x32[:, 1:W - 1], convw[:, fc, 1:2], a, ALU.mult, ALU.add)
            self.stt(a, x32[:, 2:W], convw[:, fc, 2:3], a, ALU.mult, ALU.add)
            if fc < 2:
                self.act(qkT[:, fc, 1:W - 1], a, AF.Silu)
            else:
                self.act(a, a, AF.Silu)
                self.ts(qkT[:, fc, 1:W - 1], a, 0.125, ALU.mult)
            yield
        S.free(x32, acc, convw)

        ktok = S.alloc('ktok', [128, NTILE, 256], BF16)
        for n in range(NTILE):
            ps = self.ps()
            psb = ps[:, :].bitcast(BF16)
            for kc in range(2):
                self.tr(psb[:, kc * 128:(kc + 1) * 128], qkT[:, 2 + kc, tcol(n):tcol(n) + 128], self.identb[:, :])
            self.cp(ktok[:, n, :], psb[:, 0:256], eng=('act' if n % 2 else 'dve'))
            if n % 4 == 3:
                yield

        Vp = S.alloc('Vp', [128, NTILE, 2, 4, 65], BF16)
        Xb = S.alloc('Xb', [128, NTILE, 2, 4, 65], BF16)
        v32 = S.alloc('v32', [128, 9, 256], F32)
        for t0 in range(0, NTILE, 9):
            tn = min(9, NTILE - t0)
            self.dma('sp', v32[:, 0:tn, :], d['p_tok'][t0 * 128:(t0 + tn) * 128, 0:256].rearrange('(n p) c -> p n c', p=128))
            for dd in range(2):
                self.tt(Vp[:, t0:t0 + tn, dd, :, 0:64], v32[:, 0:tn, :].rearrange('p n (h e) -> p n h e', h=4),
                        ES[:, t0:t0 + tn, dd, :].unsqueeze(3).to_broadcast([128, tn, 4, 64]), ALU.mult)
            yield
        for dd in range(2):
            self.cp(Vp[:, :, dd, :, 64:65], ES[:, :, dd, :].unsqueeze(3))
        S.free(v32, ES)

        Xf = S.alloc('Xf', [128, 2, 4, 65], F32)
        self.memset(Xf[:, :, :, :].rearrange('p d h e -> p (d h e)'), 0.0)
        order = [list(range(NTILE)), [1, 0] + list(range(NTILE - 1, 1, -1))]
        for step in range(NTILE):
            for dd in range(2):
                n = order[dd][step]
                self.cp(Xb[:, n, dd, :, :], Xf[:, dd, :, :], eng='act')
                ps = self.ps()
                psv = ps[:, 0:260].rearrange('p (h e) -> p h e', h=4)
                for h in range(4):
                    self.mm(psv[:, h, :], ktok[:, n, (h // 2) * 128:(h // 2) * 128 + 128], Vp[:, n, dd, h, :])
                self.tt(Xf[:, dd, :, :], Xf[:, dd, :, :], psv, ALU.add)
                self.tt(Xf[:, dd, :, :], Xf[:, dd, :, :], EG[:, n, dd, :].unsqueeze(2).to_broadcast([128, 4, 65]), ALU.mult)
            yield
        S.free(Xf, EG, ktok)

        mlnw = self.bcast('mlnw', d['ml_nw'][l, :], 256)
        Sms = [S.alloc(f'Sm{i}', [128, 128], BF16) for i in range(4)]
        Hd = [S.alloc(f'Hd{i}', [128, 4, 65], F32) for i in range(2)]
        den = S.alloc('den', [128, 2, 4], F32)
        hs = S.alloc('hs', [128, 4, 64], F32)
        ht = S.alloc('ht', [128, 4, 64], F32)
        ssq = S.alloc('ssq', [128, 4], F32)
        o32 = [S.alloc(f'o32{i}', [128, 256], F32) for i in range(2)]
        res = [S.alloc(f'mres{i}', [128, 256], BF16) for i in range(2)]
        resT = [S.alloc(f'mresT{i}', [128, 2, 128], BF16) for i in range(2)]
        smi = 0
        for n in range(2 if last else 0, NTILE):
            psO = [self.psacc(), self.psacc()]
            psOv = [p[:, 0:260].rearrange('p (h e) -> p h e', h=4) for p in psO]
            c0 = tcol(n)
            for h in range(4):
                pb = (h % 2) * 64
                qT = qkT[pb:pb + 64, h // 2, c0:c0 + 128]
                kT = qkT[pb:pb + 64, 2 + h // 2, c0:c0 + 128]
                psS = self.ps()
                self.mm(psS[:, 0:128], kT, qT)
                for dd in range(2):
                    Sm = Sms[smi % 4]
                    smi += 1
                    self.tt(Sm[:, :], psS[:, 0:128], (self.tri0 if dd == 0 else self.tri1)[:, :], ALU.mult)
                    self.mm(psOv[dd][:, h, :], Sm[:, :], Vp[:, n, dd, h, :], start=True, stop=False)
                    self.mm(psOv[dd][:, h, :], qT, Xb[pb:pb + 64, n, dd, h, :], start=False, stop=True)
                yield
            for dd in range(2):
                self.tt(Hd[dd][:, :, :], psOv[dd], EB[:, n, dd, :].unsqueeze(2).to_broadcast([128, 4, 65]), ALU.mult)
                self.stt(den[:, dd, :], Hd[dd][:, :, 64], -1.0, Hd[dd][:, :, 64], ALU.mult, ALU.max)
            self.ts(den[:, :, :], den[:, :, :], 1.0, ALU.max)
            self.recip(den[:, :, :], den[:, :, :])
            self.tt(hs[:, :, :], Hd[0][:, :, 0:64], den[:, 0, :].unsqueeze(2).to_broadcast([128, 4, 64]), ALU.mult)
            self.tt(ht[:, :, :], Hd[1][:, :, 0:64], den[:, 1, :].unsqueeze(2).to_broadcast([128, 4, 64]), ALU.mult)
            self.tt(hs[:, :, :], hs[:, :, :], ht[:, :, :], ALU.add)
            self.tt(ht[:, :, :], hs[:, :, :], hs[:, :, :], ALU.mult)
            self.red(ssq[:, :], ht[:, :, :], ALU.add)
            self.act(ssq[:, :], ssq[:, :], AF.Sqrt, bias=self.eps[:, 0:1], scale=1.0 / 64)
            self.recip(ssq[:, :], ssq[:, :])
            self.tt(hs[:, :, :], hs[:, :, :], ssq[:, :].unsqueeze(2).to_broadcast([128, 4, 64]), ALU.mult)
            hs2 = hs[:, :, :].rearrange('p h e -> p (h e)')
            self.tt(hs2, hs2, mlnw[:, :], ALU.mult)
            o = o32[n % 2]
            self.dma('sp', o[:, :], d['p_tok'][n * 128:(n + 1) * 128, 256:512])
            self.act(o[:, :], o[:, :], AF.Sigmoid)
            r = res[n % 2]
            self.tt(r[:, :], hs2, o[:, :], ALU.mult)
            pst = self.ps()
            pstb = pst[:, :].bitcast(BF16)
            for c in range(2):
                self.tr(pstb[:, c * 128:(c + 1) * 128], r[:, c * 128:(c + 1) * 128], self.identb[:, :])
            rT = resT[n % 2]
            self.cp(rT[:, :, :], pstb[:, 0:256].rearrange('p (c t) -> p c t', c=2), eng='act')
            self.dma('pool', d['cat'][0:256, n * 128:(n + 1) * 128].rearrange('(c p) t -> p c t', p=128), rT[:, :, :])
            yield
        S.free(mlnw, den, hs, ht, ssq, qkT, Vp, Xb, EB, *Sms, *Hd, *o32, *res, *resT)

    def attn_block(self, kT_of, qTv, nq, keys, Va_of, out_dram, bm_of=None):
        psO = self.psacc()
        nk = len(keys)

        def issue_s(i):
            kt, cfg = keys[i]
            psS = self.ps()
            self.mm(psS[:, 0:nq], kT_of(kt), qTv, start=True, stop=True)
            return psS
        D = self.cur.get('depth', 3)
        pend = [issue_s(i) for i in range(min(D, nk))]
        for i in range(nk):
            psS = pend.pop(0)
            if i + D < nk:
                pend.append(issue_s(i + D))
            P = self.Ps[self.pi % len(self.Ps)]
            self.pi += 1
            self.act(P[:, 0:nq], psS[:, 0:nq], AF.Exp)
            if keys[i][1] is not None:
                self.tt(P[:, 0:nq], P[:, 0:nq], bm_of(keys[i][1]), ALU.mult)
            self.mm(psO[:, 0:nq], Va_of(keys[i][0]), P[:, 0:nq], start=(i == 0), stop=(i == nk - 1))
            yield
        r = self.pi % 2
        rsb, rinv, ob = self.rsbs[r], self.rinvs[r], self.obs[r]
        self.act(rsb[64:65, 0:nq], psO[64:65, 0:nq], AF.Ln)
        self.act(rsb[64:65, 0:nq], rsb[64:65, 0:nq], AF.Exp, scale=-1.0)
        psR = self.ps()
        self.mm(psR[0:64, 0:nq], self.onesf[64:65, 0:64], rsb[64:65, 0:nq])
        self.cp(rinv[:, 0:nq], psR[0:64, 0:nq], eng='act')
        self.tt(ob[:, 0:nq], psO[0:64, 0:nq], rinv[:, 0:nq], ALU.mult)
        self.dma('pool', out_dram, ob[:, 0:nq])
        yield

    def attn_bufs(self):
        S = self.S
        self.Ps = [S.alloc(f'P{i}', [128, 512], BF16) for i in range(4)]
        self.rsbs = [S.alloc(f'rsb{i}', [128, 512], F32) for i in range(2)]
        self.rinvs = [S.alloc(f'rinv{i}', [64, 512], F32) for i in range(2)]
        self.obs = [S.alloc(f'ob{i}', [64, 512], BF16) for i in range(2)]
        self.pi = 0
        return [*self.Ps, *self.rsbs, *self.rinvs, *self.obs]

    def stage_na(self, l, last):
        S, d = self.S, self.d
        ab = self.attn_bufs()
        qT = S.alloc('naq', [64, NT], BF16)
        kT = S.alloc('nak', [64, NT], BF16)
        Va = S.alloc('nav', [128, NTILE, 128], BF16)
        self.memset(Va[:, :, :].rearrange('p n e -> p (n e)'), 0.0)
        bm = S.alloc('nabm', [128, 14, 256], BF16)
        ebm = S.alloc('naebm', [128, 14, 256], BF16)
        vst = S.alloc('vst', [128, NTILE, 64], F32)
        for h in range(6):
            self.dma('sp', qT[:, :], d['pT_na'][h * 64:(h + 1) * 64, :])
            self.dma('sp', kT[:, :], d['pT_na'][384 + h * 64:384 + (h + 1) * 64, :])
            self.memset(Va[:, :, 64:65], 1.0)
            self.dma('sp', vst[:, :, :], d['p_tok'][:, 528 + h * 64:528 + (h + 1) * 64].rearrange('(n p) c -> p n c', p=128))
            self.cp(Va[:, :, 0:64], vst[:, :, :], eng='act')
            for cfg in range(14):
                self.dma('pool', bm[:, cfg, :], d['na_bm'][l, h, cfg])
            self.act(ebm[:, :, :].rearrange('p c q -> p (c q)'), bm[:, :, :].rearrange('p c q -> p (c q)'), AF.Exp)
            blocks = []
            if not last:
                blocks.append((0, [(0, None), (1, None)]))
            for qb in range(16):
                if qb == 0:
                    lat = [(2 + j, 6 + j) for j in range(4)]
                elif qb == 15:
                    lat = [(2 + 28 + j, 10 + j) for j in range(4)]
                else:
                    lat = [(2 + 2 * qb - 2 + i, i) for i in range(6)]
                blocks.append((256 + qb * 256, [(0, None), (1, None)] + lat))
            for q0, keys in blocks:
                yield from self.attn_block(lambda kt: kT[:, kt * 128:(kt + 1) * 128], qT[:, q0:q0 + 256], 256, keys,
                                           lambda kt: Va[:, kt, :], d['cat'][256 + h * 64:256 + (h + 1) * 64, q0:q0 + 256],
                                           bm_of=lambda cfg: ebm[:, cfg, :])
        S.free(vst, qT, kT, Va, bm, ebm, *ab)

    def rope(self, xv, nh, cs, sn, tmp):
        x5 = xv.rearrange('p h (a b f) -> p h a b f', a=2, b=2)
        x1 = x5[:, :, :, 0, :]
        x2 = x5[:, :, :, 1, :]
        csb = cs[:, :].rearrange('p (a f) -> p a f', a=2).unsqueeze(1).to_broadcast([128, nh, 2, 8])
        snb = sn[:, :].rearrange('p (a f) -> p a f', a=2).unsqueeze(1).to_broadcast([128, nh, 2, 8])
        t = [tmp[:, i, 0:nh * 16].rearrange('p (h a f) -> p h a f', h=nh, a=2) for i in range(4)]
        self.tt(t[0], x1, csb, ALU.mult)
        self.tt(t[1], x2, snb, ALU.mult)
        self.tt(t[2], x2, csb, ALU.mult)
        self.tt(t[3], x1, snb, ALU.mult)
        self.tt(x1, t[0], t[1], ALU.subtract)
        self.tt(x2, t[2], t[3], ALU.add)

    def stage_mla(self, l, last):
        S, d = self.S, self.d
        wuq = S.alloc('wuq', [128, 4, 576], BF16)
        self.dma('pool', wuq[:, :, :], d['w_uq'][l].rearrange('(k p) n -> p k n', p=128))
        wukv = S.alloc('wukv', [128, 2, 768], BF16)
        self.dma('pool', wukv[:, :, :], d['w_ukv'][l].rearrange('(k p) n -> p k n', p=128))
        qnw = self.bcast('qnw', d['q_nw'][l, :], 512)
        kvnw = self.bcast('kvnw', d['kv_nw'][l, :], 256)
        qT = S.alloc('mqT', [96, 6, NT], BF16)
        kT = S.alloc('mkT', [96, 6, NT], BF16)
        Va = S.alloc('mVa', [128, NTILE, 6, 128], BF16)
        self.memset(Va[:, :, :, :].rearrange('p n h e -> p (n h e)'), 0.0)
        self.memset(Va[:, :, :, 64:65], 1.0)
        pms = [S.alloc(f'pm{i}', [128, 800], F32) for i in range(2)]
        rings = []
        for nm, shp, dt in (('junk', [128, 512], F32), ('ssa', [128, 2], F32), ('cn', [128, 768], BF16), ('cT', [128, 6, 128], BF16),
                            ('qf32', [128, 576], F32), ('qb16', [128, 6, 128], BF16), ('kfull', [128, 6, 128], BF16),
                            ('krr', [128, 32], F32), ('rtmp', [128, 4, 96], F32)):
            rings.append([S.alloc(f'{nm}{i}', shp, dt) for i in range(2)])
        for i in range(2):
            self.memset(rings[5][i][:, :, :].rearrange('p h e -> p (h e)'), 0.0)
            self.memset(rings[6][i][:, :, :].rearrange('p h e -> p (h e)'), 0.0)
        css = [S.alloc(f'cs{i}', [128, 16], F32) for i in range(2)]
        sns = [S.alloc(f'sn{i}', [128, 16], F32) for i in range(2)]
        scale = 96 ** -0.5
        for n in range(3 if 'mla_short' in self.flags else NTILE):
            pm = pms[n % 2]
            junk, ssa, cn, cT, qf32, qb16, kfull, krr, rtmp = [rg[n % 2] for rg in rings]
            self.dma('sp', pm[:, :], d['p_tok'][n * 128:(n + 1) * 128, 912:1712])
            cs, sn = css[n % 2], sns[n % 2]
            if n >= 2:
                self.dma('sp', cs[:, :], d['rope_cs'][(n - 2) * 128:(n - 1) * 128, :])
                self.dma('sp', sn[:, :], d['rope_sn'][(n - 2) * 128:(n - 1) * 128, :])
            self.act(junk[:, 0:512], pm[:, 0:512], AF.Square, accum=ssa[:, 0:1])
            self.act(junk[:, 0:256], pm[:, 512:768], AF.Square, accum=ssa[:, 1:2])
            self.rstd(ssa[:, 0:1], 512, 'q')
            self.rstd(ssa[:, 1:2], 256, 'kv')
            self.stt(cn[:, 0:512], pm[:, 0:512], ssa[:, 0:1], qnw[:, :], ALU.mult, ALU.mult)
            self.stt(cn[:, 512:768], pm[:, 512:768], ssa[:, 1:2], kvnw[:, :], ALU.mult, ALU.mult)
            ps = self.ps()
            psb = ps[:, :].bitcast(BF16)
            for k in range(6):
                self.tr(psb[:, k * 128:(k + 1) * 128], cn[:, k * 128:(k + 1) * 128], self.identb[:, :])
            self.cp(cT[:, :, :], psb[:, 0:768].rearrange('p (k t) -> p k t', k=6), eng='act')
            p1, p2 = self.ps(), self.ps()
            for k in range(4):
                self.mm(p1[:, 0:512], cT[:, k, :], wuq[:, k, 0:512], start=(k == 0), stop=(k == 3))
            for k in range(4):
                self.mm(p2[:, 0:64], cT[:, k, :], wuq[:, k, 512:576], start=(k == 0), stop=(k == 3))
            self.ts(qf32[:, 0:512], p1[:, 0:512], scale, ALU.mult)
            self.ts(qf32[:, 512:576], p2[:, 0:64], scale, ALU.mult)
            if n >= 2 and 'norope' not in self.flags:
                self.rope(qf32[:, :].rearrange('p (h e) -> p h e', h=6)[:, :, 64:96], 6, cs, sn, rtmp)
            self.cp(qb16[:, :, 0:96], qf32[:, :].rearrange('p (h e) -> p h e', h=6), eng='act')
            ps = self.ps()
            psb = ps[:, :].bitcast(BF16)
            if 'no_qtr' not in self.flags:
                for h in range(6):
                    self.tr(psb[:, h * 128:(h + 1) * 128], qb16[:, h, :], self.identb[:, :])
                for (pa_, pb2) in ((0, 64), (64, 96)):
                    self.cp(qT[pa_:pb2, :, n * 128:(n + 1) * 128], psb[pa_:pb2, 0:768].rearrange('p (h t) -> p h t', h=6), eng='act')
            k1, k2 = self.ps(), self.ps()
            for k in range(2):
                self.mm(k1[:, 0:512], cT[:, 4 + k, :], wukv[:, k, 0:512], start=(k == 0), stop=(k == 1))
            for k in range(2):
                self.mm(k2[:, 0:256], cT[:, 4 + k, :], wukv[:, k, 512:768], start=(k == 0), stop=(k == 1))
            k1v = k1[:, 0:512].rearrange('p (h e) -> p h e', h=4)
            k2v = k2[:, 0:256].rearrange('p (h e) -> p h e', h=2)
            self.cp(kfull[:, 0:4, 0:64], k1v[:, :, 0:64])
            self.cp(kfull[:, 4:6, 0:64], k2v[:, :, 0:64])
            self.cp(Va[:, n, 0:4, 0:64], k1v[:, :, 64:128], eng='act')
            self.cp(Va[:, n, 4:6, 0:64], k2v[:, :, 64:128], eng='act')
            self.cp(krr[:, :], pm[:, 768:800])
            if n >= 2 and 'norope' not in self.flags:
                self.rope(krr[:, :].unsqueeze(1), 1, cs, sn, rtmp)
            self.cp(kfull[:, :, 64:96], krr[:, :].unsqueeze(1).to_broadcast([128, 6, 32]))
            ps = self.ps()
            psb = ps[:, :].bitcast(BF16)
            if 'no_ktr' not in self.flags:
                for h in range(6):
                    self.tr(psb[:, h * 128:(h + 1) * 128], kfull[:, h, :], self.identb[:, :])
                for (pa_, pb2) in ((0, 64), (64, 96)):
                    self.cp(kT[pa_:pb2, :, n * 128:(n + 1) * 128], psb[pa_:pb2, 0:768].rearrange('p (h t) -> p h t', h=6), eng='dve')
        S.free(wuq, wukv, qnw, kvnw, *pms, *css, *sns, *[b for rg in rings for b in rg])

        if getattr(self, 'stop_after', None) == ('mla1', l):
            S.free(qT, kT, Va)
            return
        ab = self.attn_bufs()
        allk = [(kt, None) for kt in range(NTILE)]
        for h in range(6):
            if not last:
                for _ in self.attn_block(lambda kt: kT[:, h, kt * 128:(kt + 1) * 128], qT[:, h, 0:256], 256, [(0, None), (1, None)],
                                         lambda kt: Va[:, kt, h, :], d['cat'][640 + h * 64:640 + (h + 1) * 64, 0:256]):
                    pass
            for b in range(8):
                q0 = 256 + b * 512
                for _ in self.attn_block(lambda kt: kT[:, h, kt * 128:(kt + 1) * 128], qT[:, h, q0:q0 + 512], 512, allk,
                                         lambda kt: Va[:, kt, h, :], d['cat'][640 + h * 64:640 + (h + 1) * 64, q0:q0 + 512]):
                    pass
        S.free(qT, kT, Va, *ab)

    def stage_out(self, l, xs, last):
        S, d = self.S, self.d
        w = S.alloc('w_out', [128, 8, DM], BF16)
        wsrc = d['w_out'][l].rearrange('(k p) n -> p k n', p=128)
        for k in range(8):
            self.dma('pool', w[:, k, :], wsrc[:, k, :])
        ab = self.mod_ab(l, 1, 'norm2_w', 'b')
        g1 = [self.bcast(f'g1{r}', d['mod'][l, r, 2 * DM:3 * DM], DM) for r in range(2)]
        if last:
            rw = S.alloc('rw', [128, 8, 8], F32)
            self.dma('sp', rw[:, :, :], d['router'].rearrange('(k p) e -> p k e', p=128))
            hf = S.alloc('hf', [128, DM], F32)
            hT32 = S.alloc('hT32', [128, 8, 128], F32)
            lg = S.alloc('lg', [128, 8], F32)
            l2 = S.alloc('l2', [128, 8], F32)
            mx = S.alloc('mx', [128, 4], F32)
            gsb = S.alloc('gsb', [128, 8], F32)
            m1m = S.alloc('m1m', [128, 8], F32)
        catTs = [S.alloc(f'catT{i}', [128, 8, 128], BF16) for i in range(2)]
        xts = [S.alloc(f'xo{i}', [128, DM], F32) for i in range(2)]
        xn = [S.alloc(f'xn{i}', [128, DM], F32) for i in range(2)]
        tmps = [S.alloc(f'tmpo{i}', [128, DM], F32) for i in range(2)]
        hbos = [S.alloc(f'hbo{i}', [128, DM], BF16) for i in range(2)]
        hT = [S.alloc(f'hTo{i}', [128, 8, 128], BF16) for i in range(2)]
        sso = [S.alloc(f'sso{i}', [128, 1], F32) for i in range(2)]
        for n in range(NTILE):
            if last and n < 2:
                continue
            r = 1 if n < 2 else 0
            catT, tmp, hb, ss = catTs[n % 2], tmps[n % 2], hbos[n % 2], sso[n % 2]
            self.dma('sp', catT[:, :, :], d['cat'].rearrange('(k p) t -> p k t', p=128)[:, :, n * 128:(n + 1) * 128])
            xt = xts[n % 2]
            self.dma('sp', xt[:, :], xs[n * 128:(n + 1) * 128, :])
            x1 = xn[n % 2]
            for half in range(2):
                ps = self.ps()
                for k in range(8):
                    self.mm(ps[:, 0:512], catT[:, k, :], w[:, k, half * 512:(half + 1) * 512], start=(k == 0), stop=(k == 7))
                sl = slice(half * 512, (half + 1) * 512)
                self.tt(x1[:, sl], ps[:, 0:512], g1[r][:, sl], ALU.mult)
                self.tt(x1[:, sl], x1[:, sl], xt[:, sl], ALU.add)
            self.dma('pool', d['x1'][n * 128:(n + 1) * 128, :], x1[:, :])
            A, B = ab[r]
            if last:
                self.norm_mod(x1, A, B, hf, tmp, ss)
                self.cp(hb[:, :], hf[:, :], eng='act')
                self.dma('pool', d['h2'][n * 128:(n + 1) * 128, :], hb[:, :])
                pa, pb_ = self.ps(), self.ps()
                for k in range(8):
                    pp = pa if k < 4 else pb_
                    self.tr(pp[:, (k % 4) * 128:(k % 4 + 1) * 128], hf[:, k * 128:(k + 1) * 128], self.identf[:, :])
                self.cp(hT32[:, 0:4, :], pa[:, :].rearrange('p (k t) -> p k t', k=4))
                self.cp(hT32[:, 4:8, :], pb_[:, :].rearrange('p (k t) -> p k t', k=4), eng='act')
                pl = self.ps()
                for k in range(8):
                    self.mm(pl[:, 0:8], hT32[:, k, :], rw[:, k, :], start=(k == 0), stop=(k == 7))
                self.cp(lg[:, :], pl[:, 0:8])
                self.red(mx[:, 0:1], lg[:, :], ALU.max)
                self.ts(m1m[:, :], lg[:, :], mx[:, 0:1], ALU.is_equal)
                self.dma('pool', d['sel1'][n * 128:(n + 1) * 128, :], m1m[:, :])
                self.stt(l2[:, :], m1m[:, :], -1e30, lg[:, :], ALU.mult, ALU.add)
                self.red(mx[:, 1:2], l2[:, :], ALU.max)
                self.ts(l2[:, :], lg[:, :], mx[:, 1:2], ALU.is_ge)
                self.ts(mx[:, 2:3], mx[:, 0:1], -1.0, ALU.mult)
                self.act(gsb[:, :], lg[:, :], AF.Exp, bias=mx[:, 2:3], scale=1.0)
                self.tt(gsb[:, :], gsb[:, :], l2[:, :], ALU.mult)
                self.red(mx[:, 3:4], gsb[:, :], ALU.add)
                self.recip(mx[:, 3:4], mx[:, 3:4])
                self.ts(gsb[:, :], gsb[:, :], mx[:, 3:4], ALU.mult)
                self.dma('pool', d['gates'][n * 128:(n + 1) * 128, :], gsb[:, :])
            else:
                self.norm_mod(x1, A, B, hb, tmp, ss)
            ht = hT[n % 2]
            self.transpose8(hb, ht[:, :, :])
            self.dma('pool', d['h2T'].rearrange('(k p) t -> p k t', p=128)[:, :, n * 128:(n + 1) * 128], ht[:, :, :])
        fr = [w, *catTs, *tmps, *hbos, *sso, *xts, *xn, *hT, *g1]
        if last:
            fr += [rw, hf, hT32, lg, l2, mx, gsb, m1m]
        S.free(*fr)
        for a, b in ab:
            S.free(a, b)

    def stage_ffn(self, l, last):
        S, d = self.S, self.d
        g2 = [self.bcast(f'g2{r}', d['mod'][l, r, 5 * DM:6 * DM], DM) for r in range(2)]
        if last:
            fw = self.bcast('fw', d['fin_w'][:], DM)
            sbs = [(256 + i * 1024, 1024) for i in range(4)]
            experts = [(d['moe_w1'][e], d['moe_w3'][e], d['moe_w2'][e]) for e in range(8)]
        else:
            sbs = [(0, 256)] + [(256 + i * 1024, 1024) for i in range(4)]
            experts = [(d['ffn_w1'], d['ffn_w3'], d['ffn_w2'])]
        hTb = S.alloc('hTb', [128, 8, 1024], BF16)
        acc = S.alloc('facc', [128, 8, DM], F32)
        uT = S.alloc('uT', [128, 4, 1024], BF16)
        us = [S.alloc(f'us{i}', [128, 512], F32) for i in range(2)]
        w1s = [S.alloc(f'w1g{i}', [128, 8, 512], BF16) for i in range(2)]
        w3s = [S.alloc(f'w3g{i}', [128, 8, 512], BF16) for i in range(2)]
        w2s = [S.alloc(f'w2g{i}', [128, 4, DM], BF16) for i in range(2)]
        gt = S.alloc('fgt', [128, 8, 8], F32)
        xts = [S.alloc(f'xf{i}', [128, DM], F32) for i in range(2)]
        tmp = S.alloc('ftmp', [128, DM], F32)
        ss = S.alloc('fss', [128, 1], F32)
        groups = [(g * 512, 512) for g in range(5)] + [(2560, 256)]
        wi = 0
        for (t0, ntok) in sbs:
            ntile = ntok // 128
            self.dma('sp', hTb[:, :, 0:ntok], d['h2T'].rearrange('(k p) t -> p k t', p=128)[:, :, t0:t0 + ntok])
            if last:
                self.dma('sp', gt[:, 0:ntile, :], d['gates'][t0:t0 + ntok, :].rearrange('(n p) e -> p n e', p=128))
            first = True
            for ei, (W1, W3, W2) in enumerate(experts):
                for (g0, gs) in groups:
                    nfc = gs // 128
                    w1, w3, w2 = w1s[wi % 2], w3s[wi % 2], w2s[wi % 2]
                    wi += 1
                    self.dma('pool', w1[:, :, 0:gs], W1.rearrange('(k p) n -> p k n', p=128)[:, :, g0:g0 + gs])
                    self.dma('pool', w3[:, :, 0:gs], W3.rearrange('(k p) n -> p k n', p=128)[:, :, g0:g0 + gs])
                    self.dma('pool', w2[:, 0:nfc, :], W2[g0:g0 + gs, :].rearrange('(c p) n -> p c n', p=128))
                    for tb in range((ntok + 511) // 512):
                        tw = min(512, ntok - tb * 512)
                        for fc in range(nfc):
                            pa, pb_ = self.ps(), self.ps()
                            for k in range(8):
                                self.mm(pa[:, 0:tw], w1[:, k, fc * 128:(fc + 1) * 128], hTb[:, k, tb * 512:tb * 512 + tw], start=(k == 0), stop=(k == 7))
                            for k in range(8):
                                self.mm(pb_[:, 0:tw], w3[:, k, fc * 128:(fc + 1) * 128], hTb[:, k, tb * 512:tb * 512 + tw], start=(k == 0), stop=(k == 7))
                            u = us[fc % 2]
                            self.act(u[:, 0:tw], pa[:, 0:tw], AF.Silu)
                            self.tt(uT[:, fc, tb * 512:tb * 512 + tw], u[:, 0:tw], pb_[:, 0:tw], ALU.mult)
                    for tt_ in range(ntile):
                        for half in range(2):
                            ps = self.ps()
                            for fc in range(nfc):
                                self.mm(ps[:, 0:512], uT[:, fc, tt_ * 128:(tt_ + 1) * 128], w2[:, fc, half * 512:(half + 1) * 512],
                                        start=(fc == 0), stop=(fc == nfc - 1))
                            a = acc[:, tt_, half * 512:(half + 1) * 512]
                            if last:
                                gsc = gt[:, tt_, ei:ei + 1]
                                if first:
                                    self.ts(a, ps[:, 0:512], gsc, ALU.mult)
                                else:
                                    self.stt(a, ps[:, 0:512], gsc, a, ALU.mult, ALU.add)
                            else:
                                if first:
                                    self.cp(a, ps[:, 0:512], eng='act')
                                else:
                                    self.tt(a, a, ps[:, 0:512], ALU.add)
                    first = False
            for tt_ in range(ntile):
                n = t0 // 128 + tt_
                r = 1 if n < 2 else 0
                xt = xts[tt_ % 2]
                self.dma('sp', xt[:, :], d['x1'][n * 128:(n + 1) * 128, :])
                a = acc[:, tt_, :]
                self.tt(a, a, g2[r][:, :], ALU.mult)
                self.tt(xt[:, :], xt[:, :], a, ALU.add)
                if last:
                    self.act(tmp[:, :], xt[:, :], AF.Square, accum=ss[:, 0:1])
                    self.rstd(ss[:, 0:1], DM, 'f')
                    self.stt(xt[:, :], xt[:, :], ss[:, 0:1], fw[:, :], ALU.mult, ALU.mult)
                    self.dma('pool', d['out'][(n - 2) * 128:(n - 1) * 128, :], xt[:, :])
                else:
                    self.dma('pool', d['x2'][n * 128:(n + 1) * 128, :], xt[:, :])
        fr = [hTb, acc, uT, gt, tmp, ss, *us, *w1s, *w3s, *w2s, *xts, *g2]
        if last:
            fr.append(fw)
        S.free(*fr)


    def stage_moe(self, l):
        S, d = self.S, self.d
        NL = 32
        g2 = self.bcast('g2m', d['mod'][l, 0, 5 * DM:6 * DM], DM)
        fw = self.bcast('fwm', d['fin_w'][:], DM)
        G3 = S.alloc('G3', [128, NL, 8], F32)
        M1 = S.alloc('M1', [128, NL, 8], F32)
        self.dma('sp', G3[:, :, :], d['gates'][256:NT, :].rearrange('(n p) e -> p n e', p=128))
        self.dma('sp', M1[:, :, :], d['sel1'][256:NT, :].rearrange('(n p) e -> p n e', p=128))
        Ms = S.alloc('Ms', [128, NL, 8], F32)
        self.ts(Ms[:, :, :], G3[:, :, :], 0.0, ALU.is_gt)
        M2 = S.alloc('M2', [128, NL, 8], F32)
        self.tt(M2[:, :, :], Ms[:, :, :], M1[:, :, :], ALU.subtract)
        triS = S.alloc('triS', [128, 128], F32)
        self.tt(triS[:, :], self.tri0[:, :], self.identf[:, :], ALU.subtract)
        Ms2 = Ms[:, :, :].rearrange('p n e -> p (n e)')
        pc_, pr_ = self.ps(), self.ps()
        self.mm(pc_[:, 0:256], self.onesf[:, :], Ms2)
        self.mm(pr_[:, 0:256], triS[:, :], Ms2)
        CS = S.alloc('CS', [128, NL, 8], F32)
        self.cp(CS[:, :, :].rearrange('p n e -> p (n e)'), pc_[:, 0:256])
        pos = S.alloc('pos', [128, NL, 8], F32)
        self.cp(pos[:, :, :].rearrange('p n e -> p (n e)'), pr_[:, 0:256], eng='act')
        TB = S.alloc('TB', [128, NL + 1, 8], F32)
        self.memset(TB[:, 0, :], 0.0)
        for n in range(NL):
            self.tt(TB[:, n + 1, :], TB[:, n, :], CS[:, n, :], ALU.add)
        cth = S.alloc('cth', [128, 16], F32)
        self.dma('sp', cth[:, :], d['c_th'][:, :])
        cj = S.alloc('cj', [128, NBLK], F32)
        self.dma('sp', cj[:, :], d['c_j512'][:, :])
        cgp = S.alloc('cgp', [128, 11], F32)
        self.dma('sp', cgp[:, :], d['c_gp'][:, :])
        cmp_ = S.alloc('cmp', [128, 8, 16], F32)
        self.tt(cmp_[:, :, :], cth[:, :].unsqueeze(1).to_broadcast([128, 8, 16]),
                TB[:, NL, :].unsqueeze(2).to_broadcast([128, 8, 16]), ALU.is_lt)
        pcn = S.alloc('pcn', [128, 8], F32)
        self.red(pcn[:, :], cmp_[:, :, :], ALU.add)
        self.ts(pcn[:, :], pcn[:, :], 512.0, ALU.mult)
        end = S.alloc('end', [128, 8], F32)
        self.cp(end[:, 0:1], pcn[:, 0:1])
        for e in range(1, 8):
            self.tt(end[:, e:e + 1], end[:, e - 1:e], pcn[:, e:e + 1], ALU.add)
        st = S.alloc('st', [128, 8], F32)
        self.tt(st[:, :], end[:, :], pcn[:, :], ALU.subtract)
        self.tt(pos[:, :, :], pos[:, :, :], TB[:, 0:NL, :], ALU.add)
        self.tt(pos[:, :, :], pos[:, :, :], st[:, :].unsqueeze(1).to_broadcast([128, NL, 8]), ALU.add)
        tmp3 = S.alloc('tmp3', [128, NL, 8], F32)
        slotf = S.alloc('slotf', [128, 2, NL], F32)
        gk = S.alloc('gk', [128, 2, NL], F32)
        for k, Mk in enumerate((M1, M2)):
            self.tt(tmp3[:, :, :], Mk[:, :, :], pos[:, :, :], ALU.mult)
            self.red(slotf[:, k, :], tmp3[:, :, :], ALU.add)
            self.tt(tmp3[:, :, :], Mk[:, :, :], G3[:, :, :], ALU.mult)
            self.red(gk[:, k, :], tmp3[:, :, :], ALU.add)
        self.ts(slotf[:, :, :], slotf[:, :, :], float(NSLOT - 1), ALU.min)
        sloti = S.alloc('sloti', [128, 2, NL], I32)
        self.cp(sloti[:, :, :].rearrange('p k n -> p (k n)'), slotf[:, :, :].rearrange('p k n -> p (k n)'))
        bj = S.alloc('bj', [128, NBLK, 8], F32)
        self.tt(bj[:, :, :], end[:, :].unsqueeze(1).to_broadcast([128, NBLK, 8]),
                cj[:, :].unsqueeze(2).to_broadcast([128, NBLK, 8]), ALU.is_le)
        ej = S.alloc('ej', [128, NBLK], F32)
        self.red(ej[:, :], bj[:, :, :], ALU.add)
        self.ts(ej[:, :], ej[:, :], 7.0, ALU.min, 1408.0, ALU.mult)
        idxf = S.alloc('idxf', [128, NBLK, 11], F32)
        self.tt(idxf[:, :, :], ej[:, :].unsqueeze(2).to_broadcast([128, NBLK, 11]),
                cgp[:, :].unsqueeze(1).to_broadcast([128, NBLK, 11]), ALU.add)
        idxi = S.alloc('idxi', [128, NBLK, 11], I32)
        self.cp(idxi[:, :, :].rearrange('p j g -> p (j g)'), idxf[:, :, :].rearrange('p j g -> p (j g)'))
        S.free(G3, M1, Ms, M2, triS, CS, pos, TB, cth, cj, cgp, cmp_, pcn, end, st, tmp3, slotf, bj, ej, idxf)

        zt = S.alloc('zt', [128, 24, DM], BF16)
        self.memset(zt[:, :, :].rearrange('p a c -> p (a c)'), 0.0)
        for j in range(NBLK // 6):
            self.dma('sp', d['hs'][j * 3072:(j + 1) * 3072, :].rearrange('(a p) c -> p a c', p=128), zt[:, :, :])
        hts = [S.alloc(f'hsrc{i}', [128, DM], BF16) for i in range(2)]
        for n in range(NL):
            ht = hts[n % 2]
            self.dma('sp', ht[:, :], d['h2'][(n + 2) * 128:(n + 3) * 128, :])
            for k in range(2):
                self.dma_ind(d['hs'][:, :], ht[:, :], sloti[:, k, n:n + 1], True, NSLOT - 1)
        S.free(zt, *hts)

        hbs = [S.alloc(f'hsb{i}', [128, 4, DM], BF16) for i in range(2)]
        hTs = [S.alloc(f'hTm{i}', [128, 8, 512], BF16) for i in range(2)]
        accs = [S.alloc(f'macc{i}', [128, 4, DM], F32) for i in range(2)]
        uTs = [S.alloc(f'uTm{i}', [128, 2, 512], BF16) for i in range(2)]
        us = [S.alloc(f'usm{i}', [128, 512], F32) for i in range(2)]
        w1s = [S.alloc(f'w1m{i}', [128, 8, 256], BF16) for i in range(4)]
        w3s = [S.alloc(f'w3m{i}', [128, 8, 256], BF16) for i in range(4)]
        w2s = [S.alloc(f'w2m{i}', [128, 2, DM], BF16) for i in range(4)]
        wi = 0
        nrow = 8 * 11 * 128
        for j in range(NBLK):
            hb, hT, acc = hbs[j % 2], hTs[j % 2], accs[j % 2]
            self.dma('sp', hb[:, :, :], d['hs'][j * 512:(j + 1) * 512, :].rearrange('(a p) c -> p a c', p=128))
            for a in range(4):
                ps = self.ps()
                psb = ps[:, :].bitcast(BF16)
                for k in range(8):
                    self.tr(psb[:, k * 128:(k + 1) * 128], hb[:, a, k * 128:(k + 1) * 128], self.identb[:, :])
                self.cp(hT[:, :, a * 128:(a + 1) * 128], psb[:, :].rearrange('p (k t) -> p k t', k=8), eng=('act' if a % 2 else 'dve'))
            for gp in range(0, 11, 2):
                ws = []
                for g in [x for x in (gp, gp + 1) if x < 11]:
                    w1, w3, w2 = w1s[wi % 4], w3s[wi % 4], w2s[wi % 4]
                    wi += 1
                    ix = idxi[:, j, g:g + 1]
                    self.dma_ind(w1[:, :, :].rearrange('p k c -> p (k c)'), d['moe_w1'][:, :], ix, False, nrow - 1)
                    self.dma_ind(w3[:, :, :].rearrange('p k c -> p (k c)'), d['moe_w3'][:, :], ix, False, nrow - 1)
                    self.dma_ind(w2[:, :, :].rearrange('p k c -> p (k c)'), d['moe_w2'][:, :], ix, False, nrow - 1)
                    uT = uTs[g % 2]
                    for fc in range(2):
                        pa, pb_ = self.ps(), self.ps()
                        for k in range(8):
                            self.mm(pa[:, 0:512], w1[:, k, fc * 128:(fc + 1) * 128], hT[:, k, :], start=(k == 0), stop=(k == 7))
                        for k in range(8):
                            self.mm(pb_[:, 0:512], w3[:, k, fc * 128:(fc + 1) * 128], hT[:, k, :], start=(k == 0), stop=(k == 7))
                        u = us[fc % 2]
                        self.act(u[:, :], pa[:, 0:512], AF.Silu)
                        self.tt(uT[:, fc, :], u[:, :], pb_[:, 0:512], ALU.mult)
                    ws.append((uT, w2))
                for a in range(4):
                    for half in range(2):
                        ps = self.psacc()
                        nmm = 2 * len(ws)
                        mi = 0
                        for (uT, w2) in ws:
                            for fc in range(2):
                                self.mm(ps[:, 0:512], uT[:, fc, a * 128:(a + 1) * 128], w2[:, fc, half * 512:(half + 1) * 512],
                                        start=(mi == 0), stop=(mi == nmm - 1))
                                mi += 1
                        av = acc[:, a, half * 512:(half + 1) * 512]
                        if gp == 0:
                            self.cp(av, ps[:, 0:512], eng='act')
                        else:
                            self.tt(av, av, ps[:, 0:512], ALU.add)
            self.dma('sp', d['yb'][j * 512:(j + 1) * 512, :].rearrange('(a p) c -> p a c', p=128), acc[:, :, :])
        S.free(*hbs, *hTs, *accs, *uTs, *us, *w1s, *w3s, *w2s, idxi)

        y1s = [S.alloc(f'y1_{i}', [128, DM], F32) for i in range(2)]
        y2s = [S.alloc(f'y2_{i}', [128, DM], F32) for i in range(2)]
        xts = [S.alloc(f'xm{i}', [128, DM], F32) for i in range(2)]
        tmp = S.alloc('mtmp', [128, DM], F32)
        ss = S.alloc('mss', [128, 1], F32)
        outs = [S.alloc(f'mout{i}', [128, DM], F32) for i in range(2)]
        def loads(n):
            y1, y2, xt = y1s[n % 2], y2s[n % 2], xts[n % 2]
            self.dma_ind(y1[:, :], d['yb'][:, :], sloti[:, 0, n:n + 1], False, NSLOT - 1)
            self.dma_ind(y2[:, :], d['yb'][:, :], sloti[:, 1, n:n + 1], False, NSLOT - 1)
            self.dma('sp', xt[:, :], d['x1'][(n + 2) * 128:(n + 3) * 128, :])
        loads(0)
        for n in range(NL):
            y1, y2, xt = y1s[n % 2], y2s[n % 2], xts[n % 2]
            ob = outs[n % 2]
            if n + 1 < NL:
                loads(n + 1)
            self.ts(y1[:, :], y1[:, :], gk[:, 0, n:n + 1], ALU.mult)
            self.stt(y1[:, :], y2[:, :], gk[:, 1, n:n + 1], y1[:, :], ALU.mult, ALU.add)
            self.tt(y1[:, :], y1[:, :], g2[:, :], ALU.mult)
            self.tt(xt[:, :], xt[:, :], y1[:, :], ALU.add)
            self.act(tmp[:, :], xt[:, :], AF.Square, accum=ss[:, 0:1])
            self.rstd(ss[:, 0:1], DM, 'f')
            self.stt(ob[:, :], xt[:, :], ss[:, 0:1], fw[:, :], ALU.mult, ALU.mult)
            self.dma('sp', d['out'][n * 128:(n + 1) * 128, :], ob[:, :])
        S.free(*outs)
        S.free(g2, fw, gk, sloti, tmp, ss, *y1s, *y2s, *xts)


def _na_bias_tiles(rpb):
    L, H = rpb.shape[0], rpb.shape[1]
    NEG = np.float32(-30000.0)
    col = np.arange(64)
    c0 = np.clip(col - 8, 0, 48)
    ck = col[:, None]
    cq = col[None, :]
    cvalid = (ck >= c0[None, :]) & (ck < c0[None, :] + 16)
    cidx = np.clip(ck - cq, -15, 15) + 15
    out = np.full((L, H, 14, 128, 256), NEG, dtype=np.float32)
    cfgs = []
    for i in range(6):
        cfgs.append((4, 4 - 4 + 2 * i))
    for j in range(4):
        cfgs.append((0, 2 * j))
    for j in range(4):
        cfgs.append((60, 56 + 2 * j))
    for ci, (R, kr0) in enumerate(cfgs):
        for a in range(2):
            kr = kr0 + a
            for b in range(4):
                r = R + b
                r0 = min(max(r - 4, 0), 56)
                if not (r0 <= kr <= r0 + 7) or kr > 63:
                    continue
                drow = kr - r + 7
                g = rpb[:, :, drow, :][:, :, cidx]
                blk = np.where(cvalid[None, None], g, NEG)
                out[:, :, ci, a * 64:(a + 1) * 64, b * 64:(b + 1) * 64] = blk
    return out


def _rope_tables():
    t = np.arange(4096)
    row = (t // 64).astype(np.float32)
    colv = (t % 64).astype(np.float32)
    inv = (np.float32(10000.0) ** (-np.arange(0, 16, 2, dtype=np.float32) / np.float32(16))).astype(np.float32)
    ang = np.concatenate([row[:, None] * inv, colv[:, None] * inv], axis=1).astype(np.float32)
    return np.cos(ang).astype(np.float32), np.sin(ang).astype(np.float32)


def _w13_layout(w):
    w = w.reshape(8, 8, 128, 11, 256)
    return np.ascontiguousarray(w.transpose(0, 3, 2, 1, 4)).reshape(8 * 11 * 128, 2048)


def _w2_layout(w):
    w = w.reshape(8, 11, 2, 128, 1024)
    return np.ascontiguousarray(w.transpose(0, 1, 3, 2, 4)).reshape(8 * 11 * 128, 2048)


_NC_CACHE = {}


def _get_nc(stop_after=None):
    key = (stop_after, DEBUG)
    if key not in _NC_CACHE:
        nc = bass.Bass("TRN2", target_bir_lowering=False)
        k = K(nc)
        k.stop_after = stop_after
        k.build()
        _NC_CACHE[key] = nc
    return _NC_CACHE[key]


def make_in_maps(inputs):
    f = lambda a: np.ascontiguousarray(np.asarray(a, dtype=np.float32))
    x, c, ctx, c_ctx = f(inputs['x']), f(inputs['c']), f(inputs['ctx']), f(inputs['c_ctx'])
    cs, sn = _rope_tables()
    tri0 = np.triu(np.ones((128, 128), np.float32))
    tri1 = np.tril(np.ones((128, 128), np.float32))
    shared = {
        'ada_w': f(inputs['ada_w']), 'ada_b': f(inputs['ada_b']),
        'norm1_w': f(inputs['norm1_w']), 'norm2_w': f(inputs['norm2_w']),
        'w_in': f(inputs['w_in']), 'w_out': f(inputs['w_out']),
        'convT': f(np.transpose(np.asarray(inputs['mlstm_conv_w']), (0, 2, 1))),
        'ig_b': f(np.asarray(inputs['mlstm_ig_b']).reshape(2, 8)),
        'fg_b': f(np.asarray(inputs['mlstm_fg_b']).reshape(2, 8)),
        'ml_nw': f(inputs['mlstm_norm_w']),
        'na_bm': _na_bias_tiles(f(inputs['na_rpb'])),
        'q_nw': f(inputs['mla_q_norm_w']), 'kv_nw': f(inputs['mla_kv_norm_w']),
        'w_uq': f(inputs['mla_w_uq']), 'w_ukv': f(inputs['mla_w_ukv']),
        'ffn_w1': f(inputs['ffn_w1'])[0], 'ffn_w3': f(inputs['ffn_w3'])[0], 'ffn_w2': f(inputs['ffn_w2'])[0],
        'router': f(inputs['moe_router_w'])[0],
        'moe_w1': _w13_layout(f(inputs['moe_w1'])[0]), 'moe_w3': _w13_layout(f(inputs['moe_w3'])[0]),
        'moe_w2': _w2_layout(f(inputs['moe_w2'])[0]),
        'c_th': np.ascontiguousarray(np.broadcast_to(np.arange(16, dtype=np.float32) * 512, (128, 16))),
        'c_j512': np.ascontiguousarray(np.broadcast_to(np.arange(NBLK, dtype=np.float32) * 512, (128, NBLK))),
        'c_gp': np.ascontiguousarray((np.arange(11, dtype=np.float32)[None, :] * 128 + np.arange(128, dtype=np.float32)[:, None])),
        'fin_w': f(inputs['final_norm_w']),
        'ident': np.eye(128, dtype=np.float32), 'tri0': tri0, 'tri1': tri1,
        'rope_cs': cs, 'rope_sn': sn,
    }
    maps = []
    for b in range(8):
        m = dict(shared)
        m['xin'] = np.ascontiguousarray(np.concatenate([ctx[b], x[b]], axis=0))
        cc = np.stack([c[b].reshape(8, 128).T, c_ctx.reshape(8, 128).T], axis=2)
        m['cc'] = np.ascontiguousarray(cc.reshape(128, 16))
        maps.append(m)
    return maps


def kernel(**inputs):
    nc = _get_nc()
    maps = make_in_maps(inputs)
    res = run_bass_kernel_spmd(nc, maps, core_ids=list(range(8)))
    return np.stack([np.asarray(r['out'], dtype=np.float32) for r in res.results], axis=0)
```

```python
import numpy as np
from contextlib import ExitStack
import concourse.bass as bass
import concourse.mybir as mybir
from concourse.bass_utils import run_bass_kernel_spmd

F32 = mybir.dt.float32
BF16 = mybir.dt.bfloat16
U8 = mybir.dt.uint8
AF = mybir.ActivationFunctionType
ALU = mybir.AluOpType
AX = mybir.AxisListType
DTSIZE = {F32: 4, BF16: 2, mybir.dt.int32: 4}
COMPUTE = ('pe', 'act', 'dve', 'pool')
QUEUES = ('sp', 'act', 'pool')
NDSEM = 12
ARENA = 207 * 1024

NT = 4352
NTILE = 34
DM = 1024
NIN = 2992
DFF = 2816
EPS = 1e-6
DEBUG = False
NBLK = 24
NSLOT = NBLK * 512
I32 = mybir.dt.int32


class Op:
    __slots__ = ('eng', 'fn', 'deps', 'isdma', 'needs_inc', 'count', 'semi', 'semval', 'idx', 'q')


class V:
    __slots__ = ('buf', 'ap')

    def __init__(self, buf, ap):
        self.buf = buf
        self.ap = ap

    def __getitem__(self, k):
        return V(self.buf, self.ap[k])

    def rearrange(self, pattern_, **kw):
        return V(self.buf, self.ap.rearrange(pattern_, **kw))

    def unsqueeze(self, a):
        return V(self.buf, self.ap.unsqueeze(a))

    def to_broadcast(self, shp):
        return V(self.buf, self.ap.to_broadcast(list(shp)))

    def bitcast(self, dt):
        return V(self.buf, self.ap.bitcast(dt))

    def partition_broadcast(self, n):
        return V(self.buf, self.ap.partition_broadcast(n))


class Buf:
    def __init__(self, ap, name, off=0, size=0):
        self.ap = ap
        self.name = name
        self.w = None
        self.rc = {}
        self.rd = {}
        self.inh = []
        self.multiw = False
        self.wl = {}
        self.war = []
        self.off = off
        self.size = size

    def __getitem__(self, k):
        return V(self, self.ap[k])

    def v(self):
        return V(self, self.ap)

    def rearrange(self, pattern_, **kw):
        return V(self, self.ap.rearrange(pattern_, **kw))


class Sched:
    def __init__(self, nc):
        self.nc = nc
        self.ops = {e: [] for e in ('pe', 'act', 'dve', 'pool', 'sp')}
        self.nops = 0
        self.dsem_use = {q: [0] * NDSEM for q in QUEUES}
        self.dsem_rr = {q: 0 for q in QUEUES}
        self.arena = nc.alloc_sbuf_tensor("arena", [128, ARENA], U8)
        self.live = []
        self.dead = []
        self.psum = []
        for i in range(8):
            h = nc.alloc_psum_tensor(f"psb{i}", [128, 512], F32)
            self.psum.append(Buf(h[:, :], f"psb{i}"))

    def alloc(self, name, shape, dtype):
        n = int(np.prod(shape[1:])) * DTSIZE[dtype]
        n_al = (n + 63) // 64 * 64
        self.live.sort(key=lambda b: b.off)
        off = 0
        for b in self.live:
            if b.off - off >= n_al:
                break
            off = max(off, b.off + b.size)
        if off + n_al > ARENA:
            raise RuntimeError(f"SBUF arena OOM allocating {name} {shape}: need {n_al} at {off}; live="
                               + str([(b.name, b.off, b.size) for b in self.live]))
        ap = self.arena[0:shape[0], off:off + n].bitcast(dtype)
        if len(shape) > 2:
            names = ' '.join(f'd{i}' for i in range(1, len(shape)))
            kw = {f'd{i}': shape[i] for i in range(1, len(shape))}
            ap = ap.rearrange(f'p ({names}) -> p {names}', **kw)
        buf = Buf(ap, name, off, n_al)
        keep = []
        for d in self.dead:
            if d.off < off + n_al and off < d.off + d.size:
                acc = list(d.inh)
                if d.w is not None:
                    acc.append(d.w)
                acc.extend(d.rc.values())
                acc.extend(d.rd.values())
                buf.inh.extend(acc)
                if not (off <= d.off and d.off + d.size <= off + n_al):
                    keep.append(d)
            else:
                keep.append(d)
        self.dead = keep
        best = {}
        for o in buf.inh:
            key = (o.q, o.semi) if o.isdma else o.eng
            if key not in best or best[key].idx < o.idx:
                best[key] = o
        buf.inh = list(best.values())
        self.live.append(buf)
        return buf

    def free(self, *bufs):
        for b in bufs:
            self.live.remove(b)
            self.dead.append(b)

    def dram(self, name, shape, dtype, kind="Internal"):
        h = self.nc.dram_tensor(name, list(shape), dtype, kind=kind)
        return Buf(h.ap(), name)

    def _mk(self, eng, fn, reads, writes, isdma, q=None):
        op = Op()
        op.eng = eng
        op.fn = fn
        op.isdma = isdma
        op.needs_inc = False
        op.count = 0
        op.idx = self.nops
        op.q = q
        op.semi = -1
        self.nops += 1
        if isdma:
            i = self.dsem_rr[q]
            self.dsem_rr[q] = (i + 1) % NDSEM
            self.dsem_use[q][i] += 1
            op.semi = i
            op.semval = 16 * self.dsem_use[q][i]
        deps = []
        for b in reads:
            if b.w is not None:
                deps.append((b.w, True))
            for o in b.wl.values():
                deps.append((o, True))
        for b in writes:
            mw = b.multiw and isdma
            if mw:
                if b.rc or b.rd:
                    b.war = list(b.rc.values()) + list(b.rd.values())
                    b.wl = {}
                    b.rc = {}
                    b.rd = {}
                for o in b.war:
                    deps.append((o, False))
                if b.w is not None:
                    deps.append((b.w, False))
            else:
                if b.w is not None:
                    deps.append((b.w, False))
                for o in b.wl.values():
                    deps.append((o, False))
                for o in b.rc.values():
                    deps.append((o, False))
                for o in b.rd.values():
                    deps.append((o, False))
            for o in b.inh:
                deps.append((o, False))
        fin = []
        seen = set()
        for p, raw in deps:
            if p is op or id(p) in seen:
                continue
            if (not p.isdma) and (not isdma) and p.eng == eng and eng == 'pe':
                continue
            seen.add(id(p))
            fin.append(p)
            p.needs_inc = True
        op.deps = fin
        for b in reads:
            if isdma:
                b.rd[(q, op.semi)] = op
            else:
                b.rc[eng] = op
        for b in writes:
            if b.multiw and isdma:
                b.wl[(q, op.semi)] = op
                b.inh = []
            else:
                b.w = op
                b.wl = {}
                b.war = []
                b.rc = {}
                b.rd = {}
                b.inh = []
        self.ops[eng].append(op)
        return op

    def op(self, eng, fn, reads=(), writes=()):
        return self._mk(eng, fn, reads, writes, False)

    def dma(self, q, out, in_, **kw):
        oa, ia = out.ap, in_.ap

        def fn(e):
            return e.dma_start(out=oa, in_=ia, **kw)
        return self._mk(q, fn, [in_.buf], [out.buf], True, q=q)

    def emit(self):
        nc = self.nc
        with ExitStack() as es:
            self.csems = {e: es.enter_context(nc.semaphore(f"c_{e}")) for e in COMPUTE}
            self.dsems = {q: [es.enter_context(nc.semaphore(f"d_{q}{i}")) for i in range(NDSEM)] for q in QUEUES}
            for e in COMPUTE:
                cnt = 0
                for op in self.ops[e]:
                    if not op.isdma and op.needs_inc:
                        cnt += 1
                    op.count = cnt
            block = es.enter_context(nc.Block())

            @block.tensor
            def _(eng):
                self._emit_engine('pe', eng)

            @block.scalar
            def _(eng):
                self._emit_engine('act', eng)

            @block.vector
            def _(eng):
                self._emit_engine('dve', eng)

            @block.gpsimd
            def _(eng):
                self._emit_engine('pool', eng)

            @block.sync
            def _(eng):
                self._emit_engine('sp', eng, final=True)

    def _emit_engine(self, e, eng, final=False):
        waited = {}

        def wait(key, sem, val):
            if waited.get(key, 0) >= val:
                return
            eng.wait_ge(sem, val)
            waited[key] = val

        for op in self.ops[e]:
            for p in op.deps:
                if p.isdma:
                    wait(('d', p.q, p.semi), self.dsems[p.q][p.semi], p.semval)
                else:
                    wait(('c', p.eng), self.csems[p.eng], p.count)
            if op.isdma:
                sem = self.dsems[op.q][op.semi]
                if op.semval > 16:
                    wait(('d', op.q, op.semi), sem, op.semval - 16)
                ins = op.fn(eng)
                ins.then_inc(sem, 16)
            else:
                ins = op.fn(eng)
                if op.needs_inc:
                    ins.then_inc(self.csems[e], 1)
        if final:
            for q in QUEUES:
                for i in range(NDSEM):
                    v = 16 * self.dsem_use[q][i]
                    if v > 0:
                        wait(('d', q, i), self.dsems[q][i], v)
            for ce in COMPUTE:
                last = None
                for op in self.ops[ce]:
                    if not op.isdma and op.needs_inc:
                        last = op
                if last is not None:
                    wait(('c', ce), self.csems[ce], last.count)


def _bufs(*xs):
    out = []
    for x in xs:
        if isinstance(x, V) and x.buf not in out:
            out.append(x.buf)
    return out


def _a(x):
    return x.ap if isinstance(x, V) else x


class K:
    def __init__(self, nc):
        self.nc = nc
        self.S = Sched(nc)
        self.defctx = {'banks': [0, 1, 2, 3], 'accs': [4, 5, 6, 7], 'i': 0, 'j': 0}
        self.cur = self.defctx

    def ps(self):
        c = self.cur
        b = self.S.psum[c['banks'][c['i'] % len(c['banks'])]]
        c['i'] += 1
        return b

    def psacc(self):
        c = self.cur
        b = self.S.psum[c['accs'][c['j'] % len(c['accs'])]]
        c['j'] += 1
        return b

    def interleave(self, gens):
        live = list(gens)
        while live:
            for item in list(live):
                g, ctx, wgt = item
                self.cur = ctx
                for _ in range(wgt):
                    try:
                        next(g)
                    except StopIteration:
                        live.remove(item)
                        break
        self.cur = self.defctx

    def mm(self, out, lhsT, rhs, start=True, stop=True):
        oa, la, ra = out.ap, lhsT.ap, rhs.ap
        self.S.op('pe', lambda e: e.matmul(oa, lhsT=la, rhs=ra, start=start, stop=stop),
                  reads=_bufs(lhsT, rhs), writes=_bufs(out))

    def tr(self, out, in_, ident):
        oa, ia, da = out.ap, in_.ap, ident.ap
        self.S.op('pe', lambda e: e.transpose(oa, ia, da), reads=_bufs(in_, ident), writes=_bufs(out))

    def act(self, out, in_, func, bias=None, scale=None, accum=None):
        kw = {}
        if bias is not None:
            kw['bias'] = _a(bias)
        if scale is not None:
            kw['scale'] = _a(scale)
        if accum is not None:
            kw['accum_out'] = accum.ap
        oa, ia = out.ap, in_.ap
        self.S.op('act', lambda e: e.activation(out=oa, in_=ia, func=func, **kw),
                  reads=_bufs(in_, bias, scale), writes=_bufs(out, accum))

    def tt(self, out, in0, in1, op, eng='dve'):
        oa, a0, a1 = out.ap, in0.ap, in1.ap
        self.S.op(eng, lambda e: e.tensor_tensor(out=oa, in0=a0, in1=a1, op=op), reads=_bufs(in0, in1), writes=_bufs(out))

    def ts(self, out, in0, s1, op0, s2=None, op1=None, eng='dve'):
        oa, a0 = out.ap, in0.ap
        x1, x2 = _a(s1), _a(s2)
        if op1 is None:
            self.S.op(eng, lambda e: e.tensor_scalar(out=oa, in0=a0, scalar1=x1, scalar2=None, op0=op0),
                      reads=_bufs(in0, s1), writes=_bufs(out))
        else:
            self.S.op(eng, lambda e: e.tensor_scalar(out=oa, in0=a0, scalar1=x1, scalar2=x2, op0=op0, op1=op1),
                      reads=_bufs(in0, s1, s2), writes=_bufs(out))

    def stt(self, out, in0, scalar, in1, op0, op1, eng='dve'):
        oa, a0, a1, sc = out.ap, in0.ap, in1.ap, _a(scalar)
        self.S.op(eng, lambda e: e.scalar_tensor_tensor(out=oa, in0=a0, scalar=sc, in1=a1, op0=op0, op1=op1),
                  reads=_bufs(in0, in1, scalar), writes=_bufs(out))

    def cp(self, out, in_, eng='dve'):
        oa, ia = out.ap, in_.ap
        if eng == 'act':
            self.S.op('act', lambda e: e.copy(out=oa, in_=ia), reads=_bufs(in_), writes=_bufs(out))
        else:
            self.S.op(eng, lambda e: e.tensor_copy(out=oa, in_=ia), reads=_bufs(in_), writes=_bufs(out))

    def memset(self, out, val, eng='dve'):
        oa = out.ap
        self.S.op(eng, lambda e: e.memset(oa, val), writes=_bufs(out))

    def red(self, out, in_, op, negate=False):
        oa, ia = out.ap, in_.ap
        self.S.op('dve', lambda e: e.tensor_reduce(out=oa, in_=ia, axis=AX.X, op=op, negate=negate),
                  reads=_bufs(in_), writes=_bufs(out))

    def recip(self, out, in_):
        oa, ia = out.ap, in_.ap
        self.S.op('dve', lambda e: e.reciprocal(out=oa, in_=ia), reads=_bufs(in_), writes=_bufs(out))

    def dma(self, q, out, in_, **kw):
        self.S.dma(q, out, in_, **kw)

    def dma_ind(self, out, in_, idx, scatter, bound):
        oa, ia, xa = out.ap, in_.ap, idx.ap

        def fn(e):
            off = bass.IndirectOffsetOnAxis(ap=xa, axis=0)
            if scatter:
                return e.indirect_dma_start(out=oa, out_offset=off, in_=ia, in_offset=None, bounds_check=None, oob_is_err=False)
            return e.indirect_dma_start(out=oa, out_offset=None, in_=ia, in_offset=off, bounds_check=None, oob_is_err=False)
        self.S._mk('pool', fn, [in_.buf, idx.buf], [out.buf], True, q='pool')

    def bcast(self, name, row_view, n):
        b = self.S.alloc(name, [128, n], F32)
        self.dma('sp', b[:, :], row_view.partition_broadcast(128))
        return b

    def rstd(self, ss, n, name):
        self.act(ss, ss, AF.Sqrt, bias=self.eps[0:ss.ap.shape[0], 0:1], scale=1.0 / n)
        self.recip(ss, ss)

    def build(self):
        S = self.S
        d = {}
        self.d = d
        import os
        self.flags = set(os.environ.get('KFLAGS', '').split(','))
        om = 'only_moe' in self.flags

        def inp(name, shape):
            d[name] = S.dram(name, shape, F32, kind="ExternalInput")
        inp('xin', [NT, DM])
        inp('cc', [128, 16])
        inp('ada_w', [2, DM, 6 * DM])
        inp('ada_b', [2, 6 * DM])
        inp('norm1_w', [2, DM])
        inp('norm2_w', [2, DM])
        inp('w_in', [2, DM, NIN])
        inp('w_out', [2, DM, DM])
        inp('convT', [2, 512, 3])
        inp('ig_b', [2, 8])
        inp('fg_b', [2, 8])
        inp('ml_nw', [2, 256])
        inp('na_bm', [2, 6, 14, 128, 256])
        inp('q_nw', [2, 512])
        inp('kv_nw', [2, 256])
        inp('w_uq', [2, 512, 576])
        inp('w_ukv', [2, 256, 768])
        inp('ffn_w1', [DM, DFF])
        inp('ffn_w3', [DM, DFF])
        inp('ffn_w2', [DFF, DM])
        inp('router', [DM, 8])
        inp('moe_w1', [8 * 11 * 128, 2048])
        inp('moe_w3', [8 * 11 * 128, 2048])
        inp('moe_w2', [8 * 11 * 128, 2048])
        inp('c_th', [128, 16])
        inp('c_j512', [128, NBLK])
        inp('c_gp', [128, 11])
        inp('fin_w', [DM])
        inp('ident', [128, 128])
        inp('tri0', [128, 128])
        inp('tri1', [128, 128])
        inp('rope_cs', [4096, 16])
        inp('rope_sn', [4096, 16])
        d['out'] = S.dram('out', [4096, DM], F32, kind="ExternalOutput")
        kind = "ExternalOutput" if DEBUG else "Internal"
        d['mod'] = S.dram('mod', [2, 2, 6 * DM], F32, kind=('ExternalInput' if om else kind))
        d['pT_ml'] = S.dram('pT_ml', [512, NT], F32, kind=kind)
        d['pT_na'] = S.dram('pT_na', [768, NT], BF16, kind=kind)
        d['p_tok'] = S.dram('p_tok', [NT, 1712], F32, kind=kind)
        d['cat'] = S.dram('cat', [DM, NT], BF16, kind=kind)
        d['x1'] = S.dram('x1', [NT, DM], F32, kind=('ExternalInput' if om else kind))
        d['x2'] = S.dram('x2', [NT, DM], F32, kind=kind)
        d['h2T'] = S.dram('h2T', [DM, NT], BF16, kind=kind)
        d['gates'] = S.dram('gatesd', [NT, 8], F32, kind=('ExternalInput' if om else kind))
        d['sel1'] = S.dram('sel1d', [NT, 8], F32, kind=('ExternalInput' if om else kind))
        d['h2'] = S.dram('h2d', [NT, DM], BF16, kind=('ExternalInput' if om else kind))
        d['hs'] = S.dram('hsd', [NSLOT, DM], BF16, kind=kind)
        d['yb'] = S.dram('ybd', [NSLOT, DM], F32, kind=kind)

        for nm in ('pT_ml', 'pT_na', 'p_tok', 'cat', 'x1', 'x2', 'h2T', 'h2', 'gates', 'sel1', 'out', 'yb'):
            d[nm].multiw = True
        self.identb = S.alloc('identb', [128, 128], BF16)
        self.dma('pool', self.identb[:, :], d['ident'][:, :])
        self.identf = S.alloc('identf', [128, 128], F32)
        self.dma('sp', self.identf[:, :], d['ident'][:, :])
        self.tri0 = S.alloc('tri0', [128, 128], F32)
        self.dma('sp', self.tri0[:, :], d['tri0'][:, :])
        self.tri1 = S.alloc('tri1', [128, 128], F32)
        self.dma('sp', self.tri1[:, :], d['tri1'][:, :])
        self.onesf = S.alloc('onesf', [128, 128], F32)
        self.memset(self.onesf[:, :], 1.0)
        self.eps = S.alloc('eps', [128, 1], F32)
        self.memset(self.eps[:, :], EPS)

        stop = getattr(self, 'stop_after', None)
        xs = d['xin']
        if 'only_moe' in self.flags:
            self.stage_moe(1)
            S.emit()
            return
        for l in range(2):
            last = l == 1
            if 'only_mla' not in self.flags:
                self.adaln(l)
                if stop == ('adaln', l):
                    break
                self.stage_in(l, xs)
                if stop == ('in', l):
                    break
                if 'inter' not in self.flags:
                    for _ in self.stage_mlstm(l, last):
                        pass
                    if stop == ('ml', l):
                        break
                    for _ in self.stage_na(l, last):
                        pass
                else:
                    cm = {'banks': [3], 'accs': [4, 5], 'i': 0, 'j': 0}
                    cn = {'banks': [0, 1, 2], 'accs': [6, 7], 'i': 0, 'j': 0, 'depth': 2}
                    self.interleave([(self.stage_mlstm(l, last), cm, 1), (self.stage_na(l, last), cn, 4)])
            if stop == ('na', l):
                break
            self.stage_mla(l, last)
            if stop in (('mla', l), ('mla1', l)):
                break
            self.stage_out(l, xs, last)
            if stop == ('out', l):
                break
            if last:
                self.stage_moe(l)
            else:
                self.stage_ffn(l, last)
            if stop == ('ffn', l):
                break
            xs = d['x2']
        S.emit()

    def adaln(self, l):
        S, d = self.S, self.d
        cc = S.alloc('cc', [128, 16], F32)
        self.dma('sp', cc[:, :], d['cc'][:, :])
        scT = S.alloc('scT', [128, 16], BF16)
        self.act(scT[:, :], cc[:, :], AF.Silu)
        sc3 = scT[:, :].rearrange('p (k r) -> p k r', r=2)
        adab = S.alloc('adab', [2, 6 * DM], F32)
        self.dma('sp', adab[:, :], d['ada_b'][l, :].partition_broadcast(2))
        modrow = S.alloc('modrow', [2, 6 * DM], F32)
        wts = [S.alloc(f'adaw{i}', [128, 8, 512], BF16) for i in range(2)]
        wsrc = d['ada_w'][l].rearrange('(k p) n -> p k n', p=128)
        for j in range(12):
            wt = wts[j % 2]
            self.dma('pool', wt[:, :, :], wsrc[:, :, j * 512:(j + 1) * 512])
            ps = self.ps()
            for k in range(8):
                self.mm(ps[0:2, 0:512], sc3[:, k, :], wt[:, k, :], start=(k == 0), stop=(k == 7))
            self.tt(modrow[:, j * 512:(j + 1) * 512], ps[0:2, 0:512], adab[:, j * 512:(j + 1) * 512], ALU.add)
        self.dma('pool', d['mod'][l], modrow[:, :])
        S.free(cc, scT, adab, modrow, *wts)

    def mod_ab(self, l, which, nw_name, tag):
        S, d = self.S, self.d
        nwb = self.bcast(f'nwb{tag}', d[nw_name][l, :], DM)
        res = []
        for r in range(2):
            sc = self.bcast(f'sc{tag}{r}', d['mod'][l, r, (3 * which + 1) * DM:(3 * which + 2) * DM], DM)
            self.stt(sc[:, :], sc[:, :], 1.0, nwb[:, :], ALU.add, ALU.mult)
            sh = self.bcast(f'sh{tag}{r}', d['mod'][l, r, (3 * which) * DM:(3 * which + 1) * DM], DM)
            res.append((sc, sh))
        S.free(nwb)
        return res

    def norm_mod(self, xt, A, B, hb, tmp, ss):
        self.act(tmp[:, :], xt[:, :], AF.Square, accum=ss[:, 0:1])
        self.rstd(ss[:, 0:1], DM, 'r')
        self.stt(tmp[:, :], xt[:, :], ss[:, 0:1], A[:, :], ALU.mult, ALU.mult)
        self.tt(hb[:, :], tmp[:, :], B[:, :], ALU.add)

    def transpose8(self, hb, dst3):
        ps = self.ps()
        psb = ps[:, :].bitcast(BF16)
        for k in range(8):
            self.tr(psb[:, k * 128:(k + 1) * 128], hb[:, k * 128:(k + 1) * 128], self.identb[:, :])
        self.cp(dst3, psb[:, :].rearrange('p (k t) -> p k t', k=8), eng='act')

    def stage_in(self, l, xs):
        S, d = self.S, self.d
        w = S.alloc('w_in', [128, 8, NIN], BF16)
        wsrc = d['w_in'][l].rearrange('(k p) n -> p k n', p=128)
        for k in range(8):
            self.dma('pool', w[:, k, :], wsrc[:, k, :])
        ab = self.mod_ab(l, 0, 'norm1_w', 'a')
        hTs = [S.alloc(f'hT{i}', [128, 8, 512], BF16) for i in range(2)]
        xts = [S.alloc(f'xt{i}', [128, DM], F32) for i in range(2)]
        tmps = [S.alloc(f'tmp{i}', [128, DM], F32) for i in range(2)]
        hbs = [S.alloc(f'hb{i}', [128, DM], BF16) for i in range(2)]
        sss = [S.alloc(f'ss{i}', [128, 1], F32) for i in range(2)]
        fms = [S.alloc(f'fm{i}', [128, 512], F32) for i in range(2)]
        fnb = [S.alloc(f'fnb{i}', [128, 512], BF16) for i in range(2)]
        pts = [S.alloc(f'pt{i}', [128, 1712], F32) for i in range(2)]
        groups = [(0, 2)] + [(2 + 4 * i, 4) for i in range(8)]
        fcols = [0, 128, 256, 384] + [1040 + 128 * i for i in range(6)]
        tcols = [(512, 1024, 0), (1024, 1040, 512), (1808, 2320, 528), (2320, 2832, 1040), (2832, 2992, 1552)]
        ti = 0
        for gi, (t0, nt) in enumerate(groups):
            hT = hTs[gi % 2]
            G = nt * 128
            A, B = ab[1] if gi == 0 else ab[0]
            for j in range(nt):
                n = t0 + j
                xt = xts[ti % 2]
                hb = hbs[ti % 2]
                ss = sss[ti % 2]
                tmp = tmps[ti % 2]
                ti += 1
                self.dma('sp', xt[:, :], xs[n * 128:(n + 1) * 128, :])
                self.norm_mod(xt, A, B, hb, tmp, ss)
                self.transpose8(hb, hT[:, :, j * 128:(j + 1) * 128])
            for ci, c0 in enumerate(fcols):
                ps = self.ps()
                for k in range(8):
                    self.mm(ps[:, 0:G], w[:, k, c0:c0 + 128], hT[:, k, 0:G], start=(k == 0), stop=(k == 7))
                if ci < 4:
                    fm = fms[ci % 2]
                    self.cp(fm[:, 0:G], ps[:, 0:G], eng='act')
                    self.dma('pool', d['pT_ml'][ci * 128:(ci + 1) * 128, t0 * 128:t0 * 128 + G], fm[:, 0:G])
                else:
                    fb = fnb[ci % 2]
                    if ci < 7:
                        self.ts(fb[:, 0:G], ps[:, 0:G], 0.125, ALU.mult)
                    else:
                        self.cp(fb[:, 0:G], ps[:, 0:G], eng='act')
                    self.dma('pool', d['pT_na'][(ci - 4) * 128:(ci - 3) * 128, t0 * 128:t0 * 128 + G], fb[:, 0:G])
            for j in range(nt):
                n = t0 + j
                pt = pts[n % 2]
                for ii, (c0, c1, o0) in enumerate(tcols):
                    ps = self.ps()
                    for k in range(8):
                        self.mm(ps[:, 0:c1 - c0], hT[:, k, j * 128:(j + 1) * 128], w[:, k, c0:c1], start=(k == 0), stop=(k == 7))
                    if ii % 2 == 0:
                        self.cp(pt[:, o0:o0 + c1 - c0], ps[:, 0:c1 - c0], eng='dve')
                    else:
                        self.cp(pt[:, o0:o0 + c1 - c0], ps[:, 0:c1 - c0], eng='act')
                self.dma('pool', d['p_tok'][n * 128:(n + 1) * 128, :], pt[:, :])
        S.free(w, *tmps, *hTs, *xts, *hbs, *sss, *fms, *fnb, *pts)
        for a, b in ab:
            S.free(a, b)

    def stage_mlstm(self, l, last):
        S, d = self.S, self.d

        def tcol(n):
            return 1 + n * 128 if n < 2 else 2 + n * 128
        gt = S.alloc('gt', [128, NTILE, 16], F32)
        self.dma('sp', gt[:, :, :], d['p_tok'][:, 512:528].rearrange('(n p) c -> p n c', p=128))
        igb = self.bcast('igb', d['ig_b'][l, :], 8)
        fgb = self.bcast('fgb', d['fg_b'][l, :], 8)
        LI = S.alloc('LI', [128, NTILE, 8], F32)
        LF = S.alloc('LF', [128, NTILE, 8], F32)
        self.tt(LI[:, :, :], gt[:, :, 0:8], igb[:, :].unsqueeze(1).to_broadcast([128, NTILE, 8]), ALU.add)
        self.tt(LF[:, :, :], gt[:, :, 8:16], fgb[:, :].unsqueeze(1).to_broadcast([128, NTILE, 8]), ALU.add)
        self.act(LF[:, :, :], LF[:, :, :], AF.Exp, scale=-1.0)
        self.act(LF[:, :, :], LF[:, :, :], AF.Ln, bias=self.onesf[:, 0:1], scale=1.0)
        self.ts(LF[:, :, :], LF[:, :, :], -1.0, ALU.mult)
        LF2 = LF[:, :, :].rearrange('p n c -> p (n c)')
        NC = NTILE * 8
        Bc = S.alloc('Bc', [128, NTILE, 2, 4], F32)
        EB = S.alloc('EB', [128, NTILE, 2, 4], F32)
        ES = S.alloc('ES', [128, NTILE, 2, 4], F32)
        EG = S.alloc('EG', [128, NTILE, 2, 4], F32)
        psA = self.ps()
        self.mm(psA[:, 0:NC], self.tri0[:, :], LF2)
        self.cp(Bc[:, :, 0, :], psA[:, 0:NC].rearrange('p (n d h) -> p n d h', d=2, h=4)[:, :, 0, :])
        psB = self.ps()
        self.mm(psB[:, 0:NC], self.tri1[:, :], LF2)
        self.cp(Bc[:, :, 1, :], psB[:, 0:NC].rearrange('p (n d h) -> p n d h', d=2, h=4)[:, :, 1, :])
        psC = self.ps()
        self.mm(psC[:, 0:NC], self.onesf[:, :], LF2)
        self.act(EG[:, :, :, :].rearrange('p n d h -> p (n d h)'), psC[:, 0:NC], AF.Exp)
        Bc2 = Bc[:, :, :, :].rearrange('p n d h -> p (n d h)')
        self.act(EB[:, :, :, :].rearrange('p n d h -> p (n d h)'), Bc2, AF.Exp)
        ES2 = ES[:, :, :, :].rearrange('p n d h -> p (n d h)')
        self.tt(ES2, LI[:, :, :].rearrange('p n c -> p (n c)'), Bc2, ALU.subtract)
        self.act(ES2, ES2, AF.Exp)
        yield
        S.free(gt, igb, fgb, LI, LF, Bc)

        convw = S.alloc('convw', [128, 4, 3], F32)
        self.dma('sp', convw[:, :, :], d['convT'][l].rearrange('(c p) j -> p c j', p=128))
        W = NT + 3
        qkT = S.alloc('qkT', [128, 4, W], BF16)
        x32 = S.alloc('x32', [128, W], F32)
        acc = S.alloc('acc', [128, W], F32)
        for c in (0, 257, W - 1):
            self.memset(x32[:, c:c + 1], 0.0)
        for fc in range(4):
            self.dma('sp', x32[:, 1:257], d['pT_ml'][fc * 128:(fc + 1) * 128, 0:256])
            self.dma('sp', x32[:, 258:W - 1], d['pT_ml'][fc * 128:(fc + 1) * 128, 256:NT])
            a = acc[:, 1:W - 1]
            self.ts(a, x32[:, 0:W - 2], convw[:, fc, 0:1], ALU.mult)
            self.stt(a, x32[:, 1:W - 1], convw[:, fc, 1:2], a, ALU.mult, ALU.add)
            self.stt(a, x32[:, 2:W], convw[:, fc, 2:3], a, ALU.mult, ALU.add)
            if fc < 2:
                self.act(qkT[:, fc, 1:W - 1], a, AF.Silu)
            else:
                self.act(a, a, AF.Silu)
                self.ts(qkT[:, fc, 1:W - 1], a, 0.125, ALU.mult)
            yield
        S.free(x32, acc, convw)

        ktok = S.alloc('ktok', [128, NTILE, 256], BF16)
        for n in range(NTILE):
            ps = self.ps()
            psb = ps[:, :].bitcast(BF16)
            for kc in range(2):
                self.tr(psb[:, kc * 128:(kc + 1) * 128], qkT[:, 2 + kc, tcol(n):tcol(n) + 128], self.identb[:, :])
            self.cp(ktok[:, n, :], psb[:, 0:256], eng=('act' if n % 2 else 'dve'))
            if n % 4 == 3:
                yield

        Vp = S.alloc('Vp', [128, NTILE, 2, 4, 65], BF16)
        Xb = S.alloc('Xb', [128, NTILE, 2, 4, 65], BF16)
        v32 = S.alloc('v32', [128, 9, 256], F32)
        for t0 in range(0, NTILE, 9):
            tn = min(9, NTILE - t0)
            self.dma('sp', v32[:, 0:tn, :], d['p_tok'][t0 * 128:(t0 + tn) * 128, 0:256].rearrange('(n p) c -> p n c', p=128))
            for dd in range(2):
                self.tt(Vp[:, t0:t0 + tn, dd, :, 0:64], v32[:, 0:tn, :].rearrange('p n (h e) -> p n h e', h=4),
                        ES[:, t0:t0 + tn, dd, :].unsqueeze(3).to_broadcast([128, tn, 4, 64]), ALU.mult)
            yield
        for dd in range(2):
            self.cp(Vp[:, :, dd, :, 64:65], ES[:, :, dd, :].unsqueeze(3))
        S.free(v32, ES)

        Xf = S.alloc('Xf', [128, 2, 4, 65], F32)
        self.memset(Xf[:, :, :, :].rearrange('p d h e -> p (d h e)'), 0.0)
        order = [list(range(NTILE)), [1, 0] + list(range(NTILE - 1, 1, -1))]
        for step in range(NTILE):
            for dd in range(2):
                n = order[dd][step]
                self.cp(Xb[:, n, dd, :, :], Xf[:, dd, :, :], eng='act')
                ps = self.ps()
                psv = ps[:, 0:260].rearrange('p (h e) -> p h e', h=4)
                for h in range(4):
                    self.mm(psv[:, h, :], ktok[:, n, (h // 2) * 128:(h // 2) * 128 + 128], Vp[:, n, dd, h, :])
                self.tt(Xf[:, dd, :, :], Xf[:, dd, :, :], psv, ALU.add)
                self.tt(Xf[:, dd, :, :], Xf[:, dd, :, :], EG[:, n, dd, :].unsqueeze(2).to_broadcast([128, 4, 65]), ALU.mult)
            yield
        S.free(Xf, EG, ktok)

        mlnw = self.bcast('mlnw', d['ml_nw'][l, :], 256)
        Sms = [S.alloc(f'Sm{i}', [128, 128], BF16) for i in range(4)]
        Hd = [S.alloc(f'Hd{i}', [128, 4, 65], F32) for i in range(2)]
        den = S.alloc('den', [128, 2, 4], F32)
        hs = S.alloc('hs', [128, 4, 64], F32)
        ht = S.alloc('ht', [128, 4, 64], F32)
        ssq = S.alloc('ssq', [128, 4], F32)
        o32 = [S.alloc(f'o32{i}', [128, 256], F32) for i in range(2)]
        res = [S.alloc(f'mres{i}', [128, 256], BF16) for i in range(2)]
        resT = [S.alloc(f'mresT{i}', [128, 2, 128], BF16) for i in range(2)]
        smi = 0
        for n in range(2 if last else 0, NTILE):
            psO = [self.psacc(), self.psacc()]
            psOv = [p[:, 0:260].rearrange('p (h e) -> p h e', h=4) for p in psO]
            c0 = tcol(n)
            for h in range(4):
                pb = (h % 2) * 64
                qT = qkT[pb:pb + 64, h // 2, c0:c0 + 128]
                kT = qkT[pb:pb + 64, 2 + h // 2, c0:c0 + 128]
                psS = self.ps()
                self.mm(psS[:, 0:128], kT, qT)
                for dd in range(2):
                    Sm = Sms[smi % 4]
                    smi += 1
                    self.tt(Sm[:, :], psS[:, 0:128], (self.tri0 if dd == 0 else self.tri1)[:, :], ALU.mult)
                    self.mm(psOv[dd][:, h, :], Sm[:, :], Vp[:, n, dd, h, :], start=True, stop=False)
                    self.mm(psOv[dd][:, h, :], qT, Xb[pb:pb + 64, n, dd, h, :], start=False, stop=True)
                yield
            for dd in range(2):
                self.tt(Hd[dd][:, :, :], psOv[dd], EB[:, n, dd, :].unsqueeze(2).to_broadcast([128, 4, 65]), ALU.mult)
                self.stt(den[:, dd, :], Hd[dd][:, :, 64], -1.0, Hd[dd][:, :, 64], ALU.mult, ALU.max)
            self.ts(den[:, :, :], den[:, :, :], 1.0, ALU.max)
            self.recip(den[:, :, :], den[:, :, :])
            self.tt(hs[:, :, :], Hd[0][:, :, 0:64], den[:, 0, :].unsqueeze(2).to_broadcast([128, 4, 64]), ALU.mult)
            self.tt(ht[:, :, :], Hd[1][:, :, 0:64], den[:, 1, :].unsqueeze(2).to_broadcast([128, 4, 64]), ALU.mult)
            self.tt(hs[:, :, :], hs[:, :, :], ht[:, :, :], ALU.add)
            self.tt(ht[:, :, :], hs[:, :, :], hs[:, :, :], ALU.mult)
            self.red(ssq[:, :], ht[:, :, :], ALU.add)
            self.act(ssq[:, :], ssq[:, :], AF.Sqrt, bias=self.eps[:, 0:1], scale=1.0 / 64)
            self.recip(ssq[:, :], ssq[:, :])
            self.tt(hs[:, :, :], hs[:, :, :], ssq[:, :].unsqueeze(2).to_broadcast([128, 4, 64]), ALU.mult)
            hs2 = hs[:, :, :].rearrange('p h e -> p (h e)')
            self.tt(hs2, hs2, mlnw[:, :], ALU.mult)
            o = o32[n % 2]
            self.dma('sp', o[:, :], d['p_tok'][n * 128:(n + 1) * 128, 256:512])
            self.act(o[:, :], o[:, :], AF.Sigmoid)
            r = res[n % 2]
            self.tt(r[:, :], hs2, o[:, :], ALU.mult)
            pst = self.ps()
            pstb = pst[:, :].bitcast(BF16)
            for c in range(2):
                self.tr(pstb[:, c * 128:(c + 1) * 128], r[:, c * 128:(c + 1) * 128], self.identb[:, :])
            rT = resT[n % 2]
            self.cp(rT[:, :, :], pstb[:, 0:256].rearrange('p (c t) -> p c t', c=2), eng='act')
            self.dma('pool', d['cat'][0:256, n * 128:(n + 1) * 128].rearrange('(c p) t -> p c t', p=128), rT[:, :, :])
            yield
        S.free(mlnw, den, hs, ht, ssq, qkT, Vp, Xb, EB, *Sms, *Hd, *o32, *res, *resT)

    def attn_block(self, kT_of, qTv, nq, keys, Va_of, out_dram, bm_of=None):
        psO = self.psacc()
        nk = len(keys)

        def issue_s(i):
            kt, cfg = keys[i]
            psS = self.ps()
            self.mm(psS[:, 0:nq], kT_of(kt), qTv, start=True, stop=True)
            return psS
        D = self.cur.get('depth', 3)
        pend = [issue_s(i) for i in range(min(D, nk))]
        for i in range(nk):
            psS = pend.pop(0)
            if i + D < nk:
                pend.append(issue_s(i + D))
            P = self.Ps[self.pi % len(self.Ps)]
            self.pi += 1
            self.act(P[:, 0:nq], psS[:, 0:nq], AF.Exp)
            if keys[i][1] is not None:
                self.tt(P[:, 0:nq], P[:, 0:nq], bm_of(keys[i][1]), ALU.mult)
            self.mm(psO[:, 0:nq], Va_of(keys[i][0]), P[:, 0:nq], start=(i == 0), stop=(i == nk - 1))
            yield
        r = self.pi % 2
        rsb, rinv, ob = self.rsbs[r], self.rinvs[r], self.obs[r]
        self.act(rsb[64:65, 0:nq], psO[64:65, 0:nq], AF.Ln)
        self.act(rsb[64:65, 0:nq], rsb[64:65, 0:nq], AF.Exp, scale=-1.0)
        psR = self.ps()
        self.mm(psR[0:64, 0:nq], self.onesf[64:65, 0:64], rsb[64:65, 0:nq])
        self.cp(rinv[:, 0:nq], psR[0:64, 0:nq], eng='dve')
        self.tt(ob[:, 0:nq], psO[0:64, 0:nq], rinv[:, 0:nq], ALU.mult)
        self.dma('pool', out_dram, ob[:, 0:nq])
        yield

    def attn_bufs(self):
        S = self.S
        self.Ps = [S.alloc(f'P{i}', [128, 512], BF16) for i in range(4)]
        self.rsbs = [S.alloc(f'rsb{i}', [128, 512], F32) for i in range(2)]
        self.rinvs = [S.alloc(f'rinv{i}', [64, 512], F32) for i in range(2)]
        self.obs = [S.alloc(f'ob{i}', [64, 512], BF16) for i in range(2)]
        self.pi = 0
        return [*self.Ps, *self.rsbs, *self.rinvs, *self.obs]

    def stage_na(self, l, last):
        S, d = self.S, self.d
        ab = self.attn_bufs()
        qT = S.alloc('naq', [64, NT], BF16)
        kT = S.alloc('nak', [64, NT], BF16)
        Va = S.alloc('nav', [128, NTILE, 128], BF16)
        self.memset(Va[:, :, :].rearrange('p n e -> p (n e)'), 0.0)
        bm = S.alloc('nabm', [128, 14, 256], BF16)
        ebm = S.alloc('naebm', [128, 14, 256], BF16)
        vst = S.alloc('vst', [128, NTILE, 64], F32)
        for h in range(6):
            self.dma('sp', qT[:, :], d['pT_na'][h * 64:(h + 1) * 64, :])
            self.dma('sp', kT[:, :], d['pT_na'][384 + h * 64:384 + (h + 1) * 64, :])
            self.memset(Va[:, :, 64:65], 1.0)
            self.dma('sp', vst[:, :, :], d['p_tok'][:, 528 + h * 64:528 + (h + 1) * 64].rearrange('(n p) c -> p n c', p=128))
            self.cp(Va[:, :, 0:64], vst[:, :, :], eng='act')
            for cfg in range(14):
                self.dma('pool', bm[:, cfg, :], d['na_bm'][l, h, cfg])
            self.act(ebm[:, :, :].rearrange('p c q -> p (c q)'), bm[:, :, :].rearrange('p c q -> p (c q)'), AF.Exp)
            blocks = []
            if not last:
                blocks.append((0, [(0, None), (1, None)]))
            for qb in range(16):
                if qb == 0:
                    lat = [(2 + j, 6 + j) for j in range(4)]
                elif qb == 15:
                    lat = [(2 + 28 + j, 10 + j) for j in range(4)]
                else:
                    lat = [(2 + 2 * qb - 2 + i, i) for i in range(6)]
                blocks.append((256 + qb * 256, [(0, None), (1, None)] + lat))
            for q0, keys in blocks:
                yield from self.attn_block(lambda kt: kT[:, kt * 128:(kt + 1) * 128], qT[:, q0:q0 + 256], 256, keys,
                                           lambda kt: Va[:, kt, :], d['cat'][256 + h * 64:256 + (h + 1) * 64, q0:q0 + 256],
                                           bm_of=lambda cfg: ebm[:, cfg, :])
        S.free(vst, qT, kT, Va, bm, ebm, *ab)

    def rope(self, xv, nh, cs, sn, tmp):
        x5 = xv.rearrange('p h (a b f) -> p h a b f', a=2, b=2)
        x1 = x5[:, :, :, 0, :]
        x2 = x5[:, :, :, 1, :]
        csb = cs[:, :].rearrange('p (a f) -> p a f', a=2).unsqueeze(1).to_broadcast([128, nh, 2, 8])
        snb = sn[:, :].rearrange('p (a f) -> p a f', a=2).unsqueeze(1).to_broadcast([128, nh, 2, 8])
        t = [tmp[:, i, 0:nh * 16].rearrange('p (h a f) -> p h a f', h=nh, a=2) for i in range(4)]
        self.tt(t[0], x1, csb, ALU.mult)
        self.tt(t[1], x2, snb, ALU.mult)
        self.tt(t[2], x2, csb, ALU.mult)
        self.tt(t[3], x1, snb, ALU.mult)
        self.tt(x1, t[0], t[1], ALU.subtract)
        self.tt(x2, t[2], t[3], ALU.add)

    def stage_mla(self, l, last):
        S, d = self.S, self.d
        wuq = S.alloc('wuq', [128, 4, 576], BF16)
        self.dma('pool', wuq[:, :, :], d['w_uq'][l].rearrange('(k p) n -> p k n', p=128))
        wukv = S.alloc('wukv', [128, 2, 768], BF16)
        self.dma('pool', wukv[:, :, :], d['w_ukv'][l].rearrange('(k p) n -> p k n', p=128))
        qnw = self.bcast('qnw', d['q_nw'][l, :], 512)
        kvnw = self.bcast('kvnw', d['kv_nw'][l, :], 256)
        qT = S.alloc('mqT', [96, 6, NT], BF16)
        kT = S.alloc('mkT', [96, 6, NT], BF16)
        Va = S.alloc('mVa', [128, NTILE, 6, 128], BF16)
        self.memset(Va[:, :, :, :].rearrange('p n h e -> p (n h e)'), 0.0)
        self.memset(Va[:, :, :, 64:65], 1.0)
        pms = [S.alloc(f'pm{i}', [128, 800], F32) for i in range(2)]
        rings = []
        for nm, shp, dt in (('junk', [128, 512], F32), ('ssa', [128, 2], F32), ('cn', [128, 768], BF16), ('cT', [128, 6, 128], BF16),
                            ('qf32', [128, 576], F32), ('qb16', [128, 6, 128], BF16), ('kfull', [128, 6, 128], BF16),
                            ('krr', [128, 32], F32), ('rtmp', [128, 4, 96], F32)):
            rings.append([S.alloc(f'{nm}{i}', shp, dt) for i in range(2)])
        for i in range(2):
            self.memset(rings[5][i][:, :, :].rearrange('p h e -> p (h e)'), 0.0)
            self.memset(rings[6][i][:, :, :].rearrange('p h e -> p (h e)'), 0.0)
        css = [S.alloc(f'cs{i}', [128, 16], F32) for i in range(2)]
        sns = [S.alloc(f'sn{i}', [128, 16], F32) for i in range(2)]
        scale = 96 ** -0.5
        for n in range(3 if 'mla_short' in self.flags else NTILE):
            pm = pms[n % 2]
            junk, ssa, cn, cT, qf32, qb16, kfull, krr, rtmp = [rg[n % 2] for rg in rings]
            self.dma('sp', pm[:, :], d['p_tok'][n * 128:(n + 1) * 128, 912:1712])
            cs, sn = css[n % 2], sns[n % 2]
            if n >= 2:
                self.dma('sp', cs[:, :], d['rope_cs'][(n - 2) * 128:(n - 1) * 128, :])
                self.dma('sp', sn[:, :], d['rope_sn'][(n - 2) * 128:(n - 1) * 128, :])
            self.act(junk[:, 0:512], pm[:, 0:512], AF.Square, accum=ssa[:, 0:1])
            self.act(junk[:, 0:256], pm[:, 512:768], AF.Square, accum=ssa[:, 1:2])
            self.rstd(ssa[:, 0:1], 512, 'q')
            self.rstd(ssa[:, 1:2], 256, 'kv')
            self.stt(cn[:, 0:512], pm[:, 0:512], ssa[:, 0:1], qnw[:, :], ALU.mult, ALU.mult)
            self.stt(cn[:, 512:768], pm[:, 512:768], ssa[:, 1:2], kvnw[:, :], ALU.mult, ALU.mult)
            ps = self.ps()
            psb = ps[:, :].bitcast(BF16)
            for k in range(6):
                self.tr(psb[:, k * 128:(k + 1) * 128], cn[:, k * 128:(k + 1) * 128], self.identb[:, :])
            self.cp(cT[:, :, :], psb[:, 0:768].rearrange('p (k t) -> p k t', k=6), eng='act')
            p1, p2 = self.ps(), self.ps()
            for k in range(4):
                self.mm(p1[:, 0:512], cT[:, k, :], wuq[:, k, 0:512], start=(k == 0), stop=(k == 3))
            for k in range(4):
                self.mm(p2[:, 0:64], cT[:, k, :], wuq[:, k, 512:576], start=(k == 0), stop=(k == 3))
            self.ts(qf32[:, 0:512], p1[:, 0:512], scale, ALU.mult)
            self.ts(qf32[:, 512:576], p2[:, 0:64], scale, ALU.mult)
            if n >= 2 and 'norope' not in self.flags:
                self.rope(qf32[:, :].rearrange('p (h e) -> p h e', h=6)[:, :, 64:96], 6, cs, sn, rtmp)
            self.cp(qb16[:, :, 0:96], qf32[:, :].rearrange('p (h e) -> p h e', h=6), eng='act')
            ps = self.ps()
            psb = ps[:, :].bitcast(BF16)
            if 'no_qtr' not in self.flags:
                for h in range(6):
                    self.tr(psb[:, h * 128:(h + 1) * 128], qb16[:, h, :], self.identb[:, :])
                for (pa_, pb2) in ((0, 64), (64, 96)):
                    self.cp(qT[pa_:pb2, :, n * 128:(n + 1) * 128], psb[pa_:pb2, 0:768].rearrange('p (h t) -> p h t', h=6), eng='act')
            k1, k2 = self.ps(), self.ps()
            for k in range(2):
                self.mm(k1[:, 0:512], cT[:, 4 + k, :], wukv[:, k, 0:512], start=(k == 0), stop=(k == 1))
            for k in range(2):
                self.mm(k2[:, 0:256], cT[:, 4 + k, :], wukv[:, k, 512:768], start=(k == 0), stop=(k == 1))
            k1v = k1[:, 0:512].rearrange('p (h e) -> p h e', h=4)
            k2v = k2[:, 0:256].rearrange('p (h e) -> p h e', h=2)
            self.cp(kfull[:, 0:4, 0:64], k1v[:, :, 0:64])
            self.cp(kfull[:, 4:6, 0:64], k2v[:, :, 0:64])
            self.cp(Va[:, n, 0:4, 0:64], k1v[:, :, 64:128], eng='act')
            self.cp(Va[:, n, 4:6, 0:64], k2v[:, :, 64:128], eng='act')
            self.cp(krr[:, :], pm[:, 768:800])
            if n >= 2 and 'norope' not in self.flags:
                self.rope(krr[:, :].unsqueeze(1), 1, cs, sn, rtmp)
            self.cp(kfull[:, :, 64:96], krr[:, :].unsqueeze(1).to_broadcast([128, 6, 32]))
            ps = self.ps()
            psb = ps[:, :].bitcast(BF16)
            if 'no_ktr' not in self.flags:
                for h in range(6):
                    self.tr(psb[:, h * 128:(h + 1) * 128], kfull[:, h, :], self.identb[:, :])
                for (pa_, pb2) in ((0, 64), (64, 96)):
                    self.cp(kT[pa_:pb2, :, n * 128:(n + 1) * 128], psb[pa_:pb2, 0:768].rearrange('p (h t) -> p h t', h=6), eng='dve')
        S.free(wuq, wukv, qnw, kvnw, *pms, *css, *sns, *[b for rg in rings for b in rg])

        if getattr(self, 'stop_after', None) == ('mla1', l):
            S.free(qT, kT, Va)
            return
        ab = self.attn_bufs()
        allk = [(kt, None) for kt in range(NTILE)]
        for h in range(6):
            if not last:
                for _ in self.attn_block(lambda kt: kT[:, h, kt * 128:(kt + 1) * 128], qT[:, h, 0:256], 256, [(0, None), (1, None)],
                                         lambda kt: Va[:, kt, h, :], d['cat'][640 + h * 64:640 + (h + 1) * 64, 0:256]):
                    pass
            for b in range(8):
                q0 = 256 + b * 512
                for _ in self.attn_block(lambda kt: kT[:, h, kt * 128:(kt + 1) * 128], qT[:, h, q0:q0 + 512], 512, allk,
                                         lambda kt: Va[:, kt, h, :], d['cat'][640 + h * 64:640 + (h + 1) * 64, q0:q0 + 512]):
                    pass
        S.free(qT, kT, Va, *ab)

    def stage_out(self, l, xs, last):
        S, d = self.S, self.d
        w = S.alloc('w_out', [128, 8, DM], BF16)
        wsrc = d['w_out'][l].rearrange('(k p) n -> p k n', p=128)
        for k in range(8):
            self.dma('pool', w[:, k, :], wsrc[:, k, :])
        ab = self.mod_ab(l, 1, 'norm2_w', 'b')
        g1 = [self.bcast(f'g1{r}', d['mod'][l, r, 2 * DM:3 * DM], DM) for r in range(2)]
        if last:
            rw = S.alloc('rw', [128, 8, 8], F32)
            self.dma('sp', rw[:, :, :], d['router'].rearrange('(k p) e -> p k e', p=128))
            hf = S.alloc('hf', [128, DM], F32)
            hT32 = S.alloc('hT32', [128, 8, 128], F32)
            lg = S.alloc('lg', [128, 8], F32)
            l2 = S.alloc('l2', [128, 8], F32)
            mx = S.alloc('mx', [128, 4], F32)
            gsb = S.alloc('gsb', [128, 8], F32)
            m1m = S.alloc('m1m', [128, 8], F32)
        catTs = [S.alloc(f'catT{i}', [128, 8, 128], BF16) for i in range(2)]
        xts = [S.alloc(f'xo{i}', [128, DM], F32) for i in range(2)]
        xn = [S.alloc(f'xn{i}', [128, DM], F32) for i in range(2)]
        tmps = [S.alloc(f'tmpo{i}', [128, DM], F32) for i in range(2)]
        hbos = [S.alloc(f'hbo{i}', [128, DM], BF16) for i in range(2)]
        hT = [S.alloc(f'hTo{i}', [128, 8, 128], BF16) for i in range(2)]
        sso = [S.alloc(f'sso{i}', [128, 1], F32) for i in range(2)]
        for n in range(NTILE):
            if last and n < 2:
                continue
            r = 1 if n < 2 else 0
            catT, tmp, hb, ss = catTs[n % 2], tmps[n % 2], hbos[n % 2], sso[n % 2]
            self.dma('sp', catT[:, :, :], d['cat'].rearrange('(k p) t -> p k t', p=128)[:, :, n * 128:(n + 1) * 128])
            xt = xts[n % 2]
            self.dma('sp', xt[:, :], xs[n * 128:(n + 1) * 128, :])
            x1 = xn[n % 2]
            for half in range(2):
                ps = self.ps()
                for k in range(8):
                    self.mm(ps[:, 0:512], catT[:, k, :], w[:, k, half * 512:(half + 1) * 512], start=(k == 0), stop=(k == 7))
                sl = slice(half * 512, (half + 1) * 512)
                self.tt(x1[:, sl], ps[:, 0:512], g1[r][:, sl], ALU.mult)
                self.tt(x1[:, sl], x1[:, sl], xt[:, sl], ALU.add)
            self.dma('pool', d['x1'][n * 128:(n + 1) * 128, :], x1[:, :])
            A, B = ab[r]
            if last:
                self.norm_mod(x1, A, B, hf, tmp, ss)
                self.cp(hb[:, :], hf[:, :], eng='act')
                self.dma('pool', d['h2'][n * 128:(n + 1) * 128, :], hb[:, :])
                pa, pb_ = self.ps(), self.ps()
                for k in range(8):
                    pp = pa if k < 4 else pb_
                    self.tr(pp[:, (k % 4) * 128:(k % 4 + 1) * 128], hf[:, k * 128:(k + 1) * 128], self.identf[:, :])
                self.cp(hT32[:, 0:4, :], pa[:, :].rearrange('p (k t) -> p k t', k=4))
                self.cp(hT32[:, 4:8, :], pb_[:, :].rearrange('p (k t) -> p k t', k=4), eng='act')
                pl = self.ps()
                for k in range(8):
                    self.mm(pl[:, 0:8], hT32[:, k, :], rw[:, k, :], start=(k == 0), stop=(k == 7))
                self.cp(lg[:, :], pl[:, 0:8])
                self.red(mx[:, 0:1], lg[:, :], ALU.max)
                self.ts(m1m[:, :], lg[:, :], mx[:, 0:1], ALU.is_equal)
                self.dma('pool', d['sel1'][n * 128:(n + 1) * 128, :], m1m[:, :])
                self.stt(l2[:, :], m1m[:, :], -1e30, lg[:, :], ALU.mult, ALU.add)
                self.red(mx[:, 1:2], l2[:, :], ALU.max)
                self.ts(l2[:, :], lg[:, :], mx[:, 1:2], ALU.is_ge)
                self.ts(mx[:, 2:3], mx[:, 0:1], -1.0, ALU.mult)
                self.act(gsb[:, :], lg[:, :], AF.Exp, bias=mx[:, 2:3], scale=1.0)
                self.tt(gsb[:, :], gsb[:, :], l2[:, :], ALU.mult)
                self.red(mx[:, 3:4], gsb[:, :], ALU.add)
                self.recip(mx[:, 3:4], mx[:, 3:4])
                self.ts(gsb[:, :], gsb[:, :], mx[:, 3:4], ALU.mult)
                self.dma('pool', d['gates'][n * 128:(n + 1) * 128, :], gsb[:, :])
            else:
                self.norm_mod(x1, A, B, hb, tmp, ss)
            ht = hT[n % 2]
            self.transpose8(hb, ht[:, :, :])
            self.dma('pool', d['h2T'].rearrange('(k p) t -> p k t', p=128)[:, :, n * 128:(n + 1) * 128], ht[:, :, :])
        fr = [w, *catTs, *tmps, *hbos, *sso, *xts, *xn, *hT, *g1]
        if last:
            fr += [rw, hf, hT32, lg, l2, mx, gsb, m1m]
        S.free(*fr)
        for a, b in ab:
            S.free(a, b)

    def stage_ffn(self, l, last):
        S, d = self.S, self.d
        g2 = [self.bcast(f'g2{r}', d['mod'][l, r, 5 * DM:6 * DM], DM) for r in range(2)]
        if last:
            fw = self.bcast('fw', d['fin_w'][:], DM)
            sbs = [(256 + i * 1024, 1024) for i in range(4)]
            experts = [(d['moe_w1'][e], d['moe_w3'][e], d['moe_w2'][e]) for e in range(8)]
        else:
            sbs = [(0, 256)] + [(256 + i * 1024, 1024) for i in range(4)]
            experts = [(d['ffn_w1'], d['ffn_w3'], d['ffn_w2'])]
        hTb = S.alloc('hTb', [128, 8, 1024], BF16)
        acc = S.alloc('facc', [128, 8, DM], F32)
        uT = S.alloc('uT', [128, 4, 1024], BF16)
        us = [S.alloc(f'us{i}', [128, 512], F32) for i in range(2)]
        w1s = [S.alloc(f'w1g{i}', [128, 8, 512], BF16) for i in range(2)]
        w3s = [S.alloc(f'w3g{i}', [128, 8, 512], BF16) for i in range(2)]
        w2s = [S.alloc(f'w2g{i}', [128, 4, DM], BF16) for i in range(2)]
        gt = S.alloc('fgt', [128, 8, 8], F32)
        xts = [S.alloc(f'xf{i}', [128, DM], F32) for i in range(2)]
        tmp = S.alloc('ftmp', [128, DM], F32)
        ss = S.alloc('fss', [128, 1], F32)
        groups = [(g * 512, 512) for g in range(5)] + [(2560, 256)]
        wi = 0
        for (t0, ntok) in sbs:
            ntile = ntok // 128
            self.dma('sp', hTb[:, :, 0:ntok], d['h2T'].rearrange('(k p) t -> p k t', p=128)[:, :, t0:t0 + ntok])
            if last:
                self.dma('sp', gt[:, 0:ntile, :], d['gates'][t0:t0 + ntok, :].rearrange('(n p) e -> p n e', p=128))
            first = True
            for ei, (W1, W3, W2) in enumerate(experts):
                for (g0, gs) in groups:
                    nfc = gs // 128
                    w1, w3, w2 = w1s[wi % 2], w3s[wi % 2], w2s[wi % 2]
                    wi += 1
                    self.dma('pool', w1[:, :, 0:gs], W1.rearrange('(k p) n -> p k n', p=128)[:, :, g0:g0 + gs])
                    self.dma('pool', w3[:, :, 0:gs], W3.rearrange('(k p) n -> p k n', p=128)[:, :, g0:g0 + gs])
                    self.dma('pool', w2[:, 0:nfc, :], W2[g0:g0 + gs, :].rearrange('(c p) n -> p c n', p=128))
                    for tb in range((ntok + 511) // 512):
                        tw = min(512, ntok - tb * 512)
                        for fc in range(nfc):
                            pa, pb_ = self.ps(), self.ps()
                            for k in range(8):
                                self.mm(pa[:, 0:tw], w1[:, k, fc * 128:(fc + 1) * 128], hTb[:, k, tb * 512:tb * 512 + tw], start=(k == 0), stop=(k == 7))
                            for k in range(8):
                                self.mm(pb_[:, 0:tw], w3[:, k, fc * 128:(fc + 1) * 128], hTb[:, k, tb * 512:tb * 512 + tw], start=(k == 0), stop=(k == 7))
                            u = us[fc % 2]
                            self.act(u[:, 0:tw], pa[:, 0:tw], AF.Silu)
                            self.tt(uT[:, fc, tb * 512:tb * 512 + tw], u[:, 0:tw], pb_[:, 0:tw], ALU.mult)
                    for tt_ in range(ntile):
                        for half in range(2):
                            ps = self.ps()
                            for fc in range(nfc):
                                self.mm(ps[:, 0:512], uT[:, fc, tt_ * 128:(tt_ + 1) * 128], w2[:, fc, half * 512:(half + 1) * 512],
                                        start=(fc == 0), stop=(fc == nfc - 1))
                            a = acc[:, tt_, half * 512:(half + 1) * 512]
                            if last:
                                gsc = gt[:, tt_, ei:ei + 1]
                                if first:
                                    self.ts(a, ps[:, 0:512], gsc, ALU.mult)
                                else:
                                    self.stt(a, ps[:, 0:512], gsc, a, ALU.mult, ALU.add)
                            else:
                                if first:
                                    self.cp(a, ps[:, 0:512], eng='act')
                                else:
                                    self.tt(a, a, ps[:, 0:512], ALU.add)
                    first = False
            for tt_ in range(ntile):
                n = t0 // 128 + tt_
                r = 1 if n < 2 else 0
                xt = xts[tt_ % 2]
                self.dma('sp', xt[:, :], d['x1'][n * 128:(n + 1) * 128, :])
                a = acc[:, tt_, :]
                self.tt(a, a, g2[r][:, :], ALU.mult)
                self.tt(xt[:, :], xt[:, :], a, ALU.add)
                if last:
                    self.act(tmp[:, :], xt[:, :], AF.Square, accum=ss[:, 0:1])
                    self.rstd(ss[:, 0:1], DM, 'f')
                    self.stt(xt[:, :], xt[:, :], ss[:, 0:1], fw[:, :], ALU.mult, ALU.mult)
                    self.dma('pool', d['out'][(n - 2) * 128:(n - 1) * 128, :], xt[:, :])
                else:
                    self.dma('pool', d['x2'][n * 128:(n + 1) * 128, :], xt[:, :])
        fr = [hTb, acc, uT, gt, tmp, ss, *us, *w1s, *w3s, *w2s, *xts, *g2]
        if last:
            fr.append(fw)
        S.free(*fr)


    def stage_moe(self, l):
        S, d = self.S, self.d
        NL = 32
        g2 = self.bcast('g2m', d['mod'][l, 0, 5 * DM:6 * DM], DM)
        fw = self.bcast('fwm', d['fin_w'][:], DM)
        G3 = S.alloc('G3', [128, NL, 8], F32)
        M1 = S.alloc('M1', [128, NL, 8], F32)
        self.dma('sp', G3[:, :, :], d['gates'][256:NT, :].rearrange('(n p) e -> p n e', p=128))
        self.dma('sp', M1[:, :, :], d['sel1'][256:NT, :].rearrange('(n p) e -> p n e', p=128))
        Ms = S.alloc('Ms', [128, NL, 8], F32)
        self.ts(Ms[:, :, :], G3[:, :, :], 0.0, ALU.is_gt)
        M2 = S.alloc('M2', [128, NL, 8], F32)
        self.tt(M2[:, :, :], Ms[:, :, :], M1[:, :, :], ALU.subtract)
        triS = S.alloc('triS', [128, 128], F32)
        self.tt(triS[:, :], self.tri0[:, :], self.identf[:, :], ALU.subtract)
        Ms2 = Ms[:, :, :].rearrange('p n e -> p (n e)')
        pc_, pr_ = self.ps(), self.ps()
        self.mm(pc_[:, 0:256], self.onesf[:, :], Ms2)
        self.mm(pr_[:, 0:256], triS[:, :], Ms2)
        CS = S.alloc('CS', [128, NL, 8], F32)
        self.cp(CS[:, :, :].rearrange('p n e -> p (n e)'), pc_[:, 0:256])
        pos = S.alloc('pos', [128, NL, 8], F32)
        self.cp(pos[:, :, :].rearrange('p n e -> p (n e)'), pr_[:, 0:256], eng='act')
        TB = S.alloc('TB', [128, NL + 1, 8], F32)
        self.memset(TB[:, 0, :], 0.0)
        for n in range(NL):
            self.tt(TB[:, n + 1, :], TB[:, n, :], CS[:, n, :], ALU.add)
        cth = S.alloc('cth', [128, 16], F32)
        self.dma('sp', cth[:, :], d['c_th'][:, :])
        cj = S.alloc('cj', [128, NBLK], F32)
        self.dma('sp', cj[:, :], d['c_j512'][:, :])
        cgp = S.alloc('cgp', [128, 11], F32)
        self.dma('sp', cgp[:, :], d['c_gp'][:, :])
        cmp_ = S.alloc('cmp', [128, 8, 16], F32)
        self.tt(cmp_[:, :, :], cth[:, :].unsqueeze(1).to_broadcast([128, 8, 16]),
                TB[:, NL, :].unsqueeze(2).to_broadcast([128, 8, 16]), ALU.is_lt)
        pcn = S.alloc('pcn', [128, 8], F32)
        self.red(pcn[:, :], cmp_[:, :, :], ALU.add)
        self.ts(pcn[:, :], pcn[:, :], 512.0, ALU.mult)
        end = S.alloc('end', [128, 8], F32)
        self.cp(end[:, 0:1], pcn[:, 0:1])
        for e in range(1, 8):
            self.tt(end[:, e:e + 1], end[:, e - 1:e], pcn[:, e:e + 1], ALU.add)
        st = S.alloc('st', [128, 8], F32)
        self.tt(st[:, :], end[:, :], pcn[:, :], ALU.subtract)
        self.tt(pos[:, :, :], pos[:, :, :], TB[:, 0:NL, :], ALU.add)
        self.tt(pos[:, :, :], pos[:, :, :], st[:, :].unsqueeze(1).to_broadcast([128, NL, 8]), ALU.add)
        tmp3 = S.alloc('tmp3', [128, NL, 8], F32)
        slotf = S.alloc('slotf', [128, 2, NL], F32)
        gk = S.alloc('gk', [128, 2, NL], F32)
        for k, Mk in enumerate((M1, M2)):
            self.tt(tmp3[:, :, :], Mk[:, :, :], pos[:, :, :], ALU.mult)
            self.red(slotf[:, k, :], tmp3[:, :, :], ALU.add)
            self.tt(tmp3[:, :, :], Mk[:, :, :], G3[:, :, :], ALU.mult)
            self.red(gk[:, k, :], tmp3[:, :, :], ALU.add)
        self.ts(slotf[:, :, :], slotf[:, :, :], float(NSLOT - 1), ALU.min)
        sloti = S.alloc('sloti', [128, 2, NL], I32)
        self.cp(sloti[:, :, :].rearrange('p k n -> p (k n)'), slotf[:, :, :].rearrange('p k n -> p (k n)'))
        bj = S.alloc('bj', [128, NBLK, 8], F32)
        self.tt(bj[:, :, :], end[:, :].unsqueeze(1).to_broadcast([128, NBLK, 8]),
                cj[:, :].unsqueeze(2).to_broadcast([128, NBLK, 8]), ALU.is_le)
        ej = S.alloc('ej', [128, NBLK], F32)
        self.red(ej[:, :], bj[:, :, :], ALU.add)
        self.ts(ej[:, :], ej[:, :], 7.0, ALU.min, 1408.0, ALU.mult)
        idxf = S.alloc('idxf', [128, NBLK, 11], F32)
        self.tt(idxf[:, :, :], ej[:, :].unsqueeze(2).to_broadcast([128, NBLK, 11]),
                cgp[:, :].unsqueeze(1).to_broadcast([128, NBLK, 11]), ALU.add)
        idxi = S.alloc('idxi', [128, NBLK, 11], I32)
        self.cp(idxi[:, :, :].rearrange('p j g -> p (j g)'), idxf[:, :, :].rearrange('p j g -> p (j g)'))
        S.free(G3, M1, Ms, M2, triS, CS, pos, TB, cth, cj, cgp, cmp_, pcn, end, st, tmp3, slotf, bj, ej, idxf)

        zt = S.alloc('zt', [128, 24, DM], BF16)
        self.memset(zt[:, :, :].rearrange('p a c -> p (a c)'), 0.0)
        for j in range(NBLK // 6):
            self.dma('sp', d['hs'][j * 3072:(j + 1) * 3072, :].rearrange('(a p) c -> p a c', p=128), zt[:, :, :])
        hts = [S.alloc(f'hsrc{i}', [128, DM], BF16) for i in range(2)]
        for n in range(NL):
            ht = hts[n % 2]
            self.dma('sp', ht[:, :], d['h2'][(n + 2) * 128:(n + 3) * 128, :])
            for k in range(2):
                self.dma_ind(d['hs'][:, :], ht[:, :], sloti[:, k, n:n + 1], True, NSLOT - 1)
        S.free(zt, *hts)

        hbs = [S.alloc(f'hsb{i}', [128, 4, DM], BF16) for i in range(2)]
        hTs = [S.alloc(f'hTm{i}', [128, 8, 512], BF16) for i in range(2)]
        accs = [S.alloc(f'macc{i}', [128, 4, DM], F32) for i in range(2)]
        uTs = [S.alloc(f'uTm{i}', [128, 2, 512], BF16) for i in range(2)]
        us = [S.alloc(f'usm{i}', [128, 512], F32) for i in range(2)]
        w1s = [S.alloc(f'w1m{i}', [128, 8, 256], BF16) for i in range(4)]
        w3s = [S.alloc(f'w3m{i}', [128, 8, 256], BF16) for i in range(4)]
        w2s = [S.alloc(f'w2m{i}', [128, 2, DM], BF16) for i in range(4)]
        wi = 0
        nrow = 8 * 11 * 128
        for j in range(NBLK):
            hb, hT, acc = hbs[j % 2], hTs[j % 2], accs[j % 2]
            self.dma('sp', hb[:, :, :], d['hs'][j * 512:(j + 1) * 512, :].rearrange('(a p) c -> p a c', p=128))
            for a in range(4):
                ps = self.ps()
                psb = ps[:, :].bitcast(BF16)
                for k in range(8):
                    self.tr(psb[:, k * 128:(k + 1) * 128], hb[:, a, k * 128:(k + 1) * 128], self.identb[:, :])
                self.cp(hT[:, :, a * 128:(a + 1) * 128], psb[:, :].rearrange('p (k t) -> p k t', k=8), eng=('act' if a % 2 else 'dve'))
            for gp in range(0, 11, 2):
                ws = []
                for g in [x for x in (gp, gp + 1) if x < 11]:
                    w1, w3, w2 = w1s[wi % 4], w3s[wi % 4], w2s[wi % 4]
                    wi += 1
                    ix = idxi[:, j, g:g + 1]
                    self.dma_ind(w1[:, :, :].rearrange('p k c -> p (k c)'), d['moe_w1'][:, :], ix, False, nrow - 1)
                    self.dma_ind(w3[:, :, :].rearrange('p k c -> p (k c)'), d['moe_w3'][:, :], ix, False, nrow - 1)
                    self.dma_ind(w2[:, :, :].rearrange('p k c -> p (k c)'), d['moe_w2'][:, :], ix, False, nrow - 1)
                    uT = uTs[g % 2]
                    for fc in range(2):
                        pa, pb_ = self.ps(), self.ps()
                        for k in range(8):
                            self.mm(pa[:, 0:512], w1[:, k, fc * 128:(fc + 1) * 128], hT[:, k, :], start=(k == 0), stop=(k == 7))
                        for k in range(8):
                            self.mm(pb_[:, 0:512], w3[:, k, fc * 128:(fc + 1) * 128], hT[:, k, :], start=(k == 0), stop=(k == 7))
                        u = us[fc % 2]
                        self.act(u[:, :], pa[:, 0:512], AF.Silu)
                        self.tt(uT[:, fc, :], u[:, :], pb_[:, 0:512], ALU.mult)
                    ws.append((uT, w2))
                for a in range(4):
                    for half in range(2):
                        ps = self.psacc()
                        nmm = 2 * len(ws)
                        mi = 0
                        for (uT, w2) in ws:
                            for fc in range(2):
                                self.mm(ps[:, 0:512], uT[:, fc, a * 128:(a + 1) * 128], w2[:, fc, half * 512:(half + 1) * 512],
                                        start=(mi == 0), stop=(mi == nmm - 1))
                                mi += 1
                        av = acc[:, a, half * 512:(half + 1) * 512]
                        if gp == 0:
                            self.cp(av, ps[:, 0:512], eng='act')
                        else:
                            self.tt(av, av, ps[:, 0:512], ALU.add)
            self.dma('sp', d['yb'][j * 512:(j + 1) * 512, :].rearrange('(a p) c -> p a c', p=128), acc[:, :, :])
        S.free(*hbs, *hTs, *accs, *uTs, *us, *w1s, *w3s, *w2s, idxi)

        y1s = [S.alloc(f'y1_{i}', [128, DM], F32) for i in range(2)]
        y2s = [S.alloc(f'y2_{i}', [128, DM], F32) for i in range(2)]
        xts = [S.alloc(f'xm{i}', [128, DM], F32) for i in range(2)]
        tmp = S.alloc('mtmp', [128, DM], F32)
        ss = S.alloc('mss', [128, 1], F32)
        outs = [S.alloc(f'mout{i}', [128, DM], F32) for i in range(2)]
        def loads(n):
            y1, y2, xt = y1s[n % 2], y2s[n % 2], xts[n % 2]
            self.dma_ind(y1[:, :], d['yb'][:, :], sloti[:, 0, n:n + 1], False, NSLOT - 1)
            self.dma_ind(y2[:, :], d['yb'][:, :], sloti[:, 1, n:n + 1], False, NSLOT - 1)
            self.dma('sp', xt[:, :], d['x1'][(n + 2) * 128:(n + 3) * 128, :])
        loads(0)
        for n in range(NL):
            y1, y2, xt = y1s[n % 2], y2s[n % 2], xts[n % 2]
            ob = outs[n % 2]
            if n + 1 < NL:
                loads(n + 1)
            self.ts(y1[:, :], y1[:, :], gk[:, 0, n:n + 1], ALU.mult)
            self.stt(y1[:, :], y2[:, :], gk[:, 1, n:n + 1], y1[:, :], ALU.mult, ALU.add)
            self.tt(y1[:, :], y1[:, :], g2[:, :], ALU.mult)
            self.tt(xt[:, :], xt[:, :], y1[:, :], ALU.add)
            self.act(tmp[:, :], xt[:, :], AF.Square, accum=ss[:, 0:1])
            self.rstd(ss[:, 0:1], DM, 'f')
            self.stt(ob[:, :], xt[:, :], ss[:, 0:1], fw[:, :], ALU.mult, ALU.mult)
            self.dma('sp', d['out'][n * 128:(n + 1) * 128, :], ob[:, :])
        S.free(*outs)
        S.free(g2, fw, gk, sloti, tmp, ss, *y1s, *y2s, *xts)


def _na_bias_tiles(rpb):
    L, H = rpb.shape[0], rpb.shape[1]
    NEG = np.float32(-30000.0)
    col = np.arange(64)
    c0 = np.clip(col - 8, 0, 48)
    ck = col[:, None]
    cq = col[None, :]
    cvalid = (ck >= c0[None, :]) & (ck < c0[None, :] + 16)
    cidx = np.clip(ck - cq, -15, 15) + 15
    out = np.full((L, H, 14, 128, 256), NEG, dtype=np.float32)
    cfgs = []
    for i in range(6):
        cfgs.append((4, 4 - 4 + 2 * i))
    for j in range(4):
        cfgs.append((0, 2 * j))
    for j in range(4):
        cfgs.append((60, 56 + 2 * j))
    for ci, (R, kr0) in enumerate(cfgs):
        for a in range(2):
            kr = kr0 + a
            for b in range(4):
                r = R + b
                r0 = min(max(r - 4, 0), 56)
                if not (r0 <= kr <= r0 + 7) or kr > 63:
                    continue
                drow = kr - r + 7
                g = rpb[:, :, drow, :][:, :, cidx]
                blk = np.where(cvalid[None, None], g, NEG)
                out[:, :, ci, a * 64:(a + 1) * 64, b * 64:(b + 1) * 64] = blk
    return out


def _rope_tables():
    t = np.arange(4096)
    row = (t // 64).astype(np.float32)
    colv = (t % 64).astype(np.float32)
    inv = (np.float32(10000.0) ** (-np.arange(0, 16, 2, dtype=np.float32) / np.float32(16))).astype(np.float32)
    ang = np.concatenate([row[:, None] * inv, colv[:, None] * inv], axis=1).astype(np.float32)
    return np.cos(ang).astype(np.float32), np.sin(ang).astype(np.float32)


def _w13_layout(w):
    w = w.reshape(8, 8, 128, 11, 256)
    return np.ascontiguousarray(w.transpose(0, 3, 2, 1, 4)).reshape(8 * 11 * 128, 2048)


def _w2_layout(w):
    w = w.reshape(8, 11, 2, 128, 1024)
    return np.ascontiguousarray(w.transpose(0, 1, 3, 2, 4)).reshape(8 * 11 * 128, 2048)


_NC_CACHE = {}


def _get_nc(stop_after=None):
    key = (stop_after, DEBUG)
    if key not in _NC_CACHE:
        nc = bass.Bass("TRN2", target_bir_lowering=False)
        k = K(nc)
        k.stop_after = stop_after
        k.build()
        _NC_CACHE[key] = nc
    return _NC_CACHE[key]


def make_in_maps(inputs):
    f = lambda a: np.ascontiguousarray(np.asarray(a, dtype=np.float32))
    x, c, ctx, c_ctx = f(inputs['x']), f(inputs['c']), f(inputs['ctx']), f(inputs['c_ctx'])
    cs, sn = _rope_tables()
    tri0 = np.triu(np.ones((128, 128), np.float32))
    tri1 = np.tril(np.ones((128, 128), np.float32))
    shared = {
        'ada_w': f(inputs['ada_w']), 'ada_b': f(inputs['ada_b']),
        'norm1_w': f(inputs['norm1_w']), 'norm2_w': f(inputs['norm2_w']),
        'w_in': f(inputs['w_in']), 'w_out': f(inputs['w_out']),
        'convT': f(np.transpose(np.asarray(inputs['mlstm_conv_w']), (0, 2, 1))),
        'ig_b': f(np.asarray(inputs['mlstm_ig_b']).reshape(2, 8)),
        'fg_b': f(np.asarray(inputs['mlstm_fg_b']).reshape(2, 8)),
        'ml_nw': f(inputs['mlstm_norm_w']),
        'na_bm': _na_bias_tiles(f(inputs['na_rpb'])),
        'q_nw': f(inputs['mla_q_norm_w']), 'kv_nw': f(inputs['mla_kv_norm_w']),
        'w_uq': f(inputs['mla_w_uq']), 'w_ukv': f(inputs['mla_w_ukv']),
        'ffn_w1': f(inputs['ffn_w1'])[0], 'ffn_w3': f(inputs['ffn_w3'])[0], 'ffn_w2': f(inputs['ffn_w2'])[0],
        'router': f(inputs['moe_router_w'])[0],
        'moe_w1': _w13_layout(f(inputs['moe_w1'])[0]), 'moe_w3': _w13_layout(f(inputs['moe_w3'])[0]),
        'moe_w2': _w2_layout(f(inputs['moe_w2'])[0]),
        'c_th': np.ascontiguousarray(np.broadcast_to(np.arange(16, dtype=np.float32) * 512, (128, 16))),
        'c_j512': np.ascontiguousarray(np.broadcast_to(np.arange(NBLK, dtype=np.float32) * 512, (128, NBLK))),
        'c_gp': np.ascontiguousarray((np.arange(11, dtype=np.float32)[None, :] * 128 + np.arange(128, dtype=np.float32)[:, None])),
        'fin_w': f(inputs['final_norm_w']),
        'ident': np.eye(128, dtype=np.float32), 'tri0': tri0, 'tri1': tri1,
        'rope_cs': cs, 'rope_sn': sn,
    }
    maps = []
    for b in range(8):
        m = dict(shared)
        m['xin'] = np.ascontiguousarray(np.concatenate([ctx[b], x[b]], axis=0))
        cc = np.stack([c[b].reshape(8, 128).T, c_ctx.reshape(8, 128).T], axis=2)
        m['cc'] = np.ascontiguousarray(cc.reshape(128, 16))
        maps.append(m)
    return maps


def kernel(**inputs):
    nc = _get_nc()
    maps = make_in_maps(inputs)
    res = run_bass_kernel_spmd(nc, maps, core_ids=list(range(8)))
    return np.stack([np.asarray(r['out'], dtype=np.float32) for r in res.results], axis=0)
```
